# Optimizing a Trainium2 kernel written in Bass

```python
import math
import jax, jax.numpy as jnp
from jax import lax
import numpy as np

D_MODEL = 1024
BATCH = 4
SEQ = 4096
DEPTH = 4

PLE_DIM = 256
ATTN_HEADS = 8
ATTN_HEAD_DIM = 64
ATTN_WIDTH = ATTN_HEADS * ATTN_HEAD_DIM
Q_BLOCK = 128
POOL_WINDOWS = (2, 4, 8, 16)
POOL_GROUPS = len(POOL_WINDOWS)
POOL_GROUP_DIM = 128
POOL_WIDTH = POOL_GROUPS * POOL_GROUP_DIM
DN_HEADS = 4
DN_HEAD_DIM = 128
DN_WIDTH = DN_HEADS * DN_HEAD_DIM
DN_CONV = 4
DN_CHUNK = 64
N_GROUPS = 4
EXPERTS_PER_GROUP = 8
N_EXPERTS = N_GROUPS * EXPERTS_PER_GROUP
TOP_K = 2
D_EXPERT = 512
MOE_BLOCK = 128
DEEPNORM_ALPHA = (2 * DEPTH) ** 0.25
DEEPNORM_BETA = (8 * DEPTH) ** -0.25
LN_EPS = 1e-5
NORM_EPS = 1e-6
NEG_INF = -1e30
IN_SECTIONS = (ATTN_WIDTH, ATTN_WIDTH, ATTN_WIDTH, ATTN_HEADS,
               POOL_WIDTH,
               DN_WIDTH, DN_WIDTH, DN_WIDTH, DN_HEADS, DN_HEADS,
               DN_WIDTH,
               D_MODEL, D_MODEL, D_MODEL)
IN_WIDTH = sum(IN_SECTIONS)
IN_SPLITS = tuple(int(s) for s in np.cumsum(IN_SECTIONS)[:-1])

kernel_name = "hybrid_fox_pool_deltanet_hmoe_deepnorm"


def layer_norm(x, g, b):
    x32 = x.astype(jnp.float32)
    mu = jnp.mean(x32, axis=-1, keepdims=True)
    var = jnp.mean(jnp.square(x32 - mu), axis=-1, keepdims=True)
    y = (x32 - mu) * lax.rsqrt(var + LN_EPS) * g.astype(jnp.float32) + b.astype(jnp.float32)
    return y.astype(x.dtype)


def l2_normalize(t):
    return t * lax.rsqrt(jnp.sum(jnp.square(t), axis=-1, keepdims=True) + NORM_EPS)


def forgetting_attention(q, k, v, log_f):
    B, S, H, Dh = q.shape
    nb = S // Q_BLOCK
    c = jnp.cumsum(log_f.astype(jnp.float32), axis=1)
    c_k = jnp.transpose(c, (0, 2, 1))
    q_blocks = jnp.moveaxis(q.reshape(B, nb, Q_BLOCK, H, Dh), 1, 0)
    c_blocks = jnp.moveaxis(c_k.reshape(B, H, nb, Q_BLOCK), 2, 0)
    key_pos = jnp.arange(S)
    scale = Dh ** -0.5

    def one_block(args):
        q_i, c_i, blk = args
        query_pos = blk * Q_BLOCK + jnp.arange(Q_BLOCK)
        s = jnp.einsum("bqhd,bkhd->bhqk", q_i, k).astype(jnp.float32) * scale
        s = s + c_i[..., :, None] - c_k[:, :, None, :]
        s = jnp.where(key_pos[None, :] <= query_pos[:, None], s, NEG_INF)
        probs = jax.nn.softmax(s, axis=-1).astype(v.dtype)
        return jnp.einsum("bhqk,bkhd->bqhd", probs, v)

    out = lax.map(one_block, (q_blocks, c_blocks, jnp.arange(nb)))
    return jnp.moveaxis(out, 0, 1).reshape(B, S, H * Dh)


def multiscale_pool(u, pool_w, pool_scale):
    B, S, _ = u.shape
    u32 = u.astype(jnp.float32)
    cs = jnp.cumsum(u32, axis=1)
    n_pos = jnp.arange(1, S + 1, dtype=jnp.float32)[None, :, None]
    groups = []
    for g, w in enumerate(POOL_WINDOWS):
        sl = slice(g * POOL_GROUP_DIM, (g + 1) * POOL_GROUP_DIM)
        cs_g = cs[..., sl]
        cs_prev = jnp.pad(cs_g, ((0, 0), (w, 0), (0, 0)))[:, :S]
        mean = (cs_g - cs_prev) / jnp.minimum(n_pos, float(w))
        groups.append(mean - u32[..., sl])
    d = jnp.stack(groups, axis=2).astype(u.dtype)
    y = jnp.einsum("bsgc,gcd->bsgd", d, pool_w).reshape(B, S, POOL_WIDTH)
    return y * pool_scale


def causal_depthwise_conv(u, w):
    return lax.conv_general_dilated(
        u, w[:, None, :].astype(u.dtype), window_strides=(1,),
        padding=((DN_CONV - 1, 0),), dimension_numbers=("NWC", "WIO", "NWC"),
        feature_group_count=u.shape[-1])


def gated_delta_rule(q, k, v, g, beta):
    B, S, H, Dk = q.shape
    Dv = v.shape[-1]
    nc = S // DN_CHUNK

    def to_chunks(t):
        return jnp.transpose(t.reshape(B, nc, DN_CHUNK, H, t.shape[-1]), (0, 3, 1, 2, 4))

    q = to_chunks(q) * Dk ** -0.5
    k = to_chunks(k)
    v = to_chunks(v)
    beta = to_chunks(beta[..., None])
    gc = jnp.cumsum(to_chunks(g[..., None])[..., 0], axis=-1)
    idx = jnp.arange(DN_CHUNK)
    causal = idx[:, None] >= idx[None, :]
    strict = idx[:, None] > idx[None, :]
    decay = jnp.where(causal, jnp.exp(jnp.where(causal, gc[..., :, None] - gc[..., None, :], 0.0)), 0.0)
    kk = jnp.einsum("bhnid,bhnjd->bhnij", k, k)
    lower = jnp.where(strict, beta * kk * decay, 0.0)
    rhs = jnp.concatenate([v * beta, k * beta * jnp.exp(gc)[..., None]], axis=-1)
    sol = lax.linalg.triangular_solve(lower, rhs, left_side=True, lower=True, unit_diagonal=True)
    u_intra, w_state = sol[..., :Dv], sol[..., Dv:]
    attn = jnp.where(causal, jnp.einsum("bhnid,bhnjd->bhnij", q, k) * decay, 0.0)
    q_dec = q * jnp.exp(gc)[..., None]
    g_last = gc[..., -1]
    k_dec = k * jnp.exp(g_last[..., None] - gc)[..., None]
    xs = tuple(jnp.moveaxis(t, 2, 0) for t in (u_intra, w_state, attn, q_dec, k_dec, g_last))

    def step(state, inp):
        u_c, w_c, a_c, qd_c, kd_c, gl_c = inp
        u = u_c - jnp.einsum("bhck,bhkv->bhcv", w_c, state)
        o = jnp.einsum("bhck,bhkv->bhcv", qd_c, state) + jnp.einsum("bhij,bhjv->bhiv", a_c, u)
        state = state * jnp.exp(gl_c)[..., None, None] + jnp.einsum("bhck,bhcv->bhkv", kd_c, u)
        return state, o

    state0 = jnp.zeros((B, H, Dk, Dv), jnp.float32)
    _, o = lax.scan(step, state0, xs)
    return jnp.transpose(o, (1, 0, 3, 2, 4)).reshape(B, S, H, Dv)


def gated_deltanet(dq, dk, dv, da, db, dg, conv_w, a_log, dt_bias, norm_w):
    B, S, _ = dq.shape
    hd = (B, S, DN_HEADS, DN_HEAD_DIM)
    qkv = jax.nn.silu(causal_depthwise_conv(jnp.concatenate([dq, dk, dv], axis=-1), conv_w))
    q, k, v = jnp.split(qkv.astype(jnp.float32), 3, axis=-1)
    q = l2_normalize(q.reshape(hd))
    k = l2_normalize(k.reshape(hd))
    v = v.reshape(hd)
    beta = jax.nn.sigmoid(db.astype(jnp.float32))
    g = -jnp.exp(a_log.astype(jnp.float32)) * jax.nn.softplus(
        da.astype(jnp.float32) + dt_bias.astype(jnp.float32))
    o = gated_delta_rule(q, k, v, g, beta)
    o = o * lax.rsqrt(jnp.mean(jnp.square(o), axis=-1, keepdims=True) + NORM_EPS) * norm_w.astype(jnp.float32)
    o = o * jax.nn.silu(dg.astype(jnp.float32).reshape(hd))
    return o.reshape(B, S, DN_WIDTH).astype(dq.dtype)


def token_mixer(h, w_in, b_forget, pool_w, pool_scale, dn_conv, dn_a_log, dn_dt_bias,
                dn_norm_w, w_br_attn, w_br_pool, w_br_dn, w_out):
    B, S, _ = h.shape
    proj = h @ w_in
    (aq, ak, av, af, pu, dq, dk, dv, da, db, dg, ga, gp, gd) = jnp.split(proj, IN_SPLITS, axis=-1)
    ahd = (B, S, ATTN_HEADS, ATTN_HEAD_DIM)
    log_f = jax.nn.log_sigmoid((af + b_forget).astype(jnp.float32))
    y_attn = forgetting_attention(aq.reshape(ahd), ak.reshape(ahd), av.reshape(ahd), log_f)
    y_pool = multiscale_pool(pu, pool_w, pool_scale)
    y_dn = gated_deltanet(dq, dk, dv, da, db, dg, dn_conv, dn_a_log, dn_dt_bias, dn_norm_w)
    merged = (jax.nn.sigmoid(ga) * (y_attn @ w_br_attn)
              + jax.nn.sigmoid(gp) * (y_pool @ w_br_pool)
              + jax.nn.sigmoid(gd) * (y_dn @ w_br_dn))
    return merged @ w_out


def hierarchical_moe(h, w_rg, b_rg, w_re, b_re, w_gate, w_up, w_down):
    B, S, D = h.shape
    T = B * S
    hf = h.reshape(T, D)
    group_logits = (hf @ w_rg + b_rg).astype(jnp.float32)
    group = jnp.argmax(group_logits, axis=-1)
    p_group = jnp.take_along_axis(jax.nn.softmax(group_logits, axis=-1), group[:, None], axis=-1)
    expert_logits = (hf @ w_re + b_re).astype(jnp.float32).reshape(T, N_GROUPS, EXPERTS_PER_GROUP)
    local_logits = jnp.take_along_axis(expert_logits, group[:, None, None], axis=1)[:, 0]
    top_p, top_local = lax.top_k(jax.nn.softmax(local_logits, axis=-1), TOP_K)
    gate = p_group * top_p / jnp.sum(top_p, axis=-1, keepdims=True)
    expert = group[:, None] * EXPERTS_PER_GROUP + top_local

    n_assign = T * TOP_K
    flat_e = expert.reshape(-1)
    flat_tok = jnp.repeat(jnp.arange(T, dtype=jnp.int32), TOP_K)
    flat_gate = gate.reshape(-1)
    order = jnp.argsort(flat_e)
    sorted_e = flat_e[order]
    counts = jnp.bincount(flat_e, length=N_EXPERTS)
    padded = (counts + MOE_BLOCK - 1) // MOE_BLOCK * MOE_BLOCK
    start = jnp.cumsum(counts) - counts
    padded_end = jnp.cumsum(padded)
    padded_start = padded_end - padded
    dest = padded_start[sorted_e] + jnp.arange(n_assign) - start[sorted_e]
    n_rows = n_assign + N_EXPERTS * MOE_BLOCK
    n_blocks = n_rows // MOE_BLOCK
    row_tok = jnp.zeros((n_rows,), jnp.int32).at[dest].set(flat_tok[order])
    row_gate = jnp.zeros((n_rows,), jnp.float32).at[dest].set(flat_gate[order])
    block_start = jnp.arange(n_blocks) * MOE_BLOCK
    block_expert = jnp.minimum(jnp.sum(block_start[:, None] >= padded_end[None, :], axis=1), N_EXPERTS - 1)
    xs = hf[row_tok].reshape(n_blocks, MOE_BLOCK, D)

    def expert_block(args):
        xb, e = args
        return (jax.nn.silu(xb @ w_gate[e]) * (xb @ w_up[e])) @ w_down[e]

    ys = lax.map(expert_block, (xs, block_expert)).reshape(n_rows, D)
    out = jax.ops.segment_sum(ys * row_gate[:, None].astype(ys.dtype), row_tok, num_segments=T)
    return out.reshape(B, S, D)


def setup_inputs(seed: int = 0) -> dict:
    key = jax.random.key(seed)
    ks = jax.random.split(key, 27)
    f32 = jnp.float32
    L = DEPTH

    def nrm(k, shape, scale):
        return jax.random.normal(k, shape, f32) * scale

    dt = jnp.exp(jax.random.uniform(ks[8], (L, DN_HEADS), f32, math.log(1e-3), math.log(1e-1)))
    return {
        "x": nrm(ks[0], (BATCH, SEQ, D_MODEL), 1.0),
        "p": nrm(ks[1], (DEPTH, BATCH, SEQ, PLE_DIM), 1.0),
        "w_in": nrm(ks[2], (L, D_MODEL, IN_WIDTH), D_MODEL ** -0.5),
        "b_forget": 2.0 + nrm(ks[3], (L, ATTN_HEADS), 0.1),
        "pool_w": nrm(ks[4], (L, POOL_GROUPS, POOL_GROUP_DIM, POOL_GROUP_DIM), POOL_GROUP_DIM ** -0.5),
        "pool_scale": 1.0 + nrm(ks[5], (L, POOL_WIDTH), 0.1),
        "dn_conv": nrm(ks[6], (L, DN_CONV, 3 * DN_WIDTH), DN_CONV ** -0.5),
        "dn_a_log": jnp.log(jax.random.uniform(ks[7], (L, DN_HEADS), f32, 1.0, 16.0)),
        "dn_dt_bias": jnp.log(jnp.expm1(dt)),
        "dn_norm_w": 1.0 + nrm(ks[9], (L, DN_HEAD_DIM), 0.02),
        "w_br_attn": nrm(ks[10], (L, ATTN_WIDTH, D_MODEL), ATTN_WIDTH ** -0.5),
        "w_br_pool": nrm(ks[11], (L, POOL_WIDTH, D_MODEL), POOL_WIDTH ** -0.5),
        "w_br_dn": nrm(ks[12], (L, DN_WIDTH, D_MODEL), DN_WIDTH ** -0.5),
        "w_out": nrm(ks[13], (L, D_MODEL, D_MODEL), DEEPNORM_BETA * D_MODEL ** -0.5),
        "ln1_g": 1.0 + nrm(ks[14], (L, D_MODEL), 0.02),
        "ln1_b": nrm(ks[15], (L, D_MODEL), 0.02),
        "w_router_group": nrm(ks[16], (L, D_MODEL, N_GROUPS), D_MODEL ** -0.5),
        "b_router_group": nrm(ks[17], (L, N_GROUPS), 0.01),
        "w_router_expert": nrm(ks[18], (L, D_MODEL, N_EXPERTS), D_MODEL ** -0.5),
        "b_router_expert": nrm(ks[19], (L, N_EXPERTS), 0.01),
        "w_exp_gate": nrm(ks[20], (L, N_EXPERTS, D_MODEL, D_EXPERT), D_MODEL ** -0.5),
        "w_exp_up": nrm(ks[21], (L, N_EXPERTS, D_MODEL, D_EXPERT), D_MODEL ** -0.5),
        "w_exp_down": nrm(ks[22], (L, N_EXPERTS, D_EXPERT, D_MODEL), DEEPNORM_BETA * D_EXPERT ** -0.5),
        "w_ple_proj": nrm(ks[23], (L, PLE_DIM, D_MODEL), DEEPNORM_BETA * PLE_DIM ** -0.5),
        "w_ple_gate": nrm(ks[24], (L, D_MODEL, D_MODEL), D_MODEL ** -0.5),
        "ln2_g": 1.0 + nrm(ks[25], (L, D_MODEL), 0.02),
        "ln2_b": nrm(ks[26], (L, D_MODEL), 0.02),
    }


def reference(x, p, w_in, b_forget, pool_w, pool_scale, dn_conv, dn_a_log, dn_dt_bias, dn_norm_w,
              w_br_attn, w_br_pool, w_br_dn, w_out, ln1_g, ln1_b, w_router_group, b_router_group,
              w_router_expert, b_router_expert, w_exp_gate, w_exp_up, w_exp_down, w_ple_proj,
              w_ple_gate, ln2_g, ln2_b):
    for i in range(DEPTH):
        mix = token_mixer(x, w_in[i], b_forget[i], pool_w[i], pool_scale[i], dn_conv[i], dn_a_log[i],
                          dn_dt_bias[i], dn_norm_w[i], w_br_attn[i], w_br_pool[i], w_br_dn[i], w_out[i])
        x = layer_norm(DEEPNORM_ALPHA * x + mix, ln1_g[i], ln1_b[i])
        moe = hierarchical_moe(x, w_router_group[i], b_router_group[i], w_router_expert[i],
                               b_router_expert[i], w_exp_gate[i], w_exp_up[i], w_exp_down[i])
        ple = jax.nn.sigmoid(x @ w_ple_gate[i]) * (p[i] @ w_ple_proj[i])
        x = layer_norm(DEEPNORM_ALPHA * x + moe + ple, ln2_g[i], ln2_b[i])
    return x
```

```python
import contextlib
import numpy as np
import concourse.bass as bass
import concourse.mybir as mybir
from concourse.bass_utils import run_bass_kernel_spmd

F32 = mybir.dt.float32
BF16 = mybir.dt.bfloat16
I32 = mybir.dt.int32
AF = mybir.ActivationFunctionType
ALU = mybir.AluOpType
AX = mybir.AxisListType

ENGS = ["tensor", "vector", "scalar", "gpsimd", "sync"]
SEM_LIMIT = 30000


class Prog:
    def __init__(self, nc, stack):
        self.nc = nc
        self.stack = stack
        self.stream = {e: [] for e in ENGS}
        self.cnt = {e: 0 for e in ENGS}
        self.nsem = 0
        self.sem = {e: self._newsem("p_" + e) for e in ENGS}
        self.waited = {e: {} for e in ENGS}
        self.lastw = {}
        self.readers = {}
        self.dsem = {}
        self.nops = 0

    def _newsem(self, name):
        self.nsem += 1
        return self.stack.enter_context(self.nc.semaphore(f"{name}_{self.nsem}"))

    def _need(self, eng, toks):
        need = {}
        for (sem, val, teng), kind in toks:
            if teng == eng and (kind != "raw" or eng == "tensor"):
                continue
            k = id(sem)
            if self.waited[eng].get(k, 0) >= val:
                continue
            if k not in need or need[k][1] < val:
                need[k] = (sem, val)
        for k, (sem, val) in need.items():
            self.waited[eng][k] = val
        return list(need.values())

    def _deps(self, eng, reads, writes):
        toks = []
        for r in reads:
            t = self.lastw.get(r)
            if t is not None:
                toks.append((t, "raw"))
        for w in writes:
            t = self.lastw.get(w)
            if t is not None:
                toks.append((t, "waw"))
            for t in self.readers.get(w, {}).values():
                toks.append((t, "war"))
        return self._need(eng, toks)

    def _update(self, tok, reads, writes):
        sem, val, teng = tok
        rk = teng if teng != "dma" else ("dma", id(sem))
        for r in reads:
            self.readers.setdefault(r, {})[rk] = tok
        for w in writes:
            self.lastw[w] = tok
            self.readers[w] = {}

    def op(self, eng, fn, reads=(), writes=()):
        waits = self._deps(eng, reads, writes)
        if self.cnt[eng] >= SEM_LIMIT:
            self.sem[eng] = self._newsem("p_" + eng)
            self.cnt[eng] = 0
        self.cnt[eng] += 1
        tok = (self.sem[eng], self.cnt[eng], eng)
        self.stream[eng].append((waits, fn, self.sem[eng], 1))
        self._update(tok, reads, writes)
        self.nops += 1
        return tok

    def dma(self, eng, key, fn, reads=(), writes=()):
        waits = self._deps(eng, reads, writes)
        s = self.dsem.get(key)
        if s is None or s[1] >= SEM_LIMIT:
            s = [self._newsem("d"), 0]
            self.dsem[key] = s
        s[1] += 16
        tok = (s[0], s[1], "dma")
        self.stream[eng].append((waits, fn, s[0], 16))
        self._update(tok, reads, writes)
        self.nops += 1
        return tok

    def barrier(self):
        toks = []
        for e in ENGS:
            if self.cnt[e] > 0:
                toks.append(((self.sem[e], self.cnt[e], e), "raw"))
        for k, s in self.dsem.items():
            toks.append(((s[0], s[1], "dma"), "raw"))
        for e in ENGS:
            waits = self._need(e, [t for t in toks if t[0][2] != e])
            if waits:
                self.stream[e].append((waits, None, None, 0))
        self.lastw = {}
        self.readers = {}

    def wait_all(self, eng):
        toks = []
        for e in ENGS:
            if self.cnt[e] > 0 and e != eng:
                toks.append(((self.sem[e], self.cnt[e], e), "raw"))
        for k, s in self.dsem.items():
            toks.append(((s[0], s[1], "dma"), "raw"))
        waits = self._need(eng, toks)
        if waits:
            self.stream[eng].append((waits, None, None, 0))

    def emit(self):
        with self.nc.Block() as block:
            for eng in ENGS:
                def body(e, eng=eng):
                    for (waits, fn, sem, inc) in self.stream[eng]:
                        for (s, v) in waits:
                            e.wait_ge(s, v)
                        if fn is not None:
                            fn(e).then_inc(sem, inc)
                getattr(block, eng)(body)


S = 4096
D = 1024
NT = S // 128
DEPTH = 4
ALPHA = (2 * DEPTH) ** 0.25
C_AQ, C_AK, C_AV, C_AF, C_PU, C_DQ, C_DK, C_DV, C_DA, C_DB, C_DG, C_GA, C_GP, C_GD = (
    0, 512, 1024, 1536, 1544, 2056, 2568, 3080, 3592, 3596, 3600, 4112, 5136, 6160)
INW = 7184


class Ctx:
    pass


DBG_OUT = set()
DBG_FLAGS = set()
NH_DBG = 8
NI_DBG = 16


_SBT_N = [0]


def sbt(nc, st, name, shape, dt):
    _SBT_N[0] += 1
    return st.enter_context(nc.sbuf_tensor(f"{name}_u{_SBT_N[0]}", shape, dt))


def declare_io(nc, layers, first, last):
    L = len(layers)
    c = Ctx()
    EI = "ExternalInput"
    c.x_in = nc.dram_tensor("x_in", [S, D], F32, kind=EI).ap()
    c.p = nc.dram_tensor("p", [L, S, 256], F32, kind=EI).ap()
    c.w_in = nc.dram_tensor("w_in", [L, D, INW], F32, kind=EI).ap()
    c.b_forget = nc.dram_tensor("b_forget", [L, 8, 1], F32, kind=EI).ap()
    c.pool_w = nc.dram_tensor("pool_w", [L, 4, 128, 128], F32, kind=EI).ap()
    c.pool_scale = nc.dram_tensor("pool_scale", [L, 4, 128, 1], F32, kind=EI).ap()
    c.dn_conv = nc.dram_tensor("dn_conv", [L, 4, 1536], F32, kind=EI).ap()
    c.dn_a_log = nc.dram_tensor("dn_a_log", [L, 4, 1], F32, kind=EI).ap()
    c.dn_dt_bias = nc.dram_tensor("dn_dt_bias", [L, 4, 1], F32, kind=EI).ap()
    c.dn_norm_w = nc.dram_tensor("dn_norm_w", [L, 128, 1], F32, kind=EI).ap()
    c.w_br = nc.dram_tensor("w_br", [L, 1536, D], F32, kind=EI).ap()
    c.w_out = nc.dram_tensor("w_out", [L, D, D], F32, kind=EI).ap()
    c.ln1_g = nc.dram_tensor("ln1_g", [L, D], F32, kind=EI).ap()
    c.ln1_b = nc.dram_tensor("ln1_b", [L, D], F32, kind=EI).ap()
    c.w_r = nc.dram_tensor("w_r", [L, D, 36], F32, kind=EI).ap()
    c.b_r = nc.dram_tensor("b_r", [L, 36], F32, kind=EI).ap()
    c.w_eg = nc.dram_tensor("w_eg", [L, 32, D, 512], F32, kind=EI).ap()
    c.w_eu = nc.dram_tensor("w_eu", [L, 32, D, 512], F32, kind=EI).ap()
    c.w_ed = nc.dram_tensor("w_ed", [L, 32, 512, D], F32, kind=EI).ap()
    c.w_pp = nc.dram_tensor("w_pp", [L, 256, D], F32, kind=EI).ap()
    c.w_pg = nc.dram_tensor("w_pg", [L, D, D], F32, kind=EI).ap()
    c.ln2_g = nc.dram_tensor("ln2_g", [L, D], F32, kind=EI).ap()
    c.ln2_b = nc.dram_tensor("ln2_b", [L, D], F32, kind=EI).ap()
    c.out = nc.dram_tensor("out", [S, D], F32, kind="ExternalOutput").ap()
    def scr(name, shape, dt):
        kind = "ExternalOutput" if name in DBG_OUT else "Internal"
        return nc.dram_tensor(name, shape, dt, kind=kind).ap()
    c.xs = scr("xs", [S, D], F32)
    c.x1s = scr("x1s", [S, D], F32)
    c.qkT = scr("qkT", [1024, S], BF16)
    c.vtm = scr("vtm", [S, 512], BF16)
    c.smT = scr("smT", [16, S], F32)
    c.fT = scr("fT", [2560, S], F32)
    c.yT = scr("yT", [1536, S], BF16)
    c.dbg1 = scr("dbg1", [128, 4096], F32)
    c.dnrow = scr("dnrow", [2, 4, S], F32)
    return c


def make_consts(nc, st, P):
    k = Ctx()
    k.ones = sbt(nc, st, "c_ones", [128, 128], F32)
    k.ident = sbt(nc, st, "c_ident", [128, 128], F32)
    k.identb = sbt(nc, st, "c_identb", [128, 128], BF16)
    k.onesb = sbt(nc, st, "c_onesb", [128, 128], BF16)
    k.ps = [st.enter_context(nc.psum_tensor(f"ps{i}", [128, 512], F32)) for i in range(8)]
    P.op("gpsimd", lambda e: e.memset(k.ones[:], 1.0), writes=["c_ones"])
    P.op("gpsimd", lambda e: e.affine_select(out=k.ident[:], in_=k.ones[:], pattern=[[-1, 128]],
                                             compare_op=ALU.is_equal, fill=0.0, base=0, channel_multiplier=1),
         reads=["c_ones"], writes=["c_ident"])
    P.op("vector", lambda e: e.tensor_copy(k.identb[:], k.ident[:]), reads=["c_ident"], writes=["c_identb"])
    P.op("vector", lambda e: e.tensor_copy(k.onesb[:], k.ones[:]), reads=["c_ones"], writes=["c_onesb"])
    return k


def build_xT(nc, P, k, st, src, xT, tag):
    xin = [sbt(nc, st, f"{tag}_xin{i}", [128, D], F32) for i in range(2)]
    for t in range(NT):
        b = xin[t % 2]
        bn = f"{tag}_xin{t % 2}"
        P.dma("sync", bn, lambda e, b=b, t=t: e.dma_start(out=b[:], in_=src[t * 128:(t + 1) * 128, :]), writes=[bn])
        for half in range(2):
            pb = k.ps[(t % 2) * 2 + half]
            pn = f"ps{(t % 2) * 2 + half}"
            for q in range(4):
                kc = half * 4 + q
                P.op("tensor", lambda e, pb=pb, b=b, kc=kc, q=q: e.transpose(pb[:, q * 128:(q + 1) * 128], b[:, kc * 128:(kc + 1) * 128], k.ident[:]),
                     reads=[bn, "c_ident"], writes=[pn])
            eng = "vector" if half == 0 else "scalar"
            if eng == "vector":
                P.op("vector", lambda e, pb=pb, half=half, t=t: e.tensor_copy(
                    xT[:, half * 4:(half + 1) * 4, t * 128:(t + 1) * 128], pb[:].rearrange("p (a b) -> p a b", a=4)),
                    reads=[pn], writes=[f"{tag}_xT{t}_{half}"])
            else:
                P.op("scalar", lambda e, pb=pb, half=half, t=t: e.copy(
                    xT[:, half * 4:(half + 1) * 4, t * 128:(t + 1) * 128], pb[:].rearrange("p (a b) -> p a b", a=4)),
                    reads=[pn], writes=[f"{tag}_xT{t}_{half}"])


def stage1_proj(nc, P, k, c, li, src):
    with contextlib.ExitStack() as st:
        xT = sbt(nc, st, "s1_xT", [128, 8, S], BF16)
        build_xT(nc, P, k, st, src, xT, "s1")
        vstg = sbt(nc, st, "s1_vstg", [128, 8 * 512], BF16)
        wb = [sbt(nc, st, f"s1_w{i}", [128, 8, 512], BF16) for i in range(2)]
        stg = [sbt(nc, st, f"s1_stg{i}", [128, S], F32) for i in range(2)]
        stgb = [sbt(nc, st, f"s1_stgb{i}", [128, S], BF16) for i in range(2)]
        wv = c.w_in[li].rearrange("(kc kp) n -> kp kc n", kp=128)
        groups = [(C_AQ, 512, "q"), (C_AK, 512, "k"), (C_PU, 512, "f0"), (C_DQ, 512, "f1"), (C_DK, 512, "f2"),
                  (C_DV, 512, "f3"), (C_DG, 512, "f4"), (C_AF, 16, "sm0"), (C_DA, 8, "sm1"), (C_AV, 512, "v")]
        gi = 0
        nev = 0
        nst = 0
        for (c0, ncols, kind) in groups:
            w = wb[gi % 2]
            wn = f"s1_w{gi % 2}"
            gi += 1
            if kind == "sm0":
                P.dma("gpsimd", wn, lambda e, w=w: e.dma_start(out=w[:, :, 0:8], in_=wv[:, :, C_AF:C_AF + 8]), writes=[wn])
                P.dma("gpsimd", wn, lambda e, w=w: e.dma_start(out=w[:, :, 8:16], in_=wv[:, :, C_DA:C_DA + 8]), writes=[wn])
            elif kind == "sm1":
                gi -= 1
                continue
            else:
                P.dma("gpsimd", wn, lambda e, w=w, c0=c0, ncols=ncols: e.dma_start(out=w[:, :, 0:ncols], in_=wv[:, :, c0:c0 + ncols]), writes=[wn])
            if kind == "v":
                for t in range(NT):
                    pb = k.ps[4 + t % 4]
                    pn = f"ps{4 + t % 4}"
                    for kc in range(8):
                        P.op("tensor", lambda e, pb=pb, kc=kc, t=t, w=w: e.matmul(pb[:, :], lhsT=xT[:, kc, t * 128:(t + 1) * 128], rhs=w[:, kc, :], start=(kc == 0), stop=(kc == 7)),
                             reads=[wn, f"s1_xT{t}_{kc // 4}"], writes=[pn])
                    vslot = t % 8
                    eng = "vector" if t % 2 == 0 else "scalar"
                    fn = (lambda e, pb=pb, vslot=vslot: e.tensor_copy(vstg[:, vslot * 512:(vslot + 1) * 512], pb[:, :])) if eng == "vector" else \
                         (lambda e, pb=pb, vslot=vslot: e.copy(vstg[:, vslot * 512:(vslot + 1) * 512], pb[:, :]))
                    P.op(eng, fn, reads=[pn], writes=[f"s1_vstg_{vslot}"])
                    if vslot == 7:
                        t0 = t - 7
                        P.dma("sync", "s1_vout", lambda e, t0=t0: e.dma_start(
                            out=c.vtm[t0 * 128:(t0 + 8) * 128, :].rearrange("(a p) n -> p a n", p=128),
                            in_=vstg[:, :].rearrange("p (a n) -> p a n", a=8)),
                            reads=[f"s1_vstg_{v}" for v in range(8)])
                continue
            nch = (ncols + 127) // 128
            for ch in range(nch):
                m = min(128, ncols - ch * 128)
                isb = kind in ("q", "k")
                sbuf = (stgb if isb else stg)[nst % 2]
                sname = ("s1_stgb" if isb else "s1_stg") + str(nst % 2)
                nst += 1
                for tc in range(8):
                    pb = k.ps[4 + nev % 4]
                    pn = f"ps{4 + nev % 4}"
                    for kc in range(8):
                        P.op("tensor", lambda e, pb=pb, kc=kc, tc=tc, w=w, ch=ch, m=m: e.matmul(
                            pb[0:m, :], lhsT=w[:, kc, ch * 128:ch * 128 + m], rhs=xT[:, kc, tc * 512:(tc + 1) * 512], start=(kc == 0), stop=(kc == 7)),
                            reads=[wn] + [f"s1_xT{tt}_{kc // 4}" for tt in range(tc * 4, tc * 4 + 4)], writes=[pn])
                    sc = 0.125 if kind == "q" else 1.0
                    if nev % 2 == 0:
                        P.op("vector", lambda e, pb=pb, tc=tc, sbuf=sbuf, m=m, sc=sc: e.tensor_scalar(
                            out=sbuf[0:m, tc * 512:(tc + 1) * 512], in0=pb[0:m, :], scalar1=sc, scalar2=None, op0=ALU.mult),
                            reads=[pn], writes=[sname])
                    else:
                        P.op("scalar", lambda e, pb=pb, tc=tc, sbuf=sbuf, m=m, sc=sc: e.mul(
                            sbuf[0:m, tc * 512:(tc + 1) * 512], pb[0:m, :], sc),
                            reads=[pn], writes=[sname])
                    nev += 1
                if kind == "q":
                    dst = c.qkT[ch * 128:(ch + 1) * 128, :]
                elif kind == "k":
                    dst = c.qkT[512 + ch * 128:512 + (ch + 1) * 128, :]
                elif kind == "sm0":
                    dst = c.smT[0:16, :]
                else:
                    fi = int(kind[1])
                    dst = c.fT[fi * 512 + ch * 128:fi * 512 + (ch + 1) * 128, :]
                P.dma("sync", "s1_out", lambda e, dst=dst, sbuf=sbuf, m=m: e.dma_start(out=dst, in_=sbuf[0:m, :]), reads=[sname])
    P.barrier()


def stage2_attn(nc, P, k, c, li):
    with contextlib.ExitStack() as st:
        qT = sbt(nc, st, "s2_q", [128, 4, S], BF16)
        kT = sbt(nc, st, "s2_k", [128, 4, S], BF16)
        va = sbt(nc, st, "s2_v", [128, NT, 8, 65], BF16)
        af = sbt(nc, st, "s2_af", [8, S], F32)
        cc = sbt(nc, st, "s2_cc", [8, S], F32)
        ones8 = sbt(nc, st, "s2_ones8", [8, S], F32)
        nb = sbt(nc, st, "s2_nb", [8, 1], F32)
        ck = sbt(nc, st, "s2_ck", [128, NT, 8], F32)
        rball = sbt(nc, st, "s2_rb", [128, NT, 8], F32)
        sel0 = sbt(nc, st, "s2_sel0", [128, 128], F32)
        biasb = [sbt(nc, st, f"s2_bias{i}", [128, NT], F32) for i in range(4)]
        PT = [sbt(nc, st, f"s2_PT{i}", [128, 256], BF16) for i in range(6)]
        rden = sbt(nc, st, "s2_rden", [128, 256], F32)
        bc = sbt(nc, st, "s2_bc", [64, 256], F32)
        ystg = [sbt(nc, st, f"s2_y{i}", [64, S], BF16) for i in range(2)]
        for pr in range(4):
            P.dma("sync", f"s2_q{pr}", lambda e, pr=pr: e.dma_start(out=qT[:, pr, :], in_=c.qkT[pr * 128:(pr + 1) * 128, :]), reads=["d_qkT"], writes=[f"s2_q{pr}"])
            P.dma("sync", f"s2_k{pr}", lambda e, pr=pr: e.dma_start(out=kT[:, pr, :], in_=c.qkT[512 + pr * 128:512 + (pr + 1) * 128, :]), reads=["d_qkT"], writes=[f"s2_k{pr}"])
        P.op("gpsimd", lambda e: e.memset(va[:, :, :, 64:65], 1.0), writes=["s2_v1"])
        vsrc = c.vtm.rearrange("(t p) (h d) -> p t h d", p=128, h=8)
        for g in range(NT):
            P.dma("sync", "s2_v", lambda e, g=g: e.dma_start(out=va[:, g, :, 0:64], in_=vsrc[:, g, :, :]), reads=["d_vtm"], writes=["s2_v"])
        P.dma("sync", "s2_af", lambda e: e.dma_start(out=af[:], in_=c.smT[0:8, :]), writes=["s2_af"])
        P.dma("sync", "s2_nb", lambda e: e.dma_start(out=nb[:], in_=c.b_forget[li]), writes=["s2_nb"])
        P.op("gpsimd", lambda e: e.memset(ones8[:], 1.0), writes=["s2_ones8"])
        P.op("gpsimd", lambda e: e.memset(sel0[:], 0.0), writes=["s2_sel0"])
        P.op("gpsimd", lambda e: e.memset(sel0[0:1, :], 1.0), writes=["s2_sel0"])
        P.op("vector", lambda e: e.tensor_scalar(out=nb[:], in0=nb[:], scalar1=-1.0, scalar2=None, op0=ALU.mult), reads=["s2_nb"], writes=["s2_nb"])
        P.op("scalar", lambda e: e.activation(out=af[:], in_=af[:], func=AF.Exp, bias=nb[:, 0:1], scale=-1.0), reads=["s2_af", "s2_nb"], writes=["s2_af"])
        P.op("scalar", lambda e: e.activation(out=af[:], in_=af[:], func=AF.Ln, bias=k.ones[0:8, 0:1], scale=1.0), reads=["s2_af", "c_ones"], writes=["s2_af"])
        P.op("vector", lambda e: e.tensor_scalar(out=af[:], in0=af[:], scalar1=-1.0, scalar2=None, op0=ALU.mult), reads=["s2_af"], writes=["s2_af"])
        P.op("vector", lambda e: e.tensor_tensor_scan(out=cc[:], data0=ones8[:], data1=af[:], initial=0.0, op0=ALU.mult, op1=ALU.add),
             reads=["s2_af", "s2_ones8"], writes=["s2_cc"])
        for j in range(NT):
            P.op("tensor", lambda e, j=j: e.transpose(k.ps[7][:, j * 8:(j + 1) * 8], cc[0:8, j * 128:(j + 1) * 128], k.ident[0:8, 0:8]),
                 reads=["s2_cc", "c_ident"], writes=["ps7"])
        P.op("vector", lambda e: e.tensor_copy(ck[:].rearrange("p a b -> p (a b)"), k.ps[7][:, 0:256]), reads=["ps7"], writes=["s2_ck"])
        P.op("tensor", lambda e: e.matmul(k.ps[7][:, 256:512], lhsT=sel0[:], rhs=ck[:].rearrange("p a b -> p (a b)"), start=True, stop=True),
             reads=["s2_ck", "s2_sel0"], writes=["ps7"])
        P.op("vector", lambda e: e.tensor_copy(rball[:].rearrange("p a b -> p (a b)"), k.ps[7][:, 256:512]), reads=["ps7"], writes=["s2_rb"])

        if "s2_setup" in DBG_FLAGS:
            P.dma("sync", "dbgo", lambda e: e.dma_start(out=c.dbg1[:, 0:256], in_=ck[:].rearrange("p a b -> p (a b)")), reads=["s2_ck"])
            P.dma("sync", "dbgo", lambda e: e.dma_start(out=c.dbg1[:, 256:512], in_=rball[:].rearrange("p a b -> p (a b)")), reads=["s2_rb"])
            P.barrier()
            return
        units = [(h, i, j) for h in range(NH_DBG) for i in range(NI_DBG) for j in range(2 * i + 2)]

        def issue_S(n):
            h, i, j = units[n]
            slot = n % 4
            pb = k.ps[slot][:, 0:256]
            hp, hh = h // 2, h % 2
            bb = biasb[(h * 16 + i) % 4]
            bn = f"s2_bias{(h * 16 + i) % 4}"
            if j == 0:
                nj = 2 * i + 2
                P.op("vector", lambda e: e.tensor_scalar(out=bb[:, 0:nj], in0=ck[:, 0:nj, h], scalar1=rball[:, 2 * i + 1, h:h + 1], scalar2=-1.0,
                                                         op0=ALU.subtract, op1=ALU.mult), reads=["s2_ck", "s2_rb"], writes=[bn])
            P.op("tensor", lambda e: e.matmul(pb, lhsT=kT[hh * 64:(hh + 1) * 64, hp, j * 128:(j + 1) * 128],
                                              rhs=qT[hh * 64:(hh + 1) * 64, hp, i * 256:(i + 1) * 256], start=True, stop=True),
                 reads=[f"s2_k{hp}", f"s2_q{hp}"], writes=[f"psS{slot}"])
            pt = PT[n % 6]
            ptn = f"s2_PT{n % 6}"
            P.op("scalar", lambda e: e.activation(out=pt[:], in_=pb, func=AF.Exp, bias=bb[:, j:j + 1], scale=1.0),
                 reads=[f"psS{slot}", bn], writes=[ptn])
            if j >= 2 * i:
                P.op("gpsimd", lambda e: e.affine_select(out=pt[:], in_=pt[:], pattern=[[1, 256]], compare_op=ALU.is_ge, fill=0.0,
                                                         base=i * 256 - j * 128, channel_multiplier=-1), reads=[ptn], writes=[ptn])

        def fin_a(h, i):
            ob = k.ps[4 + (h * 16 + i) % 2]
            on = f"ps{4 + (h * 16 + i) % 2}"
            P.op("vector", lambda e: e.reciprocal(rden[64:65, :], ob[64:65, 0:256]), reads=[on], writes=["s2_rden"])

        def fin_b(h, i):
            ob = k.ps[4 + (h * 16 + i) % 2]
            on = f"ps{4 + (h * 16 + i) % 2}"
            P.op("tensor", lambda e: e.matmul(k.ps[6][0:64, 0:256], lhsT=k.ones[64:65, 0:64], rhs=rden[64:65, :], start=True, stop=True),
                 reads=["s2_rden", "c_ones"], writes=["ps6"])
            P.op("scalar", lambda e: e.copy(bc[:], k.ps[6][0:64, 0:256]), reads=["ps6"], writes=["s2_bc"])
            ys = ystg[h % 2]
            P.op("vector", lambda e: e.tensor_tensor(out=ys[:, i * 256:(i + 1) * 256], in0=ob[0:64, 0:256], in1=bc[:], op=ALU.mult),
                 reads=[on, "s2_bc"], writes=[f"s2_y{h % 2}"])
            if i == NI_DBG - 1:
                P.dma("sync", "s2_yout", lambda e: e.dma_start(out=c.yT[h * 64:(h + 1) * 64, :], in_=ys[:]), reads=[f"s2_y{h % 2}"], writes=["d_yT"])

        pending = []
        issue_S(0)
        issue_S(1)
        for n in range(len(units)):
            if n + 2 < len(units):
                issue_S(n + 2)
            h, i, j = units[n]
            ob = k.ps[4 + (h * 16 + i) % 2]
            on = f"ps{4 + (h * 16 + i) % 2}"
            pt = PT[n % 6]
            P.op("tensor", lambda e, ob=ob, pt=pt, h=h, i=i, j=j: e.matmul(ob[0:65, 0:256], lhsT=va[:, j, h, :], rhs=pt[:], start=(j == 0), stop=(j == 2 * i + 1)),
                 reads=[f"s2_PT{n % 6}", "s2_v", "s2_v1"], writes=[on])
            for (hh_, ii_) in pending:
                fin_b(hh_, ii_)
            pending = []
            if j == 2 * i + 1:
                fin_a(h, i)
                pending.append((h, i))
        for (hh_, ii_) in pending:
            fin_b(hh_, ii_)
    P.barrier()


def stage3_pool(nc, P, k, c, li):
    with contextlib.ExitStack() as st:
        u = [sbt(nc, st, f"s3_u{i}", [128, S], F32) for i in range(2)]
        ab = [sbt(nc, st, f"s3_a{i}", [128, S], F32) for i in range(2)]
        dbf = sbt(nc, st, "s3_d", [128, S], BF16)
        ys = [sbt(nc, st, f"s3_y{i}", [128, S], BF16) for i in range(2)]
        pw = sbt(nc, st, "s3_pw", [128, 4, 128], BF16)
        psc = sbt(nc, st, "s3_psc", [128, 4], F32)
        inv = sbt(nc, st, "s3_inv", [128, 4, 16], F32)
        tmp = sbt(nc, st, "s3_tmp", [128, 16], F32)
        P.dma("gpsimd", "s3_pw", lambda e: e.dma_start(out=pw[:], in_=c.pool_w[li].rearrange("g c d -> c g d")), writes=["s3_pw"])
        for g in range(4):
            P.dma("sync", "s3_psc", lambda e, g=g: e.dma_start(out=psc[:, g:g + 1], in_=c.pool_scale[li, g]), writes=["s3_psc"])
        for g in range(4):
            w = 2 ** (g + 1)
            P.op("gpsimd", lambda e, g=g: e.iota(inv[:, g, :], pattern=[[1, 16]], base=1, channel_multiplier=0, allow_small_or_imprecise_dtypes=True), writes=["s3_inv"])
            P.op("gpsimd", lambda e, g=g, w=w: e.tensor_scalar(out=inv[:, g, :], in0=inv[:, g, :], scalar1=float(w), scalar2=None, op0=ALU.min), reads=["s3_inv"], writes=["s3_inv"])
        P.op("vector", lambda e: e.reciprocal(inv[:].rearrange("p a b -> p (a b)"), inv[:].rearrange("p a b -> p (a b)")), reads=["s3_inv"], writes=["s3_inv"])
        nev = 0
        for g in range(4):
            w = 2 ** (g + 1)
            ug = u[g % 2]
            un = f"s3_u{g % 2}"
            P.dma("sync", un, lambda e, g=g, ug=ug: e.dma_start(out=ug[:], in_=c.fT[g * 128:(g + 1) * 128, :]), writes=[un])
            src, srcn = ug, un
            for m in range(g + 1):
                sh = 2 ** m
                dst, dstn = ab[m % 2], f"s3_a{m % 2}"
                P.op("vector", lambda e, dst=dst, src=src, sh=sh: e.tensor_tensor(out=dst[:, sh:], in0=src[:, sh:], in1=src[:, 0:S - sh], op=ALU.add), reads=[srcn], writes=[dstn])
                P.op("gpsimd", lambda e, dst=dst, src=src, sh=sh: e.tensor_copy(dst[:, 0:sh], src[:, 0:sh]), reads=[srcn, dstn], writes=[dstn])
                src, srcn = dst, dstn
            rs = [srcn]
            P.op("vector", lambda e, src=src, ug=ug, w=w: e.scalar_tensor_tensor(out=dbf[:], in0=src[:], scalar=1.0 / w, in1=ug[:], op0=ALU.mult, op1=ALU.subtract),
                 reads=rs + [un], writes=["s3_d"])
            P.op("vector", lambda e, src=src, g=g, w=w: e.tensor_tensor(out=tmp[:, 0:w], in0=src[:, 0:w], in1=inv[:, g, 0:w], op=ALU.mult), reads=rs + ["s3_inv"], writes=["s3_tmp"])
            P.op("vector", lambda e, ug=ug, w=w: e.tensor_tensor(out=dbf[:, 0:w], in0=tmp[:, 0:w], in1=ug[:, 0:w], op=ALU.subtract), reads=["s3_tmp", un, "s3_d"], writes=["s3_d"])
            yb, yn = ys[g % 2], f"s3_y{g % 2}"
            for tc in range(8):
                pb, pn = k.ps[nev % 4], f"ps{nev % 4}"
                nev += 1
                P.op("tensor", lambda e, pb=pb, g=g, tc=tc: e.matmul(pb[:, :], lhsT=pw[:, g, :], rhs=dbf[:, tc * 512:(tc + 1) * 512], start=True, stop=True),
                     reads=["s3_pw", "s3_d"], writes=[pn])
                P.op("scalar", lambda e, pb=pb, g=g, tc=tc, yb=yb: e.activation(out=yb[:, tc * 512:(tc + 1) * 512], in_=pb[:, :], func=AF.Copy, scale=psc[:, g:g + 1]),
                     reads=[pn, "s3_psc"], writes=[yn])
            P.dma("sync", "s3_yout", lambda e, g=g, yb=yb: e.dma_start(out=c.yT[512 + g * 128:512 + (g + 1) * 128, :], in_=yb[:]), reads=[yn])
    P.barrier()


def layer_norm_tile(P, k, z, zn, gam, bet, stats, mv, rstd, out, outn, eps_ap, tag):
    for hh in range(2):
        P.op("vector", lambda e, hh=hh: e.bn_stats(stats[:, hh, :], z[:, hh * 512:(hh + 1) * 512]), reads=[zn], writes=[tag + "_st"])
    P.op("vector", lambda e: e.bn_aggr(mv[:], stats[:]), reads=[tag + "_st"], writes=[tag + "_mv"])
    P.op("scalar", lambda e: e.activation(out=rstd[:], in_=mv[:, 1:2], func=AF.Sqrt, bias=eps_ap, scale=1.0), reads=[tag + "_mv"], writes=[tag + "_rs"])
    P.op("vector", lambda e: e.reciprocal(rstd[:], rstd[:]), reads=[tag + "_rs"], writes=[tag + "_rs"])
    P.op("vector", lambda e: e.tensor_scalar(out=z[:], in0=z[:], scalar1=mv[:, 0:1], scalar2=rstd[:, 0:1], op0=ALU.subtract, op1=ALU.mult),
         reads=[zn, tag + "_mv", tag + "_rs"], writes=[zn])
    P.op("gpsimd", lambda e: e.tensor_tensor(out=z[:], in0=z[:], in1=gam[:], op=ALU.mult), reads=[zn, "lnp"], writes=[zn])
    P.op("gpsimd", lambda e: e.tensor_tensor(out=out[:], in0=z[:], in1=bet[:], op=ALU.add), reads=[zn, "lnp"], writes=[outn])


def stage5_merge(nc, P, k, c, li, src):
    with contextlib.ExitStack() as st:
        wg = sbt(nc, st, "s5_wg", [128, 8, 3072], BF16)
        wbr = sbt(nc, st, "s5_wbr", [128, 12, 1024], BF16)
        wo = sbt(nc, st, "s5_wo", [128, 8, 1024], BF16)
        gam = sbt(nc, st, "s5_gam", [128, D], F32)
        bet = sbt(nc, st, "s5_bet", [128, D], F32)
        eps = sbt(nc, st, "s5_eps", [128, 1], F32)
        xr = sbt(nc, st, "s5_xr", [128, 4, D], F32)
        xTc = sbt(nc, st, "s5_xTc", [128, 8, 512], BF16)
        yTc = sbt(nc, st, "s5_yTc", [128, 12, 512], BF16)
        sig = [sbt(nc, st, f"s5_sig{i}", [128, 512], F32) for i in range(2)]
        macc = sbt(nc, st, "s5_macc", [128, 512], F32)
        mtmp = sbt(nc, st, "s5_mtmp", [128, 512], F32)
        mT = sbt(nc, st, "s5_mT", [128, 8, 512], BF16)
        z = [sbt(nc, st, f"s5_z{i}", [128, D], F32) for i in range(2)]
        xo = [sbt(nc, st, f"s5_xo{i}", [128, D], F32) for i in range(2)]
        stats = sbt(nc, st, "s5_stats", [128, 2, 6], F32)
        mv = sbt(nc, st, "s5_mv", [128, 2], F32)
        rstd = sbt(nc, st, "s5_rstd", [128, 1], F32)
        wv = c.w_in[li].rearrange("(kc kp) n -> kp kc n", kp=128)
        for q in range(6):
            P.dma("gpsimd", "s5_wg", lambda e, q=q: e.dma_start(out=wg[:, :, q * 512:(q + 1) * 512], in_=wv[:, :, C_GA + q * 512:C_GA + (q + 1) * 512]), writes=["s5_wg"])
        wbv = c.w_br[li].rearrange("(kc kp) n -> kp kc n", kp=128)
        for q in range(3):
            P.dma("gpsimd", "s5_wbr", lambda e, q=q: e.dma_start(out=wbr[:, q * 4:(q + 1) * 4, :], in_=wbv[:, q * 4:(q + 1) * 4, :]), writes=["s5_wbr"])
        wov = c.w_out[li].rearrange("(kc kp) n -> kp kc n", kp=128)
        for q in range(2):
            P.dma("gpsimd", "s5_wo", lambda e, q=q: e.dma_start(out=wo[:, q * 4:(q + 1) * 4, :], in_=wov[:, q * 4:(q + 1) * 4, :]), writes=["s5_wo"])
        P.dma("sync", "lnp", lambda e: e.dma_start(out=gam[:], in_=c.ln1_g[li].partition_broadcast(128)), writes=["lnp"])
        P.dma("sync", "lnp", lambda e: e.dma_start(out=bet[:], in_=c.ln1_b[li].partition_broadcast(128)), writes=["lnp"])
        P.op("vector", lambda e: e.memset(eps[:], 1e-5), writes=["s5_eps"])
        nps = 0
        for tc in range(8):
            for tt in range(4):
                t = tc * 4 + tt
                P.dma("sync", "s5_xr", lambda e, t=t, tt=tt: e.dma_start(out=xr[:, tt, :], in_=src[t * 128:(t + 1) * 128, :]), writes=[f"s5_xr{tt}"])
            P.dma("sync", "s5_yTc", lambda e, tc=tc: e.dma_start(out=yTc[:], in_=c.yT[:, tc * 512:(tc + 1) * 512].rearrange("(a p) n -> p a n", p=128)), writes=["s5_yTc"])
            for tt in range(4):
                for half in range(2):
                    pb, pn = k.ps[nps % 8], f"ps{nps % 8}"
                    nps += 1
                    for q in range(4):
                        kc = half * 4 + q
                        P.op("tensor", lambda e, pb=pb, tt=tt, kc=kc, q=q: e.transpose(pb[:, q * 128:(q + 1) * 128], xr[:, tt, kc * 128:(kc + 1) * 128], k.ident[:]),
                             reads=[f"s5_xr{tt}", "c_ident"], writes=[pn])
                    fn = (lambda e, pb=pb, half=half, tt=tt: e.tensor_copy(xTc[:, half * 4:(half + 1) * 4, tt * 128:(tt + 1) * 128], pb[:].rearrange("p (a b) -> p a b", a=4))) if half == 0 else \
                         (lambda e, pb=pb, half=half, tt=tt: e.copy(xTc[:, half * 4:(half + 1) * 4, tt * 128:(tt + 1) * 128], pb[:].rearrange("p (a b) -> p a b", a=4)))
                    P.op("vector" if half == 0 else "scalar", fn, reads=[pn], writes=[f"s5_xTc{tt}_{half}"])
            xres = [f"s5_xTc{tt}_{h}" for tt in range(4) for h in range(2)]
            for n in range(8):
                for br in range(3):
                    pg, pgn = k.ps[nps % 8], f"ps{nps % 8}"
                    nps += 1
                    pbr, pbn = k.ps[nps % 8], f"ps{nps % 8}"
                    nps += 1
                    for kc in range(8):
                        P.op("tensor", lambda e, pg=pg, kc=kc, br=br, n=n: e.matmul(pg[:, :], lhsT=wg[:, kc, br * 1024 + n * 128:br * 1024 + (n + 1) * 128], rhs=xTc[:, kc, :], start=(kc == 0), stop=(kc == 7)),
                             reads=["s5_wg"] + xres, writes=[pgn])
                    sg, sgn = sig[(n * 3 + br) % 2], f"s5_sig{(n * 3 + br) % 2}"
                    P.op("scalar", lambda e, pg=pg, sg=sg: e.activation(out=sg[:], in_=pg[:, :], func=AF.Sigmoid), reads=[pgn], writes=[sgn])
                    for c4 in range(4):
                        P.op("tensor", lambda e, pbr=pbr, c4=c4, br=br, n=n: e.matmul(pbr[:, :], lhsT=wbr[:, br * 4 + c4, n * 128:(n + 1) * 128], rhs=yTc[:, br * 4 + c4, :], start=(c4 == 0), stop=(c4 == 3)),
                             reads=["s5_wbr", "s5_yTc"], writes=[pbn])
                    if br == 0:
                        P.op("vector", lambda e, pbr=pbr, sg=sg: e.tensor_tensor(out=macc[:], in0=pbr[:, :], in1=sg[:], op=ALU.mult), reads=[pbn, sgn], writes=["s5_macc"])
                    elif br == 1:
                        P.op("vector", lambda e, pbr=pbr, sg=sg: e.tensor_tensor(out=mtmp[:], in0=pbr[:, :], in1=sg[:], op=ALU.mult), reads=[pbn, sgn], writes=["s5_mtmp"])
                        P.op("gpsimd", lambda e: e.tensor_tensor(out=macc[:], in0=macc[:], in1=mtmp[:], op=ALU.add), reads=["s5_macc", "s5_mtmp"], writes=["s5_macc"])
                    else:
                        P.op("vector", lambda e, pbr=pbr, sg=sg: e.tensor_tensor(out=mtmp[:], in0=pbr[:, :], in1=sg[:], op=ALU.mult), reads=[pbn, sgn], writes=["s5_mtmp"])
                        P.op("gpsimd", lambda e, n=n: e.tensor_tensor(out=mT[:, n, :], in0=macc[:], in1=mtmp[:], op=ALU.add), reads=["s5_macc", "s5_mtmp"], writes=[f"s5_mT{n}"])
            mres = [f"s5_mT{n}" for n in range(8)]
            for tt in range(4):
                t = tc * 4 + tt
                zb, zn = z[t % 2], f"s5_z{t % 2}"
                for nh in range(2):
                    po, pon = k.ps[nps % 8], f"ps{nps % 8}"
                    nps += 1
                    for kc in range(8):
                        P.op("tensor", lambda e, po=po, kc=kc, tt=tt, nh=nh: e.matmul(po[:, :], lhsT=mT[:, kc, tt * 128:(tt + 1) * 128], rhs=wo[:, kc, nh * 512:(nh + 1) * 512], start=(kc == 0), stop=(kc == 7)),
                             reads=["s5_wo"] + mres, writes=[pon])
                    P.op("vector", lambda e, po=po, zb=zb, tt=tt, nh=nh: e.scalar_tensor_tensor(out=zb[:, nh * 512:(nh + 1) * 512], in0=xr[:, tt, nh * 512:(nh + 1) * 512], scalar=ALPHA, in1=po[:, :], op0=ALU.mult, op1=ALU.add),
                         reads=[pon, f"s5_xr{tt}"], writes=[zn])
                ob, on = xo[t % 2], f"s5_xo{t % 2}"
                layer_norm_tile(P, k, zb, zn, gam, bet, stats, mv, rstd, ob, on, eps[:, 0:1], "s5")
                P.dma("sync", "s5_out", lambda e, t=t, ob=ob: e.dma_start(out=c.x1s[t * 128:(t + 1) * 128, :], in_=ob[:]), reads=[on])
    P.barrier()


def stage4_zero(nc, P, k, c, li):
    with contextlib.ExitStack() as st:
        zt = sbt(nc, st, "s4_z", [128, S], BF16)
        P.op("gpsimd", lambda e: e.memset(zt[:], 0.0), writes=["s4_z"])
        for h in range(4):
            P.dma("sync", "s4_out", lambda e, h=h: e.dma_start(out=c.yT[1024 + h * 128:1024 + (h + 1) * 128, :], in_=zt[:]), reads=["s4_z"])
    P.barrier()


def stage6_moe(nc, P, k, c, li, dst):
    with contextlib.ExitStack() as st:
        wr = sbt(nc, st, "s6_wr", [128, 8, 36], F32)
        brb = sbt(nc, st, "s6_brb", [128, 36], F32)
        wpg = sbt(nc, st, "s6_wpg", [128, 8, D], BF16)
        wpp = sbt(nc, st, "s6_wpp", [128, 2, D], BF16)
        gam = sbt(nc, st, "s6_gam", [128, D], F32)
        bet = sbt(nc, st, "s6_bet", [128, D], F32)
        eps = sbt(nc, st, "s6_eps", [128, 1], F32)
        acc = sbt(nc, st, "s6_acc", [128, 8, D], F32)
        x1T = sbt(nc, st, "s6_x1T", [128, 8, 1024], BF16)
        xTf = sbt(nc, st, "s6_xTf", [128, 8, 128], F32)
        xin = [sbt(nc, st, f"s6_xin{i}", [128, D], F32) for i in range(2)]
        wgu = [sbt(nc, st, f"s6_wgu{i}", [128, 8, 1024], BF16) for i in range(2)]
        wd = [sbt(nc, st, f"s6_wd{i}", [128, 4, D], BF16) for i in range(2)]
        hT = [sbt(nc, st, f"s6_hT{i}", [128, 4, 512], BF16) for i in range(2)]
        sgl = [sbt(nc, st, f"s6_sg{i}", [128, 512], F32) for i in range(2)]
        lg = sbt(nc, st, "s6_lg", [128, 8, 36], F32)
        G = sbt(nc, st, "s6_G", [128, 8, 32], F32)
        sm = sbt(nc, st, "s6_sm", [128, 16], F32)
        t4 = sbt(nc, st, "s6_t4", [128, 4], F32)
        pen = sbt(nc, st, "s6_pen", [128, 4], F32)
        mk = sbt(nc, st, "s6_mk", [128, 32], F32)
        mk2 = sbt(nc, st, "s6_mk2", [128, 32], F32)
        oh = sbt(nc, st, "s6_oh", [128, 32], F32)
        pin = sbt(nc, st, "s6_pin", [128, 256], F32)
        pT = sbt(nc, st, "s6_pT", [128, 2, 128], BF16)
        sgt = sbt(nc, st, "s6_sgt", [128, D], F32)
        z = [sbt(nc, st, f"s6_z{i}", [128, D], F32) for i in range(2)]
        xo = [sbt(nc, st, f"s6_xo{i}", [128, D], F32) for i in range(2)]
        stats = sbt(nc, st, "s6_stats", [128, 2, 6], F32)
        mv = sbt(nc, st, "s6_mv", [128, 2], F32)
        rstd = sbt(nc, st, "s6_rstd", [128, 1], F32)
        P.dma("sync", "s6_wr", lambda e: e.dma_start(out=wr[:], in_=c.w_r[li].rearrange("(kc kp) n -> kp kc n", kp=128)), writes=["s6_wr"])
        P.dma("sync", "s6_brb", lambda e: e.dma_start(out=brb[:], in_=c.b_r[li].partition_broadcast(128)), writes=["s6_brb"])
        wpgv = c.w_pg[li].rearrange("(kc kp) n -> kp kc n", kp=128)
        for q in range(2):
            P.dma("gpsimd", "s6_wpg", lambda e, q=q: e.dma_start(out=wpg[:, q * 4:(q + 1) * 4, :], in_=wpgv[:, q * 4:(q + 1) * 4, :]), writes=["s6_wpg"])
        P.dma("gpsimd", "s6_wpp", lambda e: e.dma_start(out=wpp[:], in_=c.w_pp[li].rearrange("(kc kp) n -> kp kc n", kp=128)), writes=["s6_wpp"])
        P.dma("sync", "lnp", lambda e: e.dma_start(out=gam[:], in_=c.ln2_g[li].partition_broadcast(128)), writes=["lnp"])
        P.dma("sync", "lnp", lambda e: e.dma_start(out=bet[:], in_=c.ln2_b[li].partition_broadcast(128)), writes=["lnp"])
        P.op("vector", lambda e: e.memset(eps[:], 1e-5), writes=["s6_eps"])
        nps = 0
        nw = 0
        for qt in range(4):
            for tt in range(8):
                t = qt * 8 + tt
                xb, xn = xin[t % 2], f"s6_xin{t % 2}"
                P.dma("sync", xn, lambda e, xb=xb, t=t: e.dma_start(out=xb[:], in_=c.x1s[t * 128:(t + 1) * 128, :]), writes=[xn])
                for half in range(2):
                    pb, pn = k.ps[nps % 8], f"ps{nps % 8}"
                    nps += 1
                    for q in range(4):
                        kc = half * 4 + q
                        P.op("tensor", lambda e, pb=pb, xb=xb, kc=kc, q=q: e.transpose(pb[:, q * 128:(q + 1) * 128], xb[:, kc * 128:(kc + 1) * 128], k.ident[:]),
                             reads=[xn, "c_ident"], writes=[pn])
                    P.op("vector", lambda e, pb=pb, half=half, tt=tt: e.tensor_copy(x1T[:, half * 4:(half + 1) * 4, tt * 128:(tt + 1) * 128], pb[:].rearrange("p (a b) -> p a b", a=4)),
                         reads=[pn], writes=[f"s6_x1T{tt}_{half}"])
                    P.op("scalar", lambda e, pb=pb, half=half: e.copy(xTf[:, half * 4:(half + 1) * 4, :], pb[:].rearrange("p (a b) -> p a b", a=4)),
                         reads=[pn, f"s6_x1T{tt}_{half}"], writes=[f"s6_xTf{half}"])
                pr, prn = k.ps[nps % 8], f"ps{nps % 8}"
                nps += 1
                for kc in range(8):
                    P.op("tensor", lambda e, pr=pr, kc=kc: e.matmul(pr[:, 0:36], lhsT=xTf[:, kc, :], rhs=wr[:, kc, :], start=(kc == 0), stop=(kc == 7)),
                         reads=[f"s6_xTf{kc // 4}", "s6_wr"], writes=[prn])
                P.op("vector", lambda e, pr=pr, tt=tt: e.tensor_tensor(out=lg[:, tt, :], in0=pr[:, 0:36], in1=brb[:], op=ALU.add), reads=[prn, "s6_brb"], writes=["s6_lg"])
                V = lambda fn, r, w: P.op("vector", fn, reads=r, writes=w)
                V(lambda e, tt=tt: e.reduce_max(out=sm[:, 0:1], in_=lg[:, tt, 0:4], axis=AX.X), ["s6_lg"], ["s6_sm"])
                V(lambda e, tt=tt: e.tensor_scalar(out=t4[:], in0=lg[:, tt, 0:4], scalar1=sm[:, 0:1], scalar2=None, op0=ALU.is_equal), ["s6_lg", "s6_sm"], ["s6_t4"])
                V(lambda e: e.tensor_scalar(out=pen[:], in0=t4[:], scalar1=-1.0, scalar2=1e30, op0=ALU.add, op1=ALU.mult), ["s6_t4"], ["s6_pen"])
                V(lambda e: e.tensor_scalar(out=sm[:, 1:2], in0=sm[:, 0:1], scalar1=-1.0, scalar2=None, op0=ALU.mult), ["s6_sm"], ["s6_sm"])
                P.op("scalar", lambda e, tt=tt: e.activation(out=t4[:], in_=lg[:, tt, 0:4], func=AF.Exp, bias=sm[:, 1:2], scale=1.0), reads=["s6_lg", "s6_sm", "s6_pen"], writes=["s6_t4"])
                V(lambda e: e.reduce_sum(out=sm[:, 2:3], in_=t4[:], axis=AX.X), ["s6_t4"], ["s6_sm"])
                V(lambda e: e.reciprocal(sm[:, 2:3], sm[:, 2:3]), ["s6_sm"], ["s6_sm"])
                for g in range(4):
                    V(lambda e, tt=tt, g=g: e.tensor_scalar(out=mk[:, g * 8:(g + 1) * 8], in0=lg[:, tt, 4 + g * 8:4 + (g + 1) * 8], scalar1=pen[:, g:g + 1], scalar2=None, op0=ALU.add),
                      ["s6_lg", "s6_pen"], ["s6_mk"])
                V(lambda e: e.reduce_max(out=sm[:, 3:4], in_=mk[:], axis=AX.X), ["s6_mk"], ["s6_sm"])
                V(lambda e: e.tensor_scalar(out=oh[:], in0=mk[:], scalar1=sm[:, 3:4], scalar2=None, op0=ALU.is_equal), ["s6_mk", "s6_sm"], ["s6_oh"])
                V(lambda e: e.scalar_tensor_tensor(out=mk2[:], in0=oh[:], scalar=-1e30, in1=mk[:], op0=ALU.mult, op1=ALU.add), ["s6_oh", "s6_mk"], ["s6_mk2"])
                V(lambda e: e.reduce_max(out=sm[:, 4:5], in_=mk2[:], axis=AX.X), ["s6_mk2"], ["s6_sm"])
                V(lambda e: e.tensor_tensor(out=sm[:, 5:6], in0=sm[:, 3:4], in1=sm[:, 4:5], op=ALU.subtract), ["s6_sm"], ["s6_sm"])
                P.op("scalar", lambda e: e.activation(out=sm[:, 6:7], in_=sm[:, 5:6], func=AF.Sigmoid), reads=["s6_sm"], writes=["s6_sm"])
                V(lambda e: e.tensor_tensor(out=sm[:, 7:8], in0=sm[:, 6:7], in1=sm[:, 2:3], op=ALU.mult), ["s6_sm"], ["s6_sm"])
                V(lambda e: e.tensor_tensor(out=sm[:, 8:9], in0=sm[:, 2:3], in1=sm[:, 7:8], op=ALU.subtract), ["s6_sm"], ["s6_sm"])
                V(lambda e, tt=tt: e.tensor_scalar(out=G[:, tt, :], in0=oh[:], scalar1=sm[:, 7:8], scalar2=None, op0=ALU.mult), ["s6_oh", "s6_sm"], ["s6_G"])
                V(lambda e: e.tensor_scalar(out=oh[:], in0=mk2[:], scalar1=sm[:, 4:5], scalar2=None, op0=ALU.is_equal), ["s6_mk2", "s6_sm", "s6_G"], ["s6_oh"])
                V(lambda e, tt=tt: e.scalar_tensor_tensor(out=G[:, tt, :], in0=oh[:], scalar=sm[:, 8:9], in1=G[:, tt, :], op0=ALU.mult, op1=ALU.add), ["s6_oh", "s6_sm", "s6_G"], ["s6_G"])
            x1res = [f"s6_x1T{tt}_{h}" for tt in range(8) for h in range(2)]
            for ex in range(32):
                wb, wbn = wgu[nw % 2], f"s6_wgu{nw % 2}"
                wdb, wdn = wd[nw % 2], f"s6_wd{nw % 2}"
                nw += 1
                P.dma("gpsimd", wbn, lambda e, wb=wb, ex=ex: e.dma_start(out=wb[:, :, 0:512], in_=c.w_eg[li, ex].rearrange("(kc kp) f -> kp kc f", kp=128)), writes=[wbn])
                P.dma("gpsimd", wbn, lambda e, wb=wb, ex=ex: e.dma_start(out=wb[:, :, 512:1024], in_=c.w_eu[li, ex].rearrange("(kc kp) f -> kp kc f", kp=128)), reads=[wbn], writes=[wbn])
                P.dma("gpsimd", wdn, lambda e, wdb=wdb, ex=ex: e.dma_start(out=wdb[:], in_=c.w_ed[li, ex].rearrange("(fc fp) n -> fp fc n", fp=128)), writes=[wdn])
                for tc in range(2):
                    hb, hn = hT[(ex * 2 + tc) % 2], f"s6_hT{(ex * 2 + tc) % 2}"
                    for fc in range(4):
                        pg, pgn = k.ps[nps % 8], f"ps{nps % 8}"
                        nps += 1
                        pu, pun = k.ps[nps % 8], f"ps{nps % 8}"
                        nps += 1
                        for kc in range(8):
                            P.op("tensor", lambda e, pg=pg, kc=kc, fc=fc, tc=tc, wb=wb: e.matmul(pg[:, :], lhsT=wb[:, kc, fc * 128:(fc + 1) * 128], rhs=x1T[:, kc, tc * 512:(tc + 1) * 512], start=(kc == 0), stop=(kc == 7)),
                                 reads=[wbn] + x1res[tc * 8:(tc + 1) * 8], writes=[pgn])
                        for kc in range(8):
                            P.op("tensor", lambda e, pu=pu, kc=kc, fc=fc, tc=tc, wb=wb: e.matmul(pu[:, :], lhsT=wb[:, kc, 512 + fc * 128:512 + (fc + 1) * 128], rhs=x1T[:, kc, tc * 512:(tc + 1) * 512], start=(kc == 0), stop=(kc == 7)),
                                 reads=[wbn] + x1res[tc * 8:(tc + 1) * 8], writes=[pun])
                        sg, sgn = sgl[fc % 2], f"s6_sg{fc % 2}"
                        P.op("scalar", lambda e, pg=pg, sg=sg: e.activation(out=sg[:], in_=pg[:, :], func=AF.Silu), reads=[pgn], writes=[sgn])
                        P.op("vector", lambda e, pu=pu, sg=sg, hb=hb, fc=fc: e.tensor_tensor(out=hb[:, fc, :], in0=pu[:, :], in1=sg[:], op=ALU.mult), reads=[pun, sgn], writes=[f"{hn}_{fc}"])
                    for tt4 in range(4):
                        tt = tc * 4 + tt4
                        for nh in range(2):
                            py, pyn = k.ps[nps % 8], f"ps{nps % 8}"
                            nps += 1
                            for fc in range(4):
                                P.op("tensor", lambda e, py=py, fc=fc, tt4=tt4, nh=nh, hb=hb, wdb=wdb: e.matmul(py[:, :], lhsT=hb[:, fc, tt4 * 128:(tt4 + 1) * 128], rhs=wdb[:, fc, nh * 512:(nh + 1) * 512], start=(fc == 0), stop=(fc == 3)),
                                     reads=[wdn, f"{hn}_{fc}"], writes=[pyn])
                            an = f"s6_acc{tt}_{nh}"
                            if ex == 0:
                                P.op("vector", lambda e, py=py, tt=tt, nh=nh, ex=ex: e.tensor_scalar(out=acc[:, tt, nh * 512:(nh + 1) * 512], in0=py[:, :], scalar1=G[:, tt, ex:ex + 1], scalar2=None, op0=ALU.mult),
                                     reads=[pyn, "s6_G"], writes=[an])
                            else:
                                P.op("vector", lambda e, py=py, tt=tt, nh=nh, ex=ex: e.scalar_tensor_tensor(out=acc[:, tt, nh * 512:(nh + 1) * 512], in0=py[:, :], scalar=G[:, tt, ex:ex + 1], in1=acc[:, tt, nh * 512:(nh + 1) * 512], op0=ALU.mult, op1=ALU.add),
                                     reads=[pyn, "s6_G", an], writes=[an])
            for tt in range(8):
                t = qt * 8 + tt
                xb, xn = xin[t % 2], f"s6_xin{t % 2}"
                P.dma("sync", xn, lambda e, xb=xb, t=t: e.dma_start(out=xb[:], in_=c.x1s[t * 128:(t + 1) * 128, :]), writes=[xn])
                P.dma("sync", "s6_pin", lambda e, t=t: e.dma_start(out=pin[:], in_=c.p[li, t * 128:(t + 1) * 128, :]), writes=["s6_pin"])
                pb, pn = k.ps[nps % 8], f"ps{nps % 8}"
                nps += 1
                for q in range(2):
                    P.op("tensor", lambda e, pb=pb, q=q: e.transpose(pb[:, q * 128:(q + 1) * 128], pin[:, q * 128:(q + 1) * 128], k.ident[:]), reads=["s6_pin", "c_ident"], writes=[pn])
                P.op("scalar", lambda e, pb=pb: e.copy(pT[:], pb[:, 0:256].rearrange("p (a b) -> p a b", a=2)), reads=[pn], writes=["s6_pT"])
                zb, zn = z[t % 2], f"s6_z{t % 2}"
                for nh in range(2):
                    pgt, pgtn = k.ps[nps % 8], f"ps{nps % 8}"
                    nps += 1
                    pp, ppn = k.ps[nps % 8], f"ps{nps % 8}"
                    nps += 1
                    for kc in range(8):
                        P.op("tensor", lambda e, pgt=pgt, kc=kc, tt=tt, nh=nh: e.matmul(pgt[:, :], lhsT=x1T[:, kc, tt * 128:(tt + 1) * 128], rhs=wpg[:, kc, nh * 512:(nh + 1) * 512], start=(kc == 0), stop=(kc == 7)),
                             reads=["s6_wpg", f"s6_x1T{tt}_{kc // 4}"], writes=[pgtn])
                    for kc in range(2):
                        P.op("tensor", lambda e, pp=pp, kc=kc, nh=nh: e.matmul(pp[:, :], lhsT=pT[:, kc, :], rhs=wpp[:, kc, nh * 512:(nh + 1) * 512], start=(kc == 0), stop=(kc == 1)),
                             reads=["s6_wpp", "s6_pT"], writes=[ppn])
                    P.op("scalar", lambda e, pgt=pgt, nh=nh: e.activation(out=sgt[:, nh * 512:(nh + 1) * 512], in_=pgt[:, :], func=AF.Sigmoid), reads=[pgtn], writes=[f"s6_sgt{nh}"])
                    P.op("vector", lambda e, pp=pp, nh=nh, zb=zb: e.tensor_tensor(out=zb[:, nh * 512:(nh + 1) * 512], in0=pp[:, :], in1=sgt[:, nh * 512:(nh + 1) * 512], op=ALU.mult),
                         reads=[ppn, f"s6_sgt{nh}"], writes=[zn + f"_{nh}"])
                    P.op("gpsimd", lambda e, nh=nh, zb=zb, tt=tt: e.tensor_tensor(out=zb[:, nh * 512:(nh + 1) * 512], in0=zb[:, nh * 512:(nh + 1) * 512], in1=acc[:, tt, nh * 512:(nh + 1) * 512], op=ALU.add),
                         reads=[zn + f"_{nh}", f"s6_acc{tt}_{nh}"], writes=[zn + f"_{nh}"])
                P.op("vector", lambda e, zb=zb, xb=xb: e.scalar_tensor_tensor(out=zb[:], in0=xb[:], scalar=ALPHA, in1=zb[:], op0=ALU.mult, op1=ALU.add),
                     reads=[xn, zn + "_0", zn + "_1"], writes=[zn])
                ob, on = xo[t % 2], f"s6_xo{t % 2}"
                layer_norm_tile(P, k, zb, zn, gam, bet, stats, mv, rstd, ob, on, eps[:, 0:1], "s6")
                P.dma("sync", "s6_out", lambda e, t=t, ob=ob: e.dma_start(out=dst[t * 128:(t + 1) * 128, :], in_=ob[:]), reads=[on])
    P.barrier()


def stage4_dn(nc, P, k, c, li):
    RS = 128 ** -0.5
    with contextlib.ExitStack() as st:
        da = sbt(nc, st, "d_da", [4, S], F32)
        db = sbt(nc, st, "d_db", [4, S], F32)
        rm = sbt(nc, st, "d_rm", [4, S], F32)
        gc = sbt(nc, st, "d_gc", [4, S], F32)
        par = sbt(nc, st, "d_par", [4, 4], F32)
        P.dma("sync", "d_da", lambda e: e.dma_start(out=da[:], in_=c.smT[8:12, :]), writes=["d_da"])
        P.dma("sync", "d_db", lambda e: e.dma_start(out=db[:], in_=c.smT[12:16, :]), writes=["d_db"])
        P.dma("sync", "d_par", lambda e: e.dma_start(out=par[:, 0:1], in_=c.dn_a_log[li]), writes=["d_par"])
        P.dma("sync", "d_par", lambda e: e.dma_start(out=par[:, 1:2], in_=c.dn_dt_bias[li]), writes=["d_par"])
        P.op("scalar", lambda e: e.activation(out=par[:, 2:3], in_=par[:, 0:1], func=AF.Exp), reads=["d_par"], writes=["d_par2"])
        P.op("vector", lambda e: e.tensor_scalar(out=par[:, 2:3], in0=par[:, 2:3], scalar1=-1.0, scalar2=None, op0=ALU.mult), reads=["d_par2"], writes=["d_par2"])
        P.op("scalar", lambda e: e.activation(out=da[:], in_=da[:], func=AF.Exp, bias=par[:, 1:2], scale=1.0), reads=["d_da", "d_par"], writes=["d_da"])
        P.op("scalar", lambda e: e.activation(out=da[:], in_=da[:], func=AF.Ln, bias=k.ones[0:4, 0:1], scale=1.0), reads=["d_da", "c_ones"], writes=["d_da"])
        P.op("scalar", lambda e: e.activation(out=db[:], in_=db[:], func=AF.Sigmoid), reads=["d_db"], writes=["d_db"])
        P.op("vector", lambda e: e.tensor_scalar(out=da[:], in0=da[:], scalar1=par[:, 2:3], scalar2=None, op0=ALU.mult), reads=["d_da", "d_par2"], writes=["d_da"])
        P.op("gpsimd", lambda e: e.memset(rm[:], 1.0), writes=["d_rm"])
        P.op("gpsimd", lambda e: e.memset(rm[:].rearrange("p (c j) -> p c j", j=64)[:, :, 0:1], 0.0), reads=["d_rm"], writes=["d_rm"])
        P.op("vector", lambda e: e.tensor_tensor_scan(out=gc[:], data0=rm[:], data1=da[:], initial=0.0, op0=ALU.mult, op1=ALU.add), reads=["d_rm", "d_da"], writes=["d_gc"])
        P.dma("sync", "d_rowout", lambda e: e.dma_start(out=c.dnrow[0], in_=db[:]), reads=["d_db"])
        P.dma("sync", "d_rowout", lambda e: e.dma_start(out=c.dnrow[1], in_=gc[:]), reads=["d_gc"])
    P.barrier()
    psb = k.ps[7][:, :].bitcast(BF16)
    psb6 = k.ps[6][:, :].bitcast(BF16)
    with contextlib.ExitStack() as st0:
        cw = sbt(nc, st0, "d_cw", [128, 12, 4], F32)
        nw = sbt(nc, st0, "d_nw", [128, 1], F32)
        eps6 = sbt(nc, st0, "d_eps6", [128, 1], F32)
        with contextlib.ExitStack() as st:
            cwraw = sbt(nc, st, "d_cwraw", [4, 1536], F32)
            P.dma("sync", "d_cwraw", lambda e: e.dma_start(out=cwraw[:], in_=c.dn_conv[li]), writes=["d_cwraw"])
            for idx in range(12):
                P.op("tensor", lambda e, idx=idx: e.transpose(k.ps[0][:, idx * 4:(idx + 1) * 4], cwraw[0:4, idx * 128:(idx + 1) * 128], k.ident[0:4, 0:4]),
                     reads=["d_cwraw", "c_ident"], writes=["ps0"])
            P.op("vector", lambda e: e.tensor_copy(cw[:].rearrange("p a b -> p (a b)"), k.ps[0][:, 0:48]), reads=["ps0"], writes=["d_cw"])
            P.dma("sync", "d_nw", lambda e: e.dma_start(out=nw[:], in_=c.dn_norm_w[li]), writes=["d_nw"])
            P.op("vector", lambda e: e.memset(eps6[:], 1e-6), writes=["d_eps6"])
        P.barrier()
        def do_head(h):
            with contextlib.ExitStack() as sth:
                A = lambda n, s, d: sbt(nc, sth, n, s, d)
                kT = A("d_kT", [128, S], BF16)
                qT = A("d_qT", [128, S], BF16)
                qdT = A("d_qdT", [128, S], BF16)
                vb_tm = A("d_vb", [128, NT, 128], BF16)
                kbg_tm = A("d_kbg", [128, NT, 128], BF16)
                kdec_tm = A("d_kdec", [128, NT, 128], BF16)
                AT = A("d_AT", [128, NT, 128], BF16)
                gcB = A("d_gcB", [128, S], F32)
                gc_col = A("d_gccol", [128, NT], F32)
                b_col = A("d_bcol", [128, NT], F32)
                glc = A("d_glc", [128, NT], F32)
                col_bg = A("d_colbg", [128, NT], F32)
                col_edd = A("d_coledd", [128, NT], F32)
                negb = A("d_negb", [128, NT], F32)
                neggc = A("d_neggc", [128, NT], F32)
                eglB = A("d_eglB", [128, 64], F32)
                browh = c.dnrow[0, h]
                gcrowh = c.dnrow[1, h]
                P.dma("sync", "d_cols", lambda e: e.dma_start(out=gc_col[:], in_=gcrowh.rearrange("(t p) -> p t", p=128), allow_slow_non_contiguous=True), writes=["d_gccol"])
                P.dma("sync", "d_cols", lambda e: e.dma_start(out=b_col[:], in_=browh.rearrange("(t p) -> p t", p=128), allow_slow_non_contiguous=True), writes=["d_bcol"])
                gsrc = gcrowh.rearrange("(t two j) -> two j t", two=2, j=64)
                for half in range(2):
                    P.dma("sync", "d_cols", lambda e, half=half: e.dma_start(out=glc[half * 64:(half + 1) * 64, :], in_=gsrc[half, 63, :].partition_broadcast(64), allow_slow_non_contiguous=True), writes=["d_glc"])
                P.dma("sync", "d_cols", lambda e: e.dma_start(out=eglB[:], in_=gcrowh.rearrange("(c j) -> j c", j=64)[63, :].partition_broadcast(128), allow_slow_non_contiguous=True), writes=["d_eglB"])
                P.dma("sync", "d_gcB", lambda e: e.dma_start(out=gcB[:], in_=gcrowh.partition_broadcast(128)), writes=["d_gcB"])
                P.op("scalar", lambda e: e.activation(out=eglB[:], in_=eglB[:], func=AF.Exp), reads=["d_eglB"], writes=["d_eglB"])
                P.op("scalar", lambda e: e.activation(out=col_bg[:], in_=gc_col[:], func=AF.Exp), reads=["d_gccol"], writes=["d_colbg"])
                P.op("vector", lambda e: e.tensor_tensor(out=col_bg[:], in0=col_bg[:], in1=b_col[:], op=ALU.mult), reads=["d_colbg", "d_bcol"], writes=["d_colbg"])
                P.op("vector", lambda e: e.tensor_tensor(out=col_edd[:], in0=glc[:], in1=gc_col[:], op=ALU.subtract), reads=["d_glc", "d_gccol"], writes=["d_coledd"])
                P.op("scalar", lambda e: e.activation(out=col_edd[:], in_=col_edd[:], func=AF.Exp), reads=["d_coledd"], writes=["d_coledd"])
                P.op("vector", lambda e: e.tensor_scalar(out=negb[:], in0=b_col[:], scalar1=-1.0, scalar2=None, op0=ALU.mult), reads=["d_bcol"], writes=["d_negb"])
                P.op("vector", lambda e: e.tensor_scalar(out=neggc[:], in0=gc_col[:], scalar1=-1.0, scalar2=None, op0=ALU.mult), reads=["d_gccol"], writes=["d_neggc"])
                with contextlib.ExitStack() as st:
                    u = [sbt(nc, st, f"d_u{i}", [128, S], F32) for i in range(2)]
                    acc = sbt(nc, st, "d_acc", [128, S], F32)
                    sch = [sbt(nc, st, f"d_sch{i}", [128, 512], F32) for i in range(2)]
                    sqc = [sbt(nc, st, f"d_sqc{i}", [128, 512], F32) for i in range(2)]
                    rn = [sbt(nc, st, f"d_rn{i}", [128, 512], F32) for i in range(2)]
                    vTb = sbt(nc, st, "d_vTb", [128, S], BF16)
                    if h == 0 and "dn_dbg" in DBG_FLAGS:
                        print("sbuf remaining in dn phase A:", nc.sbuf_bytes_remaining)
                        for nm_, t_ in [("kT", kT), ("qT", qT), ("qdT", qdT), ("vb", vb_tm), ("kbg", kbg_tm), ("kdec", kdec_tm), ("AT", AT), ("gcB", gcB), ("gc_col", gc_col), ("eglB", eglB),
                                        ("u0", u[0]), ("u1", u[1]), ("acc", acc), ("rn0", rn[0]), ("rn1", rn[1]), ("vTb", vTb), ("cw", cw), ("ones", k.ones)]:
                            m_ = nc.lookup_mloc(t_)
                            print("   ", nm_, m_.addr, list(m_.dims))
                    nps = 0
                    for wi, which in enumerate(("q", "k", "v")):
                        idx = wi * 4 + h
                        ub_, un = u[wi % 2], f"d_u{wi % 2}"
                        P.dma("sync", un, lambda e, ub_=ub_, idx=idx: e.dma_start(out=ub_[:], in_=c.fT[512 + idx * 128:512 + (idx + 1) * 128, :]), writes=[un])
                        P.op("vector", lambda e, ub_=ub_, idx=idx: e.tensor_scalar(out=acc[:], in0=ub_[:], scalar1=cw[:, idx, 3:4], scalar2=None, op0=ALU.mult), reads=[un, "d_cw"], writes=["d_acc"])
                        for sh, j in ((1, 2), (2, 1), (3, 0)):
                            P.op("vector", lambda e, ub_=ub_, idx=idx, sh=sh, j=j: e.scalar_tensor_tensor(out=acc[:, sh:], in0=ub_[:, 0:S - sh], scalar=cw[:, idx, j:j + 1], in1=acc[:, sh:], op0=ALU.mult, op1=ALU.add),
                                 reads=[un, "d_cw", "d_acc"], writes=["d_acc"])
                        dstT, dn_ = (qT, "d_qT") if which == "q" else ((kT, "d_kT") if which == "k" else (vTb, "d_vTb"))
                        for tc in range(8):
                            cs = slice(tc * 512, (tc + 1) * 512)
                            if which == "v":
                                P.op("scalar", lambda e, cs=cs: e.activation(out=vTb[:, cs], in_=acc[:, cs], func=AF.Silu), reads=["d_acc"], writes=["d_vTb"])
                                continue
                            pb, pn = k.ps[nps % 4], f"ps{nps % 4}"
                            rb, rbn = rn[nps % 2], f"d_rn{nps % 2}"
                            sc_, scn = sch[nps % 2], f"d_sch{nps % 2}"
                            sq_, sqn = sqc[nps % 2], f"d_sqc{nps % 2}"
                            nps += 1
                            P.op("scalar", lambda e, cs=cs, sc_=sc_: e.activation(out=sc_[:], in_=acc[:, cs], func=AF.Silu), reads=["d_acc"], writes=[scn])
                            P.op("vector", lambda e, sc_=sc_, sq_=sq_: e.tensor_tensor(out=sq_[:], in0=sc_[:], in1=sc_[:], op=ALU.mult), reads=[scn], writes=[sqn])
                            P.op("tensor", lambda e, pb=pb, sq_=sq_: e.matmul(pb[:, :], lhsT=k.ones[:], rhs=sq_[:], start=True, stop=True), reads=["c_ones", sqn], writes=[pn])
                            P.op("scalar", lambda e, pb=pb, rb=rb: e.activation(out=rb[:], in_=pb[:, :], func=AF.Sqrt, bias=eps6[:, 0:1], scale=1.0), reads=[pn, "d_eps6"], writes=[rbn])
                            P.op("vector", lambda e, rb=rb: e.reciprocal(rb[:], rb[:]), reads=[rbn], writes=[rbn])
                            sc = RS if which == "q" else 1.0
                            P.op("vector", lambda e, rb=rb, cs=cs, dstT=dstT, sc=sc, sc_=sc_: e.scalar_tensor_tensor(out=dstT[:, cs], in0=sc_[:], scalar=sc, in1=rb[:], op0=ALU.mult, op1=ALU.mult),
                                 reads=[scn, rbn], writes=[dn_])
                    for t in range(NT):
                        cs = slice(t * 128, (t + 1) * 128)
                        pq = psb if t % 2 == 0 else psb6
                        pqn = "ps7" if t % 2 == 0 else "ps6"
                        P.op("tensor", lambda e, cs=cs, pq=pq: e.transpose(pq[:, 0:128], kT[:, cs], k.identb[:]), reads=["d_kT", "c_identb"], writes=[pqn])
                        P.op("tensor", lambda e, cs=cs, pq=pq: e.transpose(pq[:, 128:256], vTb[:, cs], k.identb[:]), reads=["d_vTb", "c_identb"], writes=[pqn])
                        P.op("vector", lambda e, t=t, pq=pq: e.tensor_scalar(out=kbg_tm[:, t, :], in0=pq[:, 0:128], scalar1=col_bg[:, t:t + 1], scalar2=None, op0=ALU.mult),
                             reads=[pqn, "d_colbg"], writes=["d_kbg"])
                        P.op("vector", lambda e, t=t, pq=pq: e.tensor_scalar(out=kdec_tm[:, t, :], in0=pq[:, 0:128], scalar1=col_edd[:, t:t + 1], scalar2=None, op0=ALU.mult),
                             reads=[pqn, "d_coledd"], writes=["d_kdec"])
                        P.op("vector", lambda e, t=t, pq=pq: e.tensor_scalar(out=vb_tm[:, t, :], in0=pq[:, 128:256], scalar1=b_col[:, t:t + 1], scalar2=None, op0=ALU.mult),
                             reads=[pqn, "d_bcol"], writes=["d_vb"])
                    if "dn_dbg" in DBG_FLAGS and h == 0:
                        P.dma("gpsimd", "dbgo", lambda e: e.dma_start(out=c.dbg1[:, 2432:2560], in_=u[1][:, 2048:2176]), reads=["d_u1"])
                        P.dma("gpsimd", "dbgo", lambda e: e.dma_start(out=c.dbg1[:, 2560:2688], in_=acc[:, 2048:2176]), reads=["d_acc"])
                        P.dma("gpsimd", "dbgo", lambda e: e.dma_start(out=c.dbg1[:, 2688:2816], in_=vTb[:, 2048:2176]), reads=["d_vTb"])
                    for tc in range(8):
                        cs = slice(tc * 512, (tc + 1) * 512)
                        sc_, scn = sch[tc % 2], f"d_sch{tc % 2}"
                        P.op("scalar", lambda e, cs=cs, sc_=sc_: e.activation(out=sc_[:], in_=gcB[:, cs], func=AF.Exp), reads=["d_gcB"], writes=[scn])
                        P.op("vector", lambda e, cs=cs, sc_=sc_: e.tensor_tensor(out=qdT[:, cs], in0=qT[:, cs], in1=sc_[:], op=ALU.mult), reads=["d_qT", scn], writes=["d_qdT"])
                P.barrier()
                uin = A("d_uin", [128, NT, 128], F32)
                WT = A("d_WT", [128, S], BF16)
                oT = A("d_oT", [128, S], F32)
                with contextlib.ExitStack() as st:
                    Dm = [sbt(nc, st, f"d_Dm{i}", [128, 128], F32) for i in range(2)]
                    DTm = [sbt(nc, st, f"d_DTm{i}", [128, 128], F32) for i in range(2)]
                    Nf = [sbt(nc, st, f"d_Nf{i}", [128, 128], F32) for i in range(2)]
                    ATf = [sbt(nc, st, f"d_ATf{i}", [128, 128], F32) for i in range(2)]
                    Nl = [sbt(nc, st, f"d_Nl{i}", [128, 4, 128], BF16) for i in range(2)]
                    Ml = [sbt(nc, st, f"d_Ml{i}", [128, 4, 128], BF16) for i in range(2)]
                    Pl = [sbt(nc, st, f"d_Pl{i}", [128, 4, 128], BF16) for i in range(2)]
                    for grp in range(NT // 4):
                        for tt in range(4):
                            t = grp * 4 + tt
                            cs = slice(t * 128, (t + 1) * 128)
                            i2 = t % 2
                            pkk, pkkn = k.ps[i2], f"ps{i2}"
                            pqk, pqkn = k.ps[2 + i2], f"ps{2 + i2}"
                            P.op("tensor", lambda e, pkk=pkk, cs=cs: e.matmul(pkk[:, 0:128], lhsT=kT[:, cs], rhs=kT[:, cs], start=True, stop=True), reads=["d_kT"], writes=[pkkn])
                            P.op("tensor", lambda e, pqk=pqk, cs=cs: e.matmul(pqk[:, 0:128], lhsT=kT[:, cs], rhs=qT[:, cs], start=True, stop=True), reads=["d_kT", "d_qT"], writes=[pqkn])
                            P.op("scalar", lambda e, cs=cs, t=t, i2=i2: e.activation(out=Dm[i2][:], in_=gcB[:, cs], func=AF.Exp, bias=gc_col[:, t:t + 1], scale=-1.0), reads=["d_gcB", "d_gccol"], writes=[f"d_Dm{i2}"])
                            P.op("scalar", lambda e, cs=cs, t=t, i2=i2: e.activation(out=DTm[i2][:], in_=gcB[:, cs], func=AF.Exp, bias=neggc[:, t:t + 1], scale=1.0), reads=["d_gcB", "d_neggc"], writes=[f"d_DTm{i2}"])
                            P.op("vector", lambda e, pkk=pkk, t=t, i2=i2: e.scalar_tensor_tensor(out=Nf[i2][:], in0=pkk[:, 0:128], scalar=negb[:, t:t + 1], in1=Dm[i2][:], op0=ALU.mult, op1=ALU.mult),
                                 reads=[pkkn, "d_negb", f"d_Dm{i2}"], writes=[f"d_Nf{i2}"])
                            P.op("vector", lambda e, pqk=pqk, i2=i2: e.tensor_tensor(out=ATf[i2][:], in0=pqk[:, 0:128], in1=DTm[i2][:], op=ALU.mult),
                                 reads=[pqkn, f"d_DTm{i2}"], writes=[f"d_ATf{i2}"])
                            P.op("gpsimd", lambda e, tt=tt, i2=i2: e.affine_select(out=Nl[0][:, tt, :], in_=Nf[i2][:], pattern=[[-1, 128]], compare_op=ALU.is_gt, fill=0.0, base=0, channel_multiplier=1),
                                 reads=[f"d_Nf{i2}"], writes=[f"d_N0_{tt}"])
                            P.op("gpsimd", lambda e, tt=tt: e.memset(Nl[0][64:128, tt, 0:64], 0.0), reads=[f"d_N0_{tt}"], writes=[f"d_N0_{tt}"])
                            P.op("gpsimd", lambda e, t=t, i2=i2: e.affine_select(out=AT[:, t, :], in_=ATf[i2][:], pattern=[[1, 128]], compare_op=ALU.is_ge, fill=0.0, base=0, channel_multiplier=-1),
                                 reads=[f"d_ATf{i2}"], writes=["d_AT"])
                            P.op("gpsimd", lambda e, t=t: e.memset(AT[0:64, t, 64:128], 0.0), reads=["d_AT"], writes=["d_AT"])
                            P.op("tensor", lambda e, tt=tt: e.transpose(psb[:, tt * 128:(tt + 1) * 128], Nl[0][:, tt, :], k.identb[:]), reads=[f"d_N0_{tt}", "c_identb"], writes=["ps7"])
                        P.op("scalar", lambda e: e.copy(Ml[0][:].rearrange("p a b -> p (a b)"), psb[:, 0:512]), reads=["ps7"], writes=["d_M0"])
                        for tt in range(4):
                            P.op("vector", lambda e, tt=tt: e.tensor_tensor(out=Pl[0][:, tt, :], in0=Ml[0][:, tt, :], in1=k.identb[:], op=ALU.add), reads=["d_M0", "c_identb"], writes=[f"d_P0_{tt}"])
                        Nres = [f"d_N0_{tt}" for tt in range(4)]
                        Mres = ["d_M0"]
                        Pres = [f"d_P0_{tt}" for tt in range(4)]
                        cur = 0
                        for lvl in range(1, 6):
                            nxt = 1 - cur
                            for tt in range(4):
                                P.op("tensor", lambda e, tt=tt, cur=cur: e.matmul(k.ps[4][:, tt * 128:(tt + 1) * 128], lhsT=Ml[cur][:, tt, :], rhs=Nl[cur][:, tt, :], start=True, stop=True),
                                     reads=Nres + Mres, writes=["ps4"])
                            if lvl < 5:
                                for tt in range(4):
                                    P.op("tensor", lambda e, tt=tt, cur=cur: e.matmul(k.ps[5][:, tt * 128:(tt + 1) * 128], lhsT=Nl[cur][:, tt, :], rhs=Ml[cur][:, tt, :], start=True, stop=True),
                                         reads=Nres + Mres, writes=["ps5"])
                            P.op("scalar", lambda e, nxt=nxt: e.copy(Nl[nxt][:].rearrange("p a b -> p (a b)"), k.ps[4][:, :]), reads=["ps4"], writes=[f"d_Nn{nxt}"])
                            if lvl < 5:
                                P.op("vector", lambda e, nxt=nxt: e.tensor_copy(Ml[nxt][:].rearrange("p a b -> p (a b)"), k.ps[5][:, :]), reads=["ps5"], writes=[f"d_Mn{nxt}"])
                            for tt in range(4):
                                P.op("tensor", lambda e, tt=tt, cur=cur, nxt=nxt: e.matmul(k.ps[6][:, tt * 128:(tt + 1) * 128], lhsT=Nl[nxt][:, tt, :], rhs=Pl[cur][:, tt, :], start=True, stop=True),
                                     reads=[f"d_Nn{nxt}"] + Pres, writes=["ps6"])
                            P.op("vector", lambda e, cur=cur, nxt=nxt: e.tensor_tensor(out=Pl[nxt][:].rearrange("p a b -> p (a b)"), in0=k.ps[6][:, :], in1=Pl[cur][:].rearrange("p a b -> p (a b)"), op=ALU.add),
                                 reads=["ps6"] + Pres, writes=[f"d_Pn{nxt}"])
                            Nres = [f"d_Nn{nxt}"]
                            Mres = [f"d_Mn{nxt}"]
                            Pres = [f"d_Pn{nxt}"]
                            cur = nxt
                        for tt in range(4):
                            t = grp * 4 + tt
                            P.op("tensor", lambda e, tt=tt, t=t, cur=cur: e.matmul(k.ps[4][:, tt * 128:(tt + 1) * 128], lhsT=Pl[cur][:, tt, :], rhs=vb_tm[:, t, :], start=True, stop=True),
                                 reads=Pres + ["d_vb"], writes=["ps4"])
                            P.op("tensor", lambda e, tt=tt, t=t, cur=cur: e.matmul(k.ps[5][:, tt * 128:(tt + 1) * 128], lhsT=kbg_tm[:, t, :], rhs=Pl[cur][:, tt, :], start=True, stop=True),
                                 reads=Pres + ["d_kbg"], writes=["ps5"])
                        P.op("vector", lambda e, grp=grp: e.tensor_copy(uin[:, grp * 4:(grp + 1) * 4, :].rearrange("p a b -> p (a b)"), k.ps[4][:, :]), reads=["ps4"], writes=["d_uin"])
                        P.op("scalar", lambda e, grp=grp: e.copy(WT[:, grp * 512:(grp + 1) * 512], k.ps[5][:, :]), reads=["ps5"], writes=["d_WT"])
                P.barrier()
                with contextlib.ExitStack() as st:
                    S32 = sbt(nc, st, "d_S32", [128, 128], F32)
                    Sb = sbt(nc, st, "d_Sb", [128, 128], BF16)
                    ub = [sbt(nc, st, f"d_ub{i}", [128, 128], BF16) for i in range(2)]
                    P.op("gpsimd", lambda e: e.memset(S32[:], 0.0), writes=["d_S32"])
                    P.op("gpsimd", lambda e: e.memset(Sb[:], 0.0), writes=["d_Sb"])
                    for ci in range(64):
                        t, hb = ci // 2, ci % 2
                        r0 = hb * 64
                        cc = slice(ci * 64, (ci + 1) * 64)
                        ubb, ubn = ub[ci % 2], f"d_ub{ci % 2}"
                        po, pon = k.ps[1 + (ci // 8) % 2], f"ps{1 + (ci // 8) % 2}"
                        oc = slice((ci % 8) * 64, (ci % 8 + 1) * 64)
                        P.op("tensor", lambda e, r0=r0, cc=cc: e.matmul(k.ps[0][r0:r0 + 64, 0:128], lhsT=WT[:, cc], rhs=Sb[:], start=True, stop=True), reads=["d_WT", "d_Sb"], writes=["ps0"])
                        P.op("vector", lambda e, r0=r0, t=t, ubb=ubb: e.tensor_tensor(out=ubb[r0:r0 + 64, :], in0=uin[r0:r0 + 64, t, :], in1=k.ps[0][r0:r0 + 64, 0:128], op=ALU.subtract),
                             reads=["ps0", "d_uin"], writes=[ubn])
                        P.op("tensor", lambda e, po=po, oc=oc, cc=cc: e.matmul(po[:, oc], lhsT=Sb[:], rhs=qdT[:, cc], start=True, stop=False), reads=["d_Sb", "d_qdT"], writes=[pon])
                        P.op("tensor", lambda e, po=po, oc=oc, r0=r0, t=t, ubb=ubb: e.matmul(po[:, oc], lhsT=ubb[r0:r0 + 64, :], rhs=AT[r0:r0 + 64, t, r0:r0 + 64], start=False, stop=True),
                             reads=[ubn, "d_AT"], writes=[pon])
                        P.op("tensor", lambda e, r0=r0, t=t, ubb=ubb: e.matmul(k.ps[3][:, 0:128], lhsT=kdec_tm[r0:r0 + 64, t, :], rhs=ubb[r0:r0 + 64, :], start=True, stop=True),
                             reads=[ubn, "d_kdec"], writes=["ps3"])
                        P.op("vector", lambda e, ci=ci: e.scalar_tensor_tensor(out=S32[:], in0=S32[:], scalar=eglB[:, ci:ci + 1], in1=k.ps[3][:, 0:128], op0=ALU.mult, op1=ALU.add),
                             reads=["ps3", "d_S32", "d_eglB"], writes=["d_S32"])
                        P.op("scalar", lambda e: e.copy(Sb[:], S32[:]), reads=["d_S32"], writes=["d_Sb"])
                        if ci % 8 == 7:
                            g8 = ci // 8
                            P.op("scalar", lambda e, po=po, g8=g8: e.copy(oT[:, g8 * 512:(g8 + 1) * 512], po[:, :]), reads=[pon], writes=["d_oT"])
                P.barrier()
                if "dn_dbg" in DBG_FLAGS and h == 0:
                    dl = [(kT[:, 0:128], 0), (qT[:, 0:128], 128), (gc_col[:], 256), (b_col[:], 288), (AT[:, 0, :], 320), (uin[:, 0, :], 448),
                          (WT[:, 0:128], 576), (oT[:, 0:128], 704), (vb_tm[:, 0, :], 832), (kbg_tm[:, 0, :], 960), (kdec_tm[:, 0, :], 1088), (qdT[:, 0:128], 1216), (eglB[:], 1344), (gcB[:, 2016:2144], 1408), (kT[:, 2048:2176], 1536), (qdT[:, 2048:2176], 1664), (AT[:, 16, :], 1792), (uin[:, 16, :], 1920), (WT[:, 2048:2176], 2048), (oT[:, 2048:2176], 2176), (kdec_tm[:, 16, :], 2304)]
                    for (ap_, o_) in dl:
                        P.dma("gpsimd", "dbgo", lambda e, ap_=ap_, o_=o_: e.dma_start(out=c.dbg1[:, o_:o_ + ap_.shape[-1]], in_=ap_), reads=[])
                    P.barrier()
                with contextlib.ExitStack() as st:
                    sq = sbt(nc, st, "d_sq2", [128, S], F32)
                    dgt = sbt(nc, st, "d_dgt", [128, S], F32)
                    dgs = sbt(nc, st, "d_dgs", [128, S], BF16)
                    rn = [sbt(nc, st, f"d_rn2{i}", [128, 512], F32) for i in range(2)]
                    yst = sbt(nc, st, "d_yst", [128, S], BF16)
                    P.dma("sync", "d_dgt", lambda e: e.dma_start(out=dgt[:], in_=c.fT[2048 + h * 128:2048 + (h + 1) * 128, :]), writes=["d_dgt"])
                    for tc in range(8):
                        P.op("scalar", lambda e, tc=tc: e.activation(out=dgs[:, tc * 512:(tc + 1) * 512], in_=dgt[:, tc * 512:(tc + 1) * 512], func=AF.Silu), reads=["d_dgt"], writes=["d_dgs"])
                    P.op("vector", lambda e: e.tensor_tensor(out=sq[:], in0=oT[:], in1=oT[:], op=ALU.mult), reads=["d_oT"], writes=["d_sq2"])
                    for tc in range(8):
                        pb, pn = k.ps[tc % 4], f"ps{tc % 4}"
                        rb, rbn = rn[tc % 2], f"d_rn2{tc % 2}"
                        cs = slice(tc * 512, (tc + 1) * 512)
                        P.op("tensor", lambda e, pb=pb, cs=cs: e.matmul(pb[:, :], lhsT=k.ones[:], rhs=sq[:, cs], start=True, stop=True), reads=["c_ones", "d_sq2"], writes=[pn])
                        P.op("scalar", lambda e, pb=pb, rb=rb: e.activation(out=rb[:], in_=pb[:, :], func=AF.Sqrt, bias=eps6[:, 0:1], scale=1.0 / 128.0), reads=[pn, "d_eps6"], writes=[rbn])
                        P.op("vector", lambda e, rb=rb: e.reciprocal(rb[:], rb[:]), reads=[rbn], writes=[rbn])
                        P.op("vector", lambda e, rb=rb, cs=cs: e.scalar_tensor_tensor(out=oT[:, cs], in0=oT[:, cs], scalar=nw[:, 0:1], in1=rb[:], op0=ALU.mult, op1=ALU.mult),
                             reads=["d_oT", "d_nw", rbn], writes=[f"d_oTn{tc}"])
                        P.op("gpsimd", lambda e, cs=cs: e.tensor_tensor(out=yst[:, cs], in0=oT[:, cs], in1=dgs[:, cs], op=ALU.mult), reads=[f"d_oTn{tc}", "d_dgs"], writes=[f"d_yst{tc}"])
                    P.dma("sync", "d_yout", lambda e: e.dma_start(out=c.yT[1024 + h * 128:1024 + (h + 1) * 128, :], in_=yst[:]), reads=[f"d_yst{tc}" for tc in range(8)])
            P.barrier()
        for h in range(4):
            do_head(h)
    P.barrier()


def build_layers(nc, layers_local, first, last):
    with contextlib.ExitStack() as st:
        P = Prog(nc, st)
        c = declare_io(nc, layers_local, first, last)
        k = make_consts(nc, st, P)
        n = len(layers_local)
        for li in range(n):
            src = c.x_in if li == 0 else c.xs
            dst = c.out if li == n - 1 else c.xs
            stage1_proj(nc, P, k, c, li, src)
            stage2_attn(nc, P, k, c, li)
            stage3_pool(nc, P, k, c, li)
            stage4_dn(nc, P, k, c, li)
            stage5_merge(nc, P, k, c, li, src)
            stage6_moe(nc, P, k, c, li, dst)
        P.wait_all("sync")
        P.emit()
        return P.nops


def _core_map(inp, xb, b, layers):
    L = list(layers)
    n = len(L)
    g = lambda nm: np.ascontiguousarray(inp[nm][L])
    return {
        "x_in": np.ascontiguousarray(xb, dtype=np.float32),
        "p": np.ascontiguousarray(inp["p"][L][:, b]),
        "w_in": g("w_in"), "b_forget": g("b_forget").reshape(n, 8, 1),
        "pool_w": g("pool_w"), "pool_scale": g("pool_scale").reshape(n, 4, 128, 1),
        "dn_conv": g("dn_conv"), "dn_a_log": g("dn_a_log").reshape(n, 4, 1),
        "dn_dt_bias": g("dn_dt_bias").reshape(n, 4, 1), "dn_norm_w": g("dn_norm_w").reshape(n, 128, 1),
        "w_br": np.ascontiguousarray(np.concatenate([inp["w_br_attn"][L], inp["w_br_pool"][L], inp["w_br_dn"][L]], axis=1)),
        "w_out": g("w_out"), "ln1_g": g("ln1_g"), "ln1_b": g("ln1_b"),
        "w_r": np.ascontiguousarray(np.concatenate([inp["w_router_group"][L], inp["w_router_expert"][L]], axis=2)),
        "b_r": np.ascontiguousarray(np.concatenate([inp["b_router_group"][L], inp["b_router_expert"][L]], axis=1)),
        "w_eg": g("w_exp_gate"), "w_eu": g("w_exp_up"), "w_ed": g("w_exp_down"),
        "w_pp": g("w_ple_proj"), "w_pg": g("w_ple_gate"), "ln2_g": g("ln2_g"), "ln2_b": g("ln2_b"),
    }


LAYER_GROUPS = [[0, 1, 2, 3]]


def kernel(**inputs):
    inp = {k: np.asarray(v) for k, v in inputs.items()}
    cur = [inp["x"][b] for b in range(4)]
    for grp in LAYER_GROUPS:
        nc = bass.Bass("TRN2", target_bir_lowering=False)
        build_layers(nc, list(range(len(grp))), True, True)
        base = [_core_map(inp, cur[b], b, grp) for b in range(4)]
        maps = [base[c % 4] for c in range(8)]
        res = run_bass_kernel_spmd(nc, maps, core_ids=list(range(8)))
        cur = [np.asarray(res.results[b]["out"], dtype=np.float32) for b in range(4)]
    return np.stack(cur).astype(np.float32)
```

```python
import contextlib
import numpy as np
import concourse.bass as bass
import concourse.mybir as mybir
from concourse.bass_utils import run_bass_kernel_spmd

F32 = mybir.dt.float32
BF16 = mybir.dt.bfloat16
I32 = mybir.dt.int32
AF = mybir.ActivationFunctionType
ALU = mybir.AluOpType
AX = mybir.AxisListType

ENGS = ["tensor", "vector", "scalar", "gpsimd", "sync"]
SEM_LIMIT = 30000


class Prog:
    def __init__(self, nc, stack):
        self.nc = nc
        self.stack = stack
        self.stream = {e: [] for e in ENGS}
        self.cnt = {e: 0 for e in ENGS}
        self.nsem = 0
        self.sem = {e: self._newsem("p_" + e) for e in ENGS}
        self.waited = {e: {} for e in ENGS}
        self.lastw = {}
        self.readers = {}
        self.dsem = {}
        self.nops = 0

    def _newsem(self, name):
        self.nsem += 1
        return self.stack.enter_context(self.nc.semaphore(f"{name}_{self.nsem}"))

    def _need(self, eng, toks):
        need = {}
        for (sem, val, teng), kind in toks:
            if teng == eng and (kind != "raw" or eng == "tensor"):
                continue
            k = id(sem)
            if self.waited[eng].get(k, 0) >= val:
                continue
            if k not in need or need[k][1] < val:
                need[k] = (sem, val)
        for k, (sem, val) in need.items():
            self.waited[eng][k] = val
        return list(need.values())

    def _deps(self, eng, reads, writes):
        toks = []
        for r in reads:
            t = self.lastw.get(r)
            if t is not None:
                toks.append((t, "raw"))
        for w in writes:
            t = self.lastw.get(w)
            if t is not None:
                toks.append((t, "waw"))
            for t in self.readers.get(w, {}).values():
                toks.append((t, "war"))
        return self._need(eng, toks)

    def _update(self, tok, reads, writes):
        sem, val, teng = tok
        rk = teng if teng != "dma" else ("dma", id(sem))
        for r in reads:
            self.readers.setdefault(r, {})[rk] = tok
        for w in writes:
            self.lastw[w] = tok
            self.readers[w] = {}

    def op(self, eng, fn, reads=(), writes=()):
        waits = self._deps(eng, reads, writes)
        if self.cnt[eng] >= SEM_LIMIT:
            self.sem[eng] = self._newsem("p_" + eng)
            self.cnt[eng] = 0
        self.cnt[eng] += 1
        tok = (self.sem[eng], self.cnt[eng], eng)
        self.stream[eng].append((waits, fn, self.sem[eng], 1))
        self._update(tok, reads, writes)
        self.nops += 1
        return tok

    def dma(self, eng, key, fn, reads=(), writes=()):
        waits = self._deps(eng, reads, writes)
        s = self.dsem.get(key)
        if s is None or s[1] >= SEM_LIMIT:
            s = [self._newsem("d"), 0]
            self.dsem[key] = s
        s[1] += 16
        tok = (s[0], s[1], "dma")
        self.stream[eng].append((waits, fn, s[0], 16))
        self._update(tok, reads, writes)
        self.nops += 1
        return tok

    def barrier(self):
        toks = []
        for e in ENGS:
            if self.cnt[e] > 0:
                toks.append(((self.sem[e], self.cnt[e], e), "raw"))
        for k, s in self.dsem.items():
            toks.append(((s[0], s[1], "dma"), "raw"))
        for e in ENGS:
            waits = self._need(e, [t for t in toks if t[0][2] != e])
            if waits:
                self.stream[e].append((waits, None, None, 0))
        self.lastw = {}
        self.readers = {}

    def wait_all(self, eng):
        toks = []
        for e in ENGS:
            if self.cnt[e] > 0 and e != eng:
                toks.append(((self.sem[e], self.cnt[e], e), "raw"))
        for k, s in self.dsem.items():
            toks.append(((s[0], s[1], "dma"), "raw"))
        waits = self._need(eng, toks)
        if waits:
            self.stream[eng].append((waits, None, None, 0))

    def emit(self):
        with self.nc.Block() as block:
            for eng in ENGS:
                def body(e, eng=eng):
                    for (waits, fn, sem, inc) in self.stream[eng]:
                        for (s, v) in waits:
                            e.wait_ge(s, v)
                        if fn is not None:
                            fn(e).then_inc(sem, inc)
                getattr(block, eng)(body)


S = 4096
D = 1024
NT = S // 128
DEPTH = 4
ALPHA = (2 * DEPTH) ** 0.25
C_AQ, C_AK, C_AV, C_AF, C_PU, C_DQ, C_DK, C_DV, C_DA, C_DB, C_DG, C_GA, C_GP, C_GD = (
    0, 512, 1024, 1536, 1544, 2056, 2568, 3080, 3592, 3596, 3600, 4112, 5136, 6160)
INW = 7184


class Ctx:
    pass


DBG_OUT = set()
DBG_FLAGS = set()
NH_DBG = 8
NI_DBG = 16


_SBT_N = [0]


def sbt(nc, st, name, shape, dt):
    _SBT_N[0] += 1
    return st.enter_context(nc.sbuf_tensor(f"{name}_u{_SBT_N[0]}", shape, dt))


def declare_io(nc, layers, first, last):
    L = len(layers)
    c = Ctx()
    EI = "ExternalInput"
    c.x_in = nc.dram_tensor("x_in", [S, D], F32, kind=EI).ap()
    c.p = nc.dram_tensor("p", [L, S, 256], F32, kind=EI).ap()
    c.w_in = nc.dram_tensor("w_in", [L, D, INW], F32, kind=EI).ap()
    c.b_forget = nc.dram_tensor("b_forget", [L, 8, 1], F32, kind=EI).ap()
    c.pool_w = nc.dram_tensor("pool_w", [L, 4, 128, 128], F32, kind=EI).ap()
    c.pool_scale = nc.dram_tensor("pool_scale", [L, 4, 128, 1], F32, kind=EI).ap()
    c.dn_conv = nc.dram_tensor("dn_conv", [L, 4, 1536], F32, kind=EI).ap()
    c.dn_a_log = nc.dram_tensor("dn_a_log", [L, 4, 1], F32, kind=EI).ap()
    c.dn_dt_bias = nc.dram_tensor("dn_dt_bias", [L, 4, 1], F32, kind=EI).ap()
    c.dn_norm_w = nc.dram_tensor("dn_norm_w", [L, 128, 1], F32, kind=EI).ap()
    c.w_br = nc.dram_tensor("w_br", [L, 1536, D], F32, kind=EI).ap()
    c.w_out = nc.dram_tensor("w_out", [L, D, D], F32, kind=EI).ap()
    c.ln1_g = nc.dram_tensor("ln1_g", [L, D], F32, kind=EI).ap()
    c.ln1_b = nc.dram_tensor("ln1_b", [L, D], F32, kind=EI).ap()
    c.w_r = nc.dram_tensor("w_r", [L, D, 36], F32, kind=EI).ap()
    c.b_r = nc.dram_tensor("b_r", [L, 36], F32, kind=EI).ap()
    c.w_eg = nc.dram_tensor("w_eg", [L, 32, D, 512], F32, kind=EI).ap()
    c.w_eu = nc.dram_tensor("w_eu", [L, 32, D, 512], F32, kind=EI).ap()
    c.w_ed = nc.dram_tensor("w_ed", [L, 32, 512, D], F32, kind=EI).ap()
    c.w_pp = nc.dram_tensor("w_pp", [L, 256, D], F32, kind=EI).ap()
    c.w_pg = nc.dram_tensor("w_pg", [L, D, D], F32, kind=EI).ap()
    c.ln2_g = nc.dram_tensor("ln2_g", [L, D], F32, kind=EI).ap()
    c.ln2_b = nc.dram_tensor("ln2_b", [L, D], F32, kind=EI).ap()
    c.out = nc.dram_tensor("out", [S, D], F32, kind="ExternalOutput").ap()
    def scr(name, shape, dt):
        kind = "ExternalOutput" if name in DBG_OUT else "Internal"
        return nc.dram_tensor(name, shape, dt, kind=kind).ap()
    c.xs = scr("xs", [S, D], F32)
    c.x1s = scr("x1s", [S, D], F32)
    c.qkT = scr("qkT", [1024, S], BF16)
    c.vtm = scr("vtm", [S, 512], BF16)
    c.smT = scr("smT", [16, S], F32)
    c.fT = scr("fT", [2560, S], F32)
    c.yT = scr("yT", [1536, S], BF16)
    c.dbg1 = scr("dbg1", [128, 4096], F32)
    c.dnrow = scr("dnrow", [2, 4, S], F32)
    c.x1b = scr("x1b", [S, D], BF16)
    c.tab = scr("tab", [32 * 512, 16], I32)
    c.ybuf = scr("ybuf", [2 * S + 128, D], F32)
    c.dummy = scr("dly_scratch", [512, D], F32)
    return c


def make_consts(nc, st, P):
    k = Ctx()
    k.ones = sbt(nc, st, "c_ones", [128, 128], F32)
    k.ident = sbt(nc, st, "c_ident", [128, 128], F32)
    k.identb = sbt(nc, st, "c_identb", [128, 128], BF16)
    k.onesb = sbt(nc, st, "c_onesb", [128, 128], BF16)
    k.ps = [st.enter_context(nc.psum_tensor(f"ps{i}", [128, 512], F32)) for i in range(8)]
    P.op("gpsimd", lambda e: e.memset(k.ones[:], 1.0), writes=["c_ones"])
    P.op("gpsimd", lambda e: e.affine_select(out=k.ident[:], in_=k.ones[:], pattern=[[-1, 128]],
                                             compare_op=ALU.is_equal, fill=0.0, base=0, channel_multiplier=1),
         reads=["c_ones"], writes=["c_ident"])
    P.op("vector", lambda e: e.tensor_copy(k.identb[:], k.ident[:]), reads=["c_ident"], writes=["c_identb"])
    P.op("vector", lambda e: e.tensor_copy(k.onesb[:], k.ones[:]), reads=["c_ones"], writes=["c_onesb"])
    return k


def build_xT(nc, P, k, st, src, xT, tag):
    xin = [sbt(nc, st, f"{tag}_xin{i}", [128, D], F32) for i in range(2)]
    for t in range(NT):
        b = xin[t % 2]
        bn = f"{tag}_xin{t % 2}"
        P.dma("sync", bn, lambda e, b=b, t=t: e.dma_start(out=b[:], in_=src[t * 128:(t + 1) * 128, :]), writes=[bn])
        for half in range(2):
            pb = k.ps[(t % 2) * 2 + half]
            pn = f"ps{(t % 2) * 2 + half}"
            for q in range(4):
                kc = half * 4 + q
                P.op("tensor", lambda e, pb=pb, b=b, kc=kc, q=q: e.transpose(pb[:, q * 128:(q + 1) * 128], b[:, kc * 128:(kc + 1) * 128], k.ident[:]),
                     reads=[bn, "c_ident"], writes=[pn])
            eng = "vector" if half == 0 else "scalar"
            if eng == "vector":
                P.op("vector", lambda e, pb=pb, half=half, t=t: e.tensor_copy(
                    xT[:, half * 4:(half + 1) * 4, t * 128:(t + 1) * 128], pb[:].rearrange("p (a b) -> p a b", a=4)),
                    reads=[pn], writes=[f"{tag}_xT{t}_{half}"])
            else:
                P.op("scalar", lambda e, pb=pb, half=half, t=t: e.copy(
                    xT[:, half * 4:(half + 1) * 4, t * 128:(t + 1) * 128], pb[:].rearrange("p (a b) -> p a b", a=4)),
                    reads=[pn], writes=[f"{tag}_xT{t}_{half}"])


def stage1_proj(nc, P, k, c, li, src):
    with contextlib.ExitStack() as st:
        xT = sbt(nc, st, "s1_xT", [128, 8, S], BF16)
        build_xT(nc, P, k, st, src, xT, "s1")
        vstg = sbt(nc, st, "s1_vstg", [128, 8 * 512], BF16)
        wb = [sbt(nc, st, f"s1_w{i}", [128, 8, 512], BF16) for i in range(2)]
        stg = [sbt(nc, st, f"s1_stg{i}", [128, S], F32) for i in range(2)]
        stgb = [sbt(nc, st, f"s1_stgb{i}", [128, S], BF16) for i in range(2)]
        wv = c.w_in[li].rearrange("(kc kp) n -> kp kc n", kp=128)
        groups = [(C_AQ, 512, "q"), (C_AK, 512, "k"), (C_PU, 512, "f0"), (C_DQ, 512, "f1"), (C_DK, 512, "f2"),
                  (C_DV, 512, "f3"), (C_DG, 512, "f4"), (C_AF, 16, "sm0"), (C_DA, 8, "sm1"), (C_AV, 512, "v")]
        gi = 0
        nev = 0
        nst = 0
        for (c0, ncols, kind) in groups:
            w = wb[gi % 2]
            wn = f"s1_w{gi % 2}"
            gi += 1
            if kind == "sm0":
                P.dma("gpsimd", wn, lambda e, w=w: e.dma_start(out=w[:, :, 0:8], in_=wv[:, :, C_AF:C_AF + 8]), writes=[wn])
                P.dma("gpsimd", wn, lambda e, w=w: e.dma_start(out=w[:, :, 8:16], in_=wv[:, :, C_DA:C_DA + 8]), writes=[wn])
            elif kind == "sm1":
                gi -= 1
                continue
            else:
                P.dma("gpsimd", wn, lambda e, w=w, c0=c0, ncols=ncols: e.dma_start(out=w[:, :, 0:ncols], in_=wv[:, :, c0:c0 + ncols]), writes=[wn])
            if kind == "v":
                for t in range(NT):
                    pb = k.ps[4 + t % 4]
                    pn = f"ps{4 + t % 4}"
                    for kc in range(8):
                        P.op("tensor", lambda e, pb=pb, kc=kc, t=t, w=w: e.matmul(pb[:, :], lhsT=xT[:, kc, t * 128:(t + 1) * 128], rhs=w[:, kc, :], start=(kc == 0), stop=(kc == 7)),
                             reads=[wn, f"s1_xT{t}_{kc // 4}"], writes=[pn])
                    vslot = t % 8
                    eng = "vector" if t % 2 == 0 else "scalar"
                    fn = (lambda e, pb=pb, vslot=vslot: e.tensor_copy(vstg[:, vslot * 512:(vslot + 1) * 512], pb[:, :])) if eng == "vector" else \
                         (lambda e, pb=pb, vslot=vslot: e.copy(vstg[:, vslot * 512:(vslot + 1) * 512], pb[:, :]))
                    P.op(eng, fn, reads=[pn], writes=[f"s1_vstg_{vslot}"])
                    if vslot == 7:
                        t0 = t - 7
                        P.dma("sync", "s1_vout", lambda e, t0=t0: e.dma_start(
                            out=c.vtm[t0 * 128:(t0 + 8) * 128, :].rearrange("(a p) n -> p a n", p=128),
                            in_=vstg[:, :].rearrange("p (a n) -> p a n", a=8)),
                            reads=[f"s1_vstg_{v}" for v in range(8)])
                continue
            nch = (ncols + 127) // 128
            for ch in range(nch):
                m = min(128, ncols - ch * 128)
                isb = kind in ("q", "k")
                sbuf = (stgb if isb else stg)[nst % 2]
                sname = ("s1_stgb" if isb else "s1_stg") + str(nst % 2)
                nst += 1
                for tc in range(8):
                    pb = k.ps[4 + nev % 4]
                    pn = f"ps{4 + nev % 4}"
                    for kc in range(8):
                        P.op("tensor", lambda e, pb=pb, kc=kc, tc=tc, w=w, ch=ch, m=m: e.matmul(
                            pb[0:m, :], lhsT=w[:, kc, ch * 128:ch * 128 + m], rhs=xT[:, kc, tc * 512:(tc + 1) * 512], start=(kc == 0), stop=(kc == 7)),
                            reads=[wn] + [f"s1_xT{tt}_{kc // 4}" for tt in range(tc * 4, tc * 4 + 4)], writes=[pn])
                    sc = 0.125 if kind == "q" else 1.0
                    if nev % 2 == 0:
                        P.op("vector", lambda e, pb=pb, tc=tc, sbuf=sbuf, m=m, sc=sc: e.tensor_scalar(
                            out=sbuf[0:m, tc * 512:(tc + 1) * 512], in0=pb[0:m, :], scalar1=sc, scalar2=None, op0=ALU.mult),
                            reads=[pn], writes=[sname])
                    else:
                        P.op("scalar", lambda e, pb=pb, tc=tc, sbuf=sbuf, m=m, sc=sc: e.mul(
                            sbuf[0:m, tc * 512:(tc + 1) * 512], pb[0:m, :], sc),
                            reads=[pn], writes=[sname])
                    nev += 1
                if kind == "q":
                    dst = c.qkT[ch * 128:(ch + 1) * 128, :]
                elif kind == "k":
                    dst = c.qkT[512 + ch * 128:512 + (ch + 1) * 128, :]
                elif kind == "sm0":
                    dst = c.smT[0:16, :]
                else:
                    fi = int(kind[1])
                    dst = c.fT[fi * 512 + ch * 128:fi * 512 + (ch + 1) * 128, :]
                P.dma("sync", "s1_out", lambda e, dst=dst, sbuf=sbuf, m=m: e.dma_start(out=dst, in_=sbuf[0:m, :]), reads=[sname])
    P.barrier()


def stage2_attn(nc, P, k, c, li):
    with contextlib.ExitStack() as st:
        qT = sbt(nc, st, "s2_q", [128, 4, S], BF16)
        kT = sbt(nc, st, "s2_k", [128, 4, S], BF16)
        va = sbt(nc, st, "s2_v", [128, NT, 8, 65], BF16)
        af = sbt(nc, st, "s2_af", [8, S], F32)
        cc = sbt(nc, st, "s2_cc", [8, S], F32)
        ones8 = sbt(nc, st, "s2_ones8", [8, S], F32)
        nb = sbt(nc, st, "s2_nb", [8, 1], F32)
        ck = sbt(nc, st, "s2_ck", [128, NT, 8], F32)
        rball = sbt(nc, st, "s2_rb", [128, NT, 8], F32)
        sel0 = sbt(nc, st, "s2_sel0", [128, 128], F32)
        biasb = [sbt(nc, st, f"s2_bias{i}", [128, NT], F32) for i in range(4)]
        PT = [sbt(nc, st, f"s2_PT{i}", [128, 256], BF16) for i in range(6)]
        rden = sbt(nc, st, "s2_rden", [128, 256], F32)
        bc = sbt(nc, st, "s2_bc", [64, 256], F32)
        ystg = [sbt(nc, st, f"s2_y{i}", [64, S], BF16) for i in range(2)]
        for pr in range(4):
            P.dma("sync", f"s2_q{pr}", lambda e, pr=pr: e.dma_start(out=qT[:, pr, :], in_=c.qkT[pr * 128:(pr + 1) * 128, :]), reads=["d_qkT"], writes=[f"s2_q{pr}"])
            P.dma("sync", f"s2_k{pr}", lambda e, pr=pr: e.dma_start(out=kT[:, pr, :], in_=c.qkT[512 + pr * 128:512 + (pr + 1) * 128, :]), reads=["d_qkT"], writes=[f"s2_k{pr}"])
        P.op("gpsimd", lambda e: e.memset(va[:, :, :, 64:65], 1.0), writes=["s2_v1"])
        vsrc = c.vtm.rearrange("(t p) (h d) -> p t h d", p=128, h=8)
        for g in range(NT):
            P.dma("sync", "s2_v", lambda e, g=g: e.dma_start(out=va[:, g, :, 0:64], in_=vsrc[:, g, :, :]), reads=["d_vtm"], writes=["s2_v"])
        P.dma("sync", "s2_af", lambda e: e.dma_start(out=af[:], in_=c.smT[0:8, :]), writes=["s2_af"])
        P.dma("sync", "s2_nb", lambda e: e.dma_start(out=nb[:], in_=c.b_forget[li]), writes=["s2_nb"])
        P.op("gpsimd", lambda e: e.memset(ones8[:], 1.0), writes=["s2_ones8"])
        P.op("gpsimd", lambda e: e.memset(sel0[:], 0.0), writes=["s2_sel0"])
        P.op("gpsimd", lambda e: e.memset(sel0[0:1, :], 1.0), writes=["s2_sel0"])
        P.op("vector", lambda e: e.tensor_scalar(out=nb[:], in0=nb[:], scalar1=-1.0, scalar2=None, op0=ALU.mult), reads=["s2_nb"], writes=["s2_nb"])
        P.op("scalar", lambda e: e.activation(out=af[:], in_=af[:], func=AF.Exp, bias=nb[:, 0:1], scale=-1.0), reads=["s2_af", "s2_nb"], writes=["s2_af"])
        P.op("scalar", lambda e: e.activation(out=af[:], in_=af[:], func=AF.Ln, bias=k.ones[0:8, 0:1], scale=1.0), reads=["s2_af", "c_ones"], writes=["s2_af"])
        P.op("vector", lambda e: e.tensor_scalar(out=af[:], in0=af[:], scalar1=-1.0, scalar2=None, op0=ALU.mult), reads=["s2_af"], writes=["s2_af"])
        P.op("vector", lambda e: e.tensor_tensor_scan(out=cc[:], data0=ones8[:], data1=af[:], initial=0.0, op0=ALU.mult, op1=ALU.add),
             reads=["s2_af", "s2_ones8"], writes=["s2_cc"])
        for j in range(NT):
            P.op("tensor", lambda e, j=j: e.transpose(k.ps[7][:, j * 8:(j + 1) * 8], cc[0:8, j * 128:(j + 1) * 128], k.ident[0:8, 0:8]),
                 reads=["s2_cc", "c_ident"], writes=["ps7"])
        P.op("vector", lambda e: e.tensor_copy(ck[:].rearrange("p a b -> p (a b)"), k.ps[7][:, 0:256]), reads=["ps7"], writes=["s2_ck"])
        P.op("tensor", lambda e: e.matmul(k.ps[7][:, 256:512], lhsT=sel0[:], rhs=ck[:].rearrange("p a b -> p (a b)"), start=True, stop=True),
             reads=["s2_ck", "s2_sel0"], writes=["ps7"])
        P.op("vector", lambda e: e.tensor_copy(rball[:].rearrange("p a b -> p (a b)"), k.ps[7][:, 256:512]), reads=["ps7"], writes=["s2_rb"])

        if "s2_setup" in DBG_FLAGS:
            P.dma("sync", "dbgo", lambda e: e.dma_start(out=c.dbg1[:, 0:256], in_=ck[:].rearrange("p a b -> p (a b)")), reads=["s2_ck"])
            P.dma("sync", "dbgo", lambda e: e.dma_start(out=c.dbg1[:, 256:512], in_=rball[:].rearrange("p a b -> p (a b)")), reads=["s2_rb"])
            P.barrier()
            return
        units = [(h, i, j) for h in range(NH_DBG) for i in range(NI_DBG) for j in range(2 * i + 2)]

        def issue_S(n):
            h, i, j = units[n]
            slot = n % 4
            pb = k.ps[slot][:, 0:256]
            hp, hh = h // 2, h % 2
            bb = biasb[(h * 16 + i) % 4]
            bn = f"s2_bias{(h * 16 + i) % 4}"
            if j == 0:
                nj = 2 * i + 2
                P.op("vector", lambda e: e.tensor_scalar(out=bb[:, 0:nj], in0=ck[:, 0:nj, h], scalar1=rball[:, 2 * i + 1, h:h + 1], scalar2=-1.0,
                                                         op0=ALU.subtract, op1=ALU.mult), reads=["s2_ck", "s2_rb"], writes=[bn])
            P.op("tensor", lambda e: e.matmul(pb, lhsT=kT[hh * 64:(hh + 1) * 64, hp, j * 128:(j + 1) * 128],
                                              rhs=qT[hh * 64:(hh + 1) * 64, hp, i * 256:(i + 1) * 256], start=True, stop=True),
                 reads=[f"s2_k{hp}", f"s2_q{hp}"], writes=[f"psS{slot}"])
            pt = PT[n % 6]
            ptn = f"s2_PT{n % 6}"
            P.op("scalar", lambda e: e.activation(out=pt[:], in_=pb, func=AF.Exp, bias=bb[:, j:j + 1], scale=1.0),
                 reads=[f"psS{slot}", bn], writes=[ptn])
            if j >= 2 * i:
                P.op("gpsimd", lambda e: e.affine_select(out=pt[:], in_=pt[:], pattern=[[1, 256]], compare_op=ALU.is_ge, fill=0.0,
                                                         base=i * 256 - j * 128, channel_multiplier=-1), reads=[ptn], writes=[ptn])

        def fin_a(h, i):
            ob = k.ps[4 + (h * 16 + i) % 2]
            on = f"ps{4 + (h * 16 + i) % 2}"
            P.op("vector", lambda e: e.reciprocal(rden[64:65, :], ob[64:65, 0:256]), reads=[on], writes=["s2_rden"])

        def fin_b(h, i):
            ob = k.ps[4 + (h * 16 + i) % 2]
            on = f"ps{4 + (h * 16 + i) % 2}"
            P.op("tensor", lambda e: e.matmul(k.ps[6][0:64, 0:256], lhsT=k.ones[64:65, 0:64], rhs=rden[64:65, :], start=True, stop=True),
                 reads=["s2_rden", "c_ones"], writes=["ps6"])
            P.op("scalar", lambda e: e.copy(bc[:], k.ps[6][0:64, 0:256]), reads=["ps6"], writes=["s2_bc"])
            ys = ystg[h % 2]
            P.op("vector", lambda e: e.tensor_tensor(out=ys[:, i * 256:(i + 1) * 256], in0=ob[0:64, 0:256], in1=bc[:], op=ALU.mult),
                 reads=[on, "s2_bc"], writes=[f"s2_y{h % 2}"])
            if i == NI_DBG - 1:
                P.dma("sync", "s2_yout", lambda e: e.dma_start(out=c.yT[h * 64:(h + 1) * 64, :], in_=ys[:]), reads=[f"s2_y{h % 2}"], writes=["d_yT"])

        pending = []
        issue_S(0)
        issue_S(1)
        for n in range(len(units)):
            if n + 2 < len(units):
                issue_S(n + 2)
            h, i, j = units[n]
            ob = k.ps[4 + (h * 16 + i) % 2]
            on = f"ps{4 + (h * 16 + i) % 2}"
            pt = PT[n % 6]
            P.op("tensor", lambda e, ob=ob, pt=pt, h=h, i=i, j=j: e.matmul(ob[0:65, 0:256], lhsT=va[:, j, h, :], rhs=pt[:], start=(j == 0), stop=(j == 2 * i + 1)),
                 reads=[f"s2_PT{n % 6}", "s2_v", "s2_v1"], writes=[on])
            for (hh_, ii_) in pending:
                fin_b(hh_, ii_)
            pending = []
            if j == 2 * i + 1:
                fin_a(h, i)
                pending.append((h, i))
        for (hh_, ii_) in pending:
            fin_b(hh_, ii_)
    P.barrier()


def stage3_pool(nc, P, k, c, li):
    with contextlib.ExitStack() as st:
        u = [sbt(nc, st, f"s3_u{i}", [128, S], F32) for i in range(2)]
        ab = [sbt(nc, st, f"s3_a{i}", [128, S], F32) for i in range(2)]
        dbf = sbt(nc, st, "s3_d", [128, S], BF16)
        ys = [sbt(nc, st, f"s3_y{i}", [128, S], BF16) for i in range(2)]
        pw = sbt(nc, st, "s3_pw", [128, 4, 128], BF16)
        psc = sbt(nc, st, "s3_psc", [128, 4], F32)
        inv = sbt(nc, st, "s3_inv", [128, 4, 16], F32)
        tmp = sbt(nc, st, "s3_tmp", [128, 16], F32)
        P.dma("gpsimd", "s3_pw", lambda e: e.dma_start(out=pw[:], in_=c.pool_w[li].rearrange("g c d -> c g d")), writes=["s3_pw"])
        for g in range(4):
            P.dma("sync", "s3_psc", lambda e, g=g: e.dma_start(out=psc[:, g:g + 1], in_=c.pool_scale[li, g]), writes=["s3_psc"])
        for g in range(4):
            w = 2 ** (g + 1)
            P.op("gpsimd", lambda e, g=g: e.iota(inv[:, g, :], pattern=[[1, 16]], base=1, channel_multiplier=0, allow_small_or_imprecise_dtypes=True), writes=["s3_inv"])
            P.op("gpsimd", lambda e, g=g, w=w: e.tensor_scalar(out=inv[:, g, :], in0=inv[:, g, :], scalar1=float(w), scalar2=None, op0=ALU.min), reads=["s3_inv"], writes=["s3_inv"])
        P.op("vector", lambda e: e.reciprocal(inv[:].rearrange("p a b -> p (a b)"), inv[:].rearrange("p a b -> p (a b)")), reads=["s3_inv"], writes=["s3_inv"])
        nev = 0
        for g in range(4):
            w = 2 ** (g + 1)
            ug = u[g % 2]
            un = f"s3_u{g % 2}"
            P.dma("sync", un, lambda e, g=g, ug=ug: e.dma_start(out=ug[:], in_=c.fT[g * 128:(g + 1) * 128, :]), writes=[un])
            src, srcn = ug, un
            for m in range(g + 1):
                sh = 2 ** m
                dst, dstn = ab[m % 2], f"s3_a{m % 2}"
                P.op("vector", lambda e, dst=dst, src=src, sh=sh: e.tensor_tensor(out=dst[:, sh:], in0=src[:, sh:], in1=src[:, 0:S - sh], op=ALU.add), reads=[srcn], writes=[dstn])
                P.op("gpsimd", lambda e, dst=dst, src=src, sh=sh: e.tensor_copy(dst[:, 0:sh], src[:, 0:sh]), reads=[srcn, dstn], writes=[dstn])
                src, srcn = dst, dstn
            rs = [srcn]
            P.op("vector", lambda e, src=src, ug=ug, w=w: e.scalar_tensor_tensor(out=dbf[:], in0=src[:], scalar=1.0 / w, in1=ug[:], op0=ALU.mult, op1=ALU.subtract),
                 reads=rs + [un], writes=["s3_d"])
            P.op("vector", lambda e, src=src, g=g, w=w: e.tensor_tensor(out=tmp[:, 0:w], in0=src[:, 0:w], in1=inv[:, g, 0:w], op=ALU.mult), reads=rs + ["s3_inv"], writes=["s3_tmp"])
            P.op("vector", lambda e, ug=ug, w=w: e.tensor_tensor(out=dbf[:, 0:w], in0=tmp[:, 0:w], in1=ug[:, 0:w], op=ALU.subtract), reads=["s3_tmp", un, "s3_d"], writes=["s3_d"])
            yb, yn = ys[g % 2], f"s3_y{g % 2}"
            for tc in range(8):
                pb, pn = k.ps[nev % 4], f"ps{nev % 4}"
                nev += 1
                P.op("tensor", lambda e, pb=pb, g=g, tc=tc: e.matmul(pb[:, :], lhsT=pw[:, g, :], rhs=dbf[:, tc * 512:(tc + 1) * 512], start=True, stop=True),
                     reads=["s3_pw", "s3_d"], writes=[pn])
                P.op("scalar", lambda e, pb=pb, g=g, tc=tc, yb=yb: e.activation(out=yb[:, tc * 512:(tc + 1) * 512], in_=pb[:, :], func=AF.Copy, scale=psc[:, g:g + 1]),
                     reads=[pn, "s3_psc"], writes=[yn])
            P.dma("sync", "s3_yout", lambda e, g=g, yb=yb: e.dma_start(out=c.yT[512 + g * 128:512 + (g + 1) * 128, :], in_=yb[:]), reads=[yn])
    P.barrier()


def layer_norm_tile(P, k, z, zn, gam, bet, stats, mv, rstd, out, outn, eps_ap, tag):
    for hh in range(2):
        P.op("vector", lambda e, hh=hh: e.bn_stats(stats[:, hh, :], z[:, hh * 512:(hh + 1) * 512]), reads=[zn], writes=[tag + "_st"])
    P.op("vector", lambda e: e.bn_aggr(mv[:], stats[:]), reads=[tag + "_st"], writes=[tag + "_mv"])
    P.op("scalar", lambda e: e.activation(out=rstd[:], in_=mv[:, 1:2], func=AF.Sqrt, bias=eps_ap, scale=1.0), reads=[tag + "_mv"], writes=[tag + "_rs"])
    P.op("vector", lambda e: e.reciprocal(rstd[:], rstd[:]), reads=[tag + "_rs"], writes=[tag + "_rs"])
    P.op("vector", lambda e: e.tensor_scalar(out=z[:], in0=z[:], scalar1=mv[:, 0:1], scalar2=rstd[:, 0:1], op0=ALU.subtract, op1=ALU.mult),
         reads=[zn, tag + "_mv", tag + "_rs"], writes=[zn])
    P.op("gpsimd", lambda e: e.tensor_tensor(out=z[:], in0=z[:], in1=gam[:], op=ALU.mult), reads=[zn, "lnp"], writes=[zn])
    P.op("gpsimd", lambda e: e.tensor_tensor(out=out[:], in0=z[:], in1=bet[:], op=ALU.add), reads=[zn, "lnp"], writes=[outn])


def stage5_merge(nc, P, k, c, li, src):
    with contextlib.ExitStack() as st:
        wg = sbt(nc, st, "s5_wg", [128, 8, 3072], BF16)
        wbr = sbt(nc, st, "s5_wbr", [128, 12, 1024], BF16)
        wo = sbt(nc, st, "s5_wo", [128, 8, 1024], BF16)
        gam = sbt(nc, st, "s5_gam", [128, D], F32)
        bet = sbt(nc, st, "s5_bet", [128, D], F32)
        eps = sbt(nc, st, "s5_eps", [128, 1], F32)
        xr = sbt(nc, st, "s5_xr", [128, 4, D], F32)
        xTc = sbt(nc, st, "s5_xTc", [128, 8, 512], BF16)
        yTc = sbt(nc, st, "s5_yTc", [128, 12, 512], BF16)
        sig = [sbt(nc, st, f"s5_sig{i}", [128, 512], F32) for i in range(2)]
        macc = sbt(nc, st, "s5_macc", [128, 512], F32)
        mtmp = sbt(nc, st, "s5_mtmp", [128, 512], F32)
        mT = sbt(nc, st, "s5_mT", [128, 8, 512], BF16)
        z = [sbt(nc, st, f"s5_z{i}", [128, D], F32) for i in range(2)]
        xo = [sbt(nc, st, f"s5_xo{i}", [128, D], F32) for i in range(2)]
        stats = sbt(nc, st, "s5_stats", [128, 2, 6], F32)
        mv = sbt(nc, st, "s5_mv", [128, 2], F32)
        rstd = sbt(nc, st, "s5_rstd", [128, 1], F32)
        wv = c.w_in[li].rearrange("(kc kp) n -> kp kc n", kp=128)
        for q in range(6):
            P.dma("gpsimd", "s5_wg", lambda e, q=q: e.dma_start(out=wg[:, :, q * 512:(q + 1) * 512], in_=wv[:, :, C_GA + q * 512:C_GA + (q + 1) * 512]), writes=["s5_wg"])
        wbv = c.w_br[li].rearrange("(kc kp) n -> kp kc n", kp=128)
        for q in range(3):
            P.dma("gpsimd", "s5_wbr", lambda e, q=q: e.dma_start(out=wbr[:, q * 4:(q + 1) * 4, :], in_=wbv[:, q * 4:(q + 1) * 4, :]), writes=["s5_wbr"])
        wov = c.w_out[li].rearrange("(kc kp) n -> kp kc n", kp=128)
        for q in range(2):
            P.dma("gpsimd", "s5_wo", lambda e, q=q: e.dma_start(out=wo[:, q * 4:(q + 1) * 4, :], in_=wov[:, q * 4:(q + 1) * 4, :]), writes=["s5_wo"])
        P.dma("sync", "lnp", lambda e: e.dma_start(out=gam[:], in_=c.ln1_g[li].partition_broadcast(128)), writes=["lnp"])
        P.dma("sync", "lnp", lambda e: e.dma_start(out=bet[:], in_=c.ln1_b[li].partition_broadcast(128)), writes=["lnp"])
        P.op("vector", lambda e: e.memset(eps[:], 1e-5), writes=["s5_eps"])
        nps = 0
        for tc in range(8):
            for tt in range(4):
                t = tc * 4 + tt
                P.dma("sync", "s5_xr", lambda e, t=t, tt=tt: e.dma_start(out=xr[:, tt, :], in_=src[t * 128:(t + 1) * 128, :]), writes=[f"s5_xr{tt}"])
            P.dma("sync", "s5_yTc", lambda e, tc=tc: e.dma_start(out=yTc[:], in_=c.yT[:, tc * 512:(tc + 1) * 512].rearrange("(a p) n -> p a n", p=128)), writes=["s5_yTc"])
            for tt in range(4):
                for half in range(2):
                    pb, pn = k.ps[nps % 8], f"ps{nps % 8}"
                    nps += 1
                    for q in range(4):
                        kc = half * 4 + q
                        P.op("tensor", lambda e, pb=pb, tt=tt, kc=kc, q=q: e.transpose(pb[:, q * 128:(q + 1) * 128], xr[:, tt, kc * 128:(kc + 1) * 128], k.ident[:]),
                             reads=[f"s5_xr{tt}", "c_ident"], writes=[pn])
                    fn = (lambda e, pb=pb, half=half, tt=tt: e.tensor_copy(xTc[:, half * 4:(half + 1) * 4, tt * 128:(tt + 1) * 128], pb[:].rearrange("p (a b) -> p a b", a=4))) if half == 0 else \
                         (lambda e, pb=pb, half=half, tt=tt: e.copy(xTc[:, half * 4:(half + 1) * 4, tt * 128:(tt + 1) * 128], pb[:].rearrange("p (a b) -> p a b", a=4)))
                    P.op("vector" if half == 0 else "scalar", fn, reads=[pn], writes=[f"s5_xTc{tt}_{half}"])
            xres = [f"s5_xTc{tt}_{h}" for tt in range(4) for h in range(2)]
            for n in range(8):
                for br in range(3):
                    pg, pgn = k.ps[nps % 8], f"ps{nps % 8}"
                    nps += 1
                    pbr, pbn = k.ps[nps % 8], f"ps{nps % 8}"
                    nps += 1
                    for kc in range(8):
                        P.op("tensor", lambda e, pg=pg, kc=kc, br=br, n=n: e.matmul(pg[:, :], lhsT=wg[:, kc, br * 1024 + n * 128:br * 1024 + (n + 1) * 128], rhs=xTc[:, kc, :], start=(kc == 0), stop=(kc == 7)),
                             reads=["s5_wg"] + xres, writes=[pgn])
                    sg, sgn = sig[(n * 3 + br) % 2], f"s5_sig{(n * 3 + br) % 2}"
                    P.op("scalar", lambda e, pg=pg, sg=sg: e.activation(out=sg[:], in_=pg[:, :], func=AF.Sigmoid), reads=[pgn], writes=[sgn])
                    for c4 in range(4):
                        P.op("tensor", lambda e, pbr=pbr, c4=c4, br=br, n=n: e.matmul(pbr[:, :], lhsT=wbr[:, br * 4 + c4, n * 128:(n + 1) * 128], rhs=yTc[:, br * 4 + c4, :], start=(c4 == 0), stop=(c4 == 3)),
                             reads=["s5_wbr", "s5_yTc"], writes=[pbn])
                    if br == 0:
                        P.op("vector", lambda e, pbr=pbr, sg=sg: e.tensor_tensor(out=macc[:], in0=pbr[:, :], in1=sg[:], op=ALU.mult), reads=[pbn, sgn], writes=["s5_macc"])
                    elif br == 1:
                        P.op("vector", lambda e, pbr=pbr, sg=sg: e.tensor_tensor(out=mtmp[:], in0=pbr[:, :], in1=sg[:], op=ALU.mult), reads=[pbn, sgn], writes=["s5_mtmp"])
                        P.op("gpsimd", lambda e: e.tensor_tensor(out=macc[:], in0=macc[:], in1=mtmp[:], op=ALU.add), reads=["s5_macc", "s5_mtmp"], writes=["s5_macc"])
                    else:
                        P.op("vector", lambda e, pbr=pbr, sg=sg: e.tensor_tensor(out=mtmp[:], in0=pbr[:, :], in1=sg[:], op=ALU.mult), reads=[pbn, sgn], writes=["s5_mtmp"])
                        P.op("gpsimd", lambda e, n=n: e.tensor_tensor(out=mT[:, n, :], in0=macc[:], in1=mtmp[:], op=ALU.add), reads=["s5_macc", "s5_mtmp"], writes=[f"s5_mT{n}"])
            mres = [f"s5_mT{n}" for n in range(8)]
            for tt in range(4):
                t = tc * 4 + tt
                zb, zn = z[t % 2], f"s5_z{t % 2}"
                for nh in range(2):
                    po, pon = k.ps[nps % 8], f"ps{nps % 8}"
                    nps += 1
                    for kc in range(8):
                        P.op("tensor", lambda e, po=po, kc=kc, tt=tt, nh=nh: e.matmul(po[:, :], lhsT=mT[:, kc, tt * 128:(tt + 1) * 128], rhs=wo[:, kc, nh * 512:(nh + 1) * 512], start=(kc == 0), stop=(kc == 7)),
                             reads=["s5_wo"] + mres, writes=[pon])
                    P.op("vector", lambda e, po=po, zb=zb, tt=tt, nh=nh: e.scalar_tensor_tensor(out=zb[:, nh * 512:(nh + 1) * 512], in0=xr[:, tt, nh * 512:(nh + 1) * 512], scalar=ALPHA, in1=po[:, :], op0=ALU.mult, op1=ALU.add),
                         reads=[pon, f"s5_xr{tt}"], writes=[zn])
                ob, on = xo[t % 2], f"s5_xo{t % 2}"
                layer_norm_tile(P, k, zb, zn, gam, bet, stats, mv, rstd, ob, on, eps[:, 0:1], "s5")
                P.dma("sync", "s5_out", lambda e, t=t, ob=ob: e.dma_start(out=c.x1s[t * 128:(t + 1) * 128, :], in_=ob[:]), reads=[on])
    P.barrier()


def stage4_zero(nc, P, k, c, li):
    with contextlib.ExitStack() as st:
        zt = sbt(nc, st, "s4_z", [128, S], BF16)
        P.op("gpsimd", lambda e: e.memset(zt[:], 0.0), writes=["s4_z"])
        for h in range(4):
            P.dma("sync", "s4_out", lambda e, h=h: e.dma_start(out=c.yT[1024 + h * 128:1024 + (h + 1) * 128, :], in_=zt[:]), reads=["s4_z"])
    P.barrier()


def stage6_moe(nc, P, k, c, li, dst):
    with contextlib.ExitStack() as st:
        wr = sbt(nc, st, "s6_wr", [128, 8, 36], F32)
        brb = sbt(nc, st, "s6_brb", [128, 36], F32)
        wpg = sbt(nc, st, "s6_wpg", [128, 8, D], BF16)
        wpp = sbt(nc, st, "s6_wpp", [128, 2, D], BF16)
        gam = sbt(nc, st, "s6_gam", [128, D], F32)
        bet = sbt(nc, st, "s6_bet", [128, D], F32)
        eps = sbt(nc, st, "s6_eps", [128, 1], F32)
        acc = sbt(nc, st, "s6_acc", [128, 8, D], F32)
        x1T = sbt(nc, st, "s6_x1T", [128, 8, 1024], BF16)
        xTf = sbt(nc, st, "s6_xTf", [128, 8, 128], F32)
        xin = [sbt(nc, st, f"s6_xin{i}", [128, D], F32) for i in range(2)]
        wgu = [sbt(nc, st, f"s6_wgu{i}", [128, 8, 1024], BF16) for i in range(2)]
        wd = [sbt(nc, st, f"s6_wd{i}", [128, 4, D], BF16) for i in range(2)]
        hT = [sbt(nc, st, f"s6_hT{i}", [128, 4, 512], BF16) for i in range(2)]
        sgl = [sbt(nc, st, f"s6_sg{i}", [128, 512], F32) for i in range(2)]
        lg = sbt(nc, st, "s6_lg", [128, 8, 36], F32)
        G = sbt(nc, st, "s6_G", [128, 8, 32], F32)
        sm = sbt(nc, st, "s6_sm", [128, 16], F32)
        t4 = sbt(nc, st, "s6_t4", [128, 4], F32)
        pen = sbt(nc, st, "s6_pen", [128, 4], F32)
        mk = sbt(nc, st, "s6_mk", [128, 32], F32)
        mk2 = sbt(nc, st, "s6_mk2", [128, 32], F32)
        oh = sbt(nc, st, "s6_oh", [128, 32], F32)
        pin = sbt(nc, st, "s6_pin", [128, 256], F32)
        pT = sbt(nc, st, "s6_pT", [128, 2, 128], BF16)
        sgt = sbt(nc, st, "s6_sgt", [128, D], F32)
        z = [sbt(nc, st, f"s6_z{i}", [128, D], F32) for i in range(2)]
        xo = [sbt(nc, st, f"s6_xo{i}", [128, D], F32) for i in range(2)]
        stats = sbt(nc, st, "s6_stats", [128, 2, 6], F32)
        mv = sbt(nc, st, "s6_mv", [128, 2], F32)
        rstd = sbt(nc, st, "s6_rstd", [128, 1], F32)
        P.dma("sync", "s6_wr", lambda e: e.dma_start(out=wr[:], in_=c.w_r[li].rearrange("(kc kp) n -> kp kc n", kp=128)), writes=["s6_wr"])
        P.dma("sync", "s6_brb", lambda e: e.dma_start(out=brb[:], in_=c.b_r[li].partition_broadcast(128)), writes=["s6_brb"])
        wpgv = c.w_pg[li].rearrange("(kc kp) n -> kp kc n", kp=128)
        for q in range(2):
            P.dma("gpsimd", "s6_wpg", lambda e, q=q: e.dma_start(out=wpg[:, q * 4:(q + 1) * 4, :], in_=wpgv[:, q * 4:(q + 1) * 4, :]), writes=["s6_wpg"])
        P.dma("gpsimd", "s6_wpp", lambda e: e.dma_start(out=wpp[:], in_=c.w_pp[li].rearrange("(kc kp) n -> kp kc n", kp=128)), writes=["s6_wpp"])
        P.dma("sync", "lnp", lambda e: e.dma_start(out=gam[:], in_=c.ln2_g[li].partition_broadcast(128)), writes=["lnp"])
        P.dma("sync", "lnp", lambda e: e.dma_start(out=bet[:], in_=c.ln2_b[li].partition_broadcast(128)), writes=["lnp"])
        P.op("vector", lambda e: e.memset(eps[:], 1e-5), writes=["s6_eps"])
        nps = 0
        nw = 0
        for qt in range(4):
            for tt in range(8):
                t = qt * 8 + tt
                xb, xn = xin[t % 2], f"s6_xin{t % 2}"
                P.dma("sync", xn, lambda e, xb=xb, t=t: e.dma_start(out=xb[:], in_=c.x1s[t * 128:(t + 1) * 128, :]), writes=[xn])
                for half in range(2):
                    pb, pn = k.ps[nps % 8], f"ps{nps % 8}"
                    nps += 1
                    for q in range(4):
                        kc = half * 4 + q
                        P.op("tensor", lambda e, pb=pb, xb=xb, kc=kc, q=q: e.transpose(pb[:, q * 128:(q + 1) * 128], xb[:, kc * 128:(kc + 1) * 128], k.ident[:]),
                             reads=[xn, "c_ident"], writes=[pn])
                    P.op("vector", lambda e, pb=pb, half=half, tt=tt: e.tensor_copy(x1T[:, half * 4:(half + 1) * 4, tt * 128:(tt + 1) * 128], pb[:].rearrange("p (a b) -> p a b", a=4)),
                         reads=[pn], writes=[f"s6_x1T{tt}_{half}"])
                    P.op("scalar", lambda e, pb=pb, half=half: e.copy(xTf[:, half * 4:(half + 1) * 4, :], pb[:].rearrange("p (a b) -> p a b", a=4)),
                         reads=[pn, f"s6_x1T{tt}_{half}"], writes=[f"s6_xTf{half}"])
                pr, prn = k.ps[nps % 8], f"ps{nps % 8}"
                nps += 1
                for kc in range(8):
                    P.op("tensor", lambda e, pr=pr, kc=kc: e.matmul(pr[:, 0:36], lhsT=xTf[:, kc, :], rhs=wr[:, kc, :], start=(kc == 0), stop=(kc == 7)),
                         reads=[f"s6_xTf{kc // 4}", "s6_wr"], writes=[prn])
                P.op("vector", lambda e, pr=pr, tt=tt: e.tensor_tensor(out=lg[:, tt, :], in0=pr[:, 0:36], in1=brb[:], op=ALU.add), reads=[prn, "s6_brb"], writes=["s6_lg"])
                V = lambda fn, r, w: P.op("vector", fn, reads=r, writes=w)
                V(lambda e, tt=tt: e.reduce_max(out=sm[:, 0:1], in_=lg[:, tt, 0:4], axis=AX.X), ["s6_lg"], ["s6_sm"])
                V(lambda e, tt=tt: e.tensor_scalar(out=t4[:], in0=lg[:, tt, 0:4], scalar1=sm[:, 0:1], scalar2=None, op0=ALU.is_equal), ["s6_lg", "s6_sm"], ["s6_t4"])
                V(lambda e: e.tensor_scalar(out=pen[:], in0=t4[:], scalar1=-1.0, scalar2=1e30, op0=ALU.add, op1=ALU.mult), ["s6_t4"], ["s6_pen"])
                V(lambda e: e.tensor_scalar(out=sm[:, 1:2], in0=sm[:, 0:1], scalar1=-1.0, scalar2=None, op0=ALU.mult), ["s6_sm"], ["s6_sm"])
                P.op("scalar", lambda e, tt=tt: e.activation(out=t4[:], in_=lg[:, tt, 0:4], func=AF.Exp, bias=sm[:, 1:2], scale=1.0), reads=["s6_lg", "s6_sm", "s6_pen"], writes=["s6_t4"])
                V(lambda e: e.reduce_sum(out=sm[:, 2:3], in_=t4[:], axis=AX.X), ["s6_t4"], ["s6_sm"])
                V(lambda e: e.reciprocal(sm[:, 2:3], sm[:, 2:3]), ["s6_sm"], ["s6_sm"])
                for g in range(4):
                    V(lambda e, tt=tt, g=g: e.tensor_scalar(out=mk[:, g * 8:(g + 1) * 8], in0=lg[:, tt, 4 + g * 8:4 + (g + 1) * 8], scalar1=pen[:, g:g + 1], scalar2=None, op0=ALU.add),
                      ["s6_lg", "s6_pen"], ["s6_mk"])
                V(lambda e: e.reduce_max(out=sm[:, 3:4], in_=mk[:], axis=AX.X), ["s6_mk"], ["s6_sm"])
                V(lambda e: e.tensor_scalar(out=oh[:], in0=mk[:], scalar1=sm[:, 3:4], scalar2=None, op0=ALU.is_equal), ["s6_mk", "s6_sm"], ["s6_oh"])
                V(lambda e: e.scalar_tensor_tensor(out=mk2[:], in0=oh[:], scalar=-1e30, in1=mk[:], op0=ALU.mult, op1=ALU.add), ["s6_oh", "s6_mk"], ["s6_mk2"])
                V(lambda e: e.reduce_max(out=sm[:, 4:5], in_=mk2[:], axis=AX.X), ["s6_mk2"], ["s6_sm"])
                V(lambda e: e.tensor_tensor(out=sm[:, 5:6], in0=sm[:, 3:4], in1=sm[:, 4:5], op=ALU.subtract), ["s6_sm"], ["s6_sm"])
                P.op("scalar", lambda e: e.activation(out=sm[:, 6:7], in_=sm[:, 5:6], func=AF.Sigmoid), reads=["s6_sm"], writes=["s6_sm"])
                V(lambda e: e.tensor_tensor(out=sm[:, 7:8], in0=sm[:, 6:7], in1=sm[:, 2:3], op=ALU.mult), ["s6_sm"], ["s6_sm"])
                V(lambda e: e.tensor_tensor(out=sm[:, 8:9], in0=sm[:, 2:3], in1=sm[:, 7:8], op=ALU.subtract), ["s6_sm"], ["s6_sm"])
                V(lambda e, tt=tt: e.tensor_scalar(out=G[:, tt, :], in0=oh[:], scalar1=sm[:, 7:8], scalar2=None, op0=ALU.mult), ["s6_oh", "s6_sm"], ["s6_G"])
                V(lambda e: e.tensor_scalar(out=oh[:], in0=mk2[:], scalar1=sm[:, 4:5], scalar2=None, op0=ALU.is_equal), ["s6_mk2", "s6_sm", "s6_G"], ["s6_oh"])
                V(lambda e, tt=tt: e.scalar_tensor_tensor(out=G[:, tt, :], in0=oh[:], scalar=sm[:, 8:9], in1=G[:, tt, :], op0=ALU.mult, op1=ALU.add), ["s6_oh", "s6_sm", "s6_G"], ["s6_G"])
            x1res = [f"s6_x1T{tt}_{h}" for tt in range(8) for h in range(2)]
            for ex in range(32):
                wb, wbn = wgu[nw % 2], f"s6_wgu{nw % 2}"
                wdb, wdn = wd[nw % 2], f"s6_wd{nw % 2}"
                nw += 1
                P.dma("gpsimd", wbn, lambda e, wb=wb, ex=ex: e.dma_start(out=wb[:, :, 0:512], in_=c.w_eg[li, ex].rearrange("(kc kp) f -> kp kc f", kp=128)), writes=[wbn])
                P.dma("gpsimd", wbn, lambda e, wb=wb, ex=ex: e.dma_start(out=wb[:, :, 512:1024], in_=c.w_eu[li, ex].rearrange("(kc kp) f -> kp kc f", kp=128)), reads=[wbn], writes=[wbn])
                P.dma("gpsimd", wdn, lambda e, wdb=wdb, ex=ex: e.dma_start(out=wdb[:], in_=c.w_ed[li, ex].rearrange("(fc fp) n -> fp fc n", fp=128)), writes=[wdn])
                for tc in range(2):
                    hb, hn = hT[(ex * 2 + tc) % 2], f"s6_hT{(ex * 2 + tc) % 2}"
                    for fc in range(4):
                        pg, pgn = k.ps[nps % 8], f"ps{nps % 8}"
                        nps += 1
                        pu, pun = k.ps[nps % 8], f"ps{nps % 8}"
                        nps += 1
                        for kc in range(8):
                            P.op("tensor", lambda e, pg=pg, kc=kc, fc=fc, tc=tc, wb=wb: e.matmul(pg[:, :], lhsT=wb[:, kc, fc * 128:(fc + 1) * 128], rhs=x1T[:, kc, tc * 512:(tc + 1) * 512], start=(kc == 0), stop=(kc == 7)),
                                 reads=[wbn] + x1res[tc * 8:(tc + 1) * 8], writes=[pgn])
                        for kc in range(8):
                            P.op("tensor", lambda e, pu=pu, kc=kc, fc=fc, tc=tc, wb=wb: e.matmul(pu[:, :], lhsT=wb[:, kc, 512 + fc * 128:512 + (fc + 1) * 128], rhs=x1T[:, kc, tc * 512:(tc + 1) * 512], start=(kc == 0), stop=(kc == 7)),
                                 reads=[wbn] + x1res[tc * 8:(tc + 1) * 8], writes=[pun])
                        sg, sgn = sgl[fc % 2], f"s6_sg{fc % 2}"
                        P.op("scalar", lambda e, pg=pg, sg=sg: e.activation(out=sg[:], in_=pg[:, :], func=AF.Silu), reads=[pgn], writes=[sgn])
                        P.op("vector", lambda e, pu=pu, sg=sg, hb=hb, fc=fc: e.tensor_tensor(out=hb[:, fc, :], in0=pu[:, :], in1=sg[:], op=ALU.mult), reads=[pun, sgn], writes=[f"{hn}_{fc}"])
                    for tt4 in range(4):
                        tt = tc * 4 + tt4
                        for nh in range(2):
                            py, pyn = k.ps[nps % 8], f"ps{nps % 8}"
                            nps += 1
                            for fc in range(4):
                                P.op("tensor", lambda e, py=py, fc=fc, tt4=tt4, nh=nh, hb=hb, wdb=wdb: e.matmul(py[:, :], lhsT=hb[:, fc, tt4 * 128:(tt4 + 1) * 128], rhs=wdb[:, fc, nh * 512:(nh + 1) * 512], start=(fc == 0), stop=(fc == 3)),
                                     reads=[wdn, f"{hn}_{fc}"], writes=[pyn])
                            an = f"s6_acc{tt}_{nh}"
                            if ex == 0:
                                P.op("vector", lambda e, py=py, tt=tt, nh=nh, ex=ex: e.tensor_scalar(out=acc[:, tt, nh * 512:(nh + 1) * 512], in0=py[:, :], scalar1=G[:, tt, ex:ex + 1], scalar2=None, op0=ALU.mult),
                                     reads=[pyn, "s6_G"], writes=[an])
                            else:
                                P.op("vector", lambda e, py=py, tt=tt, nh=nh, ex=ex: e.scalar_tensor_tensor(out=acc[:, tt, nh * 512:(nh + 1) * 512], in0=py[:, :], scalar=G[:, tt, ex:ex + 1], in1=acc[:, tt, nh * 512:(nh + 1) * 512], op0=ALU.mult, op1=ALU.add),
                                     reads=[pyn, "s6_G", an], writes=[an])
            for tt in range(8):
                t = qt * 8 + tt
                xb, xn = xin[t % 2], f"s6_xin{t % 2}"
                P.dma("sync", xn, lambda e, xb=xb, t=t: e.dma_start(out=xb[:], in_=c.x1s[t * 128:(t + 1) * 128, :]), writes=[xn])
                P.dma("sync", "s6_pin", lambda e, t=t: e.dma_start(out=pin[:], in_=c.p[li, t * 128:(t + 1) * 128, :]), writes=["s6_pin"])
                pb, pn = k.ps[nps % 8], f"ps{nps % 8}"
                nps += 1
                for q in range(2):
                    P.op("tensor", lambda e, pb=pb, q=q: e.transpose(pb[:, q * 128:(q + 1) * 128], pin[:, q * 128:(q + 1) * 128], k.ident[:]), reads=["s6_pin", "c_ident"], writes=[pn])
                P.op("scalar", lambda e, pb=pb: e.copy(pT[:], pb[:, 0:256].rearrange("p (a b) -> p a b", a=2)), reads=[pn], writes=["s6_pT"])
                zb, zn = z[t % 2], f"s6_z{t % 2}"
                for nh in range(2):
                    pgt, pgtn = k.ps[nps % 8], f"ps{nps % 8}"
                    nps += 1
                    pp, ppn = k.ps[nps % 8], f"ps{nps % 8}"
                    nps += 1
                    for kc in range(8):
                        P.op("tensor", lambda e, pgt=pgt, kc=kc, tt=tt, nh=nh: e.matmul(pgt[:, :], lhsT=x1T[:, kc, tt * 128:(tt + 1) * 128], rhs=wpg[:, kc, nh * 512:(nh + 1) * 512], start=(kc == 0), stop=(kc == 7)),
                             reads=["s6_wpg", f"s6_x1T{tt}_{kc // 4}"], writes=[pgtn])
                    for kc in range(2):
                        P.op("tensor", lambda e, pp=pp, kc=kc, nh=nh: e.matmul(pp[:, :], lhsT=pT[:, kc, :], rhs=wpp[:, kc, nh * 512:(nh + 1) * 512], start=(kc == 0), stop=(kc == 1)),
                             reads=["s6_wpp", "s6_pT"], writes=[ppn])
                    P.op("scalar", lambda e, pgt=pgt, nh=nh: e.activation(out=sgt[:, nh * 512:(nh + 1) * 512], in_=pgt[:, :], func=AF.Sigmoid), reads=[pgtn], writes=[f"s6_sgt{nh}"])
                    P.op("vector", lambda e, pp=pp, nh=nh, zb=zb: e.tensor_tensor(out=zb[:, nh * 512:(nh + 1) * 512], in0=pp[:, :], in1=sgt[:, nh * 512:(nh + 1) * 512], op=ALU.mult),
                         reads=[ppn, f"s6_sgt{nh}"], writes=[zn + f"_{nh}"])
                    P.op("gpsimd", lambda e, nh=nh, zb=zb, tt=tt: e.tensor_tensor(out=zb[:, nh * 512:(nh + 1) * 512], in0=zb[:, nh * 512:(nh + 1) * 512], in1=acc[:, tt, nh * 512:(nh + 1) * 512], op=ALU.add),
                         reads=[zn + f"_{nh}", f"s6_acc{tt}_{nh}"], writes=[zn + f"_{nh}"])
                P.op("vector", lambda e, zb=zb, xb=xb: e.scalar_tensor_tensor(out=zb[:], in0=xb[:], scalar=ALPHA, in1=zb[:], op0=ALU.mult, op1=ALU.add),
                     reads=[xn, zn + "_0", zn + "_1"], writes=[zn])
                ob, on = xo[t % 2], f"s6_xo{t % 2}"
                layer_norm_tile(P, k, zb, zn, gam, bet, stats, mv, rstd, ob, on, eps[:, 0:1], "s6")
                P.dma("sync", "s6_out", lambda e, t=t, ob=ob: e.dma_start(out=dst[t * 128:(t + 1) * 128, :], in_=ob[:]), reads=[on])
    P.barrier()


def stage4_dn(nc, P, k, c, li):
    RS = 128 ** -0.5
    with contextlib.ExitStack() as st:
        da = sbt(nc, st, "d_da", [4, S], F32)
        db = sbt(nc, st, "d_db", [4, S], F32)
        rm = sbt(nc, st, "d_rm", [4, S], F32)
        gc = sbt(nc, st, "d_gc", [4, S], F32)
        par = sbt(nc, st, "d_par", [4, 4], F32)
        P.dma("sync", "d_da", lambda e: e.dma_start(out=da[:], in_=c.smT[8:12, :]), writes=["d_da"])
        P.dma("sync", "d_db", lambda e: e.dma_start(out=db[:], in_=c.smT[12:16, :]), writes=["d_db"])
        P.dma("sync", "d_par", lambda e: e.dma_start(out=par[:, 0:1], in_=c.dn_a_log[li]), writes=["d_par"])
        P.dma("sync", "d_par", lambda e: e.dma_start(out=par[:, 1:2], in_=c.dn_dt_bias[li]), writes=["d_par"])
        P.op("scalar", lambda e: e.activation(out=par[:, 2:3], in_=par[:, 0:1], func=AF.Exp), reads=["d_par"], writes=["d_par2"])
        P.op("vector", lambda e: e.tensor_scalar(out=par[:, 2:3], in0=par[:, 2:3], scalar1=-1.0, scalar2=None, op0=ALU.mult), reads=["d_par2"], writes=["d_par2"])
        P.op("scalar", lambda e: e.activation(out=da[:], in_=da[:], func=AF.Exp, bias=par[:, 1:2], scale=1.0), reads=["d_da", "d_par"], writes=["d_da"])
        P.op("scalar", lambda e: e.activation(out=da[:], in_=da[:], func=AF.Ln, bias=k.ones[0:4, 0:1], scale=1.0), reads=["d_da", "c_ones"], writes=["d_da"])
        P.op("scalar", lambda e: e.activation(out=db[:], in_=db[:], func=AF.Sigmoid), reads=["d_db"], writes=["d_db"])
        P.op("vector", lambda e: e.tensor_scalar(out=da[:], in0=da[:], scalar1=par[:, 2:3], scalar2=None, op0=ALU.mult), reads=["d_da", "d_par2"], writes=["d_da"])
        P.op("gpsimd", lambda e: e.memset(rm[:], 1.0), writes=["d_rm"])
        P.op("gpsimd", lambda e: e.memset(rm[:].rearrange("p (c j) -> p c j", j=64)[:, :, 0:1], 0.0), reads=["d_rm"], writes=["d_rm"])
        P.op("vector", lambda e: e.tensor_tensor_scan(out=gc[:], data0=rm[:], data1=da[:], initial=0.0, op0=ALU.mult, op1=ALU.add), reads=["d_rm", "d_da"], writes=["d_gc"])
        P.dma("sync", "d_rowout", lambda e: e.dma_start(out=c.dnrow[0], in_=db[:]), reads=["d_db"])
        P.dma("sync", "d_rowout", lambda e: e.dma_start(out=c.dnrow[1], in_=gc[:]), reads=["d_gc"])
    P.barrier()
    psb = k.ps[7][:, :].bitcast(BF16)
    psb6 = k.ps[6][:, :].bitcast(BF16)
    with contextlib.ExitStack() as st0:
        cw = sbt(nc, st0, "d_cw", [128, 12, 4], F32)
        nw = sbt(nc, st0, "d_nw", [128, 1], F32)
        eps6 = sbt(nc, st0, "d_eps6", [128, 1], F32)
        with contextlib.ExitStack() as st:
            cwraw = sbt(nc, st, "d_cwraw", [4, 1536], F32)
            P.dma("sync", "d_cwraw", lambda e: e.dma_start(out=cwraw[:], in_=c.dn_conv[li]), writes=["d_cwraw"])
            for idx in range(12):
                P.op("tensor", lambda e, idx=idx: e.transpose(k.ps[0][:, idx * 4:(idx + 1) * 4], cwraw[0:4, idx * 128:(idx + 1) * 128], k.ident[0:4, 0:4]),
                     reads=["d_cwraw", "c_ident"], writes=["ps0"])
            P.op("vector", lambda e: e.tensor_copy(cw[:].rearrange("p a b -> p (a b)"), k.ps[0][:, 0:48]), reads=["ps0"], writes=["d_cw"])
            P.dma("sync", "d_nw", lambda e: e.dma_start(out=nw[:], in_=c.dn_norm_w[li]), writes=["d_nw"])
            P.op("vector", lambda e: e.memset(eps6[:], 1e-6), writes=["d_eps6"])
        P.barrier()
        def do_head(h):
            with contextlib.ExitStack() as sth:
                A = lambda n, s, d: sbt(nc, sth, n, s, d)
                kT = A("d_kT", [128, S], BF16)
                qT = A("d_qT", [128, S], BF16)
                qdT = A("d_qdT", [128, S], BF16)
                vb_tm = A("d_vb", [128, NT, 128], BF16)
                kbg_tm = A("d_kbg", [128, NT, 128], BF16)
                kdec_tm = A("d_kdec", [128, NT, 128], BF16)
                AT = A("d_AT", [128, NT, 128], BF16)
                gcB = A("d_gcB", [128, S], F32)
                gc_col = A("d_gccol", [128, NT], F32)
                b_col = A("d_bcol", [128, NT], F32)
                glc = A("d_glc", [128, NT], F32)
                col_bg = A("d_colbg", [128, NT], F32)
                col_edd = A("d_coledd", [128, NT], F32)
                negb = A("d_negb", [128, NT], F32)
                neggc = A("d_neggc", [128, NT], F32)
                eglB = A("d_eglB", [128, 64], F32)
                browh = c.dnrow[0, h]
                gcrowh = c.dnrow[1, h]
                P.dma("sync", "d_cols", lambda e: e.dma_start(out=gc_col[:], in_=gcrowh.rearrange("(t p) -> p t", p=128), allow_slow_non_contiguous=True), writes=["d_gccol"])
                P.dma("sync", "d_cols", lambda e: e.dma_start(out=b_col[:], in_=browh.rearrange("(t p) -> p t", p=128), allow_slow_non_contiguous=True), writes=["d_bcol"])
                gsrc = gcrowh.rearrange("(t two j) -> two j t", two=2, j=64)
                for half in range(2):
                    P.dma("sync", "d_cols", lambda e, half=half: e.dma_start(out=glc[half * 64:(half + 1) * 64, :], in_=gsrc[half, 63, :].partition_broadcast(64), allow_slow_non_contiguous=True), writes=["d_glc"])
                P.dma("sync", "d_cols", lambda e: e.dma_start(out=eglB[:], in_=gcrowh.rearrange("(c j) -> j c", j=64)[63, :].partition_broadcast(128), allow_slow_non_contiguous=True), writes=["d_eglB"])
                P.dma("sync", "d_gcB", lambda e: e.dma_start(out=gcB[:], in_=gcrowh.partition_broadcast(128)), writes=["d_gcB"])
                P.op("scalar", lambda e: e.activation(out=eglB[:], in_=eglB[:], func=AF.Exp), reads=["d_eglB"], writes=["d_eglB"])
                P.op("scalar", lambda e: e.activation(out=col_bg[:], in_=gc_col[:], func=AF.Exp), reads=["d_gccol"], writes=["d_colbg"])
                P.op("vector", lambda e: e.tensor_tensor(out=col_bg[:], in0=col_bg[:], in1=b_col[:], op=ALU.mult), reads=["d_colbg", "d_bcol"], writes=["d_colbg"])
                P.op("vector", lambda e: e.tensor_tensor(out=col_edd[:], in0=glc[:], in1=gc_col[:], op=ALU.subtract), reads=["d_glc", "d_gccol"], writes=["d_coledd"])
                P.op("scalar", lambda e: e.activation(out=col_edd[:], in_=col_edd[:], func=AF.Exp), reads=["d_coledd"], writes=["d_coledd"])
                P.op("vector", lambda e: e.tensor_scalar(out=negb[:], in0=b_col[:], scalar1=-1.0, scalar2=None, op0=ALU.mult), reads=["d_bcol"], writes=["d_negb"])
                P.op("vector", lambda e: e.tensor_scalar(out=neggc[:], in0=gc_col[:], scalar1=-1.0, scalar2=None, op0=ALU.mult), reads=["d_gccol"], writes=["d_neggc"])
                with contextlib.ExitStack() as st:
                    u = [sbt(nc, st, f"d_u{i}", [128, S], F32) for i in range(2)]
                    acc = sbt(nc, st, "d_acc", [128, S], F32)
                    sch = [sbt(nc, st, f"d_sch{i}", [128, 512], F32) for i in range(2)]
                    sqc = [sbt(nc, st, f"d_sqc{i}", [128, 512], F32) for i in range(2)]
                    rn = [sbt(nc, st, f"d_rn{i}", [128, 512], F32) for i in range(2)]
                    vTb = sbt(nc, st, "d_vTb", [128, S], BF16)
                    if h == 0 and "dn_dbg" in DBG_FLAGS:
                        print("sbuf remaining in dn phase A:", nc.sbuf_bytes_remaining)
                        for nm_, t_ in [("kT", kT), ("qT", qT), ("qdT", qdT), ("vb", vb_tm), ("kbg", kbg_tm), ("kdec", kdec_tm), ("AT", AT), ("gcB", gcB), ("gc_col", gc_col), ("eglB", eglB),
                                        ("u0", u[0]), ("u1", u[1]), ("acc", acc), ("rn0", rn[0]), ("rn1", rn[1]), ("vTb", vTb), ("cw", cw), ("ones", k.ones)]:
                            m_ = nc.lookup_mloc(t_)
                            print("   ", nm_, m_.addr, list(m_.dims))
                    nps = 0
                    for wi, which in enumerate(("q", "k", "v")):
                        idx = wi * 4 + h
                        ub_, un = u[wi % 2], f"d_u{wi % 2}"
                        P.dma("sync", un, lambda e, ub_=ub_, idx=idx: e.dma_start(out=ub_[:], in_=c.fT[512 + idx * 128:512 + (idx + 1) * 128, :]), writes=[un])
                        P.op("vector", lambda e, ub_=ub_, idx=idx: e.tensor_scalar(out=acc[:], in0=ub_[:], scalar1=cw[:, idx, 3:4], scalar2=None, op0=ALU.mult), reads=[un, "d_cw"], writes=["d_acc"])
                        for sh, j in ((1, 2), (2, 1), (3, 0)):
                            P.op("vector", lambda e, ub_=ub_, idx=idx, sh=sh, j=j: e.scalar_tensor_tensor(out=acc[:, sh:], in0=ub_[:, 0:S - sh], scalar=cw[:, idx, j:j + 1], in1=acc[:, sh:], op0=ALU.mult, op1=ALU.add),
                                 reads=[un, "d_cw", "d_acc"], writes=["d_acc"])
                        dstT, dn_ = (qT, "d_qT") if which == "q" else ((kT, "d_kT") if which == "k" else (vTb, "d_vTb"))
                        for tc in range(8):
                            cs = slice(tc * 512, (tc + 1) * 512)
                            if which == "v":
                                P.op("scalar", lambda e, cs=cs: e.activation(out=vTb[:, cs], in_=acc[:, cs], func=AF.Silu), reads=["d_acc"], writes=["d_vTb"])
                                continue
                            pb, pn = k.ps[nps % 4], f"ps{nps % 4}"
                            rb, rbn = rn[nps % 2], f"d_rn{nps % 2}"
                            sc_, scn = sch[nps % 2], f"d_sch{nps % 2}"
                            sq_, sqn = sqc[nps % 2], f"d_sqc{nps % 2}"
                            nps += 1
                            P.op("scalar", lambda e, cs=cs, sc_=sc_: e.activation(out=sc_[:], in_=acc[:, cs], func=AF.Silu), reads=["d_acc"], writes=[scn])
                            P.op("vector", lambda e, sc_=sc_, sq_=sq_: e.tensor_tensor(out=sq_[:], in0=sc_[:], in1=sc_[:], op=ALU.mult), reads=[scn], writes=[sqn])
                            P.op("tensor", lambda e, pb=pb, sq_=sq_: e.matmul(pb[:, :], lhsT=k.ones[:], rhs=sq_[:], start=True, stop=True), reads=["c_ones", sqn], writes=[pn])
                            P.op("scalar", lambda e, pb=pb, rb=rb: e.activation(out=rb[:], in_=pb[:, :], func=AF.Sqrt, bias=eps6[:, 0:1], scale=1.0), reads=[pn, "d_eps6"], writes=[rbn])
                            P.op("vector", lambda e, rb=rb: e.reciprocal(rb[:], rb[:]), reads=[rbn], writes=[rbn])
                            sc = RS if which == "q" else 1.0
                            P.op("vector", lambda e, rb=rb, cs=cs, dstT=dstT, sc=sc, sc_=sc_: e.scalar_tensor_tensor(out=dstT[:, cs], in0=sc_[:], scalar=sc, in1=rb[:], op0=ALU.mult, op1=ALU.mult),
                                 reads=[scn, rbn], writes=[dn_])
                    for t in range(NT):
                        cs = slice(t * 128, (t + 1) * 128)
                        pq = psb if t % 2 == 0 else psb6
                        pqn = "ps7" if t % 2 == 0 else "ps6"
                        P.op("tensor", lambda e, cs=cs, pq=pq: e.transpose(pq[:, 0:128], kT[:, cs], k.identb[:]), reads=["d_kT", "c_identb"], writes=[pqn])
                        P.op("tensor", lambda e, cs=cs, pq=pq: e.transpose(pq[:, 128:256], vTb[:, cs], k.identb[:]), reads=["d_vTb", "c_identb"], writes=[pqn])
                        P.op("vector", lambda e, t=t, pq=pq: e.tensor_scalar(out=kbg_tm[:, t, :], in0=pq[:, 0:128], scalar1=col_bg[:, t:t + 1], scalar2=None, op0=ALU.mult),
                             reads=[pqn, "d_colbg"], writes=["d_kbg"])
                        P.op("vector", lambda e, t=t, pq=pq: e.tensor_scalar(out=kdec_tm[:, t, :], in0=pq[:, 0:128], scalar1=col_edd[:, t:t + 1], scalar2=None, op0=ALU.mult),
                             reads=[pqn, "d_coledd"], writes=["d_kdec"])
                        P.op("vector", lambda e, t=t, pq=pq: e.tensor_scalar(out=vb_tm[:, t, :], in0=pq[:, 128:256], scalar1=b_col[:, t:t + 1], scalar2=None, op0=ALU.mult),
                             reads=[pqn, "d_bcol"], writes=["d_vb"])
                    if "dn_dbg" in DBG_FLAGS and h == 0:
                        P.dma("gpsimd", "dbgo", lambda e: e.dma_start(out=c.dbg1[:, 2432:2560], in_=u[1][:, 2048:2176]), reads=["d_u1"])
                        P.dma("gpsimd", "dbgo", lambda e: e.dma_start(out=c.dbg1[:, 2560:2688], in_=acc[:, 2048:2176]), reads=["d_acc"])
                        P.dma("gpsimd", "dbgo", lambda e: e.dma_start(out=c.dbg1[:, 2688:2816], in_=vTb[:, 2048:2176]), reads=["d_vTb"])
                    for tc in range(8):
                        cs = slice(tc * 512, (tc + 1) * 512)
                        sc_, scn = sch[tc % 2], f"d_sch{tc % 2}"
                        P.op("scalar", lambda e, cs=cs, sc_=sc_: e.activation(out=sc_[:], in_=gcB[:, cs], func=AF.Exp), reads=["d_gcB"], writes=[scn])
                        P.op("vector", lambda e, cs=cs, sc_=sc_: e.tensor_tensor(out=qdT[:, cs], in0=qT[:, cs], in1=sc_[:], op=ALU.mult), reads=["d_qT", scn], writes=["d_qdT"])
                P.barrier()
                uin = A("d_uin", [128, NT, 128], F32)
                WT = A("d_WT", [128, S], BF16)
                oT = A("d_oT", [128, S], F32)
                with contextlib.ExitStack() as st:
                    Dm = [sbt(nc, st, f"d_Dm{i}", [128, 128], F32) for i in range(2)]
                    DTm = [sbt(nc, st, f"d_DTm{i}", [128, 128], F32) for i in range(2)]
                    Nf = [sbt(nc, st, f"d_Nf{i}", [128, 128], F32) for i in range(2)]
                    ATf = [sbt(nc, st, f"d_ATf{i}", [128, 128], F32) for i in range(2)]
                    Nl = [sbt(nc, st, f"d_Nl{i}", [128, 4, 128], BF16) for i in range(2)]
                    Ml = [sbt(nc, st, f"d_Ml{i}", [128, 4, 128], BF16) for i in range(2)]
                    Pl = [sbt(nc, st, f"d_Pl{i}", [128, 4, 128], BF16) for i in range(2)]
                    for grp in range(NT // 4):
                        for tt in range(4):
                            t = grp * 4 + tt
                            cs = slice(t * 128, (t + 1) * 128)
                            i2 = t % 2
                            pkk, pkkn = k.ps[i2], f"ps{i2}"
                            pqk, pqkn = k.ps[2 + i2], f"ps{2 + i2}"
                            P.op("tensor", lambda e, pkk=pkk, cs=cs: e.matmul(pkk[:, 0:128], lhsT=kT[:, cs], rhs=kT[:, cs], start=True, stop=True), reads=["d_kT"], writes=[pkkn])
                            P.op("tensor", lambda e, pqk=pqk, cs=cs: e.matmul(pqk[:, 0:128], lhsT=kT[:, cs], rhs=qT[:, cs], start=True, stop=True), reads=["d_kT", "d_qT"], writes=[pqkn])
                            P.op("scalar", lambda e, cs=cs, t=t, i2=i2: e.activation(out=Dm[i2][:], in_=gcB[:, cs], func=AF.Exp, bias=gc_col[:, t:t + 1], scale=-1.0), reads=["d_gcB", "d_gccol"], writes=[f"d_Dm{i2}"])
                            P.op("scalar", lambda e, cs=cs, t=t, i2=i2: e.activation(out=DTm[i2][:], in_=gcB[:, cs], func=AF.Exp, bias=neggc[:, t:t + 1], scale=1.0), reads=["d_gcB", "d_neggc"], writes=[f"d_DTm{i2}"])
                            P.op("vector", lambda e, pkk=pkk, t=t, i2=i2: e.scalar_tensor_tensor(out=Nf[i2][:], in0=pkk[:, 0:128], scalar=negb[:, t:t + 1], in1=Dm[i2][:], op0=ALU.mult, op1=ALU.mult),
                                 reads=[pkkn, "d_negb", f"d_Dm{i2}"], writes=[f"d_Nf{i2}"])
                            P.op("vector", lambda e, pqk=pqk, i2=i2: e.tensor_tensor(out=ATf[i2][:], in0=pqk[:, 0:128], in1=DTm[i2][:], op=ALU.mult),
                                 reads=[pqkn, f"d_DTm{i2}"], writes=[f"d_ATf{i2}"])
                            P.op("gpsimd", lambda e, tt=tt, i2=i2: e.affine_select(out=Nl[0][:, tt, :], in_=Nf[i2][:], pattern=[[-1, 128]], compare_op=ALU.is_gt, fill=0.0, base=0, channel_multiplier=1),
                                 reads=[f"d_Nf{i2}"], writes=[f"d_N0_{tt}"])
                            P.op("gpsimd", lambda e, tt=tt: e.memset(Nl[0][64:128, tt, 0:64], 0.0), reads=[f"d_N0_{tt}"], writes=[f"d_N0_{tt}"])
                            P.op("gpsimd", lambda e, t=t, i2=i2: e.affine_select(out=AT[:, t, :], in_=ATf[i2][:], pattern=[[1, 128]], compare_op=ALU.is_ge, fill=0.0, base=0, channel_multiplier=-1),
                                 reads=[f"d_ATf{i2}"], writes=["d_AT"])
                            P.op("gpsimd", lambda e, t=t: e.memset(AT[0:64, t, 64:128], 0.0), reads=["d_AT"], writes=["d_AT"])
                            P.op("tensor", lambda e, tt=tt: e.transpose(psb[:, tt * 128:(tt + 1) * 128], Nl[0][:, tt, :], k.identb[:]), reads=[f"d_N0_{tt}", "c_identb"], writes=["ps7"])
                        P.op("scalar", lambda e: e.copy(Ml[0][:].rearrange("p a b -> p (a b)"), psb[:, 0:512]), reads=["ps7"], writes=["d_M0"])
                        for tt in range(4):
                            P.op("vector", lambda e, tt=tt: e.tensor_tensor(out=Pl[0][:, tt, :], in0=Ml[0][:, tt, :], in1=k.identb[:], op=ALU.add), reads=["d_M0", "c_identb"], writes=[f"d_P0_{tt}"])
                        Nres = [f"d_N0_{tt}" for tt in range(4)]
                        Mres = ["d_M0"]
                        Pres = [f"d_P0_{tt}" for tt in range(4)]
                        cur = 0
                        for lvl in range(1, 6):
                            nxt = 1 - cur
                            for tt in range(4):
                                P.op("tensor", lambda e, tt=tt, cur=cur: e.matmul(k.ps[4][:, tt * 128:(tt + 1) * 128], lhsT=Ml[cur][:, tt, :], rhs=Nl[cur][:, tt, :], start=True, stop=True),
                                     reads=Nres + Mres, writes=["ps4"])
                            if lvl < 5:
                                for tt in range(4):
                                    P.op("tensor", lambda e, tt=tt, cur=cur: e.matmul(k.ps[5][:, tt * 128:(tt + 1) * 128], lhsT=Nl[cur][:, tt, :], rhs=Ml[cur][:, tt, :], start=True, stop=True),
                                         reads=Nres + Mres, writes=["ps5"])
                            P.op("scalar", lambda e, nxt=nxt: e.copy(Nl[nxt][:].rearrange("p a b -> p (a b)"), k.ps[4][:, :]), reads=["ps4"], writes=[f"d_Nn{nxt}"])
                            if lvl < 5:
                                P.op("vector", lambda e, nxt=nxt: e.tensor_copy(Ml[nxt][:].rearrange("p a b -> p (a b)"), k.ps[5][:, :]), reads=["ps5"], writes=[f"d_Mn{nxt}"])
                            for tt in range(4):
                                P.op("tensor", lambda e, tt=tt, cur=cur, nxt=nxt: e.matmul(k.ps[6][:, tt * 128:(tt + 1) * 128], lhsT=Nl[nxt][:, tt, :], rhs=Pl[cur][:, tt, :], start=True, stop=True),
                                     reads=[f"d_Nn{nxt}"] + Pres, writes=["ps6"])
                            P.op("vector", lambda e, cur=cur, nxt=nxt: e.tensor_tensor(out=Pl[nxt][:].rearrange("p a b -> p (a b)"), in0=k.ps[6][:, :], in1=Pl[cur][:].rearrange("p a b -> p (a b)"), op=ALU.add),
                                 reads=["ps6"] + Pres, writes=[f"d_Pn{nxt}"])
                            Nres = [f"d_Nn{nxt}"]
                            Mres = [f"d_Mn{nxt}"]
                            Pres = [f"d_Pn{nxt}"]
                            cur = nxt
                        for tt in range(4):
                            t = grp * 4 + tt
                            P.op("tensor", lambda e, tt=tt, t=t, cur=cur: e.matmul(k.ps[4][:, tt * 128:(tt + 1) * 128], lhsT=Pl[cur][:, tt, :], rhs=vb_tm[:, t, :], start=True, stop=True),
                                 reads=Pres + ["d_vb"], writes=["ps4"])
                            P.op("tensor", lambda e, tt=tt, t=t, cur=cur: e.matmul(k.ps[5][:, tt * 128:(tt + 1) * 128], lhsT=kbg_tm[:, t, :], rhs=Pl[cur][:, tt, :], start=True, stop=True),
                                 reads=Pres + ["d_kbg"], writes=["ps5"])
                        P.op("vector", lambda e, grp=grp: e.tensor_copy(uin[:, grp * 4:(grp + 1) * 4, :].rearrange("p a b -> p (a b)"), k.ps[4][:, :]), reads=["ps4"], writes=["d_uin"])
                        P.op("scalar", lambda e, grp=grp: e.copy(WT[:, grp * 512:(grp + 1) * 512], k.ps[5][:, :]), reads=["ps5"], writes=["d_WT"])
                P.barrier()
                with contextlib.ExitStack() as st:
                    S32 = sbt(nc, st, "d_S32", [128, 128], F32)
                    Sb = sbt(nc, st, "d_Sb", [128, 128], BF16)
                    ub = [sbt(nc, st, f"d_ub{i}", [128, 128], BF16) for i in range(2)]
                    P.op("gpsimd", lambda e: e.memset(S32[:], 0.0), writes=["d_S32"])
                    P.op("gpsimd", lambda e: e.memset(Sb[:], 0.0), writes=["d_Sb"])
                    for ci in range(64):
                        t, hb = ci // 2, ci % 2
                        r0 = hb * 64
                        cc = slice(ci * 64, (ci + 1) * 64)
                        ubb, ubn = ub[ci % 2], f"d_ub{ci % 2}"
                        po, pon = k.ps[1 + (ci // 8) % 2], f"ps{1 + (ci // 8) % 2}"
                        oc = slice((ci % 8) * 64, (ci % 8 + 1) * 64)
                        P.op("tensor", lambda e, r0=r0, cc=cc: e.matmul(k.ps[0][r0:r0 + 64, 0:128], lhsT=WT[:, cc], rhs=Sb[:], start=True, stop=True), reads=["d_WT", "d_Sb"], writes=["ps0"])
                        P.op("vector", lambda e, r0=r0, t=t, ubb=ubb: e.tensor_tensor(out=ubb[r0:r0 + 64, :], in0=uin[r0:r0 + 64, t, :], in1=k.ps[0][r0:r0 + 64, 0:128], op=ALU.subtract),
                             reads=["ps0", "d_uin"], writes=[ubn])
                        P.op("tensor", lambda e, po=po, oc=oc, cc=cc: e.matmul(po[:, oc], lhsT=Sb[:], rhs=qdT[:, cc], start=True, stop=False), reads=["d_Sb", "d_qdT"], writes=[pon])
                        P.op("tensor", lambda e, po=po, oc=oc, r0=r0, t=t, ubb=ubb: e.matmul(po[:, oc], lhsT=ubb[r0:r0 + 64, :], rhs=AT[r0:r0 + 64, t, r0:r0 + 64], start=False, stop=True),
                             reads=[ubn, "d_AT"], writes=[pon])
                        P.op("tensor", lambda e, r0=r0, t=t, ubb=ubb: e.matmul(k.ps[3][:, 0:128], lhsT=kdec_tm[r0:r0 + 64, t, :], rhs=ubb[r0:r0 + 64, :], start=True, stop=True),
                             reads=[ubn, "d_kdec"], writes=["ps3"])
                        P.op("vector", lambda e, ci=ci: e.scalar_tensor_tensor(out=S32[:], in0=S32[:], scalar=eglB[:, ci:ci + 1], in1=k.ps[3][:, 0:128], op0=ALU.mult, op1=ALU.add),
                             reads=["ps3", "d_S32", "d_eglB"], writes=["d_S32"])
                        P.op("scalar", lambda e: e.copy(Sb[:], S32[:]), reads=["d_S32"], writes=["d_Sb"])
                        if ci % 8 == 7:
                            g8 = ci // 8
                            P.op("scalar", lambda e, po=po, g8=g8: e.copy(oT[:, g8 * 512:(g8 + 1) * 512], po[:, :]), reads=[pon], writes=["d_oT"])
                P.barrier()
                if "dn_dbg" in DBG_FLAGS and h == 0:
                    dl = [(kT[:, 0:128], 0), (qT[:, 0:128], 128), (gc_col[:], 256), (b_col[:], 288), (AT[:, 0, :], 320), (uin[:, 0, :], 448),
                          (WT[:, 0:128], 576), (oT[:, 0:128], 704), (vb_tm[:, 0, :], 832), (kbg_tm[:, 0, :], 960), (kdec_tm[:, 0, :], 1088), (qdT[:, 0:128], 1216), (eglB[:], 1344), (gcB[:, 2016:2144], 1408), (kT[:, 2048:2176], 1536), (qdT[:, 2048:2176], 1664), (AT[:, 16, :], 1792), (uin[:, 16, :], 1920), (WT[:, 2048:2176], 2048), (oT[:, 2048:2176], 2176), (kdec_tm[:, 16, :], 2304)]
                    for (ap_, o_) in dl:
                        P.dma("gpsimd", "dbgo", lambda e, ap_=ap_, o_=o_: e.dma_start(out=c.dbg1[:, o_:o_ + ap_.shape[-1]], in_=ap_), reads=[])
                    P.barrier()
                with contextlib.ExitStack() as st:
                    sq = sbt(nc, st, "d_sq2", [128, S], F32)
                    dgt = sbt(nc, st, "d_dgt", [128, S], F32)
                    dgs = sbt(nc, st, "d_dgs", [128, S], BF16)
                    rn = [sbt(nc, st, f"d_rn2{i}", [128, 512], F32) for i in range(2)]
                    yst = sbt(nc, st, "d_yst", [128, S], BF16)
                    P.dma("sync", "d_dgt", lambda e: e.dma_start(out=dgt[:], in_=c.fT[2048 + h * 128:2048 + (h + 1) * 128, :]), writes=["d_dgt"])
                    for tc in range(8):
                        P.op("scalar", lambda e, tc=tc: e.activation(out=dgs[:, tc * 512:(tc + 1) * 512], in_=dgt[:, tc * 512:(tc + 1) * 512], func=AF.Silu), reads=["d_dgt"], writes=["d_dgs"])
                    P.op("vector", lambda e: e.tensor_tensor(out=sq[:], in0=oT[:], in1=oT[:], op=ALU.mult), reads=["d_oT"], writes=["d_sq2"])
                    for tc in range(8):
                        pb, pn = k.ps[tc % 4], f"ps{tc % 4}"
                        rb, rbn = rn[tc % 2], f"d_rn2{tc % 2}"
                        cs = slice(tc * 512, (tc + 1) * 512)
                        P.op("tensor", lambda e, pb=pb, cs=cs: e.matmul(pb[:, :], lhsT=k.ones[:], rhs=sq[:, cs], start=True, stop=True), reads=["c_ones", "d_sq2"], writes=[pn])
                        P.op("scalar", lambda e, pb=pb, rb=rb: e.activation(out=rb[:], in_=pb[:, :], func=AF.Sqrt, bias=eps6[:, 0:1], scale=1.0 / 128.0), reads=[pn, "d_eps6"], writes=[rbn])
                        P.op("vector", lambda e, rb=rb: e.reciprocal(rb[:], rb[:]), reads=[rbn], writes=[rbn])
                        P.op("vector", lambda e, rb=rb, cs=cs: e.scalar_tensor_tensor(out=oT[:, cs], in0=oT[:, cs], scalar=nw[:, 0:1], in1=rb[:], op0=ALU.mult, op1=ALU.mult),
                             reads=["d_oT", "d_nw", rbn], writes=[f"d_oTn{tc}"])
                        P.op("gpsimd", lambda e, cs=cs: e.tensor_tensor(out=yst[:, cs], in0=oT[:, cs], in1=dgs[:, cs], op=ALU.mult), reads=[f"d_oTn{tc}", "d_dgs"], writes=[f"d_yst{tc}"])
                    P.dma("sync", "d_yout", lambda e: e.dma_start(out=c.yT[1024 + h * 128:1024 + (h + 1) * 128, :], in_=yst[:]), reads=[f"d_yst{tc}" for tc in range(8)])
            P.barrier()
        for h in range(4):
            do_head(h)
    P.barrier()


MOE_C = 512


def stage6_moe_sparse(nc, P, k, c, li, dst):
    C = MOE_C
    NB = C // 128
    psb6 = k.ps[6][:, :].bitcast(BF16)
    psb7 = k.ps[7][:, :].bitcast(BF16)
    with contextlib.ExitStack() as st0:
        gam = sbt(nc, st0, "s6_gam", [128, D], F32)
        bet = sbt(nc, st0, "s6_bet", [128, D], F32)
        eps = sbt(nc, st0, "s6_eps", [128, 1], F32)
        P.dma("sync", "lnp", lambda e: e.dma_start(out=gam[:], in_=c.ln2_g[li].partition_broadcast(128)), writes=["lnp"])
        P.dma("sync", "lnp", lambda e: e.dma_start(out=bet[:], in_=c.ln2_b[li].partition_broadcast(128)), writes=["lnp"])
        P.op("vector", lambda e: e.memset(eps[:], 1e-5), writes=["s6_eps"])
        with contextlib.ExitStack() as st:
            wr = sbt(nc, st, "s6_wr", [128, 8, 36], F32)
            brb = sbt(nc, st, "s6_brb", [128, 36], F32)
            xin = [sbt(nc, st, f"s6_xin{i}", [128, D], F32) for i in range(2)]
            xb16 = [sbt(nc, st, f"s6_xb{i}", [128, D], BF16) for i in range(2)]
            xTf = sbt(nc, st, "s6_xTf", [128, 8, 128], F32)
            lg = sbt(nc, st, "s6_lg", [128, 36], F32)
            OH1 = sbt(nc, st, "s6_OH1", [128, NT, 32], F32)
            OH2 = sbt(nc, st, "s6_OH2", [128, NT, 32], F32)
            Gt = sbt(nc, st, "s6_Gt", [128, NT, 2], F32)
            sm = sbt(nc, st, "s6_sm", [128, 16], F32)
            t4 = sbt(nc, st, "s6_t4", [128, 4], F32)
            pen = sbt(nc, st, "s6_pen", [128, 4], F32)
            mk = sbt(nc, st, "s6_mk", [128, 32], F32)
            mk2 = sbt(nc, st, "s6_mk2", [128, 32], F32)
            P.dma("sync", "s6_wr", lambda e: e.dma_start(out=wr[:], in_=c.w_r[li].rearrange("(kc kp) n -> kp kc n", kp=128)), writes=["s6_wr"])
            P.dma("sync", "s6_brb", lambda e: e.dma_start(out=brb[:], in_=c.b_r[li].partition_broadcast(128)), writes=["s6_brb"])
            nps = 0
            V = lambda fn, r, w: P.op("vector", fn, reads=r, writes=w)
            for t in range(NT):
                xb, xn = xin[t % 2], f"s6_xin{t % 2}"
                P.dma("sync", xn, lambda e, xb=xb, t=t: e.dma_start(out=xb[:], in_=c.x1s[t * 128:(t + 1) * 128, :]), writes=[xn])
                x16, x16n = xb16[t % 2], f"s6_xb{t % 2}"
                P.op("gpsimd", lambda e, xb=xb, x16=x16: e.tensor_copy(x16[:], xb[:]), reads=[xn], writes=[x16n])
                P.dma("sync", "s6_x1bout", lambda e, x16=x16, t=t: e.dma_start(out=c.x1b[t * 128:(t + 1) * 128, :], in_=x16[:]), reads=[x16n], writes=[f"d_x1b{t}"])
                for half in range(2):
                    pb, pn = k.ps[nps % 4], f"ps{nps % 4}"
                    nps += 1
                    for q in range(4):
                        kc = half * 4 + q
                        P.op("tensor", lambda e, pb=pb, xb=xb, kc=kc, q=q: e.transpose(pb[:, q * 128:(q + 1) * 128], xb[:, kc * 128:(kc + 1) * 128], k.ident[:]),
                             reads=[xn, "c_ident"], writes=[pn])
                    P.op("scalar", lambda e, pb=pb, half=half: e.copy(xTf[:, half * 4:(half + 1) * 4, :], pb[:].rearrange("p (a b) -> p a b", a=4)),
                         reads=[pn], writes=[f"s6_xTf{half}"])
                pr, prn = k.ps[4 + t % 2], f"ps{4 + t % 2}"
                for kc in range(8):
                    P.op("tensor", lambda e, pr=pr, kc=kc: e.matmul(pr[:, 0:36], lhsT=xTf[:, kc, :], rhs=wr[:, kc, :], start=(kc == 0), stop=(kc == 7)),
                         reads=[f"s6_xTf{kc // 4}", "s6_wr"], writes=[prn])
                V(lambda e, pr=pr: e.tensor_tensor(out=lg[:], in0=pr[:, 0:36], in1=brb[:], op=ALU.add), [prn, "s6_brb"], ["s6_lg"])
                V(lambda e: e.reduce_max(out=sm[:, 0:1], in_=lg[:, 0:4], axis=AX.X), ["s6_lg"], ["s6_sm"])
                V(lambda e: e.tensor_scalar(out=t4[:], in0=lg[:, 0:4], scalar1=sm[:, 0:1], scalar2=None, op0=ALU.is_equal), ["s6_lg", "s6_sm"], ["s6_t4"])
                V(lambda e: e.tensor_scalar(out=pen[:], in0=t4[:], scalar1=-1.0, scalar2=1e30, op0=ALU.add, op1=ALU.mult), ["s6_t4"], ["s6_pen"])
                V(lambda e: e.tensor_scalar(out=sm[:, 1:2], in0=sm[:, 0:1], scalar1=-1.0, scalar2=None, op0=ALU.mult), ["s6_sm"], ["s6_sm"])
                P.op("scalar", lambda e: e.activation(out=t4[:], in_=lg[:, 0:4], func=AF.Exp, bias=sm[:, 1:2], scale=1.0), reads=["s6_lg", "s6_sm", "s6_pen"], writes=["s6_t4"])
                V(lambda e: e.reduce_sum(out=sm[:, 2:3], in_=t4[:], axis=AX.X), ["s6_t4"], ["s6_sm"])
                V(lambda e: e.reciprocal(sm[:, 2:3], sm[:, 2:3]), ["s6_sm"], ["s6_sm"])
                for g in range(4):
                    V(lambda e, g=g: e.tensor_scalar(out=mk[:, g * 8:(g + 1) * 8], in0=lg[:, 4 + g * 8:4 + (g + 1) * 8], scalar1=pen[:, g:g + 1], scalar2=None, op0=ALU.add),
                      ["s6_lg", "s6_pen"], ["s6_mk"])
                V(lambda e: e.reduce_max(out=sm[:, 3:4], in_=mk[:], axis=AX.X), ["s6_mk"], ["s6_sm"])
                V(lambda e, t=t: e.tensor_scalar(out=OH1[:, t, :], in0=mk[:], scalar1=sm[:, 3:4], scalar2=None, op0=ALU.is_equal), ["s6_mk", "s6_sm"], ["s6_OH1"])
                V(lambda e, t=t: e.scalar_tensor_tensor(out=mk2[:], in0=OH1[:, t, :], scalar=-1e30, in1=mk[:], op0=ALU.mult, op1=ALU.add), ["s6_OH1", "s6_mk"], ["s6_mk2"])
                V(lambda e: e.reduce_max(out=sm[:, 4:5], in_=mk2[:], axis=AX.X), ["s6_mk2"], ["s6_sm"])
                V(lambda e, t=t: e.tensor_scalar(out=OH2[:, t, :], in0=mk2[:], scalar1=sm[:, 4:5], scalar2=None, op0=ALU.is_equal), ["s6_mk2", "s6_sm"], ["s6_OH2"])
                V(lambda e: e.tensor_tensor(out=sm[:, 5:6], in0=sm[:, 3:4], in1=sm[:, 4:5], op=ALU.subtract), ["s6_sm"], ["s6_sm"])
                P.op("scalar", lambda e: e.activation(out=sm[:, 6:7], in_=sm[:, 5:6], func=AF.Sigmoid), reads=["s6_sm"], writes=["s6_sm"])
                V(lambda e, t=t: e.tensor_tensor(out=Gt[:, t, 0:1], in0=sm[:, 6:7], in1=sm[:, 2:3], op=ALU.mult), ["s6_sm"], ["s6_Gt"])
                V(lambda e, t=t: e.tensor_tensor(out=Gt[:, t, 1:2], in0=sm[:, 2:3], in1=Gt[:, t, 0:1], op=ALU.subtract), ["s6_sm", "s6_Gt"], ["s6_Gt"])
            A16 = sbt(nc, st, "s6_A16", [128, NT * 32], BF16)
            Tri = sbt(nc, st, "s6_Tri", [128, 128], BF16)
            INC = sbt(nc, st, "s6_INC", [128, NT, 32], F32)
            X = [sbt(nc, st, f"s6_X{i}", [128, NT, 32], F32) for i in range(2)]
            TB = sbt(nc, st, "s6_TB", [128, NT, 32], F32)
            ECf = sbt(nc, st, "s6_ECf", [128, NT, 32], F32)
            tmp3 = sbt(nc, st, "s6_tmp3", [128, NT, 32], F32)
            Sf = sbt(nc, st, "s6_Sf", [128, 2, NT], F32)
            Si = sbt(nc, st, "s6_Si", [128, 2, NT], I32)
            REC = sbt(nc, st, "s6_REC", [128, NT, 2, 16], I32)
            INIT = sbt(nc, st, "s6_INIT", [128, 32 * C // 128, 16], I32)
            io_t = sbt(nc, st, "s6_iot", [128, NT], I32)
            io_d = sbt(nc, st, "s6_iod", [128, 2, NT], I32)
            flat = lambda a: a[:].rearrange("p a b -> p (a b)")
            V(lambda e: e.tensor_tensor(out=A16[:], in0=flat(OH1), in1=flat(OH2), op=ALU.add), ["s6_OH1", "s6_OH2"], ["s6_A16"])
            P.op("gpsimd", lambda e: e.affine_select(out=Tri[:], in_=k.onesb[:], pattern=[[1, 128]], compare_op=ALU.is_ge, fill=0.0, base=0, channel_multiplier=-1), reads=["c_onesb"], writes=["s6_Tri"])
            for hf in range(2):
                P.op("tensor", lambda e, hf=hf: e.matmul(k.ps[hf][:, :], lhsT=Tri[:], rhs=A16[:, hf * 512:(hf + 1) * 512], start=True, stop=True), reads=["s6_Tri", "s6_A16"], writes=[f"ps{hf}"])
                P.op("tensor", lambda e, hf=hf: e.matmul(k.ps[2 + hf][:, :], lhsT=k.onesb[:], rhs=A16[:, hf * 512:(hf + 1) * 512], start=True, stop=True), reads=["c_onesb", "s6_A16"], writes=[f"ps{2 + hf}"])
                P.op("scalar", lambda e, hf=hf: e.copy(flat(INC)[:, hf * 512:(hf + 1) * 512], k.ps[hf][:, :]), reads=[f"ps{hf}"], writes=[f"s6_INC{hf}"])
                V(lambda e, hf=hf: e.tensor_copy(flat(TB)[:, hf * 512:(hf + 1) * 512], k.ps[2 + hf][:, :]), [f"ps{2 + hf}"], [f"s6_TB{hf}"])
            V(lambda e: e.tensor_copy(X[0][:], TB[:]), ["s6_TB0", "s6_TB1"], ["s6_X0"])
            cur = 0
            for s_ in (1, 2, 4, 8, 16):
                nxt = 1 - cur
                V(lambda e, s_=s_, cur=cur, nxt=nxt: e.tensor_tensor(out=X[nxt][:, s_:, :], in0=X[cur][:, s_:, :], in1=X[cur][:, 0:NT - s_, :], op=ALU.add), [f"s6_X{cur}"], [f"s6_X{nxt}"])
                V(lambda e, s_=s_, cur=cur, nxt=nxt: e.tensor_copy(X[nxt][:, 0:s_, :], X[cur][:, 0:s_, :]), [f"s6_X{cur}", f"s6_X{nxt}"], [f"s6_X{nxt}"])
                cur = nxt
            V(lambda e, cur=cur: e.tensor_tensor(out=tmp3[:], in0=X[cur][:], in1=TB[:], op=ALU.subtract), [f"s6_X{cur}", "s6_TB0", "s6_TB1"], ["s6_tmp3"])
            V(lambda e: e.tensor_tensor(out=tmp3[:], in0=tmp3[:], in1=INC[:], op=ALU.add), ["s6_tmp3", "s6_INC0", "s6_INC1"], ["s6_tmp3"])
            V(lambda e: e.tensor_scalar(out=tmp3[:], in0=tmp3[:], scalar1=-1.0, scalar2=float(C - 1), op0=ALU.add, op1=ALU.min), ["s6_tmp3"], ["s6_tmp3"])
            P.op("gpsimd", lambda e: e.iota(ECf[:], pattern=[[0, NT], [C, 32]], base=0, channel_multiplier=0, allow_small_or_imprecise_dtypes=True), writes=["s6_ECf"])
            V(lambda e: e.tensor_tensor(out=tmp3[:], in0=tmp3[:], in1=ECf[:], op=ALU.add), ["s6_tmp3", "s6_ECf"], ["s6_tmp3"])
            for kk, OH in ((0, OH1), (1, OH2)):
                ohn = "s6_OH1" if kk == 0 else "s6_OH2"
                V(lambda e, OH=OH: e.tensor_tensor(out=X[0][:], in0=OH[:], in1=tmp3[:], op=ALU.mult), [ohn, "s6_tmp3", "s6_X0", "s6_X1"], ["s6_X0"])
                V(lambda e, kk=kk: e.reduce_sum(out=Sf[:, kk, :], in_=X[0][:], axis=AX.X), ["s6_X0"], ["s6_Sf"])
            V(lambda e: e.tensor_copy(Si[:], Sf[:]), ["s6_Sf"], ["s6_Si"])
            P.op("gpsimd", lambda e: e.memset(REC[:], 0), writes=["s6_REC"])
            P.op("gpsimd", lambda e: e.iota(io_t[:], pattern=[[128, NT]], base=0, channel_multiplier=1), writes=["s6_iot"])
            for kk in range(2):
                P.op("gpsimd", lambda e, kk=kk: e.iota(io_d[:, kk, :], pattern=[[256, NT]], base=kk, channel_multiplier=2), writes=["s6_iod"])
            RECf = REC[:].bitcast(F32)
            for kk in range(2):
                P.op("gpsimd", lambda e, kk=kk: e.tensor_copy(REC[:, :, kk, 0], io_t[:]), reads=["s6_iot", "s6_REC"], writes=["s6_REC"])
                P.op("gpsimd", lambda e, kk=kk: e.tensor_copy(REC[:, :, kk, 1], io_d[:, kk, :]), reads=["s6_iod", "s6_REC"], writes=["s6_REC"])
                P.op("gpsimd", lambda e, kk=kk: e.tensor_copy(RECf[:, :, kk, 2], Gt[:, :, kk]), reads=["s6_Gt", "s6_REC"], writes=["s6_REC"])
            P.op("gpsimd", lambda e: e.memset(INIT[:], 0), writes=["s6_INIT"])
            P.op("gpsimd", lambda e: e.memset(INIT[:, :, 1:2], 2 * S), reads=["s6_INIT"], writes=["s6_INIT"])
            P.dma("gpsimd", "s6_tabinit", lambda e: e.dma_start(out=c.tab[:, :].rearrange("(p b) w -> p (b w)", p=128), in_=INIT[:].rearrange("p b w -> p (b w)")), reads=["s6_INIT"], writes=["d_tab"])
            for t in range(NT):
                for kk in range(2):
                    P.dma("gpsimd", "s6_tabsc", lambda e, t=t, kk=kk: e.indirect_dma_start(
                        out=c.tab[:, :], out_offset=bass.IndirectOffsetOnAxis(ap=Si[:, kk, t:t + 1], axis=0), in_=REC[:, t, kk, :], in_offset=None),
                        reads=["s6_Si", "s6_REC", "d_tab"], writes=[f"d_tabs{t}_{kk}"])
            tab_res = [f"d_tabs{t}_{kk}" for t in range(NT) for kk in range(2)]
            x1b_res = [f"d_x1b{t}" for t in range(NT)]
        P.barrier()
        wpg = sbt(nc, st0, "s6_wpg", [128, 8, D], BF16)
        wpp = sbt(nc, st0, "s6_wpp", [128, 2, D], BF16)
        wpgv = c.w_pg[li].rearrange("(kc kp) n -> kp kc n", kp=128)
        for q in range(2):
            P.dma("gpsimd", "s6_wpg", lambda e, q=q: e.dma_start(out=wpg[:, q * 4:(q + 1) * 4, :], in_=wpgv[:, q * 4:(q + 1) * 4, :]), writes=["s6_wpg"])
        P.dma("gpsimd", "s6_wpp", lambda e: e.dma_start(out=wpp[:], in_=c.w_pp[li].rearrange("(kc kp) n -> kp kc n", kp=128)), writes=["s6_wpp"])
        with contextlib.ExitStack() as st:
            wgu = [sbt(nc, st, f"s6_wgu{i}", [128, 8, 1024], BF16) for i in range(2)]
            wd = [sbt(nc, st, f"s6_wd{i}", [128, 4, D], BF16) for i in range(2)]
            tabt = [sbt(nc, st, f"s6_tabt{i}", [128, NB, 16], I32) for i in range(2)]
            xg = [sbt(nc, st, f"s6_xg{i}", [128, NB, D], BF16) for i in range(2)]
            xgT = [sbt(nc, st, f"s6_xgT{i}", [128, 8, C], BF16) for i in range(2)]
            hT = [sbt(nc, st, f"s6_hT{i}", [128, 4, C], BF16) for i in range(2)]
            sgl = [sbt(nc, st, f"s6_sg{i}", [128, C], F32) for i in range(2)]
            yg = [sbt(nc, st, f"s6_yg{i}", [128, D], F32) for i in range(2)]

            def loads(ex):
                i2 = ex % 2
                P.dma("gpsimd", f"s6_wgu{i2}", lambda e: e.dma_start(out=wgu[i2][:, :, 0:512], in_=c.w_eg[li, ex].rearrange("(kc kp) f -> kp kc f", kp=128)), writes=[f"s6_wgu{i2}"])
                P.dma("gpsimd", f"s6_wgu{i2}", lambda e: e.dma_start(out=wgu[i2][:, :, 512:1024], in_=c.w_eu[li, ex].rearrange("(kc kp) f -> kp kc f", kp=128)), reads=[f"s6_wgu{i2}"], writes=[f"s6_wgu{i2}"])
                P.dma("gpsimd", f"s6_wd{i2}", lambda e: e.dma_start(out=wd[i2][:], in_=c.w_ed[li, ex].rearrange("(fc fp) n -> fp fc n", fp=128)), writes=[f"s6_wd{i2}"])
                P.dma("gpsimd", f"s6_tabt{i2}", lambda e: e.dma_start(out=tabt[i2][:], in_=c.tab[ex * C:(ex + 1) * C, :].rearrange("(b p) w -> p b w", p=128)), reads=["s6_wpg", "s6_wpp"], writes=[f"s6_tabt{i2}"])
                for b in range(NB):
                    P.dma("gpsimd", f"s6_xg{i2}", lambda e, b=b: e.indirect_dma_start(
                        out=xg[i2][:, b, :], out_offset=None, in_=c.x1b[:, :], in_offset=bass.IndirectOffsetOnAxis(ap=tabt[i2][:, b, 0:1], axis=0)),
                        reads=[f"s6_tabt{i2}"], writes=[f"s6_xg{i2}_{b}"])

            nps = [0]

            def compute(ex):
                i2 = ex % 2
                tabf = tabt[i2][:].bitcast(F32)
                for b in range(NB):
                    pq, pqn = (psb6, "ps6") if b % 2 == 0 else (psb7, "ps7")
                    for kc in range(8):
                        P.op("tensor", lambda e, b=b, kc=kc, pq=pq: e.transpose(pq[:, kc * 128:(kc + 1) * 128], xg[i2][:, b, kc * 128:(kc + 1) * 128], k.identb[:]),
                             reads=[f"s6_xg{i2}_{b}", "c_identb"], writes=[pqn])
                    fn = (lambda e, b=b, pq=pq: e.tensor_copy(xgT[i2][:, :, b * 128:(b + 1) * 128], pq[:, :].rearrange("p (a b) -> p a b", a=8))) if b % 2 == 0 else \
                         (lambda e, b=b, pq=pq: e.copy(xgT[i2][:, :, b * 128:(b + 1) * 128], pq[:, :].rearrange("p (a b) -> p a b", a=8)))
                    P.op("vector" if b % 2 == 0 else "scalar", fn, reads=[pqn], writes=[f"s6_xgT{i2}_{b}"])
                xres = [f"s6_xgT{i2}_{b}" for b in range(NB)]
                for fc in range(4):
                    pg, pgn = k.ps[nps[0] % 6], f"ps{nps[0] % 6}"
                    nps[0] += 1
                    pu, pun = k.ps[nps[0] % 6], f"ps{nps[0] % 6}"
                    nps[0] += 1
                    for kc in range(8):
                        P.op("tensor", lambda e, pg=pg, kc=kc, fc=fc: e.matmul(pg[:, :], lhsT=wgu[i2][:, kc, fc * 128:(fc + 1) * 128], rhs=xgT[i2][:, kc, :], start=(kc == 0), stop=(kc == 7)),
                             reads=[f"s6_wgu{i2}"] + xres, writes=[pgn])
                    for kc in range(8):
                        P.op("tensor", lambda e, pu=pu, kc=kc, fc=fc: e.matmul(pu[:, :], lhsT=wgu[i2][:, kc, 512 + fc * 128:512 + (fc + 1) * 128], rhs=xgT[i2][:, kc, :], start=(kc == 0), stop=(kc == 7)),
                             reads=[f"s6_wgu{i2}"] + xres, writes=[pun])
                    sg, sgn = sgl[fc % 2], f"s6_sg{fc % 2}"
                    P.op("scalar", lambda e, pg=pg, sg=sg: e.activation(out=sg[:], in_=pg[:, :], func=AF.Silu), reads=[pgn], writes=[sgn])
                    P.op("vector", lambda e, pu=pu, sg=sg, fc=fc: e.tensor_tensor(out=hT[i2][:, fc, :], in0=pu[:, :], in1=sg[:], op=ALU.mult), reads=[pun, sgn], writes=[f"s6_hT{i2}_{fc}"])
                for b in range(NB):
                    ygb, ygn = yg[b % 2], f"s6_yg{b % 2}"
                    for nh in range(2):
                        py, pyn = k.ps[nps[0] % 6], f"ps{nps[0] % 6}"
                        nps[0] += 1
                        for fc in range(4):
                            P.op("tensor", lambda e, py=py, fc=fc, b=b, nh=nh: e.matmul(py[:, :], lhsT=hT[i2][:, fc, b * 128:(b + 1) * 128], rhs=wd[i2][:, fc, nh * 512:(nh + 1) * 512], start=(fc == 0), stop=(fc == 3)),
                                 reads=[f"s6_wd{i2}", f"s6_hT{i2}_{fc}"], writes=[pyn])
                        P.op("vector", lambda e, py=py, b=b, nh=nh, ygb=ygb: e.tensor_scalar(out=ygb[:, nh * 512:(nh + 1) * 512], in0=py[:, :], scalar1=tabf[:, b, 2:3], scalar2=None, op0=ALU.mult),
                             reads=[pyn, f"s6_tabt{i2}"], writes=[ygn + f"_{nh}"])
                    P.dma("gpsimd", "s6_ysc", lambda e, b=b, ygb=ygb: e.indirect_dma_start(
                        out=c.ybuf[:, :], out_offset=bass.IndirectOffsetOnAxis(ap=tabt[i2][:, b, 1:2], axis=0), in_=ygb[:, :], in_offset=None),
                        reads=[ygn + "_0", ygn + "_1", f"s6_tabt{i2}"], writes=[f"d_ybuf{ex}_{b}"])

            loads(0)
            for ex in range(32):
                if ex + 1 < 32:
                    loads(ex + 1)
                compute(ex)
        P.barrier()
        with contextlib.ExitStack() as st:
            xin = [sbt(nc, st, f"s6_xin2{i}", [128, D], F32) for i in range(2)]
            xTt = [sbt(nc, st, f"s6_xTt{i}", [128, 8, 128], BF16) for i in range(2)]
            ym = [sbt(nc, st, f"s6_ym{i}", [128, 2, D], F32) for i in range(2)]
            pin = [sbt(nc, st, f"s6_pin{i}", [128, 256], F32) for i in range(2)]
            pT = [sbt(nc, st, f"s6_pT{i}", [128, 2, 128], BF16) for i in range(2)]
            sgt = sbt(nc, st, "s6_sgt", [128, D], F32)
            z = [sbt(nc, st, f"s6_z{i}", [128, D], F32) for i in range(2)]
            xo = [sbt(nc, st, f"s6_xo{i}", [128, D], F32) for i in range(2)]
            stats = sbt(nc, st, "s6_stats", [128, 2, 6], F32)
            mv = sbt(nc, st, "s6_mv", [128, 2], F32)
            rstd = sbt(nc, st, "s6_rstd", [128, 1], F32)
            P.dma("sync", "s6_dummy", lambda e: e.dma_start(out=c.dummy[:, :], in_=c.x1s[0:512, :]), writes=["d_dummy"])
            nps = 0
            for t in range(NT):
                i2 = t % 2
                xb, xn = xin[i2], f"s6_xin2{i2}"
                P.dma("sync", xn, lambda e, xb=xb, t=t: e.dma_start(out=xb[:], in_=c.x1s[t * 128:(t + 1) * 128, :]), writes=[xn])
                P.dma("sync", f"s6_pin{i2}", lambda e, t=t, i2=i2: e.dma_start(out=pin[i2][:], in_=c.p[li, t * 128:(t + 1) * 128, :]), writes=[f"s6_pin{i2}"])
                P.dma("sync", f"s6_ym{i2}", lambda e, t=t, i2=i2: e.dma_start(out=ym[i2][:], in_=c.ybuf[t * 256:(t + 1) * 256, :].rearrange("(p two) n -> p two n", two=2)), reads=["d_dummy"], writes=[f"s6_ym{i2}"])
                for half in range(2):
                    pb, pn = k.ps[nps % 6], f"ps{nps % 6}"
                    nps += 1
                    for q in range(4):
                        kc = half * 4 + q
                        P.op("tensor", lambda e, pb=pb, xb=xb, kc=kc, q=q: e.transpose(pb[:, q * 128:(q + 1) * 128], xb[:, kc * 128:(kc + 1) * 128], k.ident[:]),
                             reads=[xn, "c_ident"], writes=[pn])
                    fn = (lambda e, pb=pb, half=half, i2=i2: e.tensor_copy(xTt[i2][:, half * 4:(half + 1) * 4, :], pb[:].rearrange("p (a b) -> p a b", a=4))) if half == 0 else \
                         (lambda e, pb=pb, half=half, i2=i2: e.copy(xTt[i2][:, half * 4:(half + 1) * 4, :], pb[:].rearrange("p (a b) -> p a b", a=4)))
                    P.op("vector" if half == 0 else "scalar", fn, reads=[pn], writes=[f"s6_xTt{i2}_{half}"])
                pb, pn = k.ps[nps % 6], f"ps{nps % 6}"
                nps += 1
                for q in range(2):
                    P.op("tensor", lambda e, pb=pb, q=q, i2=i2: e.transpose(pb[:, q * 128:(q + 1) * 128], pin[i2][:, q * 128:(q + 1) * 128], k.ident[:]), reads=[f"s6_pin{i2}", "c_ident"], writes=[pn])
                P.op("scalar", lambda e, pb=pb, i2=i2: e.copy(pT[i2][:], pb[:, 0:256].rearrange("p (a b) -> p a b", a=2)), reads=[pn], writes=[f"s6_pT{i2}"])
                zb, zn = z[i2], f"s6_z{i2}"
                for nh in range(2):
                    pgt, pgtn = k.ps[nps % 6], f"ps{nps % 6}"
                    nps += 1
                    pp, ppn = k.ps[nps % 6], f"ps{nps % 6}"
                    nps += 1
                    for kc in range(8):
                        P.op("tensor", lambda e, pgt=pgt, kc=kc, nh=nh, i2=i2: e.matmul(pgt[:, :], lhsT=xTt[i2][:, kc, :], rhs=wpg[:, kc, nh * 512:(nh + 1) * 512], start=(kc == 0), stop=(kc == 7)),
                             reads=["s6_wpg", f"s6_xTt{i2}_{kc // 4}"], writes=[pgtn])
                    for kc in range(2):
                        P.op("tensor", lambda e, pp=pp, kc=kc, nh=nh, i2=i2: e.matmul(pp[:, :], lhsT=pT[i2][:, kc, :], rhs=wpp[:, kc, nh * 512:(nh + 1) * 512], start=(kc == 0), stop=(kc == 1)),
                             reads=["s6_wpp", f"s6_pT{i2}"], writes=[ppn])
                    P.op("scalar", lambda e, pgt=pgt, nh=nh: e.activation(out=sgt[:, nh * 512:(nh + 1) * 512], in_=pgt[:, :], func=AF.Sigmoid), reads=[pgtn], writes=[f"s6_sgt{nh}"])
                    P.op("vector", lambda e, pp=pp, nh=nh, zb=zb: e.tensor_tensor(out=zb[:, nh * 512:(nh + 1) * 512], in0=pp[:, :], in1=sgt[:, nh * 512:(nh + 1) * 512], op=ALU.mult),
                         reads=[ppn, f"s6_sgt{nh}"], writes=[zn + f"_{nh}"])
                P.op("gpsimd", lambda e, zb=zb, i2=i2: e.tensor_tensor(out=zb[:], in0=zb[:], in1=ym[i2][:, 0, :], op=ALU.add), reads=[zn + "_0", zn + "_1", f"s6_ym{i2}"], writes=[zn])
                P.op("gpsimd", lambda e, zb=zb, i2=i2: e.tensor_tensor(out=zb[:], in0=zb[:], in1=ym[i2][:, 1, :], op=ALU.add), reads=[zn, f"s6_ym{i2}"], writes=[zn])
                P.op("vector", lambda e, zb=zb, xb=xb: e.scalar_tensor_tensor(out=zb[:], in0=xb[:], scalar=ALPHA, in1=zb[:], op0=ALU.mult, op1=ALU.add), reads=[xn, zn], writes=[zn])
                ob, on = xo[i2], f"s6_xo{i2}"
                layer_norm_tile(P, k, zb, zn, gam, bet, stats, mv, rstd, ob, on, eps[:, 0:1], "s6")
                P.dma("sync", "s6_out", lambda e, t=t, ob=ob: e.dma_start(out=dst[t * 128:(t + 1) * 128, :], in_=ob[:]), reads=[on])
    P.barrier()


def build_layers(nc, layers_local, first, last):
    with contextlib.ExitStack() as st:
        P = Prog(nc, st)
        c = declare_io(nc, layers_local, first, last)
        k = make_consts(nc, st, P)
        n = len(layers_local)
        for li in range(n):
            src = c.x_in if li == 0 else c.xs
            dst = c.out if li == n - 1 else c.xs
            stage1_proj(nc, P, k, c, li, src)
            stage2_attn(nc, P, k, c, li)
            stage3_pool(nc, P, k, c, li)
            stage4_dn(nc, P, k, c, li)
            stage5_merge(nc, P, k, c, li, src)
            stage6_moe_sparse(nc, P, k, c, li, dst)
        P.wait_all("sync")
        P.emit()
        return P.nops


def _core_map(inp, xb, b, layers):
    L = list(layers)
    n = len(L)
    g = lambda nm: np.ascontiguousarray(inp[nm][L])
    return {
        "x_in": np.ascontiguousarray(xb, dtype=np.float32),
        "p": np.ascontiguousarray(inp["p"][L][:, b]),
        "w_in": g("w_in"), "b_forget": g("b_forget").reshape(n, 8, 1),
        "pool_w": g("pool_w"), "pool_scale": g("pool_scale").reshape(n, 4, 128, 1),
        "dn_conv": g("dn_conv"), "dn_a_log": g("dn_a_log").reshape(n, 4, 1),
        "dn_dt_bias": g("dn_dt_bias").reshape(n, 4, 1), "dn_norm_w": g("dn_norm_w").reshape(n, 128, 1),
        "w_br": np.ascontiguousarray(np.concatenate([inp["w_br_attn"][L], inp["w_br_pool"][L], inp["w_br_dn"][L]], axis=1)),
        "w_out": g("w_out"), "ln1_g": g("ln1_g"), "ln1_b": g("ln1_b"),
        "w_r": np.ascontiguousarray(np.concatenate([inp["w_router_group"][L], inp["w_router_expert"][L]], axis=2)),
        "b_r": np.ascontiguousarray(np.concatenate([inp["b_router_group"][L], inp["b_router_expert"][L]], axis=1)),
        "w_eg": g("w_exp_gate"), "w_eu": g("w_exp_up"), "w_ed": g("w_exp_down"),
        "w_pp": g("w_ple_proj"), "w_pg": g("w_ple_gate"), "ln2_g": g("ln2_g"), "ln2_b": g("ln2_b"),
    }


LAYER_GROUPS = [[0, 1, 2, 3]]


def kernel(**inputs):
    inp = {k: np.asarray(v) for k, v in inputs.items()}
    cur = [inp["x"][b] for b in range(4)]
    for grp in LAYER_GROUPS:
        nc = bass.Bass("TRN2", target_bir_lowering=False)
        build_layers(nc, list(range(len(grp))), True, True)
        base = [_core_map(inp, cur[b], b, grp) for b in range(4)]
        maps = [base[c % 4] for c in range(8)]
        res = run_bass_kernel_spmd(nc, maps, core_ids=list(range(8)))
        cur = [np.asarray(res.results[b]["out"], dtype=np.float32) for b in range(4)]
    return np.stack(cur).astype(np.float32)
```

```python
import contextlib
import numpy as np
import concourse.bass as bass
import concourse.mybir as mybir
from concourse.bass_utils import run_bass_kernel_spmd

F32 = mybir.dt.float32
BF16 = mybir.dt.bfloat16
I32 = mybir.dt.int32
AF = mybir.ActivationFunctionType
ALU = mybir.AluOpType
AX = mybir.AxisListType

ENGS = ["tensor", "vector", "scalar", "gpsimd", "sync"]
SEM_LIMIT = 30000


class Prog:
    def __init__(self, nc, stack):
        self.nc = nc
        self.stack = stack
        self.stream = {e: [] for e in ENGS}
        self.cnt = {e: 0 for e in ENGS}
        self.nsem = 0
        self.sem = {e: self._newsem("p_" + e) for e in ENGS}
        self.waited = {e: {} for e in ENGS}
        self.lastw = {}
        self.readers = {}
        self.dsem = {}
        self.nops = 0

    def _newsem(self, name):
        self.nsem += 1
        return self.stack.enter_context(self.nc.semaphore(f"{name}_{self.nsem}"))

    def _need(self, eng, toks):
        need = {}
        for (sem, val, teng), kind in toks:
            if teng == eng and (kind != "raw" or eng == "tensor"):
                continue
            k = id(sem)
            if self.waited[eng].get(k, 0) >= val:
                continue
            if k not in need or need[k][1] < val:
                need[k] = (sem, val)
        for k, (sem, val) in need.items():
            self.waited[eng][k] = val
        return list(need.values())

    def _deps(self, eng, reads, writes):
        toks = []
        for r in reads:
            t = self.lastw.get(r)
            if t is not None:
                toks.append((t, "raw"))
        for w in writes:
            t = self.lastw.get(w)
            if t is not None:
                toks.append((t, "waw"))
            for t in self.readers.get(w, {}).values():
                toks.append((t, "war"))
        return self._need(eng, toks)

    def _update(self, tok, reads, writes):
        sem, val, teng = tok
        rk = teng if teng != "dma" else ("dma", id(sem))
        for r in reads:
            self.readers.setdefault(r, {})[rk] = tok
        for w in writes:
            self.lastw[w] = tok
            self.readers[w] = {}

    def op(self, eng, fn, reads=(), writes=()):
        waits = self._deps(eng, reads, writes)
        if self.cnt[eng] >= SEM_LIMIT:
            self.sem[eng] = self._newsem("p_" + eng)
            self.cnt[eng] = 0
        self.cnt[eng] += 1
        tok = (self.sem[eng], self.cnt[eng], eng)
        self.stream[eng].append((waits, fn, self.sem[eng], 1))
        self._update(tok, reads, writes)
        self.nops += 1
        return tok

    def dma(self, eng, key, fn, reads=(), writes=()):
        waits = self._deps(eng, reads, writes)
        s = self.dsem.get(key)
        if s is None or s[1] >= SEM_LIMIT:
            s = [self._newsem("d"), 0]
            self.dsem[key] = s
        s[1] += 16
        tok = (s[0], s[1], "dma")
        self.stream[eng].append((waits, fn, s[0], 16))
        self._update(tok, reads, writes)
        self.nops += 1
        return tok

    def barrier(self):
        toks = []
        for e in ENGS:
            if self.cnt[e] > 0:
                toks.append(((self.sem[e], self.cnt[e], e), "raw"))
        for k, s in self.dsem.items():
            toks.append(((s[0], s[1], "dma"), "raw"))
        for e in ENGS:
            waits = self._need(e, [t for t in toks if t[0][2] != e])
            if waits:
                self.stream[e].append((waits, None, None, 0))
        self.lastw = {}
        self.readers = {}

    def wait_all(self, eng):
        toks = []
        for e in ENGS:
            if self.cnt[e] > 0 and e != eng:
                toks.append(((self.sem[e], self.cnt[e], e), "raw"))
        for k, s in self.dsem.items():
            toks.append(((s[0], s[1], "dma"), "raw"))
        waits = self._need(eng, toks)
        if waits:
            self.stream[eng].append((waits, None, None, 0))

    def emit(self):
        with self.nc.Block() as block:
            for eng in ENGS:
                def body(e, eng=eng):
                    for (waits, fn, sem, inc) in self.stream[eng]:
                        for (s, v) in waits:
                            e.wait_ge(s, v)
                        if fn is not None:
                            fn(e).then_inc(sem, inc)
                getattr(block, eng)(body)


S = 4096
D = 1024
NT = S // 128
DEPTH = 4
ALPHA = (2 * DEPTH) ** 0.25
C_AQ, C_AK, C_AV, C_AF, C_PU, C_DQ, C_DK, C_DV, C_DA, C_DB, C_DG, C_GA, C_GP, C_GD = (
    0, 512, 1024, 1536, 1544, 2056, 2568, 3080, 3592, 3596, 3600, 4112, 5136, 6160)
INW = 7184


class Ctx:
    pass


DBG_OUT = set()
DBG_FLAGS = set()
NH_DBG = 8
NI_DBG = 16


_SBT_N = [0]


def sbt(nc, st, name, shape, dt):
    _SBT_N[0] += 1
    return st.enter_context(nc.sbuf_tensor(f"{name}_u{_SBT_N[0]}", shape, dt))


def declare_io(nc, layers, first, last):
    L = len(layers)
    c = Ctx()
    EI = "ExternalInput"
    c.x_in = nc.dram_tensor("x_in", [S, D], F32, kind=EI).ap()
    c.p = nc.dram_tensor("p", [L, S, 256], F32, kind=EI).ap()
    c.w_in = nc.dram_tensor("w_in", [L, D, INW], F32, kind=EI).ap()
    c.b_forget = nc.dram_tensor("b_forget", [L, 8, 1], F32, kind=EI).ap()
    c.pool_w = nc.dram_tensor("pool_w", [L, 4, 128, 128], F32, kind=EI).ap()
    c.pool_scale = nc.dram_tensor("pool_scale", [L, 4, 128, 1], F32, kind=EI).ap()
    c.dn_conv = nc.dram_tensor("dn_conv", [L, 4, 1536], F32, kind=EI).ap()
    c.dn_a_log = nc.dram_tensor("dn_a_log", [L, 4, 1], F32, kind=EI).ap()
    c.dn_dt_bias = nc.dram_tensor("dn_dt_bias", [L, 4, 1], F32, kind=EI).ap()
    c.dn_norm_w = nc.dram_tensor("dn_norm_w", [L, 128, 1], F32, kind=EI).ap()
    c.w_br = nc.dram_tensor("w_br", [L, 1536, D], F32, kind=EI).ap()
    c.w_out = nc.dram_tensor("w_out", [L, D, D], F32, kind=EI).ap()
    c.ln1_g = nc.dram_tensor("ln1_g", [L, D], F32, kind=EI).ap()
    c.ln1_b = nc.dram_tensor("ln1_b", [L, D], F32, kind=EI).ap()
    c.w_r = nc.dram_tensor("w_r", [L, D, 36], F32, kind=EI).ap()
    c.b_r = nc.dram_tensor("b_r", [L, 36], F32, kind=EI).ap()
    c.w_eg = nc.dram_tensor("w_eg", [L, 32, D, 512], F32, kind=EI).ap()
    c.w_eu = nc.dram_tensor("w_eu", [L, 32, D, 512], F32, kind=EI).ap()
    c.w_ed = nc.dram_tensor("w_ed", [L, 32, 512, D], F32, kind=EI).ap()
    c.w_pp = nc.dram_tensor("w_pp", [L, 256, D], F32, kind=EI).ap()
    c.w_pg = nc.dram_tensor("w_pg", [L, D, D], F32, kind=EI).ap()
    c.ln2_g = nc.dram_tensor("ln2_g", [L, D], F32, kind=EI).ap()
    c.ln2_b = nc.dram_tensor("ln2_b", [L, D], F32, kind=EI).ap()
    c.out = nc.dram_tensor("out", [S, D], F32, kind="ExternalOutput").ap()
    def scr(name, shape, dt):
        kind = "ExternalOutput" if name in DBG_OUT else "Internal"
        return nc.dram_tensor(name, shape, dt, kind=kind).ap()
    c.xs = scr("xs", [S, D], F32)
    c.x1s = scr("x1s", [S, D], F32)
    c.qkT = scr("qkT", [1024, S], BF16)
    c.vtm = scr("vtm", [S, 512], BF16)
    c.smT = scr("smT", [16, S], F32)
    c.fT = scr("fT", [2560, S], F32)
    c.yT = scr("yT", [1536, S], BF16)
    c.dbg1 = scr("dbg1", [128, 4096], F32)
    c.dnrow = scr("dnrow", [2, 4, S], F32)
    c.x1b = scr("x1b", [S, D], BF16)
    c.tab = scr("tab", [32 * 512, 16], I32)
    c.ybuf = scr("ybuf", [2 * S + 128, D], F32)
    c.dummy = scr("dly_scratch", [512, D], F32)
    return c


def make_consts(nc, st, P):
    k = Ctx()
    k.ones = sbt(nc, st, "c_ones", [128, 128], F32)
    k.ident = sbt(nc, st, "c_ident", [128, 128], F32)
    k.identb = sbt(nc, st, "c_identb", [128, 128], BF16)
    k.onesb = sbt(nc, st, "c_onesb", [128, 128], BF16)
    k.ps = [st.enter_context(nc.psum_tensor(f"ps{i}", [128, 512], F32)) for i in range(8)]
    P.op("gpsimd", lambda e: e.memset(k.ones[:], 1.0), writes=["c_ones"])
    P.op("gpsimd", lambda e: e.affine_select(out=k.ident[:], in_=k.ones[:], pattern=[[-1, 128]],
                                             compare_op=ALU.is_equal, fill=0.0, base=0, channel_multiplier=1),
         reads=["c_ones"], writes=["c_ident"])
    P.op("vector", lambda e: e.tensor_copy(k.identb[:], k.ident[:]), reads=["c_ident"], writes=["c_identb"])
    P.op("vector", lambda e: e.tensor_copy(k.onesb[:], k.ones[:]), reads=["c_ones"], writes=["c_onesb"])
    return k


def build_xT(nc, P, k, st, src, xT, tag):
    xin = [sbt(nc, st, f"{tag}_xin{i}", [128, D], F32) for i in range(2)]
    for t in range(NT):
        b = xin[t % 2]
        bn = f"{tag}_xin{t % 2}"
        P.dma("sync", bn, lambda e, b=b, t=t: e.dma_start(out=b[:], in_=src[t * 128:(t + 1) * 128, :]), writes=[bn])
        for half in range(2):
            pb = k.ps[(t % 2) * 2 + half]
            pn = f"ps{(t % 2) * 2 + half}"
            for q in range(4):
                kc = half * 4 + q
                P.op("tensor", lambda e, pb=pb, b=b, kc=kc, q=q: e.transpose(pb[:, q * 128:(q + 1) * 128], b[:, kc * 128:(kc + 1) * 128], k.ident[:]),
                     reads=[bn, "c_ident"], writes=[pn])
            eng = "vector" if half == 0 else "scalar"
            if eng == "vector":
                P.op("vector", lambda e, pb=pb, half=half, t=t: e.tensor_copy(
                    xT[:, half * 4:(half + 1) * 4, t * 128:(t + 1) * 128], pb[:].rearrange("p (a b) -> p a b", a=4)),
                    reads=[pn], writes=[f"{tag}_xT{t}_{half}"])
            else:
                P.op("scalar", lambda e, pb=pb, half=half, t=t: e.copy(
                    xT[:, half * 4:(half + 1) * 4, t * 128:(t + 1) * 128], pb[:].rearrange("p (a b) -> p a b", a=4)),
                    reads=[pn], writes=[f"{tag}_xT{t}_{half}"])


def stage1_proj(nc, P, k, c, li, src):
    with contextlib.ExitStack() as st:
        xT = sbt(nc, st, "s1_xT", [128, 8, S], BF16)
        build_xT(nc, P, k, st, src, xT, "s1")
        vstg = sbt(nc, st, "s1_vstg", [128, 8 * 512], BF16)
        wb = [sbt(nc, st, f"s1_w{i}", [128, 8, 512], BF16) for i in range(2)]
        stg = [sbt(nc, st, f"s1_stg{i}", [128, S], F32) for i in range(2)]
        stgb = [sbt(nc, st, f"s1_stgb{i}", [128, S], BF16) for i in range(2)]
        wv = c.w_in[li].rearrange("(kc kp) n -> kp kc n", kp=128)
        groups = [(C_AQ, 512, "q"), (C_AK, 512, "k"), (C_PU, 512, "f0"), (C_DQ, 512, "f1"), (C_DK, 512, "f2"),
                  (C_DV, 512, "f3"), (C_DG, 512, "f4"), (C_AF, 16, "sm0"), (C_DA, 8, "sm1"), (C_AV, 512, "v")]
        gi = 0
        nev = 0
        nst = 0
        for (c0, ncols, kind) in groups:
            w = wb[gi % 2]
            wn = f"s1_w{gi % 2}"
            gi += 1
            if kind == "sm0":
                P.dma("gpsimd", wn, lambda e, w=w: e.dma_start(out=w[:, :, 0:8], in_=wv[:, :, C_AF:C_AF + 8]), writes=[wn])
                P.dma("gpsimd", wn, lambda e, w=w: e.dma_start(out=w[:, :, 8:16], in_=wv[:, :, C_DA:C_DA + 8]), writes=[wn])
            elif kind == "sm1":
                gi -= 1
                continue
            else:
                P.dma("gpsimd", wn, lambda e, w=w, c0=c0, ncols=ncols: e.dma_start(out=w[:, :, 0:ncols], in_=wv[:, :, c0:c0 + ncols]), writes=[wn])
            if kind == "v":
                for t in range(NT):
                    pb = k.ps[4 + t % 4]
                    pn = f"ps{4 + t % 4}"
                    for kc in range(8):
                        P.op("tensor", lambda e, pb=pb, kc=kc, t=t, w=w: e.matmul(pb[:, :], lhsT=xT[:, kc, t * 128:(t + 1) * 128], rhs=w[:, kc, :], start=(kc == 0), stop=(kc == 7)),
                             reads=[wn, f"s1_xT{t}_{kc // 4}"], writes=[pn])
                    vslot = t % 8
                    eng = "vector" if t % 2 == 0 else "scalar"
                    fn = (lambda e, pb=pb, vslot=vslot: e.tensor_copy(vstg[:, vslot * 512:(vslot + 1) * 512], pb[:, :])) if eng == "vector" else \
                         (lambda e, pb=pb, vslot=vslot: e.copy(vstg[:, vslot * 512:(vslot + 1) * 512], pb[:, :]))
                    P.op(eng, fn, reads=[pn], writes=[f"s1_vstg_{vslot}"])
                    if vslot == 7:
                        t0 = t - 7
                        P.dma("sync", "s1_vout", lambda e, t0=t0: e.dma_start(
                            out=c.vtm[t0 * 128:(t0 + 8) * 128, :].rearrange("(a p) n -> p a n", p=128),
                            in_=vstg[:, :].rearrange("p (a n) -> p a n", a=8)),
                            reads=[f"s1_vstg_{v}" for v in range(8)])
                continue
            nch = (ncols + 127) // 128
            for ch in range(nch):
                m = min(128, ncols - ch * 128)
                isb = kind in ("q", "k")
                sbuf = (stgb if isb else stg)[nst % 2]
                sname = ("s1_stgb" if isb else "s1_stg") + str(nst % 2)
                nst += 1
                for tc in range(8):
                    pb = k.ps[4 + nev % 4]
                    pn = f"ps{4 + nev % 4}"
                    for kc in range(8):
                        P.op("tensor", lambda e, pb=pb, kc=kc, tc=tc, w=w, ch=ch, m=m: e.matmul(
                            pb[0:m, :], lhsT=w[:, kc, ch * 128:ch * 128 + m], rhs=xT[:, kc, tc * 512:(tc + 1) * 512], start=(kc == 0), stop=(kc == 7)),
                            reads=[wn] + [f"s1_xT{tt}_{kc // 4}" for tt in range(tc * 4, tc * 4 + 4)], writes=[pn])
                    sc = 0.125 if kind == "q" else 1.0
                    if nev % 2 == 0:
                        P.op("vector", lambda e, pb=pb, tc=tc, sbuf=sbuf, m=m, sc=sc: e.tensor_scalar(
                            out=sbuf[0:m, tc * 512:(tc + 1) * 512], in0=pb[0:m, :], scalar1=sc, scalar2=None, op0=ALU.mult),
                            reads=[pn], writes=[sname])
                    else:
                        P.op("scalar", lambda e, pb=pb, tc=tc, sbuf=sbuf, m=m, sc=sc: e.mul(
                            sbuf[0:m, tc * 512:(tc + 1) * 512], pb[0:m, :], sc),
                            reads=[pn], writes=[sname])
                    nev += 1
                if kind == "q":
                    dst = c.qkT[ch * 128:(ch + 1) * 128, :]
                elif kind == "k":
                    dst = c.qkT[512 + ch * 128:512 + (ch + 1) * 128, :]
                elif kind == "sm0":
                    dst = c.smT[0:16, :]
                else:
                    fi = int(kind[1])
                    dst = c.fT[fi * 512 + ch * 128:fi * 512 + (ch + 1) * 128, :]
                P.dma("sync", "s1_out", lambda e, dst=dst, sbuf=sbuf, m=m: e.dma_start(out=dst, in_=sbuf[0:m, :]), reads=[sname])
    P.barrier()


def stage2_attn(nc, P, k, c, li):
    with contextlib.ExitStack() as st:
        qT = sbt(nc, st, "s2_q", [128, 4, S], BF16)
        kT = sbt(nc, st, "s2_k", [128, 4, S], BF16)
        va = sbt(nc, st, "s2_v", [128, NT, 8, 65], BF16)
        af = sbt(nc, st, "s2_af", [8, S], F32)
        cc = sbt(nc, st, "s2_cc", [8, S], F32)
        ones8 = sbt(nc, st, "s2_ones8", [8, S], F32)
        nb = sbt(nc, st, "s2_nb", [8, 1], F32)
        ck = sbt(nc, st, "s2_ck", [128, NT, 8], F32)
        rball = sbt(nc, st, "s2_rb", [128, NT, 8], F32)
        sel0 = sbt(nc, st, "s2_sel0", [128, 128], F32)
        biasb = [sbt(nc, st, f"s2_bias{i}", [128, NT], F32) for i in range(4)]
        PT = [sbt(nc, st, f"s2_PT{i}", [128, 256], BF16) for i in range(8)]
        rden = sbt(nc, st, "s2_rden", [128, 256], F32)
        bc = sbt(nc, st, "s2_bc", [64, 256], F32)
        ystg = [sbt(nc, st, f"s2_y{i}", [64, S], BF16) for i in range(2)]
        for pr in range(4):
            P.dma("sync", f"s2_q{pr}", lambda e, pr=pr: e.dma_start(out=qT[:, pr, :], in_=c.qkT[pr * 128:(pr + 1) * 128, :]), reads=["d_qkT"], writes=[f"s2_q{pr}"])
            P.dma("sync", f"s2_k{pr}", lambda e, pr=pr: e.dma_start(out=kT[:, pr, :], in_=c.qkT[512 + pr * 128:512 + (pr + 1) * 128, :]), reads=["d_qkT"], writes=[f"s2_k{pr}"])
        P.op("gpsimd", lambda e: e.memset(va[:, :, :, 64:65], 1.0), writes=["s2_v1"])
        vsrc = c.vtm.rearrange("(t p) (h d) -> p t h d", p=128, h=8)
        for g in range(NT):
            P.dma("sync", "s2_v", lambda e, g=g: e.dma_start(out=va[:, g, :, 0:64], in_=vsrc[:, g, :, :]), reads=["d_vtm"], writes=["s2_v"])
        P.dma("sync", "s2_af", lambda e: e.dma_start(out=af[:], in_=c.smT[0:8, :]), writes=["s2_af"])
        P.dma("sync", "s2_nb", lambda e: e.dma_start(out=nb[:], in_=c.b_forget[li]), writes=["s2_nb"])
        P.op("gpsimd", lambda e: e.memset(ones8[:], 1.0), writes=["s2_ones8"])
        P.op("gpsimd", lambda e: e.memset(sel0[:], 0.0), writes=["s2_sel0"])
        P.op("gpsimd", lambda e: e.memset(sel0[0:1, :], 1.0), writes=["s2_sel0"])
        P.op("vector", lambda e: e.tensor_scalar(out=nb[:], in0=nb[:], scalar1=-1.0, scalar2=None, op0=ALU.mult), reads=["s2_nb"], writes=["s2_nb"])
        P.op("scalar", lambda e: e.activation(out=af[:], in_=af[:], func=AF.Exp, bias=nb[:, 0:1], scale=-1.0), reads=["s2_af", "s2_nb"], writes=["s2_af"])
        P.op("scalar", lambda e: e.activation(out=af[:], in_=af[:], func=AF.Ln, bias=k.ones[0:8, 0:1], scale=1.0), reads=["s2_af", "c_ones"], writes=["s2_af"])
        P.op("vector", lambda e: e.tensor_scalar(out=af[:], in0=af[:], scalar1=-1.0, scalar2=None, op0=ALU.mult), reads=["s2_af"], writes=["s2_af"])
        P.op("vector", lambda e: e.tensor_tensor_scan(out=cc[:], data0=ones8[:], data1=af[:], initial=0.0, op0=ALU.mult, op1=ALU.add),
             reads=["s2_af", "s2_ones8"], writes=["s2_cc"])
        for j in range(NT):
            P.op("tensor", lambda e, j=j: e.transpose(k.ps[7][:, j * 8:(j + 1) * 8], cc[0:8, j * 128:(j + 1) * 128], k.ident[0:8, 0:8]),
                 reads=["s2_cc", "c_ident"], writes=["ps7"])
        P.op("vector", lambda e: e.tensor_copy(ck[:].rearrange("p a b -> p (a b)"), k.ps[7][:, 0:256]), reads=["ps7"], writes=["s2_ck"])
        P.op("tensor", lambda e: e.matmul(k.ps[7][:, 256:512], lhsT=sel0[:], rhs=ck[:].rearrange("p a b -> p (a b)"), start=True, stop=True),
             reads=["s2_ck", "s2_sel0"], writes=["ps7"])
        P.op("vector", lambda e: e.tensor_copy(rball[:].rearrange("p a b -> p (a b)"), k.ps[7][:, 256:512]), reads=["ps7"], writes=["s2_rb"])

        if "s2_setup" in DBG_FLAGS:
            P.dma("sync", "dbgo", lambda e: e.dma_start(out=c.dbg1[:, 0:256], in_=ck[:].rearrange("p a b -> p (a b)")), reads=["s2_ck"])
            P.dma("sync", "dbgo", lambda e: e.dma_start(out=c.dbg1[:, 256:512], in_=rball[:].rearrange("p a b -> p (a b)")), reads=["s2_rb"])
            P.barrier()
            return
        units = [(h, i, j) for h in range(NH_DBG) for i in range(NI_DBG) for j in range(2 * i + 2)]

        def issue_S(n):
            h, i, j = units[n]
            slot = n % 4
            pb = k.ps[slot][:, 0:256]
            hp, hh = h // 2, h % 2
            bb = biasb[(h * 16 + i) % 4]
            bn = f"s2_bias{(h * 16 + i) % 4}"
            if j == 0:
                nj = 2 * i + 2
                P.op("vector", lambda e: e.tensor_scalar(out=bb[:, 0:nj], in0=ck[:, 0:nj, h], scalar1=rball[:, 2 * i + 1, h:h + 1], scalar2=-1.0,
                                                         op0=ALU.subtract, op1=ALU.mult), reads=["s2_ck", "s2_rb"], writes=[bn])
            P.op("tensor", lambda e: e.matmul(pb, lhsT=kT[hh * 64:(hh + 1) * 64, hp, j * 128:(j + 1) * 128],
                                              rhs=qT[hh * 64:(hh + 1) * 64, hp, i * 256:(i + 1) * 256], start=True, stop=True),
                 reads=[f"s2_k{hp}", f"s2_q{hp}"], writes=[f"psS{slot}"])
            pt = PT[n % 8]
            ptn = f"s2_PT{n % 8}"
            P.op("scalar", lambda e: e.activation(out=pt[:], in_=pb, func=AF.Exp, bias=bb[:, j:j + 1], scale=1.0),
                 reads=[f"psS{slot}", bn], writes=[ptn])
            if j >= 2 * i:
                P.op("gpsimd", lambda e: e.affine_select(out=pt[:], in_=pt[:], pattern=[[1, 256]], compare_op=ALU.is_ge, fill=0.0,
                                                         base=i * 256 - j * 128, channel_multiplier=-1), reads=[ptn], writes=[ptn])

        def fin_a(h, i):
            ob = k.ps[4 + (h * 16 + i) % 2]
            on = f"ps{4 + (h * 16 + i) % 2}"
            P.op("vector", lambda e: e.reciprocal(rden[64:65, :], ob[64:65, 0:256]), reads=[on], writes=["s2_rden"])

        def fin_b(h, i):
            ob = k.ps[4 + (h * 16 + i) % 2]
            on = f"ps{4 + (h * 16 + i) % 2}"
            P.op("tensor", lambda e: e.matmul(k.ps[6][0:64, 0:256], lhsT=k.ones[64:65, 0:64], rhs=rden[64:65, :], start=True, stop=True),
                 reads=["s2_rden", "c_ones"], writes=["ps6"])
            P.op("scalar", lambda e: e.copy(bc[:], k.ps[6][0:64, 0:256]), reads=["ps6"], writes=["s2_bc"])
            ys = ystg[h % 2]
            P.op("vector", lambda e: e.tensor_tensor(out=ys[:, i * 256:(i + 1) * 256], in0=ob[0:64, 0:256], in1=bc[:], op=ALU.mult),
                 reads=[on, "s2_bc"], writes=[f"s2_y{h % 2}"])
            if i == NI_DBG - 1:
                P.dma("sync", "s2_yout", lambda e: e.dma_start(out=c.yT[h * 64:(h + 1) * 64, :], in_=ys[:]), reads=[f"s2_y{h % 2}"], writes=["d_yT"])

        pending = []
        LA = 3
        for n0 in range(LA):
            issue_S(n0)
        for n in range(len(units)):
            if n + LA < len(units):
                issue_S(n + LA)
            h, i, j = units[n]
            ob = k.ps[4 + (h * 16 + i) % 2]
            on = f"ps{4 + (h * 16 + i) % 2}"
            pt = PT[n % 8]
            P.op("tensor", lambda e, ob=ob, pt=pt, h=h, i=i, j=j: e.matmul(ob[0:65, 0:256], lhsT=va[:, j, h, :], rhs=pt[:], start=(j == 0), stop=(j == 2 * i + 1)),
                 reads=[f"s2_PT{n % 8}", "s2_v", "s2_v1"], writes=[on])
            for (hh_, ii_) in pending:
                fin_b(hh_, ii_)
            pending = []
            if j == 2 * i + 1:
                fin_a(h, i)
                pending.append((h, i))
        for (hh_, ii_) in pending:
            fin_b(hh_, ii_)
    P.barrier()


def stage3_pool(nc, P, k, c, li):
    with contextlib.ExitStack() as st:
        u = [sbt(nc, st, f"s3_u{i}", [128, S], F32) for i in range(2)]
        ab = [sbt(nc, st, f"s3_a{i}", [128, S], F32) for i in range(2)]
        dbf = sbt(nc, st, "s3_d", [128, S], BF16)
        ys = [sbt(nc, st, f"s3_y{i}", [128, S], BF16) for i in range(2)]
        pw = sbt(nc, st, "s3_pw", [128, 4, 128], BF16)
        psc = sbt(nc, st, "s3_psc", [128, 4], F32)
        inv = sbt(nc, st, "s3_inv", [128, 4, 16], F32)
        tmp = sbt(nc, st, "s3_tmp", [128, 16], F32)
        P.dma("gpsimd", "s3_pw", lambda e: e.dma_start(out=pw[:], in_=c.pool_w[li].rearrange("g c d -> c g d")), writes=["s3_pw"])
        for g in range(4):
            P.dma("sync", "s3_psc", lambda e, g=g: e.dma_start(out=psc[:, g:g + 1], in_=c.pool_scale[li, g]), writes=["s3_psc"])
        for g in range(4):
            w = 2 ** (g + 1)
            P.op("gpsimd", lambda e, g=g: e.iota(inv[:, g, :], pattern=[[1, 16]], base=1, channel_multiplier=0, allow_small_or_imprecise_dtypes=True), writes=["s3_inv"])
            P.op("gpsimd", lambda e, g=g, w=w: e.tensor_scalar(out=inv[:, g, :], in0=inv[:, g, :], scalar1=float(w), scalar2=None, op0=ALU.min), reads=["s3_inv"], writes=["s3_inv"])
        P.op("vector", lambda e: e.reciprocal(inv[:].rearrange("p a b -> p (a b)"), inv[:].rearrange("p a b -> p (a b)")), reads=["s3_inv"], writes=["s3_inv"])
        nev = 0
        for g in range(4):
            w = 2 ** (g + 1)
            ug = u[g % 2]
            un = f"s3_u{g % 2}"
            P.dma("sync", un, lambda e, g=g, ug=ug: e.dma_start(out=ug[:], in_=c.fT[g * 128:(g + 1) * 128, :]), writes=[un])
            src, srcn = ug, un
            for m in range(g + 1):
                sh = 2 ** m
                dst, dstn = ab[m % 2], f"s3_a{m % 2}"
                P.op("vector", lambda e, dst=dst, src=src, sh=sh: e.tensor_tensor(out=dst[:, sh:], in0=src[:, sh:], in1=src[:, 0:S - sh], op=ALU.add), reads=[srcn], writes=[dstn])
                P.op("gpsimd", lambda e, dst=dst, src=src, sh=sh: e.tensor_copy(dst[:, 0:sh], src[:, 0:sh]), reads=[srcn, dstn], writes=[dstn])
                src, srcn = dst, dstn
            rs = [srcn]
            P.op("vector", lambda e, src=src, ug=ug, w=w: e.scalar_tensor_tensor(out=dbf[:], in0=src[:], scalar=1.0 / w, in1=ug[:], op0=ALU.mult, op1=ALU.subtract),
                 reads=rs + [un], writes=["s3_d"])
            P.op("vector", lambda e, src=src, g=g, w=w: e.tensor_tensor(out=tmp[:, 0:w], in0=src[:, 0:w], in1=inv[:, g, 0:w], op=ALU.mult), reads=rs + ["s3_inv"], writes=["s3_tmp"])
            P.op("vector", lambda e, ug=ug, w=w: e.tensor_tensor(out=dbf[:, 0:w], in0=tmp[:, 0:w], in1=ug[:, 0:w], op=ALU.subtract), reads=["s3_tmp", un, "s3_d"], writes=["s3_d"])
            yb, yn = ys[g % 2], f"s3_y{g % 2}"
            for tc in range(8):
                pb, pn = k.ps[nev % 4], f"ps{nev % 4}"
                nev += 1
                P.op("tensor", lambda e, pb=pb, g=g, tc=tc: e.matmul(pb[:, :], lhsT=pw[:, g, :], rhs=dbf[:, tc * 512:(tc + 1) * 512], start=True, stop=True),
                     reads=["s3_pw", "s3_d"], writes=[pn])
                P.op("scalar", lambda e, pb=pb, g=g, tc=tc, yb=yb: e.activation(out=yb[:, tc * 512:(tc + 1) * 512], in_=pb[:, :], func=AF.Copy, scale=psc[:, g:g + 1]),
                     reads=[pn, "s3_psc"], writes=[yn])
            P.dma("sync", "s3_yout", lambda e, g=g, yb=yb: e.dma_start(out=c.yT[512 + g * 128:512 + (g + 1) * 128, :], in_=yb[:]), reads=[yn])
    P.barrier()


def layer_norm_tile(P, k, z, zn, gam, bet, stats, mv, rstd, out, outn, eps_ap, tag):
    for hh in range(2):
        P.op("vector", lambda e, hh=hh: e.bn_stats(stats[:, hh, :], z[:, hh * 512:(hh + 1) * 512]), reads=[zn], writes=[tag + "_st"])
    P.op("vector", lambda e: e.bn_aggr(mv[:], stats[:]), reads=[tag + "_st"], writes=[tag + "_mv"])
    P.op("scalar", lambda e: e.activation(out=rstd[:], in_=mv[:, 1:2], func=AF.Sqrt, bias=eps_ap, scale=1.0), reads=[tag + "_mv"], writes=[tag + "_rs"])
    P.op("vector", lambda e: e.reciprocal(rstd[:], rstd[:]), reads=[tag + "_rs"], writes=[tag + "_rs"])
    P.op("vector", lambda e: e.tensor_scalar(out=z[:], in0=z[:], scalar1=mv[:, 0:1], scalar2=rstd[:, 0:1], op0=ALU.subtract, op1=ALU.mult),
         reads=[zn, tag + "_mv", tag + "_rs"], writes=[zn])
    P.op("gpsimd", lambda e: e.tensor_tensor(out=z[:], in0=z[:], in1=gam[:], op=ALU.mult), reads=[zn, "lnp"], writes=[zn])
    P.op("gpsimd", lambda e: e.tensor_tensor(out=out[:], in0=z[:], in1=bet[:], op=ALU.add), reads=[zn, "lnp"], writes=[outn])


def stage5_merge(nc, P, k, c, li, src):
    with contextlib.ExitStack() as st:
        wg = sbt(nc, st, "s5_wg", [128, 8, 3072], BF16)
        wbr = sbt(nc, st, "s5_wbr", [128, 12, 1024], BF16)
        wo = sbt(nc, st, "s5_wo", [128, 8, 1024], BF16)
        gam = sbt(nc, st, "s5_gam", [128, D], F32)
        bet = sbt(nc, st, "s5_bet", [128, D], F32)
        eps = sbt(nc, st, "s5_eps", [128, 1], F32)
        xr = sbt(nc, st, "s5_xr", [128, 4, D], F32)
        xTc = sbt(nc, st, "s5_xTc", [128, 8, 512], BF16)
        yTc = sbt(nc, st, "s5_yTc", [128, 12, 512], BF16)
        sig = [sbt(nc, st, f"s5_sig{i}", [128, 512], F32) for i in range(2)]
        macc = sbt(nc, st, "s5_macc", [128, 512], F32)
        mtmp = sbt(nc, st, "s5_mtmp", [128, 512], F32)
        mT = sbt(nc, st, "s5_mT", [128, 8, 512], BF16)
        z = [sbt(nc, st, f"s5_z{i}", [128, D], F32) for i in range(2)]
        xo = [sbt(nc, st, f"s5_xo{i}", [128, D], F32) for i in range(2)]
        stats = sbt(nc, st, "s5_stats", [128, 2, 6], F32)
        mv = sbt(nc, st, "s5_mv", [128, 2], F32)
        rstd = sbt(nc, st, "s5_rstd", [128, 1], F32)
        wv = c.w_in[li].rearrange("(kc kp) n -> kp kc n", kp=128)
        for q in range(6):
            P.dma("gpsimd", "s5_wg", lambda e, q=q: e.dma_start(out=wg[:, :, q * 512:(q + 1) * 512], in_=wv[:, :, C_GA + q * 512:C_GA + (q + 1) * 512]), writes=["s5_wg"])
        wbv = c.w_br[li].rearrange("(kc kp) n -> kp kc n", kp=128)
        for q in range(3):
            P.dma("gpsimd", "s5_wbr", lambda e, q=q: e.dma_start(out=wbr[:, q * 4:(q + 1) * 4, :], in_=wbv[:, q * 4:(q + 1) * 4, :]), writes=["s5_wbr"])
        wov = c.w_out[li].rearrange("(kc kp) n -> kp kc n", kp=128)
        for q in range(2):
            P.dma("gpsimd", "s5_wo", lambda e, q=q: e.dma_start(out=wo[:, q * 4:(q + 1) * 4, :], in_=wov[:, q * 4:(q + 1) * 4, :]), writes=["s5_wo"])
        P.dma("sync", "lnp", lambda e: e.dma_start(out=gam[:], in_=c.ln1_g[li].partition_broadcast(128)), writes=["lnp"])
        P.dma("sync", "lnp", lambda e: e.dma_start(out=bet[:], in_=c.ln1_b[li].partition_broadcast(128)), writes=["lnp"])
        P.op("vector", lambda e: e.memset(eps[:], 1e-5), writes=["s5_eps"])
        nps = 0
        for tc in range(8):
            for tt in range(4):
                t = tc * 4 + tt
                P.dma("sync", "s5_xr", lambda e, t=t, tt=tt: e.dma_start(out=xr[:, tt, :], in_=src[t * 128:(t + 1) * 128, :]), writes=[f"s5_xr{tt}"])
            P.dma("sync", "s5_yTc", lambda e, tc=tc: e.dma_start(out=yTc[:], in_=c.yT[:, tc * 512:(tc + 1) * 512].rearrange("(a p) n -> p a n", p=128)), writes=["s5_yTc"])
            for tt in range(4):
                for half in range(2):
                    pb, pn = k.ps[nps % 8], f"ps{nps % 8}"
                    nps += 1
                    for q in range(4):
                        kc = half * 4 + q
                        P.op("tensor", lambda e, pb=pb, tt=tt, kc=kc, q=q: e.transpose(pb[:, q * 128:(q + 1) * 128], xr[:, tt, kc * 128:(kc + 1) * 128], k.ident[:]),
                             reads=[f"s5_xr{tt}", "c_ident"], writes=[pn])
                    fn = (lambda e, pb=pb, half=half, tt=tt: e.tensor_copy(xTc[:, half * 4:(half + 1) * 4, tt * 128:(tt + 1) * 128], pb[:].rearrange("p (a b) -> p a b", a=4))) if half == 0 else \
                         (lambda e, pb=pb, half=half, tt=tt: e.copy(xTc[:, half * 4:(half + 1) * 4, tt * 128:(tt + 1) * 128], pb[:].rearrange("p (a b) -> p a b", a=4)))
                    P.op("vector" if half == 0 else "scalar", fn, reads=[pn], writes=[f"s5_xTc{tt}_{half}"])
            xres = [f"s5_xTc{tt}_{h}" for tt in range(4) for h in range(2)]
            for n in range(8):
                for br in range(3):
                    pg, pgn = k.ps[nps % 8], f"ps{nps % 8}"
                    nps += 1
                    pbr, pbn = k.ps[nps % 8], f"ps{nps % 8}"
                    nps += 1
                    for kc in range(8):
                        P.op("tensor", lambda e, pg=pg, kc=kc, br=br, n=n: e.matmul(pg[:, :], lhsT=wg[:, kc, br * 1024 + n * 128:br * 1024 + (n + 1) * 128], rhs=xTc[:, kc, :], start=(kc == 0), stop=(kc == 7)),
                             reads=["s5_wg"] + xres, writes=[pgn])
                    sg, sgn = sig[(n * 3 + br) % 2], f"s5_sig{(n * 3 + br) % 2}"
                    P.op("scalar", lambda e, pg=pg, sg=sg: e.activation(out=sg[:], in_=pg[:, :], func=AF.Sigmoid), reads=[pgn], writes=[sgn])
                    for c4 in range(4):
                        P.op("tensor", lambda e, pbr=pbr, c4=c4, br=br, n=n: e.matmul(pbr[:, :], lhsT=wbr[:, br * 4 + c4, n * 128:(n + 1) * 128], rhs=yTc[:, br * 4 + c4, :], start=(c4 == 0), stop=(c4 == 3)),
                             reads=["s5_wbr", "s5_yTc"], writes=[pbn])
                    if br == 0:
                        P.op("vector", lambda e, pbr=pbr, sg=sg: e.tensor_tensor(out=macc[:], in0=pbr[:, :], in1=sg[:], op=ALU.mult), reads=[pbn, sgn], writes=["s5_macc"])
                    elif br == 1:
                        P.op("vector", lambda e, pbr=pbr, sg=sg: e.tensor_tensor(out=mtmp[:], in0=pbr[:, :], in1=sg[:], op=ALU.mult), reads=[pbn, sgn], writes=["s5_mtmp"])
                        P.op("gpsimd", lambda e: e.tensor_tensor(out=macc[:], in0=macc[:], in1=mtmp[:], op=ALU.add), reads=["s5_macc", "s5_mtmp"], writes=["s5_macc"])
                    else:
                        P.op("vector", lambda e, pbr=pbr, sg=sg: e.tensor_tensor(out=mtmp[:], in0=pbr[:, :], in1=sg[:], op=ALU.mult), reads=[pbn, sgn], writes=["s5_mtmp"])
                        P.op("gpsimd", lambda e, n=n: e.tensor_tensor(out=mT[:, n, :], in0=macc[:], in1=mtmp[:], op=ALU.add), reads=["s5_macc", "s5_mtmp"], writes=[f"s5_mT{n}"])
            mres = [f"s5_mT{n}" for n in range(8)]
            for tt in range(4):
                t = tc * 4 + tt
                zb, zn = z[t % 2], f"s5_z{t % 2}"
                for nh in range(2):
                    po, pon = k.ps[nps % 8], f"ps{nps % 8}"
                    nps += 1
                    for kc in range(8):
                        P.op("tensor", lambda e, po=po, kc=kc, tt=tt, nh=nh: e.matmul(po[:, :], lhsT=mT[:, kc, tt * 128:(tt + 1) * 128], rhs=wo[:, kc, nh * 512:(nh + 1) * 512], start=(kc == 0), stop=(kc == 7)),
                             reads=["s5_wo"] + mres, writes=[pon])
                    P.op("vector", lambda e, po=po, zb=zb, tt=tt, nh=nh: e.scalar_tensor_tensor(out=zb[:, nh * 512:(nh + 1) * 512], in0=xr[:, tt, nh * 512:(nh + 1) * 512], scalar=ALPHA, in1=po[:, :], op0=ALU.mult, op1=ALU.add),
                         reads=[pon, f"s5_xr{tt}"], writes=[zn])
                ob, on = xo[t % 2], f"s5_xo{t % 2}"
                layer_norm_tile(P, k, zb, zn, gam, bet, stats, mv, rstd, ob, on, eps[:, 0:1], "s5")
                P.dma("sync", "s5_out", lambda e, t=t, ob=ob: e.dma_start(out=c.x1s[t * 128:(t + 1) * 128, :], in_=ob[:]), reads=[on])
    P.barrier()


def stage4_zero(nc, P, k, c, li):
    with contextlib.ExitStack() as st:
        zt = sbt(nc, st, "s4_z", [128, S], BF16)
        P.op("gpsimd", lambda e: e.memset(zt[:], 0.0), writes=["s4_z"])
        for h in range(4):
            P.dma("sync", "s4_out", lambda e, h=h: e.dma_start(out=c.yT[1024 + h * 128:1024 + (h + 1) * 128, :], in_=zt[:]), reads=["s4_z"])
    P.barrier()


def stage6_moe(nc, P, k, c, li, dst):
    with contextlib.ExitStack() as st:
        wr = sbt(nc, st, "s6_wr", [128, 8, 36], F32)
        brb = sbt(nc, st, "s6_brb", [128, 36], F32)
        wpg = sbt(nc, st, "s6_wpg", [128, 8, D], BF16)
        wpp = sbt(nc, st, "s6_wpp", [128, 2, D], BF16)
        gam = sbt(nc, st, "s6_gam", [128, D], F32)
        bet = sbt(nc, st, "s6_bet", [128, D], F32)
        eps = sbt(nc, st, "s6_eps", [128, 1], F32)
        acc = sbt(nc, st, "s6_acc", [128, 8, D], F32)
        x1T = sbt(nc, st, "s6_x1T", [128, 8, 1024], BF16)
        xTf = sbt(nc, st, "s6_xTf", [128, 8, 128], F32)
        xin = [sbt(nc, st, f"s6_xin{i}", [128, D], F32) for i in range(2)]
        wgu = [sbt(nc, st, f"s6_wgu{i}", [128, 8, 1024], BF16) for i in range(2)]
        wd = [sbt(nc, st, f"s6_wd{i}", [128, 4, D], BF16) for i in range(2)]
        hT = [sbt(nc, st, f"s6_hT{i}", [128, 4, 512], BF16) for i in range(2)]
        sgl = [sbt(nc, st, f"s6_sg{i}", [128, 512], F32) for i in range(2)]
        lg = sbt(nc, st, "s6_lg", [128, 8, 36], F32)
        G = sbt(nc, st, "s6_G", [128, 8, 32], F32)
        sm = sbt(nc, st, "s6_sm", [128, 16], F32)
        t4 = sbt(nc, st, "s6_t4", [128, 4], F32)
        pen = sbt(nc, st, "s6_pen", [128, 4], F32)
        mk = sbt(nc, st, "s6_mk", [128, 32], F32)
        mk2 = sbt(nc, st, "s6_mk2", [128, 32], F32)
        oh = sbt(nc, st, "s6_oh", [128, 32], F32)
        pin = sbt(nc, st, "s6_pin", [128, 256], F32)
        pT = sbt(nc, st, "s6_pT", [128, 2, 128], BF16)
        sgt = sbt(nc, st, "s6_sgt", [128, D], F32)
        z = [sbt(nc, st, f"s6_z{i}", [128, D], F32) for i in range(2)]
        xo = [sbt(nc, st, f"s6_xo{i}", [128, D], F32) for i in range(2)]
        stats = sbt(nc, st, "s6_stats", [128, 2, 6], F32)
        mv = sbt(nc, st, "s6_mv", [128, 2], F32)
        rstd = sbt(nc, st, "s6_rstd", [128, 1], F32)
        P.dma("sync", "s6_wr", lambda e: e.dma_start(out=wr[:], in_=c.w_r[li].rearrange("(kc kp) n -> kp kc n", kp=128)), writes=["s6_wr"])
        P.dma("sync", "s6_brb", lambda e: e.dma_start(out=brb[:], in_=c.b_r[li].partition_broadcast(128)), writes=["s6_brb"])
        wpgv = c.w_pg[li].rearrange("(kc kp) n -> kp kc n", kp=128)
        for q in range(2):
            P.dma("gpsimd", "s6_wpg", lambda e, q=q: e.dma_start(out=wpg[:, q * 4:(q + 1) * 4, :], in_=wpgv[:, q * 4:(q + 1) * 4, :]), writes=["s6_wpg"])
        P.dma("gpsimd", "s6_wpp", lambda e: e.dma_start(out=wpp[:], in_=c.w_pp[li].rearrange("(kc kp) n -> kp kc n", kp=128)), writes=["s6_wpp"])
        P.dma("sync", "lnp", lambda e: e.dma_start(out=gam[:], in_=c.ln2_g[li].partition_broadcast(128)), writes=["lnp"])
        P.dma("sync", "lnp", lambda e: e.dma_start(out=bet[:], in_=c.ln2_b[li].partition_broadcast(128)), writes=["lnp"])
        P.op("vector", lambda e: e.memset(eps[:], 1e-5), writes=["s6_eps"])
        nps = 0
        nw = 0
        for qt in range(4):
            for tt in range(8):
                t = qt * 8 + tt
                xb, xn = xin[t % 2], f"s6_xin{t % 2}"
                P.dma("sync", xn, lambda e, xb=xb, t=t: e.dma_start(out=xb[:], in_=c.x1s[t * 128:(t + 1) * 128, :]), writes=[xn])
                for half in range(2):
                    pb, pn = k.ps[nps % 8], f"ps{nps % 8}"
                    nps += 1
                    for q in range(4):
                        kc = half * 4 + q
                        P.op("tensor", lambda e, pb=pb, xb=xb, kc=kc, q=q: e.transpose(pb[:, q * 128:(q + 1) * 128], xb[:, kc * 128:(kc + 1) * 128], k.ident[:]),
                             reads=[xn, "c_ident"], writes=[pn])
                    P.op("vector", lambda e, pb=pb, half=half, tt=tt: e.tensor_copy(x1T[:, half * 4:(half + 1) * 4, tt * 128:(tt + 1) * 128], pb[:].rearrange("p (a b) -> p a b", a=4)),
                         reads=[pn], writes=[f"s6_x1T{tt}_{half}"])
                    P.op("scalar", lambda e, pb=pb, half=half: e.copy(xTf[:, half * 4:(half + 1) * 4, :], pb[:].rearrange("p (a b) -> p a b", a=4)),
                         reads=[pn, f"s6_x1T{tt}_{half}"], writes=[f"s6_xTf{half}"])
                pr, prn = k.ps[nps % 8], f"ps{nps % 8}"
                nps += 1
                for kc in range(8):
                    P.op("tensor", lambda e, pr=pr, kc=kc: e.matmul(pr[:, 0:36], lhsT=xTf[:, kc, :], rhs=wr[:, kc, :], start=(kc == 0), stop=(kc == 7)),
                         reads=[f"s6_xTf{kc // 4}", "s6_wr"], writes=[prn])
                P.op("vector", lambda e, pr=pr, tt=tt: e.tensor_tensor(out=lg[:, tt, :], in0=pr[:, 0:36], in1=brb[:], op=ALU.add), reads=[prn, "s6_brb"], writes=["s6_lg"])
                V = lambda fn, r, w: P.op("vector", fn, reads=r, writes=w)
                V(lambda e, tt=tt: e.reduce_max(out=sm[:, 0:1], in_=lg[:, tt, 0:4], axis=AX.X), ["s6_lg"], ["s6_sm"])
                V(lambda e, tt=tt: e.tensor_scalar(out=t4[:], in0=lg[:, tt, 0:4], scalar1=sm[:, 0:1], scalar2=None, op0=ALU.is_equal), ["s6_lg", "s6_sm"], ["s6_t4"])
                V(lambda e: e.tensor_scalar(out=pen[:], in0=t4[:], scalar1=-1.0, scalar2=1e30, op0=ALU.add, op1=ALU.mult), ["s6_t4"], ["s6_pen"])
                V(lambda e: e.tensor_scalar(out=sm[:, 1:2], in0=sm[:, 0:1], scalar1=-1.0, scalar2=None, op0=ALU.mult), ["s6_sm"], ["s6_sm"])
                P.op("scalar", lambda e, tt=tt: e.activation(out=t4[:], in_=lg[:, tt, 0:4], func=AF.Exp, bias=sm[:, 1:2], scale=1.0), reads=["s6_lg", "s6_sm", "s6_pen"], writes=["s6_t4"])
                V(lambda e: e.reduce_sum(out=sm[:, 2:3], in_=t4[:], axis=AX.X), ["s6_t4"], ["s6_sm"])
                V(lambda e: e.reciprocal(sm[:, 2:3], sm[:, 2:3]), ["s6_sm"], ["s6_sm"])
                for g in range(4):
                    V(lambda e, tt=tt, g=g: e.tensor_scalar(out=mk[:, g * 8:(g + 1) * 8], in0=lg[:, tt, 4 + g * 8:4 + (g + 1) * 8], scalar1=pen[:, g:g + 1], scalar2=None, op0=ALU.add),
                      ["s6_lg", "s6_pen"], ["s6_mk"])
                V(lambda e: e.reduce_max(out=sm[:, 3:4], in_=mk[:], axis=AX.X), ["s6_mk"], ["s6_sm"])
                V(lambda e: e.tensor_scalar(out=oh[:], in0=mk[:], scalar1=sm[:, 3:4], scalar2=None, op0=ALU.is_equal), ["s6_mk", "s6_sm"], ["s6_oh"])
                V(lambda e: e.scalar_tensor_tensor(out=mk2[:], in0=oh[:], scalar=-1e30, in1=mk[:], op0=ALU.mult, op1=ALU.add), ["s6_oh", "s6_mk"], ["s6_mk2"])
                V(lambda e: e.reduce_max(out=sm[:, 4:5], in_=mk2[:], axis=AX.X), ["s6_mk2"], ["s6_sm"])
                V(lambda e: e.tensor_tensor(out=sm[:, 5:6], in0=sm[:, 3:4], in1=sm[:, 4:5], op=ALU.subtract), ["s6_sm"], ["s6_sm"])
                P.op("scalar", lambda e: e.activation(out=sm[:, 6:7], in_=sm[:, 5:6], func=AF.Sigmoid), reads=["s6_sm"], writes=["s6_sm"])
                V(lambda e: e.tensor_tensor(out=sm[:, 7:8], in0=sm[:, 6:7], in1=sm[:, 2:3], op=ALU.mult), ["s6_sm"], ["s6_sm"])
                V(lambda e: e.tensor_tensor(out=sm[:, 8:9], in0=sm[:, 2:3], in1=sm[:, 7:8], op=ALU.subtract), ["s6_sm"], ["s6_sm"])
                V(lambda e, tt=tt: e.tensor_scalar(out=G[:, tt, :], in0=oh[:], scalar1=sm[:, 7:8], scalar2=None, op0=ALU.mult), ["s6_oh", "s6_sm"], ["s6_G"])
                V(lambda e: e.tensor_scalar(out=oh[:], in0=mk2[:], scalar1=sm[:, 4:5], scalar2=None, op0=ALU.is_equal), ["s6_mk2", "s6_sm", "s6_G"], ["s6_oh"])
                V(lambda e, tt=tt: e.scalar_tensor_tensor(out=G[:, tt, :], in0=oh[:], scalar=sm[:, 8:9], in1=G[:, tt, :], op0=ALU.mult, op1=ALU.add), ["s6_oh", "s6_sm", "s6_G"], ["s6_G"])
            x1res = [f"s6_x1T{tt}_{h}" for tt in range(8) for h in range(2)]
            for ex in range(32):
                wb, wbn = wgu[nw % 2], f"s6_wgu{nw % 2}"
                wdb, wdn = wd[nw % 2], f"s6_wd{nw % 2}"
                nw += 1
                P.dma("gpsimd", wbn, lambda e, wb=wb, ex=ex: e.dma_start(out=wb[:, :, 0:512], in_=c.w_eg[li, ex].rearrange("(kc kp) f -> kp kc f", kp=128)), writes=[wbn])
                P.dma("gpsimd", wbn, lambda e, wb=wb, ex=ex: e.dma_start(out=wb[:, :, 512:1024], in_=c.w_eu[li, ex].rearrange("(kc kp) f -> kp kc f", kp=128)), reads=[wbn], writes=[wbn])
                P.dma("gpsimd", wdn, lambda e, wdb=wdb, ex=ex: e.dma_start(out=wdb[:], in_=c.w_ed[li, ex].rearrange("(fc fp) n -> fp fc n", fp=128)), writes=[wdn])
                for tc in range(2):
                    hb, hn = hT[(ex * 2 + tc) % 2], f"s6_hT{(ex * 2 + tc) % 2}"
                    for fc in range(4):
                        pg, pgn = k.ps[nps % 8], f"ps{nps % 8}"
                        nps += 1
                        pu, pun = k.ps[nps % 8], f"ps{nps % 8}"
                        nps += 1
                        for kc in range(8):
                            P.op("tensor", lambda e, pg=pg, kc=kc, fc=fc, tc=tc, wb=wb: e.matmul(pg[:, :], lhsT=wb[:, kc, fc * 128:(fc + 1) * 128], rhs=x1T[:, kc, tc * 512:(tc + 1) * 512], start=(kc == 0), stop=(kc == 7)),
                                 reads=[wbn] + x1res[tc * 8:(tc + 1) * 8], writes=[pgn])
                        for kc in range(8):
                            P.op("tensor", lambda e, pu=pu, kc=kc, fc=fc, tc=tc, wb=wb: e.matmul(pu[:, :], lhsT=wb[:, kc, 512 + fc * 128:512 + (fc + 1) * 128], rhs=x1T[:, kc, tc * 512:(tc + 1) * 512], start=(kc == 0), stop=(kc == 7)),
                                 reads=[wbn] + x1res[tc * 8:(tc + 1) * 8], writes=[pun])
                        sg, sgn = sgl[fc % 2], f"s6_sg{fc % 2}"
                        P.op("scalar", lambda e, pg=pg, sg=sg: e.activation(out=sg[:], in_=pg[:, :], func=AF.Silu), reads=[pgn], writes=[sgn])
                        P.op("vector", lambda e, pu=pu, sg=sg, hb=hb, fc=fc: e.tensor_tensor(out=hb[:, fc, :], in0=pu[:, :], in1=sg[:], op=ALU.mult), reads=[pun, sgn], writes=[f"{hn}_{fc}"])
                    for tt4 in range(4):
                        tt = tc * 4 + tt4
                        for nh in range(2):
                            py, pyn = k.ps[nps % 8], f"ps{nps % 8}"
                            nps += 1
                            for fc in range(4):
                                P.op("tensor", lambda e, py=py, fc=fc, tt4=tt4, nh=nh, hb=hb, wdb=wdb: e.matmul(py[:, :], lhsT=hb[:, fc, tt4 * 128:(tt4 + 1) * 128], rhs=wdb[:, fc, nh * 512:(nh + 1) * 512], start=(fc == 0), stop=(fc == 3)),
                                     reads=[wdn, f"{hn}_{fc}"], writes=[pyn])
                            an = f"s6_acc{tt}_{nh}"
                            if ex == 0:
                                P.op("vector", lambda e, py=py, tt=tt, nh=nh, ex=ex: e.tensor_scalar(out=acc[:, tt, nh * 512:(nh + 1) * 512], in0=py[:, :], scalar1=G[:, tt, ex:ex + 1], scalar2=None, op0=ALU.mult),
                                     reads=[pyn, "s6_G"], writes=[an])
                            else:
                                P.op("vector", lambda e, py=py, tt=tt, nh=nh, ex=ex: e.scalar_tensor_tensor(out=acc[:, tt, nh * 512:(nh + 1) * 512], in0=py[:, :], scalar=G[:, tt, ex:ex + 1], in1=acc[:, tt, nh * 512:(nh + 1) * 512], op0=ALU.mult, op1=ALU.add),
                                     reads=[pyn, "s6_G", an], writes=[an])
            for tt in range(8):
                t = qt * 8 + tt
                xb, xn = xin[t % 2], f"s6_xin{t % 2}"
                P.dma("sync", xn, lambda e, xb=xb, t=t: e.dma_start(out=xb[:], in_=c.x1s[t * 128:(t + 1) * 128, :]), writes=[xn])
                P.dma("sync", "s6_pin", lambda e, t=t: e.dma_start(out=pin[:], in_=c.p[li, t * 128:(t + 1) * 128, :]), writes=["s6_pin"])
                pb, pn = k.ps[nps % 8], f"ps{nps % 8}"
                nps += 1
                for q in range(2):
                    P.op("tensor", lambda e, pb=pb, q=q: e.transpose(pb[:, q * 128:(q + 1) * 128], pin[:, q * 128:(q + 1) * 128], k.ident[:]), reads=["s6_pin", "c_ident"], writes=[pn])
                P.op("scalar", lambda e, pb=pb: e.copy(pT[:], pb[:, 0:256].rearrange("p (a b) -> p a b", a=2)), reads=[pn], writes=["s6_pT"])
                zb, zn = z[t % 2], f"s6_z{t % 2}"
                for nh in range(2):
                    pgt, pgtn = k.ps[nps % 8], f"ps{nps % 8}"
                    nps += 1
                    pp, ppn = k.ps[nps % 8], f"ps{nps % 8}"
                    nps += 1
                    for kc in range(8):
                        P.op("tensor", lambda e, pgt=pgt, kc=kc, tt=tt, nh=nh: e.matmul(pgt[:, :], lhsT=x1T[:, kc, tt * 128:(tt + 1) * 128], rhs=wpg[:, kc, nh * 512:(nh + 1) * 512], start=(kc == 0), stop=(kc == 7)),
                             reads=["s6_wpg", f"s6_x1T{tt}_{kc // 4}"], writes=[pgtn])
                    for kc in range(2):
                        P.op("tensor", lambda e, pp=pp, kc=kc, nh=nh: e.matmul(pp[:, :], lhsT=pT[:, kc, :], rhs=wpp[:, kc, nh * 512:(nh + 1) * 512], start=(kc == 0), stop=(kc == 1)),
                             reads=["s6_wpp", "s6_pT"], writes=[ppn])
                    P.op("scalar", lambda e, pgt=pgt, nh=nh: e.activation(out=sgt[:, nh * 512:(nh + 1) * 512], in_=pgt[:, :], func=AF.Sigmoid), reads=[pgtn], writes=[f"s6_sgt{nh}"])
                    P.op("vector", lambda e, pp=pp, nh=nh, zb=zb: e.tensor_tensor(out=zb[:, nh * 512:(nh + 1) * 512], in0=pp[:, :], in1=sgt[:, nh * 512:(nh + 1) * 512], op=ALU.mult),
                         reads=[ppn, f"s6_sgt{nh}"], writes=[zn + f"_{nh}"])
                    P.op("gpsimd", lambda e, nh=nh, zb=zb, tt=tt: e.tensor_tensor(out=zb[:, nh * 512:(nh + 1) * 512], in0=zb[:, nh * 512:(nh + 1) * 512], in1=acc[:, tt, nh * 512:(nh + 1) * 512], op=ALU.add),
                         reads=[zn + f"_{nh}", f"s6_acc{tt}_{nh}"], writes=[zn + f"_{nh}"])
                P.op("vector", lambda e, zb=zb, xb=xb: e.scalar_tensor_tensor(out=zb[:], in0=xb[:], scalar=ALPHA, in1=zb[:], op0=ALU.mult, op1=ALU.add),
                     reads=[xn, zn + "_0", zn + "_1"], writes=[zn])
                ob, on = xo[t % 2], f"s6_xo{t % 2}"
                layer_norm_tile(P, k, zb, zn, gam, bet, stats, mv, rstd, ob, on, eps[:, 0:1], "s6")
                P.dma("sync", "s6_out", lambda e, t=t, ob=ob: e.dma_start(out=dst[t * 128:(t + 1) * 128, :], in_=ob[:]), reads=[on])
    P.barrier()


def stage4_dn(nc, P, k, c, li):
    RS = 128 ** -0.5
    with contextlib.ExitStack() as st:
        da = sbt(nc, st, "d_da", [4, S], F32)
        db = sbt(nc, st, "d_db", [4, S], F32)
        rm = sbt(nc, st, "d_rm", [4, S], F32)
        gc = sbt(nc, st, "d_gc", [4, S], F32)
        par = sbt(nc, st, "d_par", [4, 4], F32)
        P.dma("sync", "d_da", lambda e: e.dma_start(out=da[:], in_=c.smT[8:12, :]), writes=["d_da"])
        P.dma("sync", "d_db", lambda e: e.dma_start(out=db[:], in_=c.smT[12:16, :]), writes=["d_db"])
        P.dma("sync", "d_par", lambda e: e.dma_start(out=par[:, 0:1], in_=c.dn_a_log[li]), writes=["d_par"])
        P.dma("sync", "d_par", lambda e: e.dma_start(out=par[:, 1:2], in_=c.dn_dt_bias[li]), writes=["d_par"])
        P.op("scalar", lambda e: e.activation(out=par[:, 2:3], in_=par[:, 0:1], func=AF.Exp), reads=["d_par"], writes=["d_par2"])
        P.op("vector", lambda e: e.tensor_scalar(out=par[:, 2:3], in0=par[:, 2:3], scalar1=-1.0, scalar2=None, op0=ALU.mult), reads=["d_par2"], writes=["d_par2"])
        P.op("scalar", lambda e: e.activation(out=da[:], in_=da[:], func=AF.Exp, bias=par[:, 1:2], scale=1.0), reads=["d_da", "d_par"], writes=["d_da"])
        P.op("scalar", lambda e: e.activation(out=da[:], in_=da[:], func=AF.Ln, bias=k.ones[0:4, 0:1], scale=1.0), reads=["d_da", "c_ones"], writes=["d_da"])
        P.op("scalar", lambda e: e.activation(out=db[:], in_=db[:], func=AF.Sigmoid), reads=["d_db"], writes=["d_db"])
        P.op("vector", lambda e: e.tensor_scalar(out=da[:], in0=da[:], scalar1=par[:, 2:3], scalar2=None, op0=ALU.mult), reads=["d_da", "d_par2"], writes=["d_da"])
        P.op("gpsimd", lambda e: e.memset(rm[:], 1.0), writes=["d_rm"])
        P.op("gpsimd", lambda e: e.memset(rm[:].rearrange("p (c j) -> p c j", j=64)[:, :, 0:1], 0.0), reads=["d_rm"], writes=["d_rm"])
        P.op("vector", lambda e: e.tensor_tensor_scan(out=gc[:], data0=rm[:], data1=da[:], initial=0.0, op0=ALU.mult, op1=ALU.add), reads=["d_rm", "d_da"], writes=["d_gc"])
        P.dma("sync", "d_rowout", lambda e: e.dma_start(out=c.dnrow[0], in_=db[:]), reads=["d_db"])
        P.dma("sync", "d_rowout", lambda e: e.dma_start(out=c.dnrow[1], in_=gc[:]), reads=["d_gc"])
    P.barrier()
    psb = k.ps[7][:, :].bitcast(BF16)
    psb6 = k.ps[6][:, :].bitcast(BF16)
    with contextlib.ExitStack() as st0:
        cw = sbt(nc, st0, "d_cw", [128, 12, 4], F32)
        nw = sbt(nc, st0, "d_nw", [128, 1], F32)
        eps6 = sbt(nc, st0, "d_eps6", [128, 1], F32)
        with contextlib.ExitStack() as st:
            cwraw = sbt(nc, st, "d_cwraw", [4, 1536], F32)
            P.dma("sync", "d_cwraw", lambda e: e.dma_start(out=cwraw[:], in_=c.dn_conv[li]), writes=["d_cwraw"])
            for idx in range(12):
                P.op("tensor", lambda e, idx=idx: e.transpose(k.ps[0][:, idx * 4:(idx + 1) * 4], cwraw[0:4, idx * 128:(idx + 1) * 128], k.ident[0:4, 0:4]),
                     reads=["d_cwraw", "c_ident"], writes=["ps0"])
            P.op("vector", lambda e: e.tensor_copy(cw[:].rearrange("p a b -> p (a b)"), k.ps[0][:, 0:48]), reads=["ps0"], writes=["d_cw"])
            P.dma("sync", "d_nw", lambda e: e.dma_start(out=nw[:], in_=c.dn_norm_w[li]), writes=["d_nw"])
            P.op("vector", lambda e: e.memset(eps6[:], 1e-6), writes=["d_eps6"])
        P.barrier()
        def do_head(h):
            with contextlib.ExitStack() as sth:
                A = lambda n, s, d: sbt(nc, sth, n, s, d)
                kT = A("d_kT", [128, S], BF16)
                qT = A("d_qT", [128, S], BF16)
                qdT = A("d_qdT", [128, S], BF16)
                vb_tm = A("d_vb", [128, NT, 128], BF16)
                kbg_tm = A("d_kbg", [128, NT, 128], BF16)
                kdec_tm = A("d_kdec", [128, NT, 128], BF16)
                AT = A("d_AT", [128, NT, 128], BF16)
                gcB = A("d_gcB", [128, S], F32)
                gc_col = A("d_gccol", [128, NT], F32)
                b_col = A("d_bcol", [128, NT], F32)
                glc = A("d_glc", [128, NT], F32)
                col_bg = A("d_colbg", [128, NT], F32)
                col_edd = A("d_coledd", [128, NT], F32)
                negb = A("d_negb", [128, NT], F32)
                neggc = A("d_neggc", [128, NT], F32)
                eglB = A("d_eglB", [128, 64], F32)
                browh = c.dnrow[0, h]
                gcrowh = c.dnrow[1, h]
                P.dma("sync", "d_cols", lambda e: e.dma_start(out=gc_col[:], in_=gcrowh.rearrange("(t p) -> p t", p=128), allow_slow_non_contiguous=True), writes=["d_gccol"])
                P.dma("sync", "d_cols", lambda e: e.dma_start(out=b_col[:], in_=browh.rearrange("(t p) -> p t", p=128), allow_slow_non_contiguous=True), writes=["d_bcol"])
                gsrc = gcrowh.rearrange("(t two j) -> two j t", two=2, j=64)
                for half in range(2):
                    P.dma("sync", "d_cols", lambda e, half=half: e.dma_start(out=glc[half * 64:(half + 1) * 64, :], in_=gsrc[half, 63, :].partition_broadcast(64), allow_slow_non_contiguous=True), writes=["d_glc"])
                P.dma("sync", "d_cols", lambda e: e.dma_start(out=eglB[:], in_=gcrowh.rearrange("(c j) -> j c", j=64)[63, :].partition_broadcast(128), allow_slow_non_contiguous=True), writes=["d_eglB"])
                P.dma("sync", "d_gcB", lambda e: e.dma_start(out=gcB[:], in_=gcrowh.partition_broadcast(128)), writes=["d_gcB"])
                P.op("scalar", lambda e: e.activation(out=eglB[:], in_=eglB[:], func=AF.Exp), reads=["d_eglB"], writes=["d_eglB"])
                P.op("scalar", lambda e: e.activation(out=col_bg[:], in_=gc_col[:], func=AF.Exp), reads=["d_gccol"], writes=["d_colbg"])
                P.op("vector", lambda e: e.tensor_tensor(out=col_bg[:], in0=col_bg[:], in1=b_col[:], op=ALU.mult), reads=["d_colbg", "d_bcol"], writes=["d_colbg"])
                P.op("vector", lambda e: e.tensor_tensor(out=col_edd[:], in0=glc[:], in1=gc_col[:], op=ALU.subtract), reads=["d_glc", "d_gccol"], writes=["d_coledd"])
                P.op("scalar", lambda e: e.activation(out=col_edd[:], in_=col_edd[:], func=AF.Exp), reads=["d_coledd"], writes=["d_coledd"])
                P.op("vector", lambda e: e.tensor_scalar(out=negb[:], in0=b_col[:], scalar1=-1.0, scalar2=None, op0=ALU.mult), reads=["d_bcol"], writes=["d_negb"])
                P.op("vector", lambda e: e.tensor_scalar(out=neggc[:], in0=gc_col[:], scalar1=-1.0, scalar2=None, op0=ALU.mult), reads=["d_gccol"], writes=["d_neggc"])
                with contextlib.ExitStack() as st:
                    u = [sbt(nc, st, f"d_u{i}", [128, S], F32) for i in range(2)]
                    acc = sbt(nc, st, "d_acc", [128, S], F32)
                    sch = [sbt(nc, st, f"d_sch{i}", [128, 512], F32) for i in range(2)]
                    sqc = [sbt(nc, st, f"d_sqc{i}", [128, 512], F32) for i in range(2)]
                    rn = [sbt(nc, st, f"d_rn{i}", [128, 512], F32) for i in range(2)]
                    vTb = sbt(nc, st, "d_vTb", [128, S], BF16)
                    if h == 0 and "dn_dbg" in DBG_FLAGS:
                        print("sbuf remaining in dn phase A:", nc.sbuf_bytes_remaining)
                        for nm_, t_ in [("kT", kT), ("qT", qT), ("qdT", qdT), ("vb", vb_tm), ("kbg", kbg_tm), ("kdec", kdec_tm), ("AT", AT), ("gcB", gcB), ("gc_col", gc_col), ("eglB", eglB),
                                        ("u0", u[0]), ("u1", u[1]), ("acc", acc), ("rn0", rn[0]), ("rn1", rn[1]), ("vTb", vTb), ("cw", cw), ("ones", k.ones)]:
                            m_ = nc.lookup_mloc(t_)
                            print("   ", nm_, m_.addr, list(m_.dims))
                    nps = 0
                    for wi, which in enumerate(("q", "k", "v")):
                        idx = wi * 4 + h
                        ub_, un = u[wi % 2], f"d_u{wi % 2}"
                        P.dma("sync", un, lambda e, ub_=ub_, idx=idx: e.dma_start(out=ub_[:], in_=c.fT[512 + idx * 128:512 + (idx + 1) * 128, :]), writes=[un])
                        P.op("vector", lambda e, ub_=ub_, idx=idx: e.tensor_scalar(out=acc[:], in0=ub_[:], scalar1=cw[:, idx, 3:4], scalar2=None, op0=ALU.mult), reads=[un, "d_cw"], writes=["d_acc"])
                        for sh, j in ((1, 2), (2, 1), (3, 0)):
                            P.op("vector", lambda e, ub_=ub_, idx=idx, sh=sh, j=j: e.scalar_tensor_tensor(out=acc[:, sh:], in0=ub_[:, 0:S - sh], scalar=cw[:, idx, j:j + 1], in1=acc[:, sh:], op0=ALU.mult, op1=ALU.add),
                                 reads=[un, "d_cw", "d_acc"], writes=["d_acc"])
                        dstT, dn_ = (qT, "d_qT") if which == "q" else ((kT, "d_kT") if which == "k" else (vTb, "d_vTb"))
                        for tc in range(8):
                            cs = slice(tc * 512, (tc + 1) * 512)
                            if which == "v":
                                P.op("scalar", lambda e, cs=cs: e.activation(out=vTb[:, cs], in_=acc[:, cs], func=AF.Silu), reads=["d_acc"], writes=["d_vTb"])
                                continue
                            pb, pn = k.ps[nps % 4], f"ps{nps % 4}"
                            rb, rbn = rn[nps % 2], f"d_rn{nps % 2}"
                            sc_, scn = sch[nps % 2], f"d_sch{nps % 2}"
                            sq_, sqn = sqc[nps % 2], f"d_sqc{nps % 2}"
                            nps += 1
                            P.op("scalar", lambda e, cs=cs, sc_=sc_: e.activation(out=sc_[:], in_=acc[:, cs], func=AF.Silu), reads=["d_acc"], writes=[scn])
                            P.op("vector", lambda e, sc_=sc_, sq_=sq_: e.tensor_tensor(out=sq_[:], in0=sc_[:], in1=sc_[:], op=ALU.mult), reads=[scn], writes=[sqn])
                            P.op("tensor", lambda e, pb=pb, sq_=sq_: e.matmul(pb[:, :], lhsT=k.ones[:], rhs=sq_[:], start=True, stop=True), reads=["c_ones", sqn], writes=[pn])
                            P.op("scalar", lambda e, pb=pb, rb=rb: e.activation(out=rb[:], in_=pb[:, :], func=AF.Sqrt, bias=eps6[:, 0:1], scale=1.0), reads=[pn, "d_eps6"], writes=[rbn])
                            P.op("vector", lambda e, rb=rb: e.reciprocal(rb[:], rb[:]), reads=[rbn], writes=[rbn])
                            sc = RS if which == "q" else 1.0
                            P.op("vector", lambda e, rb=rb, cs=cs, dstT=dstT, sc=sc, sc_=sc_: e.scalar_tensor_tensor(out=dstT[:, cs], in0=sc_[:], scalar=sc, in1=rb[:], op0=ALU.mult, op1=ALU.mult),
                                 reads=[scn, rbn], writes=[dn_])
                    for t in range(NT):
                        cs = slice(t * 128, (t + 1) * 128)
                        pq = psb if t % 2 == 0 else psb6
                        pqn = "ps7" if t % 2 == 0 else "ps6"
                        P.op("tensor", lambda e, cs=cs, pq=pq: e.transpose(pq[:, 0:128], kT[:, cs], k.identb[:]), reads=["d_kT", "c_identb"], writes=[pqn])
                        P.op("tensor", lambda e, cs=cs, pq=pq: e.transpose(pq[:, 128:256], vTb[:, cs], k.identb[:]), reads=["d_vTb", "c_identb"], writes=[pqn])
                        P.op("vector", lambda e, t=t, pq=pq: e.tensor_scalar(out=kbg_tm[:, t, :], in0=pq[:, 0:128], scalar1=col_bg[:, t:t + 1], scalar2=None, op0=ALU.mult),
                             reads=[pqn, "d_colbg"], writes=["d_kbg"])
                        P.op("vector", lambda e, t=t, pq=pq: e.tensor_scalar(out=kdec_tm[:, t, :], in0=pq[:, 0:128], scalar1=col_edd[:, t:t + 1], scalar2=None, op0=ALU.mult),
                             reads=[pqn, "d_coledd"], writes=["d_kdec"])
                        P.op("vector", lambda e, t=t, pq=pq: e.tensor_scalar(out=vb_tm[:, t, :], in0=pq[:, 128:256], scalar1=b_col[:, t:t + 1], scalar2=None, op0=ALU.mult),
                             reads=[pqn, "d_bcol"], writes=["d_vb"])
                    if "dn_dbg" in DBG_FLAGS and h == 0:
                        P.dma("gpsimd", "dbgo", lambda e: e.dma_start(out=c.dbg1[:, 2432:2560], in_=u[1][:, 2048:2176]), reads=["d_u1"])
                        P.dma("gpsimd", "dbgo", lambda e: e.dma_start(out=c.dbg1[:, 2560:2688], in_=acc[:, 2048:2176]), reads=["d_acc"])
                        P.dma("gpsimd", "dbgo", lambda e: e.dma_start(out=c.dbg1[:, 2688:2816], in_=vTb[:, 2048:2176]), reads=["d_vTb"])
                    for tc in range(8):
                        cs = slice(tc * 512, (tc + 1) * 512)
                        sc_, scn = sch[tc % 2], f"d_sch{tc % 2}"
                        P.op("scalar", lambda e, cs=cs, sc_=sc_: e.activation(out=sc_[:], in_=gcB[:, cs], func=AF.Exp), reads=["d_gcB"], writes=[scn])
                        P.op("vector", lambda e, cs=cs, sc_=sc_: e.tensor_tensor(out=qdT[:, cs], in0=qT[:, cs], in1=sc_[:], op=ALU.mult), reads=["d_qT", scn], writes=["d_qdT"])
                P.barrier()
                uin = A("d_uin", [128, NT, 128], F32)
                WT = A("d_WT", [128, S], BF16)
                oT = A("d_oT", [128, S], F32)
                with contextlib.ExitStack() as st:
                    Dm = [sbt(nc, st, f"d_Dm{i}", [128, 128], F32) for i in range(2)]
                    DTm = [sbt(nc, st, f"d_DTm{i}", [128, 128], F32) for i in range(2)]
                    Nf = [sbt(nc, st, f"d_Nf{i}", [128, 128], F32) for i in range(2)]
                    ATf = [sbt(nc, st, f"d_ATf{i}", [128, 128], F32) for i in range(2)]
                    Nl = [sbt(nc, st, f"d_Nl{i}", [128, 4, 128], BF16) for i in range(2)]
                    Ml = [sbt(nc, st, f"d_Ml{i}", [128, 4, 128], BF16) for i in range(2)]
                    Pl = [sbt(nc, st, f"d_Pl{i}", [128, 4, 128], BF16) for i in range(2)]
                    for grp in range(NT // 4):
                        for tt in range(4):
                            t = grp * 4 + tt
                            cs = slice(t * 128, (t + 1) * 128)
                            i2 = t % 2
                            pkk, pkkn = k.ps[i2], f"ps{i2}"
                            pqk, pqkn = k.ps[2 + i2], f"ps{2 + i2}"
                            P.op("tensor", lambda e, pkk=pkk, cs=cs: e.matmul(pkk[:, 0:128], lhsT=kT[:, cs], rhs=kT[:, cs], start=True, stop=True), reads=["d_kT"], writes=[pkkn])
                            P.op("tensor", lambda e, pqk=pqk, cs=cs: e.matmul(pqk[:, 0:128], lhsT=kT[:, cs], rhs=qT[:, cs], start=True, stop=True), reads=["d_kT", "d_qT"], writes=[pqkn])
                            P.op("scalar", lambda e, cs=cs, t=t, i2=i2: e.activation(out=Dm[i2][:], in_=gcB[:, cs], func=AF.Exp, bias=gc_col[:, t:t + 1], scale=-1.0), reads=["d_gcB", "d_gccol"], writes=[f"d_Dm{i2}"])
                            P.op("scalar", lambda e, cs=cs, t=t, i2=i2: e.activation(out=DTm[i2][:], in_=gcB[:, cs], func=AF.Exp, bias=neggc[:, t:t + 1], scale=1.0), reads=["d_gcB", "d_neggc"], writes=[f"d_DTm{i2}"])
                            P.op("vector", lambda e, pkk=pkk, t=t, i2=i2: e.scalar_tensor_tensor(out=Nf[i2][:], in0=pkk[:, 0:128], scalar=negb[:, t:t + 1], in1=Dm[i2][:], op0=ALU.mult, op1=ALU.mult),
                                 reads=[pkkn, "d_negb", f"d_Dm{i2}"], writes=[f"d_Nf{i2}"])
                            P.op("vector", lambda e, pqk=pqk, i2=i2: e.tensor_tensor(out=ATf[i2][:], in0=pqk[:, 0:128], in1=DTm[i2][:], op=ALU.mult),
                                 reads=[pqkn, f"d_DTm{i2}"], writes=[f"d_ATf{i2}"])
                            P.op("gpsimd", lambda e, tt=tt, i2=i2: e.affine_select(out=Nl[0][:, tt, :], in_=Nf[i2][:], pattern=[[-1, 128]], compare_op=ALU.is_gt, fill=0.0, base=0, channel_multiplier=1),
                                 reads=[f"d_Nf{i2}"], writes=[f"d_N0_{tt}"])
                            P.op("gpsimd", lambda e, tt=tt: e.memset(Nl[0][64:128, tt, 0:64], 0.0), reads=[f"d_N0_{tt}"], writes=[f"d_N0_{tt}"])
                            P.op("gpsimd", lambda e, t=t, i2=i2: e.affine_select(out=AT[:, t, :], in_=ATf[i2][:], pattern=[[1, 128]], compare_op=ALU.is_ge, fill=0.0, base=0, channel_multiplier=-1),
                                 reads=[f"d_ATf{i2}"], writes=["d_AT"])
                            P.op("gpsimd", lambda e, t=t: e.memset(AT[0:64, t, 64:128], 0.0), reads=["d_AT"], writes=["d_AT"])
                            P.op("tensor", lambda e, tt=tt: e.transpose(psb[:, tt * 128:(tt + 1) * 128], Nl[0][:, tt, :], k.identb[:]), reads=[f"d_N0_{tt}", "c_identb"], writes=["ps7"])
                        P.op("scalar", lambda e: e.copy(Ml[0][:].rearrange("p a b -> p (a b)"), psb[:, 0:512]), reads=["ps7"], writes=["d_M0"])
                        for tt in range(4):
                            P.op("vector", lambda e, tt=tt: e.tensor_tensor(out=Pl[0][:, tt, :], in0=Ml[0][:, tt, :], in1=k.identb[:], op=ALU.add), reads=["d_M0", "c_identb"], writes=[f"d_P0_{tt}"])
                        Nres = [f"d_N0_{tt}" for tt in range(4)]
                        Mres = ["d_M0"]
                        Pres = [f"d_P0_{tt}" for tt in range(4)]
                        cur = 0
                        for lvl in range(1, 6):
                            nxt = 1 - cur
                            for tt in range(4):
                                P.op("tensor", lambda e, tt=tt, cur=cur: e.matmul(k.ps[4][:, tt * 128:(tt + 1) * 128], lhsT=Ml[cur][:, tt, :], rhs=Nl[cur][:, tt, :], start=True, stop=True),
                                     reads=Nres + Mres, writes=["ps4"])
                            if lvl < 5:
                                for tt in range(4):
                                    P.op("tensor", lambda e, tt=tt, cur=cur: e.matmul(k.ps[5][:, tt * 128:(tt + 1) * 128], lhsT=Nl[cur][:, tt, :], rhs=Ml[cur][:, tt, :], start=True, stop=True),
                                         reads=Nres + Mres, writes=["ps5"])
                            P.op("scalar", lambda e, nxt=nxt: e.copy(Nl[nxt][:].rearrange("p a b -> p (a b)"), k.ps[4][:, :]), reads=["ps4"], writes=[f"d_Nn{nxt}"])
                            if lvl < 5:
                                P.op("vector", lambda e, nxt=nxt: e.tensor_copy(Ml[nxt][:].rearrange("p a b -> p (a b)"), k.ps[5][:, :]), reads=["ps5"], writes=[f"d_Mn{nxt}"])
                            for tt in range(4):
                                P.op("tensor", lambda e, tt=tt, cur=cur, nxt=nxt: e.matmul(k.ps[6][:, tt * 128:(tt + 1) * 128], lhsT=Nl[nxt][:, tt, :], rhs=Pl[cur][:, tt, :], start=True, stop=True),
                                     reads=[f"d_Nn{nxt}"] + Pres, writes=["ps6"])
                            P.op("vector", lambda e, cur=cur, nxt=nxt: e.tensor_tensor(out=Pl[nxt][:].rearrange("p a b -> p (a b)"), in0=k.ps[6][:, :], in1=Pl[cur][:].rearrange("p a b -> p (a b)"), op=ALU.add),
                                 reads=["ps6"] + Pres, writes=[f"d_Pn{nxt}"])
                            Nres = [f"d_Nn{nxt}"]
                            Mres = [f"d_Mn{nxt}"]
                            Pres = [f"d_Pn{nxt}"]
                            cur = nxt
                        for tt in range(4):
                            t = grp * 4 + tt
                            P.op("tensor", lambda e, tt=tt, t=t, cur=cur: e.matmul(k.ps[4][:, tt * 128:(tt + 1) * 128], lhsT=Pl[cur][:, tt, :], rhs=vb_tm[:, t, :], start=True, stop=True),
                                 reads=Pres + ["d_vb"], writes=["ps4"])
                            P.op("tensor", lambda e, tt=tt, t=t, cur=cur: e.matmul(k.ps[5][:, tt * 128:(tt + 1) * 128], lhsT=kbg_tm[:, t, :], rhs=Pl[cur][:, tt, :], start=True, stop=True),
                                 reads=Pres + ["d_kbg"], writes=["ps5"])
                        P.op("vector", lambda e, grp=grp: e.tensor_copy(uin[:, grp * 4:(grp + 1) * 4, :].rearrange("p a b -> p (a b)"), k.ps[4][:, :]), reads=["ps4"], writes=["d_uin"])
                        P.op("scalar", lambda e, grp=grp: e.copy(WT[:, grp * 512:(grp + 1) * 512], k.ps[5][:, :]), reads=["ps5"], writes=["d_WT"])
                P.barrier()
                with contextlib.ExitStack() as st:
                    S32 = sbt(nc, st, "d_S32", [128, 128], F32)
                    Sb = sbt(nc, st, "d_Sb", [128, 128], BF16)
                    ub = [sbt(nc, st, f"d_ub{i}", [128, 128], BF16) for i in range(2)]
                    P.op("gpsimd", lambda e: e.memset(S32[:], 0.0), writes=["d_S32"])
                    P.op("gpsimd", lambda e: e.memset(Sb[:], 0.0), writes=["d_Sb"])
                    for ci in range(64):
                        t, hb = ci // 2, ci % 2
                        r0 = hb * 64
                        cc = slice(ci * 64, (ci + 1) * 64)
                        ubb, ubn = ub[ci % 2], f"d_ub{ci % 2}"
                        po, pon = k.ps[1 + (ci // 8) % 2], f"ps{1 + (ci // 8) % 2}"
                        oc = slice((ci % 8) * 64, (ci % 8 + 1) * 64)
                        P.op("tensor", lambda e, r0=r0, cc=cc: e.matmul(k.ps[0][r0:r0 + 64, 0:128], lhsT=WT[:, cc], rhs=Sb[:], start=True, stop=True), reads=["d_WT", "d_Sb"], writes=["ps0"])
                        P.op("vector", lambda e, r0=r0, t=t, ubb=ubb: e.tensor_tensor(out=ubb[r0:r0 + 64, :], in0=uin[r0:r0 + 64, t, :], in1=k.ps[0][r0:r0 + 64, 0:128], op=ALU.subtract),
                             reads=["ps0", "d_uin"], writes=[ubn])
                        P.op("tensor", lambda e, po=po, oc=oc, cc=cc: e.matmul(po[:, oc], lhsT=Sb[:], rhs=qdT[:, cc], start=True, stop=False), reads=["d_Sb", "d_qdT"], writes=[pon])
                        P.op("tensor", lambda e, po=po, oc=oc, r0=r0, t=t, ubb=ubb: e.matmul(po[:, oc], lhsT=ubb[r0:r0 + 64, :], rhs=AT[r0:r0 + 64, t, r0:r0 + 64], start=False, stop=True),
                             reads=[ubn, "d_AT"], writes=[pon])
                        P.op("tensor", lambda e, r0=r0, t=t, ubb=ubb: e.matmul(k.ps[3][:, 0:128], lhsT=kdec_tm[r0:r0 + 64, t, :], rhs=ubb[r0:r0 + 64, :], start=True, stop=True),
                             reads=[ubn, "d_kdec"], writes=["ps3"])
                        P.op("vector", lambda e, ci=ci: e.scalar_tensor_tensor(out=S32[:], in0=S32[:], scalar=eglB[:, ci:ci + 1], in1=k.ps[3][:, 0:128], op0=ALU.mult, op1=ALU.add),
                             reads=["ps3", "d_S32", "d_eglB"], writes=["d_S32"])
                        P.op("scalar", lambda e: e.copy(Sb[:], S32[:]), reads=["d_S32"], writes=["d_Sb"])
                        if ci % 8 == 7:
                            g8 = ci // 8
                            P.op("scalar", lambda e, po=po, g8=g8: e.copy(oT[:, g8 * 512:(g8 + 1) * 512], po[:, :]), reads=[pon], writes=["d_oT"])
                P.barrier()
                if "dn_dbg" in DBG_FLAGS and h == 0:
                    dl = [(kT[:, 0:128], 0), (qT[:, 0:128], 128), (gc_col[:], 256), (b_col[:], 288), (AT[:, 0, :], 320), (uin[:, 0, :], 448),
                          (WT[:, 0:128], 576), (oT[:, 0:128], 704), (vb_tm[:, 0, :], 832), (kbg_tm[:, 0, :], 960), (kdec_tm[:, 0, :], 1088), (qdT[:, 0:128], 1216), (eglB[:], 1344), (gcB[:, 2016:2144], 1408), (kT[:, 2048:2176], 1536), (qdT[:, 2048:2176], 1664), (AT[:, 16, :], 1792), (uin[:, 16, :], 1920), (WT[:, 2048:2176], 2048), (oT[:, 2048:2176], 2176), (kdec_tm[:, 16, :], 2304)]
                    for (ap_, o_) in dl:
                        P.dma("gpsimd", "dbgo", lambda e, ap_=ap_, o_=o_: e.dma_start(out=c.dbg1[:, o_:o_ + ap_.shape[-1]], in_=ap_), reads=[])
                    P.barrier()
                with contextlib.ExitStack() as st:
                    sq = sbt(nc, st, "d_sq2", [128, S], F32)
                    dgt = sbt(nc, st, "d_dgt", [128, S], F32)
                    dgs = sbt(nc, st, "d_dgs", [128, S], BF16)
                    rn = [sbt(nc, st, f"d_rn2{i}", [128, 512], F32) for i in range(2)]
                    yst = sbt(nc, st, "d_yst", [128, S], BF16)
                    P.dma("sync", "d_dgt", lambda e: e.dma_start(out=dgt[:], in_=c.fT[2048 + h * 128:2048 + (h + 1) * 128, :]), writes=["d_dgt"])
                    for tc in range(8):
                        P.op("scalar", lambda e, tc=tc: e.activation(out=dgs[:, tc * 512:(tc + 1) * 512], in_=dgt[:, tc * 512:(tc + 1) * 512], func=AF.Silu), reads=["d_dgt"], writes=["d_dgs"])
                    P.op("vector", lambda e: e.tensor_tensor(out=sq[:], in0=oT[:], in1=oT[:], op=ALU.mult), reads=["d_oT"], writes=["d_sq2"])
                    for tc in range(8):
                        pb, pn = k.ps[tc % 4], f"ps{tc % 4}"
                        rb, rbn = rn[tc % 2], f"d_rn2{tc % 2}"
                        cs = slice(tc * 512, (tc + 1) * 512)
                        P.op("tensor", lambda e, pb=pb, cs=cs: e.matmul(pb[:, :], lhsT=k.ones[:], rhs=sq[:, cs], start=True, stop=True), reads=["c_ones", "d_sq2"], writes=[pn])
                        P.op("scalar", lambda e, pb=pb, rb=rb: e.activation(out=rb[:], in_=pb[:, :], func=AF.Sqrt, bias=eps6[:, 0:1], scale=1.0 / 128.0), reads=[pn, "d_eps6"], writes=[rbn])
                        P.op("vector", lambda e, rb=rb: e.reciprocal(rb[:], rb[:]), reads=[rbn], writes=[rbn])
                        P.op("vector", lambda e, rb=rb, cs=cs: e.scalar_tensor_tensor(out=oT[:, cs], in0=oT[:, cs], scalar=nw[:, 0:1], in1=rb[:], op0=ALU.mult, op1=ALU.mult),
                             reads=["d_oT", "d_nw", rbn], writes=[f"d_oTn{tc}"])
                        P.op("gpsimd", lambda e, cs=cs: e.tensor_tensor(out=yst[:, cs], in0=oT[:, cs], in1=dgs[:, cs], op=ALU.mult), reads=[f"d_oTn{tc}", "d_dgs"], writes=[f"d_yst{tc}"])
                    P.dma("sync", "d_yout", lambda e: e.dma_start(out=c.yT[1024 + h * 128:1024 + (h + 1) * 128, :], in_=yst[:]), reads=[f"d_yst{tc}" for tc in range(8)])
            P.barrier()
        for h in range(4):
            do_head(h)
    P.barrier()


MOE_C = 512


def stage6_moe_sparse(nc, P, k, c, li, dst):
    C = MOE_C
    NB = C // 128
    psb6 = k.ps[6][:, :].bitcast(BF16)
    psb7 = k.ps[7][:, :].bitcast(BF16)
    with contextlib.ExitStack() as st0:
        gam = sbt(nc, st0, "s6_gam", [128, D], F32)
        bet = sbt(nc, st0, "s6_bet", [128, D], F32)
        eps = sbt(nc, st0, "s6_eps", [128, 1], F32)
        P.dma("sync", "lnp", lambda e: e.dma_start(out=gam[:], in_=c.ln2_g[li].partition_broadcast(128)), writes=["lnp"])
        P.dma("sync", "lnp", lambda e: e.dma_start(out=bet[:], in_=c.ln2_b[li].partition_broadcast(128)), writes=["lnp"])
        P.op("vector", lambda e: e.memset(eps[:], 1e-5), writes=["s6_eps"])
        with contextlib.ExitStack() as st:
            wr = sbt(nc, st, "s6_wr", [128, 8, 36], F32)
            brb = sbt(nc, st, "s6_brb", [128, 36], F32)
            xin = [sbt(nc, st, f"s6_xin{i}", [128, D], F32) for i in range(2)]
            xb16 = [sbt(nc, st, f"s6_xb{i}", [128, D], BF16) for i in range(2)]
            xTf = sbt(nc, st, "s6_xTf", [128, 8, 128], F32)
            LG = sbt(nc, st, "s6_LG", [128, NT, 36], F32)
            mg = sbt(nc, st, "s6_mg", [128, NT], F32)
            ohg = sbt(nc, st, "s6_ohg", [128, NT, 4], F32)
            e4 = sbt(nc, st, "s6_e4", [128, NT, 4], F32)
            pgp = sbt(nc, st, "s6_pgp", [128, NT], F32)
            MK = sbt(nc, st, "s6_MK", [128, NT, 32], F32)
            MK2 = sbt(nc, st, "s6_MK2", [128, NT, 32], F32)
            m1 = sbt(nc, st, "s6_m1", [128, NT], F32)
            m2 = sbt(nc, st, "s6_m2", [128, NT], F32)
            OH1 = sbt(nc, st, "s6_OH1", [128, NT, 32], F32)
            OH2 = sbt(nc, st, "s6_OH2", [128, NT, 32], F32)
            Gt = sbt(nc, st, "s6_Gt", [128, NT, 2], F32)
            sm = sbt(nc, st, "s6_sm", [128, 16], F32)
            t4 = sbt(nc, st, "s6_t4", [128, 4], F32)
            pen = sbt(nc, st, "s6_pen", [128, NT, 4], F32)
            P.dma("sync", "s6_wr", lambda e: e.dma_start(out=wr[:], in_=c.w_r[li].rearrange("(kc kp) n -> kp kc n", kp=128)), writes=["s6_wr"])
            P.dma("sync", "s6_brb", lambda e: e.dma_start(out=brb[:], in_=c.b_r[li].partition_broadcast(128)), writes=["s6_brb"])
            nps = 0
            V = lambda fn, r, w: P.op("vector", fn, reads=r, writes=w)
            for t in range(NT):
                xb, xn = xin[t % 2], f"s6_xin{t % 2}"
                P.dma("sync", xn, lambda e, xb=xb, t=t: e.dma_start(out=xb[:], in_=c.x1s[t * 128:(t + 1) * 128, :]), writes=[xn])
                x16, x16n = xb16[t % 2], f"s6_xb{t % 2}"
                P.op("gpsimd", lambda e, xb=xb, x16=x16: e.tensor_copy(x16[:], xb[:]), reads=[xn], writes=[x16n])
                P.dma("sync", "s6_x1bout", lambda e, x16=x16, t=t: e.dma_start(out=c.x1b[t * 128:(t + 1) * 128, :], in_=x16[:]), reads=[x16n], writes=[f"d_x1b{t}"])
                for half in range(2):
                    pb, pn = k.ps[nps % 4], f"ps{nps % 4}"
                    nps += 1
                    for q in range(4):
                        kc = half * 4 + q
                        P.op("tensor", lambda e, pb=pb, xb=xb, kc=kc, q=q: e.transpose(pb[:, q * 128:(q + 1) * 128], xb[:, kc * 128:(kc + 1) * 128], k.ident[:]),
                             reads=[xn, "c_ident"], writes=[pn])
                    P.op("scalar", lambda e, pb=pb, half=half: e.copy(xTf[:, half * 4:(half + 1) * 4, :], pb[:].rearrange("p (a b) -> p a b", a=4)),
                         reads=[pn], writes=[f"s6_xTf{half}"])
                pr, prn = k.ps[4 + t % 2], f"ps{4 + t % 2}"
                for kc in range(8):
                    P.op("tensor", lambda e, pr=pr, kc=kc: e.matmul(pr[:, 0:36], lhsT=xTf[:, kc, :], rhs=wr[:, kc, :], start=(kc == 0), stop=(kc == 7)),
                         reads=[f"s6_xTf{kc // 4}", "s6_wr"], writes=[prn])
                V(lambda e, pr=pr, t=t: e.tensor_tensor(out=LG[:, t, :], in0=pr[:, 0:36], in1=brb[:], op=ALU.add), [prn, "s6_brb"], ["s6_LG"])
            bc = lambda a, n: a.unsqueeze(2).to_broadcast([128, NT, n])
            LGg = LG[:, :, 0:4]
            V(lambda e: e.reduce_max(out=mg[:], in_=LGg, axis=AX.X), ["s6_LG"], ["s6_mg"])
            V(lambda e: e.tensor_tensor(out=ohg[:], in0=LGg, in1=bc(mg[:, :], 4), op=ALU.is_equal), ["s6_LG", "s6_mg"], ["s6_ohg"])
            V(lambda e: e.tensor_scalar(out=pen[:], in0=ohg[:], scalar1=-1.0, scalar2=1e30, op0=ALU.add, op1=ALU.mult), ["s6_ohg"], ["s6_pen"])
            V(lambda e: e.tensor_tensor(out=e4[:], in0=LGg, in1=bc(mg[:, :], 4), op=ALU.subtract), ["s6_LG", "s6_mg"], ["s6_e4"])
            P.op("scalar", lambda e: e.activation(out=e4[:], in_=e4[:], func=AF.Exp), reads=["s6_e4"], writes=["s6_e4"])
            V(lambda e: e.reduce_sum(out=pgp[:], in_=e4[:], axis=AX.X), ["s6_e4"], ["s6_pgp"])
            V(lambda e: e.reciprocal(pgp[:], pgp[:]), ["s6_pgp"], ["s6_pgp"])
            for g in range(4):
                V(lambda e, g=g: e.tensor_tensor(out=MK[:, :, g * 8:(g + 1) * 8], in0=LG[:, :, 4 + g * 8:4 + (g + 1) * 8], in1=pen[:, :, g:g + 1].to_broadcast([128, NT, 8]), op=ALU.add),
                  ["s6_LG", "s6_pen"], [f"s6_MK{g}"])
            mkres = [f"s6_MK{g}" for g in range(4)]
            V(lambda e: e.reduce_max(out=m1[:], in_=MK[:], axis=AX.X), mkres, ["s6_m1"])
            V(lambda e: e.tensor_tensor(out=OH1[:], in0=MK[:], in1=bc(m1[:, :], 32), op=ALU.is_equal), mkres + ["s6_m1"], ["s6_OH1"])
            V(lambda e: e.scalar_tensor_tensor(out=MK2[:], in0=OH1[:], scalar=-1e30, in1=MK[:], op0=ALU.mult, op1=ALU.add), mkres + ["s6_OH1"], ["s6_MK2"])
            V(lambda e: e.reduce_max(out=m2[:], in_=MK2[:], axis=AX.X), ["s6_MK2"], ["s6_m2"])
            V(lambda e: e.tensor_tensor(out=OH2[:], in0=MK2[:], in1=bc(m2[:, :], 32), op=ALU.is_equal), ["s6_MK2", "s6_m2"], ["s6_OH2"])
            V(lambda e: e.tensor_tensor(out=m1[:], in0=m1[:], in1=m2[:], op=ALU.subtract), ["s6_m1", "s6_m2", "s6_OH1"], ["s6_m1"])
            P.op("scalar", lambda e: e.activation(out=m1[:], in_=m1[:], func=AF.Sigmoid), reads=["s6_m1"], writes=["s6_m1"])
            V(lambda e: e.tensor_tensor(out=Gt[:, :, 0], in0=m1[:], in1=pgp[:], op=ALU.mult), ["s6_m1", "s6_pgp"], ["s6_Gt"])
            V(lambda e: e.tensor_tensor(out=Gt[:, :, 1], in0=pgp[:], in1=Gt[:, :, 0], op=ALU.subtract), ["s6_pgp", "s6_Gt"], ["s6_Gt"])
            A16 = sbt(nc, st, "s6_A16", [128, NT * 32], BF16)
            Tri = sbt(nc, st, "s6_Tri", [128, 128], BF16)
            INC = sbt(nc, st, "s6_INC", [128, NT, 32], F32)
            X = [sbt(nc, st, f"s6_X{i}", [128, NT, 32], F32) for i in range(2)]
            TB = sbt(nc, st, "s6_TB", [128, NT, 32], F32)
            ECf = sbt(nc, st, "s6_ECf", [128, NT, 32], F32)
            tmp3 = sbt(nc, st, "s6_tmp3", [128, NT, 32], F32)
            Sf = sbt(nc, st, "s6_Sf", [128, 2, NT], F32)
            Si = sbt(nc, st, "s6_Si", [128, 2, NT], I32)
            REC = sbt(nc, st, "s6_REC", [128, NT, 2, 16], I32)
            INIT = sbt(nc, st, "s6_INIT", [128, 32 * C // 128, 16], I32)
            io_t = sbt(nc, st, "s6_iot", [128, NT], I32)
            io_d = sbt(nc, st, "s6_iod", [128, 2, NT], I32)
            flat = lambda a: a[:].rearrange("p a b -> p (a b)")
            V(lambda e: e.tensor_tensor(out=A16[:], in0=flat(OH1), in1=flat(OH2), op=ALU.add), ["s6_OH1", "s6_OH2"], ["s6_A16"])
            P.op("gpsimd", lambda e: e.affine_select(out=Tri[:], in_=k.onesb[:], pattern=[[1, 128]], compare_op=ALU.is_ge, fill=0.0, base=0, channel_multiplier=-1), reads=["c_onesb"], writes=["s6_Tri"])
            for hf in range(2):
                P.op("tensor", lambda e, hf=hf: e.matmul(k.ps[hf][:, :], lhsT=Tri[:], rhs=A16[:, hf * 512:(hf + 1) * 512], start=True, stop=True), reads=["s6_Tri", "s6_A16"], writes=[f"ps{hf}"])
                P.op("tensor", lambda e, hf=hf: e.matmul(k.ps[2 + hf][:, :], lhsT=k.onesb[:], rhs=A16[:, hf * 512:(hf + 1) * 512], start=True, stop=True), reads=["c_onesb", "s6_A16"], writes=[f"ps{2 + hf}"])
                P.op("scalar", lambda e, hf=hf: e.copy(flat(INC)[:, hf * 512:(hf + 1) * 512], k.ps[hf][:, :]), reads=[f"ps{hf}"], writes=[f"s6_INC{hf}"])
                V(lambda e, hf=hf: e.tensor_copy(flat(TB)[:, hf * 512:(hf + 1) * 512], k.ps[2 + hf][:, :]), [f"ps{2 + hf}"], [f"s6_TB{hf}"])
            V(lambda e: e.tensor_copy(X[0][:], TB[:]), ["s6_TB0", "s6_TB1"], ["s6_X0"])
            cur = 0
            for s_ in (1, 2, 4, 8, 16):
                nxt = 1 - cur
                V(lambda e, s_=s_, cur=cur, nxt=nxt: e.tensor_tensor(out=X[nxt][:, s_:, :], in0=X[cur][:, s_:, :], in1=X[cur][:, 0:NT - s_, :], op=ALU.add), [f"s6_X{cur}"], [f"s6_X{nxt}"])
                V(lambda e, s_=s_, cur=cur, nxt=nxt: e.tensor_copy(X[nxt][:, 0:s_, :], X[cur][:, 0:s_, :]), [f"s6_X{cur}", f"s6_X{nxt}"], [f"s6_X{nxt}"])
                cur = nxt
            V(lambda e, cur=cur: e.tensor_tensor(out=tmp3[:], in0=X[cur][:], in1=TB[:], op=ALU.subtract), [f"s6_X{cur}", "s6_TB0", "s6_TB1"], ["s6_tmp3"])
            V(lambda e: e.tensor_tensor(out=tmp3[:], in0=tmp3[:], in1=INC[:], op=ALU.add), ["s6_tmp3", "s6_INC0", "s6_INC1"], ["s6_tmp3"])
            V(lambda e: e.tensor_scalar(out=tmp3[:], in0=tmp3[:], scalar1=-1.0, scalar2=float(C - 1), op0=ALU.add, op1=ALU.min), ["s6_tmp3"], ["s6_tmp3"])
            P.op("gpsimd", lambda e: e.iota(ECf[:], pattern=[[0, NT], [C, 32]], base=0, channel_multiplier=0, allow_small_or_imprecise_dtypes=True), writes=["s6_ECf"])
            V(lambda e: e.tensor_tensor(out=tmp3[:], in0=tmp3[:], in1=ECf[:], op=ALU.add), ["s6_tmp3", "s6_ECf"], ["s6_tmp3"])
            for kk, OH in ((0, OH1), (1, OH2)):
                ohn = "s6_OH1" if kk == 0 else "s6_OH2"
                V(lambda e, OH=OH: e.tensor_tensor(out=X[0][:], in0=OH[:], in1=tmp3[:], op=ALU.mult), [ohn, "s6_tmp3", "s6_X0", "s6_X1"], ["s6_X0"])
                V(lambda e, kk=kk: e.reduce_sum(out=Sf[:, kk, :], in_=X[0][:], axis=AX.X), ["s6_X0"], ["s6_Sf"])
            V(lambda e: e.tensor_copy(Si[:], Sf[:]), ["s6_Sf"], ["s6_Si"])
            P.op("gpsimd", lambda e: e.memset(REC[:], 0), writes=["s6_REC"])
            P.op("gpsimd", lambda e: e.iota(io_t[:], pattern=[[128, NT]], base=0, channel_multiplier=1), writes=["s6_iot"])
            for kk in range(2):
                P.op("gpsimd", lambda e, kk=kk: e.iota(io_d[:, kk, :], pattern=[[256, NT]], base=kk, channel_multiplier=2), writes=["s6_iod"])
            RECf = REC[:].bitcast(F32)
            for kk in range(2):
                P.op("gpsimd", lambda e, kk=kk: e.tensor_copy(REC[:, :, kk, 0], io_t[:]), reads=["s6_iot", "s6_REC"], writes=["s6_REC"])
                P.op("gpsimd", lambda e, kk=kk: e.tensor_copy(REC[:, :, kk, 1], io_d[:, kk, :]), reads=["s6_iod", "s6_REC"], writes=["s6_REC"])
                P.op("gpsimd", lambda e, kk=kk: e.tensor_copy(RECf[:, :, kk, 2], Gt[:, :, kk]), reads=["s6_Gt", "s6_REC"], writes=["s6_REC"])
            P.op("gpsimd", lambda e: e.memset(INIT[:], 0), writes=["s6_INIT"])
            P.op("gpsimd", lambda e: e.memset(INIT[:, :, 1:2], 2 * S), reads=["s6_INIT"], writes=["s6_INIT"])
            P.dma("gpsimd", "s6_tabinit", lambda e: e.dma_start(out=c.tab[:, :].rearrange("(p b) w -> p (b w)", p=128), in_=INIT[:].rearrange("p b w -> p (b w)")), reads=["s6_INIT"], writes=["d_tab"])
            for t in range(NT):
                for kk in range(2):
                    P.dma("gpsimd", "s6_tabsc", lambda e, t=t, kk=kk: e.indirect_dma_start(
                        out=c.tab[:, :], out_offset=bass.IndirectOffsetOnAxis(ap=Si[:, kk, t:t + 1], axis=0), in_=REC[:, t, kk, :], in_offset=None),
                        reads=["s6_Si", "s6_REC", "d_tab"], writes=[f"d_tabs{t}_{kk}"])
            tab_res = [f"d_tabs{t}_{kk}" for t in range(NT) for kk in range(2)]
            x1b_res = [f"d_x1b{t}" for t in range(NT)]
        P.barrier()
        wpg = sbt(nc, st0, "s6_wpg", [128, 8, D], BF16)
        wpp = sbt(nc, st0, "s6_wpp", [128, 2, D], BF16)
        wpgv = c.w_pg[li].rearrange("(kc kp) n -> kp kc n", kp=128)
        for q in range(2):
            P.dma("gpsimd", "s6_wpg", lambda e, q=q: e.dma_start(out=wpg[:, q * 4:(q + 1) * 4, :], in_=wpgv[:, q * 4:(q + 1) * 4, :]), writes=["s6_wpg"])
        P.dma("gpsimd", "s6_wpp", lambda e: e.dma_start(out=wpp[:], in_=c.w_pp[li].rearrange("(kc kp) n -> kp kc n", kp=128)), writes=["s6_wpp"])
        with contextlib.ExitStack() as st:
            wgu = [sbt(nc, st, f"s6_wgu{i}", [128, 8, 1024], BF16) for i in range(2)]
            wd = [sbt(nc, st, f"s6_wd{i}", [128, 4, D], BF16) for i in range(2)]
            tabt = [sbt(nc, st, f"s6_tabt{i}", [128, NB, 16], I32) for i in range(2)]
            xg = [sbt(nc, st, f"s6_xg{i}", [128, NB, D], BF16) for i in range(2)]
            xgT = [sbt(nc, st, f"s6_xgT{i}", [128, 8, C], BF16) for i in range(2)]
            hT = [sbt(nc, st, f"s6_hT{i}", [128, 4, C], BF16) for i in range(2)]
            sgl = [sbt(nc, st, f"s6_sg{i}", [128, C], F32) for i in range(2)]
            yg = [sbt(nc, st, f"s6_yg{i}", [128, D], F32) for i in range(2)]

            def loads(ex):
                i2 = ex % 2
                P.dma("gpsimd", f"s6_wgu{i2}", lambda e: e.dma_start(out=wgu[i2][:, :, 0:512], in_=c.w_eg[li, ex].rearrange("(kc kp) f -> kp kc f", kp=128)), writes=[f"s6_wgu{i2}"])
                P.dma("gpsimd", f"s6_wgu{i2}", lambda e: e.dma_start(out=wgu[i2][:, :, 512:1024], in_=c.w_eu[li, ex].rearrange("(kc kp) f -> kp kc f", kp=128)), reads=[f"s6_wgu{i2}"], writes=[f"s6_wgu{i2}"])
                P.dma("gpsimd", f"s6_wd{i2}", lambda e: e.dma_start(out=wd[i2][:], in_=c.w_ed[li, ex].rearrange("(fc fp) n -> fp fc n", fp=128)), writes=[f"s6_wd{i2}"])
                P.dma("gpsimd", f"s6_tabt{i2}", lambda e: e.dma_start(out=tabt[i2][:], in_=c.tab[ex * C:(ex + 1) * C, :].rearrange("(b p) w -> p b w", p=128)), reads=["s6_wpg", "s6_wpp"], writes=[f"s6_tabt{i2}"])
                for b in range(NB):
                    P.dma("gpsimd", f"s6_xg{i2}", lambda e, b=b: e.indirect_dma_start(
                        out=xg[i2][:, b, :], out_offset=None, in_=c.x1b[:, :], in_offset=bass.IndirectOffsetOnAxis(ap=tabt[i2][:, b, 0:1], axis=0)),
                        reads=[f"s6_tabt{i2}"], writes=[f"s6_xg{i2}_{b}"])

            nps = [0]

            def compute(ex):
                i2 = ex % 2
                tabf = tabt[i2][:].bitcast(F32)
                for b in range(NB):
                    pq, pqn = (psb6, "ps6") if b % 2 == 0 else (psb7, "ps7")
                    for kc in range(8):
                        P.op("tensor", lambda e, b=b, kc=kc, pq=pq: e.transpose(pq[:, kc * 128:(kc + 1) * 128], xg[i2][:, b, kc * 128:(kc + 1) * 128], k.identb[:]),
                             reads=[f"s6_xg{i2}_{b}", "c_identb"], writes=[pqn])
                    fn = (lambda e, b=b, pq=pq: e.tensor_copy(xgT[i2][:, :, b * 128:(b + 1) * 128], pq[:, :].rearrange("p (a b) -> p a b", a=8))) if b % 2 == 0 else \
                         (lambda e, b=b, pq=pq: e.copy(xgT[i2][:, :, b * 128:(b + 1) * 128], pq[:, :].rearrange("p (a b) -> p a b", a=8)))
                    P.op("vector" if b % 2 == 0 else "scalar", fn, reads=[pqn], writes=[f"s6_xgT{i2}_{b}"])
                xres = [f"s6_xgT{i2}_{b}" for b in range(NB)]
                for fc in range(4):
                    pg, pgn = k.ps[nps[0] % 6], f"ps{nps[0] % 6}"
                    nps[0] += 1
                    pu, pun = k.ps[nps[0] % 6], f"ps{nps[0] % 6}"
                    nps[0] += 1
                    for kc in range(8):
                        P.op("tensor", lambda e, pg=pg, kc=kc, fc=fc: e.matmul(pg[:, :], lhsT=wgu[i2][:, kc, fc * 128:(fc + 1) * 128], rhs=xgT[i2][:, kc, :], start=(kc == 0), stop=(kc == 7)),
                             reads=[f"s6_wgu{i2}"] + xres, writes=[pgn])
                    for kc in range(8):
                        P.op("tensor", lambda e, pu=pu, kc=kc, fc=fc: e.matmul(pu[:, :], lhsT=wgu[i2][:, kc, 512 + fc * 128:512 + (fc + 1) * 128], rhs=xgT[i2][:, kc, :], start=(kc == 0), stop=(kc == 7)),
                             reads=[f"s6_wgu{i2}"] + xres, writes=[pun])
                    sg, sgn = sgl[fc % 2], f"s6_sg{fc % 2}"
                    P.op("scalar", lambda e, pg=pg, sg=sg: e.activation(out=sg[:], in_=pg[:, :], func=AF.Silu), reads=[pgn], writes=[sgn])
                    P.op("vector", lambda e, pu=pu, sg=sg, fc=fc: e.tensor_tensor(out=hT[i2][:, fc, :], in0=pu[:, :], in1=sg[:], op=ALU.mult), reads=[pun, sgn], writes=[f"s6_hT{i2}_{fc}"])
                for b in range(NB):
                    ygb, ygn = yg[b % 2], f"s6_yg{b % 2}"
                    for nh in range(2):
                        py, pyn = k.ps[nps[0] % 6], f"ps{nps[0] % 6}"
                        nps[0] += 1
                        for fc in range(4):
                            P.op("tensor", lambda e, py=py, fc=fc, b=b, nh=nh: e.matmul(py[:, :], lhsT=hT[i2][:, fc, b * 128:(b + 1) * 128], rhs=wd[i2][:, fc, nh * 512:(nh + 1) * 512], start=(fc == 0), stop=(fc == 3)),
                                 reads=[f"s6_wd{i2}", f"s6_hT{i2}_{fc}"], writes=[pyn])
                        P.op("vector", lambda e, py=py, b=b, nh=nh, ygb=ygb: e.tensor_scalar(out=ygb[:, nh * 512:(nh + 1) * 512], in0=py[:, :], scalar1=tabf[:, b, 2:3], scalar2=None, op0=ALU.mult),
                             reads=[pyn, f"s6_tabt{i2}"], writes=[ygn + f"_{nh}"])
                    P.dma("gpsimd", "s6_ysc", lambda e, b=b, ygb=ygb: e.indirect_dma_start(
                        out=c.ybuf[:, :], out_offset=bass.IndirectOffsetOnAxis(ap=tabt[i2][:, b, 1:2], axis=0), in_=ygb[:, :], in_offset=None),
                        reads=[ygn + "_0", ygn + "_1", f"s6_tabt{i2}"], writes=[f"d_ybuf{ex}_{b}"])

            loads(0)
            for ex in range(32):
                if ex + 1 < 32:
                    loads(ex + 1)
                compute(ex)
        P.barrier()
        with contextlib.ExitStack() as st:
            xin = [sbt(nc, st, f"s6_xin2{i}", [128, D], F32) for i in range(2)]
            xTt = [sbt(nc, st, f"s6_xTt{i}", [128, 8, 128], BF16) for i in range(2)]
            ym = [sbt(nc, st, f"s6_ym{i}", [128, 2, D], F32) for i in range(2)]
            pin = [sbt(nc, st, f"s6_pin{i}", [128, 256], F32) for i in range(2)]
            pT = [sbt(nc, st, f"s6_pT{i}", [128, 2, 128], BF16) for i in range(2)]
            sgt = sbt(nc, st, "s6_sgt", [128, D], F32)
            z = [sbt(nc, st, f"s6_z{i}", [128, D], F32) for i in range(2)]
            xo = [sbt(nc, st, f"s6_xo{i}", [128, D], F32) for i in range(2)]
            stats = sbt(nc, st, "s6_stats", [128, 2, 6], F32)
            mv = sbt(nc, st, "s6_mv", [128, 2], F32)
            rstd = sbt(nc, st, "s6_rstd", [128, 1], F32)
            P.dma("sync", "s6_dummy", lambda e: e.dma_start(out=c.dummy[:, :], in_=c.x1s[0:512, :]), writes=["d_dummy"])
            nps = 0
            for t in range(NT):
                i2 = t % 2
                xb, xn = xin[i2], f"s6_xin2{i2}"
                P.dma("sync", xn, lambda e, xb=xb, t=t: e.dma_start(out=xb[:], in_=c.x1s[t * 128:(t + 1) * 128, :]), writes=[xn])
                P.dma("sync", f"s6_pin{i2}", lambda e, t=t, i2=i2: e.dma_start(out=pin[i2][:], in_=c.p[li, t * 128:(t + 1) * 128, :]), writes=[f"s6_pin{i2}"])
                P.dma("sync", f"s6_ym{i2}", lambda e, t=t, i2=i2: e.dma_start(out=ym[i2][:], in_=c.ybuf[t * 256:(t + 1) * 256, :].rearrange("(p two) n -> p two n", two=2)), reads=["d_dummy"], writes=[f"s6_ym{i2}"])
                for half in range(2):
                    pb, pn = k.ps[nps % 6], f"ps{nps % 6}"
                    nps += 1
                    for q in range(4):
                        kc = half * 4 + q
                        P.op("tensor", lambda e, pb=pb, xb=xb, kc=kc, q=q: e.transpose(pb[:, q * 128:(q + 1) * 128], xb[:, kc * 128:(kc + 1) * 128], k.ident[:]),
                             reads=[xn, "c_ident"], writes=[pn])
                    fn = (lambda e, pb=pb, half=half, i2=i2: e.tensor_copy(xTt[i2][:, half * 4:(half + 1) * 4, :], pb[:].rearrange("p (a b) -> p a b", a=4))) if half == 0 else \
                         (lambda e, pb=pb, half=half, i2=i2: e.copy(xTt[i2][:, half * 4:(half + 1) * 4, :], pb[:].rearrange("p (a b) -> p a b", a=4)))
                    P.op("vector" if half == 0 else "scalar", fn, reads=[pn], writes=[f"s6_xTt{i2}_{half}"])
                pb, pn = k.ps[nps % 6], f"ps{nps % 6}"
                nps += 1
                for q in range(2):
                    P.op("tensor", lambda e, pb=pb, q=q, i2=i2: e.transpose(pb[:, q * 128:(q + 1) * 128], pin[i2][:, q * 128:(q + 1) * 128], k.ident[:]), reads=[f"s6_pin{i2}", "c_ident"], writes=[pn])
                P.op("scalar", lambda e, pb=pb, i2=i2: e.copy(pT[i2][:], pb[:, 0:256].rearrange("p (a b) -> p a b", a=2)), reads=[pn], writes=[f"s6_pT{i2}"])
                zb, zn = z[i2], f"s6_z{i2}"
                for nh in range(2):
                    pgt, pgtn = k.ps[nps % 6], f"ps{nps % 6}"
                    nps += 1
                    pp, ppn = k.ps[nps % 6], f"ps{nps % 6}"
                    nps += 1
                    for kc in range(8):
                        P.op("tensor", lambda e, pgt=pgt, kc=kc, nh=nh, i2=i2: e.matmul(pgt[:, :], lhsT=xTt[i2][:, kc, :], rhs=wpg[:, kc, nh * 512:(nh + 1) * 512], start=(kc == 0), stop=(kc == 7)),
                             reads=["s6_wpg", f"s6_xTt{i2}_{kc // 4}"], writes=[pgtn])
                    for kc in range(2):
                        P.op("tensor", lambda e, pp=pp, kc=kc, nh=nh, i2=i2: e.matmul(pp[:, :], lhsT=pT[i2][:, kc, :], rhs=wpp[:, kc, nh * 512:(nh + 1) * 512], start=(kc == 0), stop=(kc == 1)),
                             reads=["s6_wpp", f"s6_pT{i2}"], writes=[ppn])
                    P.op("scalar", lambda e, pgt=pgt, nh=nh: e.activation(out=sgt[:, nh * 512:(nh + 1) * 512], in_=pgt[:, :], func=AF.Sigmoid), reads=[pgtn], writes=[f"s6_sgt{nh}"])
                    P.op("vector", lambda e, pp=pp, nh=nh, zb=zb: e.tensor_tensor(out=zb[:, nh * 512:(nh + 1) * 512], in0=pp[:, :], in1=sgt[:, nh * 512:(nh + 1) * 512], op=ALU.mult),
                         reads=[ppn, f"s6_sgt{nh}"], writes=[zn + f"_{nh}"])
                P.op("gpsimd", lambda e, zb=zb, i2=i2: e.tensor_tensor(out=zb[:], in0=zb[:], in1=ym[i2][:, 0, :], op=ALU.add), reads=[zn + "_0", zn + "_1", f"s6_ym{i2}"], writes=[zn])
                P.op("gpsimd", lambda e, zb=zb, i2=i2: e.tensor_tensor(out=zb[:], in0=zb[:], in1=ym[i2][:, 1, :], op=ALU.add), reads=[zn, f"s6_ym{i2}"], writes=[zn])
                P.op("vector", lambda e, zb=zb, xb=xb: e.scalar_tensor_tensor(out=zb[:], in0=xb[:], scalar=ALPHA, in1=zb[:], op0=ALU.mult, op1=ALU.add), reads=[xn, zn], writes=[zn])
                ob, on = xo[i2], f"s6_xo{i2}"
                layer_norm_tile(P, k, zb, zn, gam, bet, stats, mv, rstd, ob, on, eps[:, 0:1], "s6")
                P.dma("sync", "s6_out", lambda e, t=t, ob=ob: e.dma_start(out=dst[t * 128:(t + 1) * 128, :], in_=ob[:]), reads=[on])
    P.barrier()


def build_layers(nc, layers_local, first, last):
    with contextlib.ExitStack() as st:
        P = Prog(nc, st)
        c = declare_io(nc, layers_local, first, last)
        k = make_consts(nc, st, P)
        n = len(layers_local)
        for li in range(n):
            src = c.x_in if li == 0 else c.xs
            dst = c.out if li == n - 1 else c.xs
            stage1_proj(nc, P, k, c, li, src)
            stage2_attn(nc, P, k, c, li)
            stage3_pool(nc, P, k, c, li)
            stage4_dn(nc, P, k, c, li)
            stage5_merge(nc, P, k, c, li, src)
            stage6_moe_sparse(nc, P, k, c, li, dst)
        P.wait_all("sync")
        P.emit()
        return P.nops


def _core_map(inp, xb, b, layers):
    L = list(layers)
    n = len(L)
    g = lambda nm: np.ascontiguousarray(inp[nm][L])
    return {
        "x_in": np.ascontiguousarray(xb, dtype=np.float32),
        "p": np.ascontiguousarray(inp["p"][L][:, b]),
        "w_in": g("w_in"), "b_forget": g("b_forget").reshape(n, 8, 1),
        "pool_w": g("pool_w"), "pool_scale": g("pool_scale").reshape(n, 4, 128, 1),
        "dn_conv": g("dn_conv"), "dn_a_log": g("dn_a_log").reshape(n, 4, 1),
        "dn_dt_bias": g("dn_dt_bias").reshape(n, 4, 1), "dn_norm_w": g("dn_norm_w").reshape(n, 128, 1),
        "w_br": np.ascontiguousarray(np.concatenate([inp["w_br_attn"][L], inp["w_br_pool"][L], inp["w_br_dn"][L]], axis=1)),
        "w_out": g("w_out"), "ln1_g": g("ln1_g"), "ln1_b": g("ln1_b"),
        "w_r": np.ascontiguousarray(np.concatenate([inp["w_router_group"][L], inp["w_router_expert"][L]], axis=2)),
        "b_r": np.ascontiguousarray(np.concatenate([inp["b_router_group"][L], inp["b_router_expert"][L]], axis=1)),
        "w_eg": g("w_exp_gate"), "w_eu": g("w_exp_up"), "w_ed": g("w_exp_down"),
        "w_pp": g("w_ple_proj"), "w_pg": g("w_ple_gate"), "ln2_g": g("ln2_g"), "ln2_b": g("ln2_b"),
    }


LAYER_GROUPS = [[0, 1, 2, 3]]


def kernel(**inputs):
    inp = {k: np.asarray(v) for k, v in inputs.items()}
    cur = [inp["x"][b] for b in range(4)]
    for grp in LAYER_GROUPS:
        nc = bass.Bass("TRN2", target_bir_lowering=False)
        build_layers(nc, list(range(len(grp))), True, True)
        base = [_core_map(inp, cur[b], b, grp) for b in range(4)]
        maps = [base[c % 4] for c in range(8)]
        res = run_bass_kernel_spmd(nc, maps, core_ids=list(range(8)))
        cur = [np.asarray(res.results[b]["out"], dtype=np.float32) for b in range(4)]
    return np.stack(cur).astype(np.float32)
```

```python
import contextlib
import numpy as np
import concourse.bass as bass
import concourse.mybir as mybir
from concourse.bass_utils import run_bass_kernel_spmd

F32 = mybir.dt.float32
BF16 = mybir.dt.bfloat16
I32 = mybir.dt.int32
AF = mybir.ActivationFunctionType
ALU = mybir.AluOpType
AX = mybir.AxisListType

ENGS = ["tensor", "vector", "scalar", "gpsimd", "sync"]
SEM_LIMIT = 30000


class Prog:
    def __init__(self, nc, stack):
        self.nc = nc
        self.stack = stack
        self.stream = {e: [] for e in ENGS}
        self.cnt = {e: 0 for e in ENGS}
        self.nsem = 0
        self.sem = {e: self._newsem("p_" + e) for e in ENGS}
        self.waited = {e: {} for e in ENGS}
        self.lastw = {}
        self.readers = {}
        self.dsem = {}
        self.nops = 0

    def _newsem(self, name):
        self.nsem += 1
        return self.stack.enter_context(self.nc.semaphore(f"{name}_{self.nsem}"))

    def _need(self, eng, toks):
        need = {}
        for (sem, val, teng), kind in toks:
            if teng == eng and (kind != "raw" or eng == "tensor"):
                continue
            k = id(sem)
            if self.waited[eng].get(k, 0) >= val:
                continue
            if k not in need or need[k][1] < val:
                need[k] = (sem, val)
        for k, (sem, val) in need.items():
            self.waited[eng][k] = val
        return list(need.values())

    def _deps(self, eng, reads, writes):
        toks = []
        for r in reads:
            t = self.lastw.get(r)
            if t is not None:
                toks.append((t, "raw"))
        for w in writes:
            t = self.lastw.get(w)
            if t is not None:
                toks.append((t, "waw"))
            for t in self.readers.get(w, {}).values():
                toks.append((t, "war"))
        return self._need(eng, toks)

    def _update(self, tok, reads, writes):
        sem, val, teng = tok
        rk = teng if teng != "dma" else ("dma", id(sem))
        for r in reads:
            self.readers.setdefault(r, {})[rk] = tok
        for w in writes:
            self.lastw[w] = tok
            self.readers[w] = {}

    def op(self, eng, fn, reads=(), writes=()):
        waits = self._deps(eng, reads, writes)
        if self.cnt[eng] >= SEM_LIMIT:
            self.sem[eng] = self._newsem("p_" + eng)
            self.cnt[eng] = 0
        self.cnt[eng] += 1
        tok = (self.sem[eng], self.cnt[eng], eng)
        self.stream[eng].append((waits, fn, self.sem[eng], 1))
        self._update(tok, reads, writes)
        self.nops += 1
        return tok

    def dma(self, eng, key, fn, reads=(), writes=()):
        waits = self._deps(eng, reads, writes)
        s = self.dsem.get(key)
        if s is None or s[1] >= SEM_LIMIT:
            s = [self._newsem("d"), 0]
            self.dsem[key] = s
        s[1] += 16
        tok = (s[0], s[1], "dma")
        self.stream[eng].append((waits, fn, s[0], 16))
        self._update(tok, reads, writes)
        self.nops += 1
        return tok

    def barrier(self):
        toks = []
        for e in ENGS:
            if self.cnt[e] > 0:
                toks.append(((self.sem[e], self.cnt[e], e), "raw"))
        for k, s in self.dsem.items():
            toks.append(((s[0], s[1], "dma"), "raw"))
        for e in ENGS:
            waits = self._need(e, [t for t in toks if t[0][2] != e])
            if waits:
                self.stream[e].append((waits, None, None, 0))
        self.lastw = {}
        self.readers = {}

    def wait_all(self, eng):
        toks = []
        for e in ENGS:
            if self.cnt[e] > 0 and e != eng:
                toks.append(((self.sem[e], self.cnt[e], e), "raw"))
        for k, s in self.dsem.items():
            toks.append(((s[0], s[1], "dma"), "raw"))
        waits = self._need(eng, toks)
        if waits:
            self.stream[eng].append((waits, None, None, 0))

    def emit(self):
        with self.nc.Block() as block:
            for eng in ENGS:
                def body(e, eng=eng):
                    for (waits, fn, sem, inc) in self.stream[eng]:
                        for (s, v) in waits:
                            e.wait_ge(s, v)
                        if fn is not None:
                            fn(e).then_inc(sem, inc)
                getattr(block, eng)(body)


S = 4096
D = 1024
NT = S // 128
DEPTH = 4
ALPHA = (2 * DEPTH) ** 0.25
C_AQ, C_AK, C_AV, C_AF, C_PU, C_DQ, C_DK, C_DV, C_DA, C_DB, C_DG, C_GA, C_GP, C_GD = (
    0, 512, 1024, 1536, 1544, 2056, 2568, 3080, 3592, 3596, 3600, 4112, 5136, 6160)
INW = 7184


class Ctx:
    pass


DBG_OUT = set()
DBG_FLAGS = set()
NH_DBG = 8
NI_DBG = 16


_SBT_N = [0]


def sbt(nc, st, name, shape, dt):
    _SBT_N[0] += 1
    return st.enter_context(nc.sbuf_tensor(f"{name}_u{_SBT_N[0]}", shape, dt))


def declare_io(nc, layers, first, last):
    L = len(layers)
    c = Ctx()
    EI = "ExternalInput"
    c.x_in = nc.dram_tensor("x_in", [S, D], F32, kind=EI).ap()
    c.p = nc.dram_tensor("p", [L, S, 256], F32, kind=EI).ap()
    c.w_in = nc.dram_tensor("w_in", [L, D, INW], F32, kind=EI).ap()
    c.b_forget = nc.dram_tensor("b_forget", [L, 8, 1], F32, kind=EI).ap()
    c.pool_w = nc.dram_tensor("pool_w", [L, 4, 128, 128], F32, kind=EI).ap()
    c.pool_scale = nc.dram_tensor("pool_scale", [L, 4, 128, 1], F32, kind=EI).ap()
    c.dn_conv = nc.dram_tensor("dn_conv", [L, 4, 1536], F32, kind=EI).ap()
    c.dn_a_log = nc.dram_tensor("dn_a_log", [L, 4, 1], F32, kind=EI).ap()
    c.dn_dt_bias = nc.dram_tensor("dn_dt_bias", [L, 4, 1], F32, kind=EI).ap()
    c.dn_norm_w = nc.dram_tensor("dn_norm_w", [L, 128, 1], F32, kind=EI).ap()
    c.w_br = nc.dram_tensor("w_br", [L, 1536, D], F32, kind=EI).ap()
    c.w_out = nc.dram_tensor("w_out", [L, D, D], F32, kind=EI).ap()
    c.ln1_g = nc.dram_tensor("ln1_g", [L, D], F32, kind=EI).ap()
    c.ln1_b = nc.dram_tensor("ln1_b", [L, D], F32, kind=EI).ap()
    c.w_r = nc.dram_tensor("w_r", [L, D, 36], F32, kind=EI).ap()
    c.b_r = nc.dram_tensor("b_r", [L, 36], F32, kind=EI).ap()
    c.w_eg = nc.dram_tensor("w_eg", [L, 32, 128, 8 * 512], F32, kind=EI).ap()
    c.w_eu = nc.dram_tensor("w_eu", [L, 32, 128, 8 * 512], F32, kind=EI).ap()
    c.w_ed = nc.dram_tensor("w_ed", [L, 32, 128, 4 * D], F32, kind=EI).ap()
    c.w_pp = nc.dram_tensor("w_pp", [L, 256, D], F32, kind=EI).ap()
    c.w_pg = nc.dram_tensor("w_pg", [L, D, D], F32, kind=EI).ap()
    c.ln2_g = nc.dram_tensor("ln2_g", [L, D], F32, kind=EI).ap()
    c.ln2_b = nc.dram_tensor("ln2_b", [L, D], F32, kind=EI).ap()
    c.out = nc.dram_tensor("out", [S, D], F32, kind="ExternalOutput").ap()
    def scr(name, shape, dt):
        kind = "ExternalOutput" if name in DBG_OUT else "Internal"
        return nc.dram_tensor(name, shape, dt, kind=kind).ap()
    c.xs = scr("xs", [S, D], F32)
    c.x1s = scr("x1s", [S, D], F32)
    c.qkT = scr("qkT", [1024, S], BF16)
    c.vtm = scr("vtm", [S, 512], BF16)
    c.smT = scr("smT", [16, S], F32)
    c.fT = scr("fT", [2560, S], F32)
    c.yT = scr("yT", [1536, S], BF16)
    c.dbg1 = scr("dbg1", [128, 4096], F32)
    c.dnrow = scr("dnrow", [2, 4, S], F32)
    c.x1b = scr("x1b", [S, D], BF16)
    c.tab = scr("tab", [32 * 512, 16], I32)
    c.ybuf = scr("ybuf", [2 * S + 128, D], F32)
    c.dummy = scr("dly_scratch", [512, D], F32)
    return c


def make_consts(nc, st, P):
    k = Ctx()
    k.ones = sbt(nc, st, "c_ones", [128, 128], F32)
    k.ident = sbt(nc, st, "c_ident", [128, 128], F32)
    k.identb = sbt(nc, st, "c_identb", [128, 128], BF16)
    k.onesb = sbt(nc, st, "c_onesb", [128, 128], BF16)
    k.ps = [st.enter_context(nc.psum_tensor(f"ps{i}", [128, 512], F32)) for i in range(8)]
    P.op("gpsimd", lambda e: e.memset(k.ones[:], 1.0), writes=["c_ones"])
    P.op("gpsimd", lambda e: e.affine_select(out=k.ident[:], in_=k.ones[:], pattern=[[-1, 128]],
                                             compare_op=ALU.is_equal, fill=0.0, base=0, channel_multiplier=1),
         reads=["c_ones"], writes=["c_ident"])
    P.op("vector", lambda e: e.tensor_copy(k.identb[:], k.ident[:]), reads=["c_ident"], writes=["c_identb"])
    P.op("vector", lambda e: e.tensor_copy(k.onesb[:], k.ones[:]), reads=["c_ones"], writes=["c_onesb"])
    return k


def build_xT(nc, P, k, st, src, xT, tag):
    xin = [sbt(nc, st, f"{tag}_xin{i}", [128, D], F32) for i in range(2)]
    for t in range(NT):
        b = xin[t % 2]
        bn = f"{tag}_xin{t % 2}"
        P.dma("sync", bn, lambda e, b=b, t=t: e.dma_start(out=b[:], in_=src[t * 128:(t + 1) * 128, :]), writes=[bn])
        for half in range(2):
            pb = k.ps[(t % 2) * 2 + half]
            pn = f"ps{(t % 2) * 2 + half}"
            for q in range(4):
                kc = half * 4 + q
                P.op("tensor", lambda e, pb=pb, b=b, kc=kc, q=q: e.transpose(pb[:, q * 128:(q + 1) * 128], b[:, kc * 128:(kc + 1) * 128], k.ident[:]),
                     reads=[bn, "c_ident"], writes=[pn])
            eng = "vector" if half == 0 else "scalar"
            if eng == "vector":
                P.op("vector", lambda e, pb=pb, half=half, t=t: e.tensor_copy(
                    xT[:, half * 4:(half + 1) * 4, t * 128:(t + 1) * 128], pb[:].rearrange("p (a b) -> p a b", a=4)),
                    reads=[pn], writes=[f"{tag}_xT{t}_{half}"])
            else:
                P.op("scalar", lambda e, pb=pb, half=half, t=t: e.copy(
                    xT[:, half * 4:(half + 1) * 4, t * 128:(t + 1) * 128], pb[:].rearrange("p (a b) -> p a b", a=4)),
                    reads=[pn], writes=[f"{tag}_xT{t}_{half}"])


def stage1_proj(nc, P, k, c, li, src):
    with contextlib.ExitStack() as st:
        xT = sbt(nc, st, "s1_xT", [128, 8, S], BF16)
        build_xT(nc, P, k, st, src, xT, "s1")
        vstg = sbt(nc, st, "s1_vstg", [128, 8 * 512], BF16)
        wb = [sbt(nc, st, f"s1_w{i}", [128, 8, 512], BF16) for i in range(2)]
        stg = [sbt(nc, st, f"s1_stg{i}", [128, S], F32) for i in range(2)]
        stgb = [sbt(nc, st, f"s1_stgb{i}", [128, S], BF16) for i in range(2)]
        wv = c.w_in[li].rearrange("(kc kp) n -> kp kc n", kp=128)
        groups = [(C_AQ, 512, "q"), (C_AK, 512, "k"), (C_PU, 512, "f0"), (C_DQ, 512, "f1"), (C_DK, 512, "f2"),
                  (C_DV, 512, "f3"), (C_DG, 512, "f4"), (C_AF, 16, "sm0"), (C_DA, 8, "sm1"), (C_AV, 512, "v")]
        gi = 0
        nev = 0
        nst = 0
        for (c0, ncols, kind) in groups:
            w = wb[gi % 2]
            wn = f"s1_w{gi % 2}"
            gi += 1
            if kind == "sm0":
                P.dma("gpsimd", wn, lambda e, w=w: e.dma_start(out=w[:, :, 0:8], in_=wv[:, :, C_AF:C_AF + 8]), writes=[wn])
                P.dma("gpsimd", wn, lambda e, w=w: e.dma_start(out=w[:, :, 8:16], in_=wv[:, :, C_DA:C_DA + 8]), writes=[wn])
            elif kind == "sm1":
                gi -= 1
                continue
            else:
                P.dma("gpsimd", wn, lambda e, w=w, c0=c0, ncols=ncols: e.dma_start(out=w[:, :, 0:ncols], in_=wv[:, :, c0:c0 + ncols]), writes=[wn])
            if kind == "v":
                for t in range(NT):
                    pb = k.ps[4 + t % 4]
                    pn = f"ps{4 + t % 4}"
                    for kc in range(8):
                        P.op("tensor", lambda e, pb=pb, kc=kc, t=t, w=w: e.matmul(pb[:, :], lhsT=xT[:, kc, t * 128:(t + 1) * 128], rhs=w[:, kc, :], start=(kc == 0), stop=(kc == 7)),
                             reads=[wn, f"s1_xT{t}_{kc // 4}"], writes=[pn])
                    vslot = t % 8
                    eng = "vector" if t % 2 == 0 else "scalar"
                    fn = (lambda e, pb=pb, vslot=vslot: e.tensor_copy(vstg[:, vslot * 512:(vslot + 1) * 512], pb[:, :])) if eng == "vector" else \
                         (lambda e, pb=pb, vslot=vslot: e.copy(vstg[:, vslot * 512:(vslot + 1) * 512], pb[:, :]))
                    P.op(eng, fn, reads=[pn], writes=[f"s1_vstg_{vslot}"])
                    if vslot == 7:
                        t0 = t - 7
                        P.dma("sync", "s1_vout", lambda e, t0=t0: e.dma_start(
                            out=c.vtm[t0 * 128:(t0 + 8) * 128, :].rearrange("(a p) n -> p a n", p=128),
                            in_=vstg[:, :].rearrange("p (a n) -> p a n", a=8)),
                            reads=[f"s1_vstg_{v}" for v in range(8)])
                continue
            nch = (ncols + 127) // 128
            for ch in range(nch):
                m = min(128, ncols - ch * 128)
                isb = kind in ("q", "k")
                sbuf = (stgb if isb else stg)[nst % 2]
                sname = ("s1_stgb" if isb else "s1_stg") + str(nst % 2)
                nst += 1
                for tc in range(8):
                    pb = k.ps[4 + nev % 4]
                    pn = f"ps{4 + nev % 4}"
                    for kc in range(8):
                        P.op("tensor", lambda e, pb=pb, kc=kc, tc=tc, w=w, ch=ch, m=m: e.matmul(
                            pb[0:m, :], lhsT=w[:, kc, ch * 128:ch * 128 + m], rhs=xT[:, kc, tc * 512:(tc + 1) * 512], start=(kc == 0), stop=(kc == 7)),
                            reads=[wn] + [f"s1_xT{tt}_{kc // 4}" for tt in range(tc * 4, tc * 4 + 4)], writes=[pn])
                    sc = 0.125 if kind == "q" else 1.0
                    if nev % 2 == 0:
                        P.op("vector", lambda e, pb=pb, tc=tc, sbuf=sbuf, m=m, sc=sc: e.tensor_scalar(
                            out=sbuf[0:m, tc * 512:(tc + 1) * 512], in0=pb[0:m, :], scalar1=sc, scalar2=None, op0=ALU.mult),
                            reads=[pn], writes=[sname])
                    else:
                        P.op("scalar", lambda e, pb=pb, tc=tc, sbuf=sbuf, m=m, sc=sc: e.mul(
                            sbuf[0:m, tc * 512:(tc + 1) * 512], pb[0:m, :], sc),
                            reads=[pn], writes=[sname])
                    nev += 1
                if kind == "q":
                    dst = c.qkT[ch * 128:(ch + 1) * 128, :]
                elif kind == "k":
                    dst = c.qkT[512 + ch * 128:512 + (ch + 1) * 128, :]
                elif kind == "sm0":
                    dst = c.smT[0:16, :]
                else:
                    fi = int(kind[1])
                    dst = c.fT[fi * 512 + ch * 128:fi * 512 + (ch + 1) * 128, :]
                P.dma("sync", "s1_out", lambda e, dst=dst, sbuf=sbuf, m=m: e.dma_start(out=dst, in_=sbuf[0:m, :]), reads=[sname])
    P.barrier()


def stage2_attn(nc, P, k, c, li):
    QC = 512
    QB = QC // 128
    NI = S // QC
    with contextlib.ExitStack() as st:
        qT = sbt(nc, st, "s2_q", [128, 4, S], BF16)
        kT = sbt(nc, st, "s2_k", [128, 4, S], BF16)
        va = sbt(nc, st, "s2_v", [128, NT, 8, 65], BF16)
        af = sbt(nc, st, "s2_af", [8, S], F32)
        cc = sbt(nc, st, "s2_cc", [8, S], F32)
        ones8 = sbt(nc, st, "s2_ones8", [8, S], F32)
        nb = sbt(nc, st, "s2_nb", [8, 1], F32)
        ck = sbt(nc, st, "s2_ck", [128, NT, 8], F32)
        rball = sbt(nc, st, "s2_rb", [128, NT, 8], F32)
        sel0 = sbt(nc, st, "s2_sel0", [128, 128], F32)
        biasb = [sbt(nc, st, f"s2_bias{i}", [128, NT], F32) for i in range(4)]
        PT = [sbt(nc, st, f"s2_PT{i}", [128, QC], BF16) for i in range(8)]
        rden = sbt(nc, st, "s2_rden", [128, QC], F32)
        bc = sbt(nc, st, "s2_bc", [64, QC], F32)
        ystg = [sbt(nc, st, f"s2_y{i}", [64, S], BF16) for i in range(2)]
        for pr in range(4):
            P.dma("sync", f"s2_q{pr}", lambda e, pr=pr: e.dma_start(out=qT[:, pr, :], in_=c.qkT[pr * 128:(pr + 1) * 128, :]), reads=["d_qkT"], writes=[f"s2_q{pr}"])
            P.dma("sync", f"s2_k{pr}", lambda e, pr=pr: e.dma_start(out=kT[:, pr, :], in_=c.qkT[512 + pr * 128:512 + (pr + 1) * 128, :]), reads=["d_qkT"], writes=[f"s2_k{pr}"])
        P.op("gpsimd", lambda e: e.memset(va[:, :, :, 64:65], 1.0), writes=["s2_v1"])
        vsrc = c.vtm.rearrange("(t p) (h d) -> p t h d", p=128, h=8)
        for g in range(NT):
            P.dma("sync", "s2_v", lambda e, g=g: e.dma_start(out=va[:, g, :, 0:64], in_=vsrc[:, g, :, :]), reads=["d_vtm"], writes=["s2_v"])
        P.dma("sync", "s2_af", lambda e: e.dma_start(out=af[:], in_=c.smT[0:8, :]), writes=["s2_af"])
        P.dma("sync", "s2_nb", lambda e: e.dma_start(out=nb[:], in_=c.b_forget[li]), writes=["s2_nb"])
        P.op("gpsimd", lambda e: e.memset(ones8[:], 1.0), writes=["s2_ones8"])
        P.op("gpsimd", lambda e: e.memset(sel0[:], 0.0), writes=["s2_sel0"])
        P.op("gpsimd", lambda e: e.memset(sel0[0:1, :], 1.0), writes=["s2_sel0"])
        P.op("vector", lambda e: e.tensor_scalar(out=nb[:], in0=nb[:], scalar1=-1.0, scalar2=None, op0=ALU.mult), reads=["s2_nb"], writes=["s2_nb"])
        P.op("scalar", lambda e: e.activation(out=af[:], in_=af[:], func=AF.Exp, bias=nb[:, 0:1], scale=-1.0), reads=["s2_af", "s2_nb"], writes=["s2_af"])
        P.op("scalar", lambda e: e.activation(out=af[:], in_=af[:], func=AF.Ln, bias=k.ones[0:8, 0:1], scale=1.0), reads=["s2_af", "c_ones"], writes=["s2_af"])
        P.op("vector", lambda e: e.tensor_scalar(out=af[:], in0=af[:], scalar1=-1.0, scalar2=None, op0=ALU.mult), reads=["s2_af"], writes=["s2_af"])
        P.op("vector", lambda e: e.tensor_tensor_scan(out=cc[:], data0=ones8[:], data1=af[:], initial=0.0, op0=ALU.mult, op1=ALU.add),
             reads=["s2_af", "s2_ones8"], writes=["s2_cc"])
        for j in range(NT):
            P.op("tensor", lambda e, j=j: e.transpose(k.ps[7][:, j * 8:(j + 1) * 8], cc[0:8, j * 128:(j + 1) * 128], k.ident[0:8, 0:8]),
                 reads=["s2_cc", "c_ident"], writes=["ps7"])
        P.op("vector", lambda e: e.tensor_copy(ck[:].rearrange("p a b -> p (a b)"), k.ps[7][:, 0:256]), reads=["ps7"], writes=["s2_ck"])
        P.op("tensor", lambda e: e.matmul(k.ps[7][:, 256:512], lhsT=sel0[:], rhs=ck[:].rearrange("p a b -> p (a b)"), start=True, stop=True),
             reads=["s2_ck", "s2_sel0"], writes=["ps7"])
        P.op("vector", lambda e: e.tensor_copy(rball[:].rearrange("p a b -> p (a b)"), k.ps[7][:, 256:512]), reads=["ps7"], writes=["s2_rb"])

        if "s2_setup" in DBG_FLAGS:
            P.dma("sync", "dbgo", lambda e: e.dma_start(out=c.dbg1[:, 0:256], in_=ck[:].rearrange("p a b -> p (a b)")), reads=["s2_ck"])
            P.dma("sync", "dbgo", lambda e: e.dma_start(out=c.dbg1[:, 256:512], in_=rball[:].rearrange("p a b -> p (a b)")), reads=["s2_rb"])
            P.barrier()
            return
        units = [(h, i, j) for h in range(NH_DBG) for i in range(NI) for j in range(QB * i + QB)]

        def issue_S(n):
            h, i, j = units[n]
            slot = n % 4
            pb = k.ps[slot][:, 0:QC]
            hp, hh = h // 2, h % 2
            bb = biasb[(h * 16 + i) % 4]
            bn = f"s2_bias{(h * 16 + i) % 4}"
            if j == 0:
                nj = QB * i + QB
                P.op("vector", lambda e: e.tensor_scalar(out=bb[:, 0:nj], in0=ck[:, 0:nj, h], scalar1=rball[:, QB * i + QB // 2, h:h + 1], scalar2=-1.0,
                                                         op0=ALU.subtract, op1=ALU.mult), reads=["s2_ck", "s2_rb"], writes=[bn])
            P.op("tensor", lambda e: e.matmul(pb, lhsT=kT[hh * 64:(hh + 1) * 64, hp, j * 128:(j + 1) * 128],
                                              rhs=qT[hh * 64:(hh + 1) * 64, hp, i * QC:(i + 1) * QC], start=True, stop=True),
                 reads=[f"s2_k{hp}", f"s2_q{hp}"], writes=[f"psS{slot}"])
            pt = PT[n % 8]
            ptn = f"s2_PT{n % 8}"
            P.op("scalar", lambda e: e.activation(out=pt[:], in_=pb, func=AF.Exp, bias=bb[:, j:j + 1], scale=1.0),
                 reads=[f"psS{slot}", bn], writes=[ptn])
            if j >= QB * i:
                P.op("gpsimd", lambda e: e.affine_select(out=pt[:], in_=pt[:], pattern=[[1, QC]], compare_op=ALU.is_ge, fill=0.0,
                                                         base=i * QC - j * 128, channel_multiplier=-1), reads=[ptn], writes=[ptn])

        def fin_a(h, i):
            ob = k.ps[4 + (h * 16 + i) % 2]
            on = f"ps{4 + (h * 16 + i) % 2}"
            P.op("vector", lambda e: e.reciprocal(rden[64:65, :], ob[64:65, 0:QC]), reads=[on], writes=["s2_rden"])

        def fin_b(h, i):
            ob = k.ps[4 + (h * 16 + i) % 2]
            on = f"ps{4 + (h * 16 + i) % 2}"
            P.op("tensor", lambda e: e.matmul(k.ps[6][0:64, 0:QC], lhsT=k.ones[64:65, 0:64], rhs=rden[64:65, :], start=True, stop=True),
                 reads=["s2_rden", "c_ones"], writes=["ps6"])
            P.op("scalar", lambda e: e.copy(bc[:], k.ps[6][0:64, 0:QC]), reads=["ps6"], writes=["s2_bc"])
            ys = ystg[h % 2]
            P.op("vector", lambda e: e.tensor_tensor(out=ys[:, i * QC:(i + 1) * QC], in0=ob[0:64, 0:QC], in1=bc[:], op=ALU.mult),
                 reads=[on, "s2_bc"], writes=[f"s2_y{h % 2}"])
            if i == NI - 1:
                P.dma("sync", "s2_yout", lambda e: e.dma_start(out=c.yT[h * 64:(h + 1) * 64, :], in_=ys[:]), reads=[f"s2_y{h % 2}"], writes=["d_yT"])

        pending = []
        LA = 3
        for n0 in range(LA):
            issue_S(n0)
        for n in range(len(units)):
            if n + LA < len(units):
                issue_S(n + LA)
            h, i, j = units[n]
            ob = k.ps[4 + (h * 16 + i) % 2]
            on = f"ps{4 + (h * 16 + i) % 2}"
            pt = PT[n % 8]
            P.op("tensor", lambda e, ob=ob, pt=pt, h=h, i=i, j=j: e.matmul(ob[0:65, 0:QC], lhsT=va[:, j, h, :], rhs=pt[:], start=(j == 0), stop=(j == QB * i + QB - 1)),
                 reads=[f"s2_PT{n % 8}", "s2_v", "s2_v1"], writes=[on])
            for (hh_, ii_) in pending:
                fin_b(hh_, ii_)
            pending = []
            if j == QB * i + QB - 1:
                fin_a(h, i)
                pending.append((h, i))
        for (hh_, ii_) in pending:
            fin_b(hh_, ii_)
    P.barrier()


def stage3_pool(nc, P, k, c, li):
    with contextlib.ExitStack() as st:
        u = [sbt(nc, st, f"s3_u{i}", [128, S], F32) for i in range(2)]
        ab = [sbt(nc, st, f"s3_a{i}", [128, S], F32) for i in range(2)]
        dbf = sbt(nc, st, "s3_d", [128, S], BF16)
        ys = [sbt(nc, st, f"s3_y{i}", [128, S], BF16) for i in range(2)]
        pw = sbt(nc, st, "s3_pw", [128, 4, 128], BF16)
        psc = sbt(nc, st, "s3_psc", [128, 4], F32)
        inv = sbt(nc, st, "s3_inv", [128, 4, 16], F32)
        tmp = sbt(nc, st, "s3_tmp", [128, 16], F32)
        P.dma("gpsimd", "s3_pw", lambda e: e.dma_start(out=pw[:], in_=c.pool_w[li].rearrange("g c d -> c g d")), writes=["s3_pw"])
        for g in range(4):
            P.dma("sync", "s3_psc", lambda e, g=g: e.dma_start(out=psc[:, g:g + 1], in_=c.pool_scale[li, g]), writes=["s3_psc"])
        for g in range(4):
            w = 2 ** (g + 1)
            P.op("gpsimd", lambda e, g=g: e.iota(inv[:, g, :], pattern=[[1, 16]], base=1, channel_multiplier=0, allow_small_or_imprecise_dtypes=True), writes=["s3_inv"])
            P.op("gpsimd", lambda e, g=g, w=w: e.tensor_scalar(out=inv[:, g, :], in0=inv[:, g, :], scalar1=float(w), scalar2=None, op0=ALU.min), reads=["s3_inv"], writes=["s3_inv"])
        P.op("vector", lambda e: e.reciprocal(inv[:].rearrange("p a b -> p (a b)"), inv[:].rearrange("p a b -> p (a b)")), reads=["s3_inv"], writes=["s3_inv"])
        nev = 0
        for g in range(4):
            w = 2 ** (g + 1)
            ug = u[g % 2]
            un = f"s3_u{g % 2}"
            P.dma("sync", un, lambda e, g=g, ug=ug: e.dma_start(out=ug[:], in_=c.fT[g * 128:(g + 1) * 128, :]), writes=[un])
            src, srcn = ug, un
            for m in range(g + 1):
                sh = 2 ** m
                dst, dstn = ab[m % 2], f"s3_a{m % 2}"
                P.op("vector", lambda e, dst=dst, src=src, sh=sh: e.tensor_tensor(out=dst[:, sh:], in0=src[:, sh:], in1=src[:, 0:S - sh], op=ALU.add), reads=[srcn], writes=[dstn])
                P.op("gpsimd", lambda e, dst=dst, src=src, sh=sh: e.tensor_copy(dst[:, 0:sh], src[:, 0:sh]), reads=[srcn, dstn], writes=[dstn])
                src, srcn = dst, dstn
            rs = [srcn]
            P.op("vector", lambda e, src=src, ug=ug, w=w: e.scalar_tensor_tensor(out=dbf[:], in0=src[:], scalar=1.0 / w, in1=ug[:], op0=ALU.mult, op1=ALU.subtract),
                 reads=rs + [un], writes=["s3_d"])
            P.op("vector", lambda e, src=src, g=g, w=w: e.tensor_tensor(out=tmp[:, 0:w], in0=src[:, 0:w], in1=inv[:, g, 0:w], op=ALU.mult), reads=rs + ["s3_inv"], writes=["s3_tmp"])
            P.op("vector", lambda e, ug=ug, w=w: e.tensor_tensor(out=dbf[:, 0:w], in0=tmp[:, 0:w], in1=ug[:, 0:w], op=ALU.subtract), reads=["s3_tmp", un, "s3_d"], writes=["s3_d"])
            yb, yn = ys[g % 2], f"s3_y{g % 2}"
            for tc in range(8):
                pb, pn = k.ps[nev % 4], f"ps{nev % 4}"
                nev += 1
                P.op("tensor", lambda e, pb=pb, g=g, tc=tc: e.matmul(pb[:, :], lhsT=pw[:, g, :], rhs=dbf[:, tc * 512:(tc + 1) * 512], start=True, stop=True),
                     reads=["s3_pw", "s3_d"], writes=[pn])
                P.op("scalar", lambda e, pb=pb, g=g, tc=tc, yb=yb: e.activation(out=yb[:, tc * 512:(tc + 1) * 512], in_=pb[:, :], func=AF.Copy, scale=psc[:, g:g + 1]),
                     reads=[pn, "s3_psc"], writes=[yn])
            P.dma("sync", "s3_yout", lambda e, g=g, yb=yb: e.dma_start(out=c.yT[512 + g * 128:512 + (g + 1) * 128, :], in_=yb[:]), reads=[yn])
    P.barrier()


def layer_norm_tile(P, k, z, zn, gam, bet, stats, mv, rstd, out, outn, eps_ap, tag):
    for hh in range(2):
        P.op("vector", lambda e, hh=hh: e.bn_stats(stats[:, hh, :], z[:, hh * 512:(hh + 1) * 512]), reads=[zn], writes=[tag + "_st"])
    P.op("vector", lambda e: e.bn_aggr(mv[:], stats[:]), reads=[tag + "_st"], writes=[tag + "_mv"])
    P.op("scalar", lambda e: e.activation(out=rstd[:], in_=mv[:, 1:2], func=AF.Sqrt, bias=eps_ap, scale=1.0), reads=[tag + "_mv"], writes=[tag + "_rs"])
    P.op("vector", lambda e: e.reciprocal(rstd[:], rstd[:]), reads=[tag + "_rs"], writes=[tag + "_rs"])
    P.op("vector", lambda e: e.tensor_scalar(out=z[:], in0=z[:], scalar1=mv[:, 0:1], scalar2=rstd[:, 0:1], op0=ALU.subtract, op1=ALU.mult),
         reads=[zn, tag + "_mv", tag + "_rs"], writes=[zn])
    P.op("gpsimd", lambda e: e.tensor_tensor(out=z[:], in0=z[:], in1=gam[:], op=ALU.mult), reads=[zn, "lnp"], writes=[zn])
    P.op("gpsimd", lambda e: e.tensor_tensor(out=out[:], in0=z[:], in1=bet[:], op=ALU.add), reads=[zn, "lnp"], writes=[outn])


def stage5_merge(nc, P, k, c, li, src):
    with contextlib.ExitStack() as st:
        wg = sbt(nc, st, "s5_wg", [128, 8, 3072], BF16)
        wbr = sbt(nc, st, "s5_wbr", [128, 12, 1024], BF16)
        wo = sbt(nc, st, "s5_wo", [128, 8, 1024], BF16)
        gam = sbt(nc, st, "s5_gam", [128, D], F32)
        bet = sbt(nc, st, "s5_bet", [128, D], F32)
        eps = sbt(nc, st, "s5_eps", [128, 1], F32)
        xr = sbt(nc, st, "s5_xr", [128, 4, D], F32)
        xTc = sbt(nc, st, "s5_xTc", [128, 8, 512], BF16)
        yTc = sbt(nc, st, "s5_yTc", [128, 12, 512], BF16)
        sig = [sbt(nc, st, f"s5_sig{i}", [128, 512], F32) for i in range(2)]
        macc = sbt(nc, st, "s5_macc", [128, 512], F32)
        mtmp = sbt(nc, st, "s5_mtmp", [128, 512], F32)
        mT = sbt(nc, st, "s5_mT", [128, 8, 512], BF16)
        z = [sbt(nc, st, f"s5_z{i}", [128, D], F32) for i in range(2)]
        xo = [sbt(nc, st, f"s5_xo{i}", [128, D], F32) for i in range(2)]
        stats = sbt(nc, st, "s5_stats", [128, 2, 6], F32)
        mv = sbt(nc, st, "s5_mv", [128, 2], F32)
        rstd = sbt(nc, st, "s5_rstd", [128, 1], F32)
        wv = c.w_in[li].rearrange("(kc kp) n -> kp kc n", kp=128)
        for q in range(6):
            P.dma("gpsimd", "s5_wg", lambda e, q=q: e.dma_start(out=wg[:, :, q * 512:(q + 1) * 512], in_=wv[:, :, C_GA + q * 512:C_GA + (q + 1) * 512]), writes=["s5_wg"])
        wbv = c.w_br[li].rearrange("(kc kp) n -> kp kc n", kp=128)
        for q in range(3):
            P.dma("gpsimd", "s5_wbr", lambda e, q=q: e.dma_start(out=wbr[:, q * 4:(q + 1) * 4, :], in_=wbv[:, q * 4:(q + 1) * 4, :]), writes=["s5_wbr"])
        wov = c.w_out[li].rearrange("(kc kp) n -> kp kc n", kp=128)
        for q in range(2):
            P.dma("gpsimd", "s5_wo", lambda e, q=q: e.dma_start(out=wo[:, q * 4:(q + 1) * 4, :], in_=wov[:, q * 4:(q + 1) * 4, :]), writes=["s5_wo"])
        P.dma("sync", "lnp", lambda e: e.dma_start(out=gam[:], in_=c.ln1_g[li].partition_broadcast(128)), writes=["lnp"])
        P.dma("sync", "lnp", lambda e: e.dma_start(out=bet[:], in_=c.ln1_b[li].partition_broadcast(128)), writes=["lnp"])
        P.op("vector", lambda e: e.memset(eps[:], 1e-5), writes=["s5_eps"])
        nps = 0
        for tc in range(8):
            for tt in range(4):
                t = tc * 4 + tt
                P.dma("sync", "s5_xr", lambda e, t=t, tt=tt: e.dma_start(out=xr[:, tt, :], in_=src[t * 128:(t + 1) * 128, :]), writes=[f"s5_xr{tt}"])
            P.dma("sync", "s5_yTc", lambda e, tc=tc: e.dma_start(out=yTc[:], in_=c.yT[:, tc * 512:(tc + 1) * 512].rearrange("(a p) n -> p a n", p=128)), writes=["s5_yTc"])
            for tt in range(4):
                for half in range(2):
                    pb, pn = k.ps[nps % 8], f"ps{nps % 8}"
                    nps += 1
                    for q in range(4):
                        kc = half * 4 + q
                        P.op("tensor", lambda e, pb=pb, tt=tt, kc=kc, q=q: e.transpose(pb[:, q * 128:(q + 1) * 128], xr[:, tt, kc * 128:(kc + 1) * 128], k.ident[:]),
                             reads=[f"s5_xr{tt}", "c_ident"], writes=[pn])
                    fn = (lambda e, pb=pb, half=half, tt=tt: e.tensor_copy(xTc[:, half * 4:(half + 1) * 4, tt * 128:(tt + 1) * 128], pb[:].rearrange("p (a b) -> p a b", a=4))) if half == 0 else \
                         (lambda e, pb=pb, half=half, tt=tt: e.copy(xTc[:, half * 4:(half + 1) * 4, tt * 128:(tt + 1) * 128], pb[:].rearrange("p (a b) -> p a b", a=4)))
                    P.op("vector" if half == 0 else "scalar", fn, reads=[pn], writes=[f"s5_xTc{tt}_{half}"])
            xres = [f"s5_xTc{tt}_{h}" for tt in range(4) for h in range(2)]
            for n in range(8):
                for br in range(3):
                    pg, pgn = k.ps[nps % 8], f"ps{nps % 8}"
                    nps += 1
                    pbr, pbn = k.ps[nps % 8], f"ps{nps % 8}"
                    nps += 1
                    for kc in range(8):
                        P.op("tensor", lambda e, pg=pg, kc=kc, br=br, n=n: e.matmul(pg[:, :], lhsT=wg[:, kc, br * 1024 + n * 128:br * 1024 + (n + 1) * 128], rhs=xTc[:, kc, :], start=(kc == 0), stop=(kc == 7)),
                             reads=["s5_wg"] + xres, writes=[pgn])
                    sg, sgn = sig[(n * 3 + br) % 2], f"s5_sig{(n * 3 + br) % 2}"
                    P.op("scalar", lambda e, pg=pg, sg=sg: e.activation(out=sg[:], in_=pg[:, :], func=AF.Sigmoid), reads=[pgn], writes=[sgn])
                    for c4 in range(4):
                        P.op("tensor", lambda e, pbr=pbr, c4=c4, br=br, n=n: e.matmul(pbr[:, :], lhsT=wbr[:, br * 4 + c4, n * 128:(n + 1) * 128], rhs=yTc[:, br * 4 + c4, :], start=(c4 == 0), stop=(c4 == 3)),
                             reads=["s5_wbr", "s5_yTc"], writes=[pbn])
                    if br == 0:
                        P.op("vector", lambda e, pbr=pbr, sg=sg: e.tensor_tensor(out=macc[:], in0=pbr[:, :], in1=sg[:], op=ALU.mult), reads=[pbn, sgn], writes=["s5_macc"])
                    elif br == 1:
                        P.op("vector", lambda e, pbr=pbr, sg=sg: e.tensor_tensor(out=mtmp[:], in0=pbr[:, :], in1=sg[:], op=ALU.mult), reads=[pbn, sgn], writes=["s5_mtmp"])
                        P.op("gpsimd", lambda e: e.tensor_tensor(out=macc[:], in0=macc[:], in1=mtmp[:], op=ALU.add), reads=["s5_macc", "s5_mtmp"], writes=["s5_macc"])
                    else:
                        P.op("vector", lambda e, pbr=pbr, sg=sg: e.tensor_tensor(out=mtmp[:], in0=pbr[:, :], in1=sg[:], op=ALU.mult), reads=[pbn, sgn], writes=["s5_mtmp"])
                        P.op("gpsimd", lambda e, n=n: e.tensor_tensor(out=mT[:, n, :], in0=macc[:], in1=mtmp[:], op=ALU.add), reads=["s5_macc", "s5_mtmp"], writes=[f"s5_mT{n}"])
            mres = [f"s5_mT{n}" for n in range(8)]
            for tt in range(4):
                t = tc * 4 + tt
                zb, zn = z[t % 2], f"s5_z{t % 2}"
                for nh in range(2):
                    po, pon = k.ps[nps % 8], f"ps{nps % 8}"
                    nps += 1
                    for kc in range(8):
                        P.op("tensor", lambda e, po=po, kc=kc, tt=tt, nh=nh: e.matmul(po[:, :], lhsT=mT[:, kc, tt * 128:(tt + 1) * 128], rhs=wo[:, kc, nh * 512:(nh + 1) * 512], start=(kc == 0), stop=(kc == 7)),
                             reads=["s5_wo"] + mres, writes=[pon])
                    P.op("vector", lambda e, po=po, zb=zb, tt=tt, nh=nh: e.scalar_tensor_tensor(out=zb[:, nh * 512:(nh + 1) * 512], in0=xr[:, tt, nh * 512:(nh + 1) * 512], scalar=ALPHA, in1=po[:, :], op0=ALU.mult, op1=ALU.add),
                         reads=[pon, f"s5_xr{tt}"], writes=[zn])
                ob, on = xo[t % 2], f"s5_xo{t % 2}"
                layer_norm_tile(P, k, zb, zn, gam, bet, stats, mv, rstd, ob, on, eps[:, 0:1], "s5")
                P.dma("sync", "s5_out", lambda e, t=t, ob=ob: e.dma_start(out=c.x1s[t * 128:(t + 1) * 128, :], in_=ob[:]), reads=[on])
    P.barrier()


def stage4_zero(nc, P, k, c, li):
    with contextlib.ExitStack() as st:
        zt = sbt(nc, st, "s4_z", [128, S], BF16)
        P.op("gpsimd", lambda e: e.memset(zt[:], 0.0), writes=["s4_z"])
        for h in range(4):
            P.dma("sync", "s4_out", lambda e, h=h: e.dma_start(out=c.yT[1024 + h * 128:1024 + (h + 1) * 128, :], in_=zt[:]), reads=["s4_z"])
    P.barrier()


def stage6_moe(nc, P, k, c, li, dst):
    with contextlib.ExitStack() as st:
        wr = sbt(nc, st, "s6_wr", [128, 8, 36], F32)
        brb = sbt(nc, st, "s6_brb", [128, 36], F32)
        wpg = sbt(nc, st, "s6_wpg", [128, 8, D], BF16)
        wpp = sbt(nc, st, "s6_wpp", [128, 2, D], BF16)
        gam = sbt(nc, st, "s6_gam", [128, D], F32)
        bet = sbt(nc, st, "s6_bet", [128, D], F32)
        eps = sbt(nc, st, "s6_eps", [128, 1], F32)
        acc = sbt(nc, st, "s6_acc", [128, 8, D], F32)
        x1T = sbt(nc, st, "s6_x1T", [128, 8, 1024], BF16)
        xTf = sbt(nc, st, "s6_xTf", [128, 8, 128], F32)
        xin = [sbt(nc, st, f"s6_xin{i}", [128, D], F32) for i in range(2)]
        wgu = [sbt(nc, st, f"s6_wgu{i}", [128, 8, 1024], BF16) for i in range(2)]
        wd = [sbt(nc, st, f"s6_wd{i}", [128, 4, D], BF16) for i in range(2)]
        hT = [sbt(nc, st, f"s6_hT{i}", [128, 4, 512], BF16) for i in range(2)]
        sgl = [sbt(nc, st, f"s6_sg{i}", [128, 512], F32) for i in range(2)]
        lg = sbt(nc, st, "s6_lg", [128, 8, 36], F32)
        G = sbt(nc, st, "s6_G", [128, 8, 32], F32)
        sm = sbt(nc, st, "s6_sm", [128, 16], F32)
        t4 = sbt(nc, st, "s6_t4", [128, 4], F32)
        pen = sbt(nc, st, "s6_pen", [128, 4], F32)
        mk = sbt(nc, st, "s6_mk", [128, 32], F32)
        mk2 = sbt(nc, st, "s6_mk2", [128, 32], F32)
        oh = sbt(nc, st, "s6_oh", [128, 32], F32)
        pin = sbt(nc, st, "s6_pin", [128, 256], F32)
        pT = sbt(nc, st, "s6_pT", [128, 2, 128], BF16)
        sgt = sbt(nc, st, "s6_sgt", [128, D], F32)
        z = [sbt(nc, st, f"s6_z{i}", [128, D], F32) for i in range(2)]
        xo = [sbt(nc, st, f"s6_xo{i}", [128, D], F32) for i in range(2)]
        stats = sbt(nc, st, "s6_stats", [128, 2, 6], F32)
        mv = sbt(nc, st, "s6_mv", [128, 2], F32)
        rstd = sbt(nc, st, "s6_rstd", [128, 1], F32)
        P.dma("sync", "s6_wr", lambda e: e.dma_start(out=wr[:], in_=c.w_r[li].rearrange("(kc kp) n -> kp kc n", kp=128)), writes=["s6_wr"])
        P.dma("sync", "s6_brb", lambda e: e.dma_start(out=brb[:], in_=c.b_r[li].partition_broadcast(128)), writes=["s6_brb"])
        wpgv = c.w_pg[li].rearrange("(kc kp) n -> kp kc n", kp=128)
        for q in range(2):
            P.dma("gpsimd", "s6_wpg", lambda e, q=q: e.dma_start(out=wpg[:, q * 4:(q + 1) * 4, :], in_=wpgv[:, q * 4:(q + 1) * 4, :]), writes=["s6_wpg"])
        P.dma("gpsimd", "s6_wpp", lambda e: e.dma_start(out=wpp[:], in_=c.w_pp[li].rearrange("(kc kp) n -> kp kc n", kp=128)), writes=["s6_wpp"])
        P.dma("sync", "lnp", lambda e: e.dma_start(out=gam[:], in_=c.ln2_g[li].partition_broadcast(128)), writes=["lnp"])
        P.dma("sync", "lnp", lambda e: e.dma_start(out=bet[:], in_=c.ln2_b[li].partition_broadcast(128)), writes=["lnp"])
        P.op("vector", lambda e: e.memset(eps[:], 1e-5), writes=["s6_eps"])
        nps = 0
        nw = 0
        for qt in range(4):
            for tt in range(8):
                t = qt * 8 + tt
                xb, xn = xin[t % 2], f"s6_xin{t % 2}"
                P.dma("sync", xn, lambda e, xb=xb, t=t: e.dma_start(out=xb[:], in_=c.x1s[t * 128:(t + 1) * 128, :]), writes=[xn])
                for half in range(2):
                    pb, pn = k.ps[nps % 8], f"ps{nps % 8}"
                    nps += 1
                    for q in range(4):
                        kc = half * 4 + q
                        P.op("tensor", lambda e, pb=pb, xb=xb, kc=kc, q=q: e.transpose(pb[:, q * 128:(q + 1) * 128], xb[:, kc * 128:(kc + 1) * 128], k.ident[:]),
                             reads=[xn, "c_ident"], writes=[pn])
                    P.op("vector", lambda e, pb=pb, half=half, tt=tt: e.tensor_copy(x1T[:, half * 4:(half + 1) * 4, tt * 128:(tt + 1) * 128], pb[:].rearrange("p (a b) -> p a b", a=4)),
                         reads=[pn], writes=[f"s6_x1T{tt}_{half}"])
                    P.op("scalar", lambda e, pb=pb, half=half: e.copy(xTf[:, half * 4:(half + 1) * 4, :], pb[:].rearrange("p (a b) -> p a b", a=4)),
                         reads=[pn, f"s6_x1T{tt}_{half}"], writes=[f"s6_xTf{half}"])
                pr, prn = k.ps[nps % 8], f"ps{nps % 8}"
                nps += 1
                for kc in range(8):
                    P.op("tensor", lambda e, pr=pr, kc=kc: e.matmul(pr[:, 0:36], lhsT=xTf[:, kc, :], rhs=wr[:, kc, :], start=(kc == 0), stop=(kc == 7)),
                         reads=[f"s6_xTf{kc // 4}", "s6_wr"], writes=[prn])
                P.op("vector", lambda e, pr=pr, tt=tt: e.tensor_tensor(out=lg[:, tt, :], in0=pr[:, 0:36], in1=brb[:], op=ALU.add), reads=[prn, "s6_brb"], writes=["s6_lg"])
                V = lambda fn, r, w: P.op("vector", fn, reads=r, writes=w)
                V(lambda e, tt=tt: e.reduce_max(out=sm[:, 0:1], in_=lg[:, tt, 0:4], axis=AX.X), ["s6_lg"], ["s6_sm"])
                V(lambda e, tt=tt: e.tensor_scalar(out=t4[:], in0=lg[:, tt, 0:4], scalar1=sm[:, 0:1], scalar2=None, op0=ALU.is_equal), ["s6_lg", "s6_sm"], ["s6_t4"])
                V(lambda e: e.tensor_scalar(out=pen[:], in0=t4[:], scalar1=-1.0, scalar2=1e30, op0=ALU.add, op1=ALU.mult), ["s6_t4"], ["s6_pen"])
                V(lambda e: e.tensor_scalar(out=sm[:, 1:2], in0=sm[:, 0:1], scalar1=-1.0, scalar2=None, op0=ALU.mult), ["s6_sm"], ["s6_sm"])
                P.op("scalar", lambda e, tt=tt: e.activation(out=t4[:], in_=lg[:, tt, 0:4], func=AF.Exp, bias=sm[:, 1:2], scale=1.0), reads=["s6_lg", "s6_sm", "s6_pen"], writes=["s6_t4"])
                V(lambda e: e.reduce_sum(out=sm[:, 2:3], in_=t4[:], axis=AX.X), ["s6_t4"], ["s6_sm"])
                V(lambda e: e.reciprocal(sm[:, 2:3], sm[:, 2:3]), ["s6_sm"], ["s6_sm"])
                for g in range(4):
                    V(lambda e, tt=tt, g=g: e.tensor_scalar(out=mk[:, g * 8:(g + 1) * 8], in0=lg[:, tt, 4 + g * 8:4 + (g + 1) * 8], scalar1=pen[:, g:g + 1], scalar2=None, op0=ALU.add),
                      ["s6_lg", "s6_pen"], ["s6_mk"])
                V(lambda e: e.reduce_max(out=sm[:, 3:4], in_=mk[:], axis=AX.X), ["s6_mk"], ["s6_sm"])
                V(lambda e: e.tensor_scalar(out=oh[:], in0=mk[:], scalar1=sm[:, 3:4], scalar2=None, op0=ALU.is_equal), ["s6_mk", "s6_sm"], ["s6_oh"])
                V(lambda e: e.scalar_tensor_tensor(out=mk2[:], in0=oh[:], scalar=-1e30, in1=mk[:], op0=ALU.mult, op1=ALU.add), ["s6_oh", "s6_mk"], ["s6_mk2"])
                V(lambda e: e.reduce_max(out=sm[:, 4:5], in_=mk2[:], axis=AX.X), ["s6_mk2"], ["s6_sm"])
                V(lambda e: e.tensor_tensor(out=sm[:, 5:6], in0=sm[:, 3:4], in1=sm[:, 4:5], op=ALU.subtract), ["s6_sm"], ["s6_sm"])
                P.op("scalar", lambda e: e.activation(out=sm[:, 6:7], in_=sm[:, 5:6], func=AF.Sigmoid), reads=["s6_sm"], writes=["s6_sm"])
                V(lambda e: e.tensor_tensor(out=sm[:, 7:8], in0=sm[:, 6:7], in1=sm[:, 2:3], op=ALU.mult), ["s6_sm"], ["s6_sm"])
                V(lambda e: e.tensor_tensor(out=sm[:, 8:9], in0=sm[:, 2:3], in1=sm[:, 7:8], op=ALU.subtract), ["s6_sm"], ["s6_sm"])
                V(lambda e, tt=tt: e.tensor_scalar(out=G[:, tt, :], in0=oh[:], scalar1=sm[:, 7:8], scalar2=None, op0=ALU.mult), ["s6_oh", "s6_sm"], ["s6_G"])
                V(lambda e: e.tensor_scalar(out=oh[:], in0=mk2[:], scalar1=sm[:, 4:5], scalar2=None, op0=ALU.is_equal), ["s6_mk2", "s6_sm", "s6_G"], ["s6_oh"])
                V(lambda e, tt=tt: e.scalar_tensor_tensor(out=G[:, tt, :], in0=oh[:], scalar=sm[:, 8:9], in1=G[:, tt, :], op0=ALU.mult, op1=ALU.add), ["s6_oh", "s6_sm", "s6_G"], ["s6_G"])
            x1res = [f"s6_x1T{tt}_{h}" for tt in range(8) for h in range(2)]
            for ex in range(32):
                wb, wbn = wgu[nw % 2], f"s6_wgu{nw % 2}"
                wdb, wdn = wd[nw % 2], f"s6_wd{nw % 2}"
                nw += 1
                P.dma("gpsimd", wbn, lambda e, wb=wb, ex=ex: e.dma_start(out=wb[:, :, 0:512], in_=c.w_eg[li, ex].rearrange("(kc kp) f -> kp kc f", kp=128)), writes=[wbn])
                P.dma("gpsimd", wbn, lambda e, wb=wb, ex=ex: e.dma_start(out=wb[:, :, 512:1024], in_=c.w_eu[li, ex].rearrange("(kc kp) f -> kp kc f", kp=128)), reads=[wbn], writes=[wbn])
                P.dma("gpsimd", wdn, lambda e, wdb=wdb, ex=ex: e.dma_start(out=wdb[:], in_=c.w_ed[li, ex].rearrange("(fc fp) n -> fp fc n", fp=128)), writes=[wdn])
                for tc in range(2):
                    hb, hn = hT[(ex * 2 + tc) % 2], f"s6_hT{(ex * 2 + tc) % 2}"
                    for fc in range(4):
                        pg, pgn = k.ps[nps % 8], f"ps{nps % 8}"
                        nps += 1
                        pu, pun = k.ps[nps % 8], f"ps{nps % 8}"
                        nps += 1
                        for kc in range(8):
                            P.op("tensor", lambda e, pg=pg, kc=kc, fc=fc, tc=tc, wb=wb: e.matmul(pg[:, :], lhsT=wb[:, kc, fc * 128:(fc + 1) * 128], rhs=x1T[:, kc, tc * 512:(tc + 1) * 512], start=(kc == 0), stop=(kc == 7)),
                                 reads=[wbn] + x1res[tc * 8:(tc + 1) * 8], writes=[pgn])
                        for kc in range(8):
                            P.op("tensor", lambda e, pu=pu, kc=kc, fc=fc, tc=tc, wb=wb: e.matmul(pu[:, :], lhsT=wb[:, kc, 512 + fc * 128:512 + (fc + 1) * 128], rhs=x1T[:, kc, tc * 512:(tc + 1) * 512], start=(kc == 0), stop=(kc == 7)),
                                 reads=[wbn] + x1res[tc * 8:(tc + 1) * 8], writes=[pun])
                        sg, sgn = sgl[fc % 2], f"s6_sg{fc % 2}"
                        P.op("scalar", lambda e, pg=pg, sg=sg: e.activation(out=sg[:], in_=pg[:, :], func=AF.Silu), reads=[pgn], writes=[sgn])
                        P.op("vector", lambda e, pu=pu, sg=sg, hb=hb, fc=fc: e.tensor_tensor(out=hb[:, fc, :], in0=pu[:, :], in1=sg[:], op=ALU.mult), reads=[pun, sgn], writes=[f"{hn}_{fc}"])
                    for tt4 in range(4):
                        tt = tc * 4 + tt4
                        for nh in range(2):
                            py, pyn = k.ps[nps % 8], f"ps{nps % 8}"
                            nps += 1
                            for fc in range(4):
                                P.op("tensor", lambda e, py=py, fc=fc, tt4=tt4, nh=nh, hb=hb, wdb=wdb: e.matmul(py[:, :], lhsT=hb[:, fc, tt4 * 128:(tt4 + 1) * 128], rhs=wdb[:, fc, nh * 512:(nh + 1) * 512], start=(fc == 0), stop=(fc == 3)),
                                     reads=[wdn, f"{hn}_{fc}"], writes=[pyn])
                            an = f"s6_acc{tt}_{nh}"
                            if ex == 0:
                                P.op("vector", lambda e, py=py, tt=tt, nh=nh, ex=ex: e.tensor_scalar(out=acc[:, tt, nh * 512:(nh + 1) * 512], in0=py[:, :], scalar1=G[:, tt, ex:ex + 1], scalar2=None, op0=ALU.mult),
                                     reads=[pyn, "s6_G"], writes=[an])
                            else:
                                P.op("vector", lambda e, py=py, tt=tt, nh=nh, ex=ex: e.scalar_tensor_tensor(out=acc[:, tt, nh * 512:(nh + 1) * 512], in0=py[:, :], scalar=G[:, tt, ex:ex + 1], in1=acc[:, tt, nh * 512:(nh + 1) * 512], op0=ALU.mult, op1=ALU.add),
                                     reads=[pyn, "s6_G", an], writes=[an])
            for tt in range(8):
                t = qt * 8 + tt
                xb, xn = xin[t % 2], f"s6_xin{t % 2}"
                P.dma("sync", xn, lambda e, xb=xb, t=t: e.dma_start(out=xb[:], in_=c.x1s[t * 128:(t + 1) * 128, :]), writes=[xn])
                P.dma("sync", "s6_pin", lambda e, t=t: e.dma_start(out=pin[:], in_=c.p[li, t * 128:(t + 1) * 128, :]), writes=["s6_pin"])
                pb, pn = k.ps[nps % 8], f"ps{nps % 8}"
                nps += 1
                for q in range(2):
                    P.op("tensor", lambda e, pb=pb, q=q: e.transpose(pb[:, q * 128:(q + 1) * 128], pin[:, q * 128:(q + 1) * 128], k.ident[:]), reads=["s6_pin", "c_ident"], writes=[pn])
                P.op("scalar", lambda e, pb=pb: e.copy(pT[:], pb[:, 0:256].rearrange("p (a b) -> p a b", a=2)), reads=[pn], writes=["s6_pT"])
                zb, zn = z[t % 2], f"s6_z{t % 2}"
                for nh in range(2):
                    pgt, pgtn = k.ps[nps % 8], f"ps{nps % 8}"
                    nps += 1
                    pp, ppn = k.ps[nps % 8], f"ps{nps % 8}"
                    nps += 1
                    for kc in range(8):
                        P.op("tensor", lambda e, pgt=pgt, kc=kc, tt=tt, nh=nh: e.matmul(pgt[:, :], lhsT=x1T[:, kc, tt * 128:(tt + 1) * 128], rhs=wpg[:, kc, nh * 512:(nh + 1) * 512], start=(kc == 0), stop=(kc == 7)),
                             reads=["s6_wpg", f"s6_x1T{tt}_{kc // 4}"], writes=[pgtn])
                    for kc in range(2):
                        P.op("tensor", lambda e, pp=pp, kc=kc, nh=nh: e.matmul(pp[:, :], lhsT=pT[:, kc, :], rhs=wpp[:, kc, nh * 512:(nh + 1) * 512], start=(kc == 0), stop=(kc == 1)),
                             reads=["s6_wpp", "s6_pT"], writes=[ppn])
                    P.op("scalar", lambda e, pgt=pgt, nh=nh: e.activation(out=sgt[:, nh * 512:(nh + 1) * 512], in_=pgt[:, :], func=AF.Sigmoid), reads=[pgtn], writes=[f"s6_sgt{nh}"])
                    P.op("vector", lambda e, pp=pp, nh=nh, zb=zb: e.tensor_tensor(out=zb[:, nh * 512:(nh + 1) * 512], in0=pp[:, :], in1=sgt[:, nh * 512:(nh + 1) * 512], op=ALU.mult),
                         reads=[ppn, f"s6_sgt{nh}"], writes=[zn + f"_{nh}"])
                    P.op("gpsimd", lambda e, nh=nh, zb=zb, tt=tt: e.tensor_tensor(out=zb[:, nh * 512:(nh + 1) * 512], in0=zb[:, nh * 512:(nh + 1) * 512], in1=acc[:, tt, nh * 512:(nh + 1) * 512], op=ALU.add),
                         reads=[zn + f"_{nh}", f"s6_acc{tt}_{nh}"], writes=[zn + f"_{nh}"])
                P.op("vector", lambda e, zb=zb, xb=xb: e.scalar_tensor_tensor(out=zb[:], in0=xb[:], scalar=ALPHA, in1=zb[:], op0=ALU.mult, op1=ALU.add),
                     reads=[xn, zn + "_0", zn + "_1"], writes=[zn])
                ob, on = xo[t % 2], f"s6_xo{t % 2}"
                layer_norm_tile(P, k, zb, zn, gam, bet, stats, mv, rstd, ob, on, eps[:, 0:1], "s6")
                P.dma("sync", "s6_out", lambda e, t=t, ob=ob: e.dma_start(out=dst[t * 128:(t + 1) * 128, :], in_=ob[:]), reads=[on])
    P.barrier()


def stage4_dn(nc, P, k, c, li):
    RS = 128 ** -0.5
    with contextlib.ExitStack() as st:
        da = sbt(nc, st, "d_da", [4, S], F32)
        db = sbt(nc, st, "d_db", [4, S], F32)
        rm = sbt(nc, st, "d_rm", [4, S], F32)
        gc = sbt(nc, st, "d_gc", [4, S], F32)
        par = sbt(nc, st, "d_par", [4, 4], F32)
        P.dma("sync", "d_da", lambda e: e.dma_start(out=da[:], in_=c.smT[8:12, :]), writes=["d_da"])
        P.dma("sync", "d_db", lambda e: e.dma_start(out=db[:], in_=c.smT[12:16, :]), writes=["d_db"])
        P.dma("sync", "d_par", lambda e: e.dma_start(out=par[:, 0:1], in_=c.dn_a_log[li]), writes=["d_par"])
        P.dma("sync", "d_par", lambda e: e.dma_start(out=par[:, 1:2], in_=c.dn_dt_bias[li]), writes=["d_par"])
        P.op("scalar", lambda e: e.activation(out=par[:, 2:3], in_=par[:, 0:1], func=AF.Exp), reads=["d_par"], writes=["d_par2"])
        P.op("vector", lambda e: e.tensor_scalar(out=par[:, 2:3], in0=par[:, 2:3], scalar1=-1.0, scalar2=None, op0=ALU.mult), reads=["d_par2"], writes=["d_par2"])
        P.op("scalar", lambda e: e.activation(out=da[:], in_=da[:], func=AF.Exp, bias=par[:, 1:2], scale=1.0), reads=["d_da", "d_par"], writes=["d_da"])
        P.op("scalar", lambda e: e.activation(out=da[:], in_=da[:], func=AF.Ln, bias=k.ones[0:4, 0:1], scale=1.0), reads=["d_da", "c_ones"], writes=["d_da"])
        P.op("scalar", lambda e: e.activation(out=db[:], in_=db[:], func=AF.Sigmoid), reads=["d_db"], writes=["d_db"])
        P.op("vector", lambda e: e.tensor_scalar(out=da[:], in0=da[:], scalar1=par[:, 2:3], scalar2=None, op0=ALU.mult), reads=["d_da", "d_par2"], writes=["d_da"])
        P.op("gpsimd", lambda e: e.memset(rm[:], 1.0), writes=["d_rm"])
        P.op("gpsimd", lambda e: e.memset(rm[:].rearrange("p (c j) -> p c j", j=64)[:, :, 0:1], 0.0), reads=["d_rm"], writes=["d_rm"])
        P.op("vector", lambda e: e.tensor_tensor_scan(out=gc[:], data0=rm[:], data1=da[:], initial=0.0, op0=ALU.mult, op1=ALU.add), reads=["d_rm", "d_da"], writes=["d_gc"])
        P.dma("sync", "d_rowout", lambda e: e.dma_start(out=c.dnrow[0], in_=db[:]), reads=["d_db"])
        P.dma("sync", "d_rowout", lambda e: e.dma_start(out=c.dnrow[1], in_=gc[:]), reads=["d_gc"])
    P.barrier()
    psb = k.ps[7][:, :].bitcast(BF16)
    psb6 = k.ps[6][:, :].bitcast(BF16)
    with contextlib.ExitStack() as st0:
        cw = sbt(nc, st0, "d_cw", [128, 12, 4], F32)
        nw = sbt(nc, st0, "d_nw", [128, 1], F32)
        eps6 = sbt(nc, st0, "d_eps6", [128, 1], F32)
        with contextlib.ExitStack() as st:
            cwraw = sbt(nc, st, "d_cwraw", [4, 1536], F32)
            P.dma("sync", "d_cwraw", lambda e: e.dma_start(out=cwraw[:], in_=c.dn_conv[li]), writes=["d_cwraw"])
            for idx in range(12):
                P.op("tensor", lambda e, idx=idx: e.transpose(k.ps[0][:, idx * 4:(idx + 1) * 4], cwraw[0:4, idx * 128:(idx + 1) * 128], k.ident[0:4, 0:4]),
                     reads=["d_cwraw", "c_ident"], writes=["ps0"])
            P.op("vector", lambda e: e.tensor_copy(cw[:].rearrange("p a b -> p (a b)"), k.ps[0][:, 0:48]), reads=["ps0"], writes=["d_cw"])
            P.dma("sync", "d_nw", lambda e: e.dma_start(out=nw[:], in_=c.dn_norm_w[li]), writes=["d_nw"])
            P.op("vector", lambda e: e.memset(eps6[:], 1e-6), writes=["d_eps6"])
        P.barrier()
        def do_head(h):
            with contextlib.ExitStack() as sth:
                A = lambda n, s, d: sbt(nc, sth, n, s, d)
                kT = A("d_kT", [128, S], BF16)
                qT = A("d_qT", [128, S], BF16)
                qdT = A("d_qdT", [128, S], BF16)
                vb_tm = A("d_vb", [128, NT, 128], BF16)
                kbg_tm = A("d_kbg", [128, NT, 128], BF16)
                kdec_tm = A("d_kdec", [128, NT, 128], BF16)
                AT = A("d_AT", [128, NT, 128], BF16)
                gcB = A("d_gcB", [128, S], F32)
                gc_col = A("d_gccol", [128, NT], F32)
                b_col = A("d_bcol", [128, NT], F32)
                glc = A("d_glc", [128, NT], F32)
                col_bg = A("d_colbg", [128, NT], F32)
                col_edd = A("d_coledd", [128, NT], F32)
                negb = A("d_negb", [128, NT], F32)
                neggc = A("d_neggc", [128, NT], F32)
                eglB = A("d_eglB", [128, 64], F32)
                browh = c.dnrow[0, h]
                gcrowh = c.dnrow[1, h]
                P.dma("sync", "d_cols", lambda e: e.dma_start(out=gc_col[:], in_=gcrowh.rearrange("(t p) -> p t", p=128), allow_slow_non_contiguous=True), writes=["d_gccol"])
                P.dma("sync", "d_cols", lambda e: e.dma_start(out=b_col[:], in_=browh.rearrange("(t p) -> p t", p=128), allow_slow_non_contiguous=True), writes=["d_bcol"])
                gsrc = gcrowh.rearrange("(t two j) -> two j t", two=2, j=64)
                for half in range(2):
                    P.dma("sync", "d_cols", lambda e, half=half: e.dma_start(out=glc[half * 64:(half + 1) * 64, :], in_=gsrc[half, 63, :].partition_broadcast(64), allow_slow_non_contiguous=True), writes=["d_glc"])
                P.dma("sync", "d_cols", lambda e: e.dma_start(out=eglB[:], in_=gcrowh.rearrange("(c j) -> j c", j=64)[63, :].partition_broadcast(128), allow_slow_non_contiguous=True), writes=["d_eglB"])
                P.dma("sync", "d_gcB", lambda e: e.dma_start(out=gcB[:], in_=gcrowh.partition_broadcast(128)), writes=["d_gcB"])
                P.op("scalar", lambda e: e.activation(out=eglB[:], in_=eglB[:], func=AF.Exp), reads=["d_eglB"], writes=["d_eglB"])
                P.op("scalar", lambda e: e.activation(out=col_bg[:], in_=gc_col[:], func=AF.Exp), reads=["d_gccol"], writes=["d_colbg"])
                P.op("vector", lambda e: e.tensor_tensor(out=col_bg[:], in0=col_bg[:], in1=b_col[:], op=ALU.mult), reads=["d_colbg", "d_bcol"], writes=["d_colbg"])
                P.op("vector", lambda e: e.tensor_tensor(out=col_edd[:], in0=glc[:], in1=gc_col[:], op=ALU.subtract), reads=["d_glc", "d_gccol"], writes=["d_coledd"])
                P.op("scalar", lambda e: e.activation(out=col_edd[:], in_=col_edd[:], func=AF.Exp), reads=["d_coledd"], writes=["d_coledd"])
                P.op("vector", lambda e: e.tensor_scalar(out=negb[:], in0=b_col[:], scalar1=-1.0, scalar2=None, op0=ALU.mult), reads=["d_bcol"], writes=["d_negb"])
                P.op("vector", lambda e: e.tensor_scalar(out=neggc[:], in0=gc_col[:], scalar1=-1.0, scalar2=None, op0=ALU.mult), reads=["d_gccol"], writes=["d_neggc"])
                if "dn_stop0" in DBG_FLAGS:
                    P.barrier()
                    return
                with contextlib.ExitStack() as st:
                    u = [sbt(nc, st, f"d_u{i}", [128, S], F32) for i in range(2)]
                    acc = sbt(nc, st, "d_acc", [128, S], F32)
                    sch = [sbt(nc, st, f"d_sch{i}", [128, 512], F32) for i in range(4)]
                    sqc = [sbt(nc, st, f"d_sqc{i}", [128, 512], F32) for i in range(4)]
                    rn = [sbt(nc, st, f"d_rn{i}", [128, 512], F32) for i in range(4)]
                    vTb = sbt(nc, st, "d_vTb", [128, S], BF16)
                    if h == 0 and "dn_dbg" in DBG_FLAGS:
                        print("sbuf remaining in dn phase A:", nc.sbuf_bytes_remaining)
                        for nm_, t_ in [("kT", kT), ("qT", qT), ("qdT", qdT), ("vb", vb_tm), ("kbg", kbg_tm), ("kdec", kdec_tm), ("AT", AT), ("gcB", gcB), ("gc_col", gc_col), ("eglB", eglB),
                                        ("u0", u[0]), ("u1", u[1]), ("acc", acc), ("vTb", vTb), ("cw", cw), ("ones", k.ones)]:
                            m_ = nc.lookup_mloc(t_)
                            print("   ", nm_, m_.addr, list(m_.dims))
                    nps = 0
                    for wi, which in enumerate(("q", "k", "v")):
                        idx = wi * 4 + h
                        ub_, un = u[wi % 2], f"d_u{wi % 2}"
                        P.dma("sync", un, lambda e, ub_=ub_, idx=idx: e.dma_start(out=ub_[:], in_=c.fT[512 + idx * 128:512 + (idx + 1) * 128, :]), writes=[un])
                        P.op("vector", lambda e, ub_=ub_, idx=idx: e.tensor_scalar(out=acc[:], in0=ub_[:], scalar1=cw[:, idx, 3:4], scalar2=None, op0=ALU.mult), reads=[un, "d_cw"], writes=["d_acc"])
                        for sh, j in ((1, 2), (2, 1), (3, 0)):
                            P.op("vector", lambda e, ub_=ub_, idx=idx, sh=sh, j=j: e.scalar_tensor_tensor(out=acc[:, sh:], in0=ub_[:, 0:S - sh], scalar=cw[:, idx, j:j + 1], in1=acc[:, sh:], op0=ALU.mult, op1=ALU.add),
                                 reads=[un, "d_cw", "d_acc"], writes=["d_acc"])
                        dstT, dn_ = (qT, "d_qT") if which == "q" else ((kT, "d_kT") if which == "k" else (vTb, "d_vTb"))
                        for tc in range(8):
                            cs = slice(tc * 512, (tc + 1) * 512)
                            if which == "v":
                                P.op("scalar", lambda e, cs=cs: e.activation(out=vTb[:, cs], in_=acc[:, cs], func=AF.Silu), reads=["d_acc"], writes=["d_vTb"])
                                continue
                            pb, pn = k.ps[nps % 4], f"ps{nps % 4}"
                            rb, rbn = rn[nps % 4], f"d_rn{nps % 4}"
                            sc_, scn = sch[nps % 4], f"d_sch{nps % 4}"
                            sq_, sqn = sqc[nps % 4], f"d_sqc{nps % 4}"
                            nps += 1
                            P.op("scalar", lambda e, cs=cs, sc_=sc_: e.activation(out=sc_[:], in_=acc[:, cs], func=AF.Silu), reads=["d_acc"], writes=[scn])
                            P.op("vector", lambda e, sc_=sc_, sq_=sq_: e.tensor_tensor(out=sq_[:], in0=sc_[:], in1=sc_[:], op=ALU.mult), reads=[scn], writes=[sqn])
                            P.op("tensor", lambda e, pb=pb, sq_=sq_: e.matmul(pb[:, :], lhsT=k.ones[:], rhs=sq_[:], start=True, stop=True), reads=["c_ones", sqn], writes=[pn])
                            P.op("scalar", lambda e, pb=pb, rb=rb: e.activation(out=rb[:], in_=pb[:, :], func=AF.Sqrt, bias=eps6[:, 0:1], scale=1.0), reads=[pn, "d_eps6"], writes=[rbn])
                            P.op("vector", lambda e, rb=rb: e.reciprocal(rb[:], rb[:]), reads=[rbn], writes=[rbn])
                            sc = RS if which == "q" else 1.0
                            P.op("vector", lambda e, rb=rb, cs=cs, dstT=dstT, sc=sc, sc_=sc_: e.scalar_tensor_tensor(out=dstT[:, cs], in0=sc_[:], scalar=sc, in1=rb[:], op0=ALU.mult, op1=ALU.mult),
                                 reads=[scn, rbn], writes=[dn_])
                    for t in range(NT):
                        cs = slice(t * 128, (t + 1) * 128)
                        pq = psb if t % 2 == 0 else psb6
                        pqn = "ps7" if t % 2 == 0 else "ps6"
                        P.op("tensor", lambda e, cs=cs, pq=pq: e.transpose(pq[:, 0:128], kT[:, cs], k.identb[:]), reads=["d_kT", "c_identb"], writes=[pqn])
                        P.op("tensor", lambda e, cs=cs, pq=pq: e.transpose(pq[:, 128:256], vTb[:, cs], k.identb[:]), reads=["d_vTb", "c_identb"], writes=[pqn])
                        P.op("vector", lambda e, t=t, pq=pq: e.tensor_scalar(out=kbg_tm[:, t, :], in0=pq[:, 0:128], scalar1=col_bg[:, t:t + 1], scalar2=None, op0=ALU.mult),
                             reads=[pqn, "d_colbg"], writes=["d_kbg"])
                        P.op("vector", lambda e, t=t, pq=pq: e.tensor_scalar(out=kdec_tm[:, t, :], in0=pq[:, 0:128], scalar1=col_edd[:, t:t + 1], scalar2=None, op0=ALU.mult),
                             reads=[pqn, "d_coledd"], writes=["d_kdec"])
                        P.op("vector", lambda e, t=t, pq=pq: e.tensor_scalar(out=vb_tm[:, t, :], in0=pq[:, 128:256], scalar1=b_col[:, t:t + 1], scalar2=None, op0=ALU.mult),
                             reads=[pqn, "d_bcol"], writes=["d_vb"])
                    if "dn_dbg" in DBG_FLAGS and h == 0:
                        P.dma("gpsimd", "dbgo", lambda e: e.dma_start(out=c.dbg1[:, 2432:2560], in_=u[1][:, 2048:2176]), reads=["d_u1"])
                        P.dma("gpsimd", "dbgo", lambda e: e.dma_start(out=c.dbg1[:, 2560:2688], in_=acc[:, 2048:2176]), reads=["d_acc"])
                        P.dma("gpsimd", "dbgo", lambda e: e.dma_start(out=c.dbg1[:, 2688:2816], in_=vTb[:, 2048:2176]), reads=["d_vTb"])
                    for tc in range(8):
                        cs = slice(tc * 512, (tc + 1) * 512)
                        sc_, scn = sch[tc % 2], f"d_sch{tc % 2}"
                        P.op("scalar", lambda e, cs=cs, sc_=sc_: e.activation(out=sc_[:], in_=gcB[:, cs], func=AF.Exp), reads=["d_gcB"], writes=[scn])
                        P.op("vector", lambda e, cs=cs, sc_=sc_: e.tensor_tensor(out=qdT[:, cs], in0=qT[:, cs], in1=sc_[:], op=ALU.mult), reads=["d_qT", scn], writes=["d_qdT"])
                P.barrier()
                if "dn_stopA" in DBG_FLAGS:
                    return
                uin = A("d_uin", [128, NT, 128], F32)
                WT = A("d_WT", [128, S], BF16)
                oT = A("d_oT", [128, S], F32)
                with contextlib.ExitStack() as st:
                    Dm = [sbt(nc, st, f"d_Dm{i}", [128, 128], F32) for i in range(2)]
                    DTm = [sbt(nc, st, f"d_DTm{i}", [128, 128], F32) for i in range(2)]
                    Nf = [sbt(nc, st, f"d_Nf{i}", [128, 128], F32) for i in range(2)]
                    ATf = [sbt(nc, st, f"d_ATf{i}", [128, 128], F32) for i in range(2)]
                    Nl = [sbt(nc, st, f"d_Nl{i}", [128, 4, 128], BF16) for i in range(2)]
                    Ml = [sbt(nc, st, f"d_Ml{i}", [128, 4, 128], BF16) for i in range(2)]
                    Pl = [sbt(nc, st, f"d_Pl{i}", [128, 4, 128], BF16) for i in range(2)]
                    for grp in range(NT // 4):
                        for tt in range(4):
                            t = grp * 4 + tt
                            cs = slice(t * 128, (t + 1) * 128)
                            i2 = t % 2
                            pkk, pkkn = k.ps[i2], f"ps{i2}"
                            pqk, pqkn = k.ps[2 + i2], f"ps{2 + i2}"
                            P.op("tensor", lambda e, pkk=pkk, cs=cs: e.matmul(pkk[:, 0:128], lhsT=kT[:, cs], rhs=kT[:, cs], start=True, stop=True), reads=["d_kT"], writes=[pkkn])
                            P.op("tensor", lambda e, pqk=pqk, cs=cs: e.matmul(pqk[:, 0:128], lhsT=kT[:, cs], rhs=qT[:, cs], start=True, stop=True), reads=["d_kT", "d_qT"], writes=[pqkn])
                            P.op("scalar", lambda e, cs=cs, t=t, i2=i2: e.activation(out=Dm[i2][:], in_=gcB[:, cs], func=AF.Exp, bias=gc_col[:, t:t + 1], scale=-1.0), reads=["d_gcB", "d_gccol"], writes=[f"d_Dm{i2}"])
                            P.op("scalar", lambda e, cs=cs, t=t, i2=i2: e.activation(out=DTm[i2][:], in_=gcB[:, cs], func=AF.Exp, bias=neggc[:, t:t + 1], scale=1.0), reads=["d_gcB", "d_neggc"], writes=[f"d_DTm{i2}"])
                            P.op("vector", lambda e, pkk=pkk, t=t, i2=i2: e.scalar_tensor_tensor(out=Nf[i2][:], in0=pkk[:, 0:128], scalar=negb[:, t:t + 1], in1=Dm[i2][:], op0=ALU.mult, op1=ALU.mult),
                                 reads=[pkkn, "d_negb", f"d_Dm{i2}"], writes=[f"d_Nf{i2}"])
                            P.op("vector", lambda e, pqk=pqk, i2=i2: e.tensor_tensor(out=ATf[i2][:], in0=pqk[:, 0:128], in1=DTm[i2][:], op=ALU.mult),
                                 reads=[pqkn, f"d_DTm{i2}"], writes=[f"d_ATf{i2}"])
                            P.op("gpsimd", lambda e, tt=tt, i2=i2: e.affine_select(out=Nl[0][:, tt, :], in_=Nf[i2][:], pattern=[[-1, 128]], compare_op=ALU.is_gt, fill=0.0, base=0, channel_multiplier=1),
                                 reads=[f"d_Nf{i2}"], writes=[f"d_N0_{tt}"])
                            P.op("gpsimd", lambda e, tt=tt: e.memset(Nl[0][64:128, tt, 0:64], 0.0), reads=[f"d_N0_{tt}"], writes=[f"d_N0_{tt}"])
                            P.op("gpsimd", lambda e, t=t, i2=i2: e.affine_select(out=AT[:, t, :], in_=ATf[i2][:], pattern=[[1, 128]], compare_op=ALU.is_ge, fill=0.0, base=0, channel_multiplier=-1),
                                 reads=[f"d_ATf{i2}"], writes=["d_AT"])
                            P.op("gpsimd", lambda e, t=t: e.memset(AT[0:64, t, 64:128], 0.0), reads=["d_AT"], writes=["d_AT"])
                            P.op("tensor", lambda e, tt=tt: e.transpose(psb[:, tt * 128:(tt + 1) * 128], Nl[0][:, tt, :], k.identb[:]), reads=[f"d_N0_{tt}", "c_identb"], writes=["ps7"])
                        P.op("scalar", lambda e: e.copy(Ml[0][:].rearrange("p a b -> p (a b)"), psb[:, 0:512]), reads=["ps7"], writes=["d_M0"])
                        for tt in range(4):
                            P.op("vector", lambda e, tt=tt: e.tensor_tensor(out=Pl[0][:, tt, :], in0=Ml[0][:, tt, :], in1=k.identb[:], op=ALU.add), reads=["d_M0", "c_identb"], writes=[f"d_P0_{tt}"])
                        Nres = [f"d_N0_{tt}" for tt in range(4)]
                        Mres = ["d_M0"]
                        Pres = [f"d_P0_{tt}" for tt in range(4)]
                        cur = 0
                        for lvl in range(1, 6):
                            nxt = 1 - cur
                            for tt in range(4):
                                P.op("tensor", lambda e, tt=tt, cur=cur: e.matmul(k.ps[4][:, tt * 128:(tt + 1) * 128], lhsT=Ml[cur][:, tt, :], rhs=Nl[cur][:, tt, :], start=True, stop=True),
                                     reads=Nres + Mres, writes=["ps4"])
                            if lvl < 5:
                                for tt in range(4):
                                    P.op("tensor", lambda e, tt=tt, cur=cur: e.matmul(k.ps[5][:, tt * 128:(tt + 1) * 128], lhsT=Nl[cur][:, tt, :], rhs=Ml[cur][:, tt, :], start=True, stop=True),
                                         reads=Nres + Mres, writes=["ps5"])
                            P.op("scalar", lambda e, nxt=nxt: e.copy(Nl[nxt][:].rearrange("p a b -> p (a b)"), k.ps[4][:, :]), reads=["ps4"], writes=[f"d_Nn{nxt}"])
                            if lvl < 5:
                                P.op("vector", lambda e, nxt=nxt: e.tensor_copy(Ml[nxt][:].rearrange("p a b -> p (a b)"), k.ps[5][:, :]), reads=["ps5"], writes=[f"d_Mn{nxt}"])
                            for tt in range(4):
                                P.op("tensor", lambda e, tt=tt, cur=cur, nxt=nxt: e.matmul(k.ps[6][:, tt * 128:(tt + 1) * 128], lhsT=Nl[nxt][:, tt, :], rhs=Pl[cur][:, tt, :], start=True, stop=True),
                                     reads=[f"d_Nn{nxt}"] + Pres, writes=["ps6"])
                            P.op("vector", lambda e, cur=cur, nxt=nxt: e.tensor_tensor(out=Pl[nxt][:].rearrange("p a b -> p (a b)"), in0=k.ps[6][:, :], in1=Pl[cur][:].rearrange("p a b -> p (a b)"), op=ALU.add),
                                 reads=["ps6"] + Pres, writes=[f"d_Pn{nxt}"])
                            Nres = [f"d_Nn{nxt}"]
                            Mres = [f"d_Mn{nxt}"]
                            Pres = [f"d_Pn{nxt}"]
                            cur = nxt
                        for tt in range(4):
                            t = grp * 4 + tt
                            P.op("tensor", lambda e, tt=tt, t=t, cur=cur: e.matmul(k.ps[4][:, tt * 128:(tt + 1) * 128], lhsT=Pl[cur][:, tt, :], rhs=vb_tm[:, t, :], start=True, stop=True),
                                 reads=Pres + ["d_vb"], writes=["ps4"])
                            P.op("tensor", lambda e, tt=tt, t=t, cur=cur: e.matmul(k.ps[5][:, tt * 128:(tt + 1) * 128], lhsT=kbg_tm[:, t, :], rhs=Pl[cur][:, tt, :], start=True, stop=True),
                                 reads=Pres + ["d_kbg"], writes=["ps5"])
                        P.op("vector", lambda e, grp=grp: e.tensor_copy(uin[:, grp * 4:(grp + 1) * 4, :].rearrange("p a b -> p (a b)"), k.ps[4][:, :]), reads=["ps4"], writes=["d_uin"])
                        P.op("scalar", lambda e, grp=grp: e.copy(WT[:, grp * 512:(grp + 1) * 512], k.ps[5][:, :]), reads=["ps5"], writes=["d_WT"])
                P.barrier()
                if "dn_stopC" in DBG_FLAGS:
                    return
                with contextlib.ExitStack() as st:
                    S32 = sbt(nc, st, "d_S32", [128, 128], F32)
                    Sb = sbt(nc, st, "d_Sb", [128, 128], BF16)
                    ub = [sbt(nc, st, f"d_ub{i}", [128, 128], BF16) for i in range(2)]
                    P.op("gpsimd", lambda e: e.memset(S32[:], 0.0), writes=["d_S32"])
                    P.op("gpsimd", lambda e: e.memset(Sb[:], 0.0), writes=["d_Sb"])
                    for ci in range(64):
                        t, hb = ci // 2, ci % 2
                        r0 = hb * 64
                        cc = slice(ci * 64, (ci + 1) * 64)
                        ubb, ubn = ub[ci % 2], f"d_ub{ci % 2}"
                        po, pon = k.ps[1 + (ci // 8) % 2], f"ps{1 + (ci // 8) % 2}"
                        oc = slice((ci % 8) * 64, (ci % 8 + 1) * 64)
                        P.op("tensor", lambda e, r0=r0, cc=cc: e.matmul(k.ps[0][r0:r0 + 64, 0:128], lhsT=WT[:, cc], rhs=Sb[:], start=True, stop=True), reads=["d_WT", "d_Sb"], writes=["ps0"])
                        P.op("vector", lambda e, r0=r0, t=t, ubb=ubb: e.tensor_tensor(out=ubb[r0:r0 + 64, :], in0=uin[r0:r0 + 64, t, :], in1=k.ps[0][r0:r0 + 64, 0:128], op=ALU.subtract),
                             reads=["ps0", "d_uin"], writes=[ubn])
                        P.op("tensor", lambda e, po=po, oc=oc, cc=cc: e.matmul(po[:, oc], lhsT=Sb[:], rhs=qdT[:, cc], start=True, stop=False), reads=["d_Sb", "d_qdT"], writes=[pon])
                        P.op("tensor", lambda e, po=po, oc=oc, r0=r0, t=t, ubb=ubb: e.matmul(po[:, oc], lhsT=ubb[r0:r0 + 64, :], rhs=AT[r0:r0 + 64, t, r0:r0 + 64], start=False, stop=True),
                             reads=[ubn, "d_AT"], writes=[pon])
                        P.op("tensor", lambda e, r0=r0, t=t, ubb=ubb: e.matmul(k.ps[3][:, 0:128], lhsT=kdec_tm[r0:r0 + 64, t, :], rhs=ubb[r0:r0 + 64, :], start=True, stop=True),
                             reads=[ubn, "d_kdec"], writes=["ps3"])
                        P.op("vector", lambda e, ci=ci: e.scalar_tensor_tensor(out=Sb[:], in0=S32[:], scalar=eglB[:, ci:ci + 1], in1=k.ps[3][:, 0:128], op0=ALU.mult, op1=ALU.add),
                             reads=["ps3", "d_S32", "d_eglB"], writes=["d_Sb"])
                        P.op("vector", lambda e, ci=ci: e.scalar_tensor_tensor(out=S32[:], in0=S32[:], scalar=eglB[:, ci:ci + 1], in1=k.ps[3][:, 0:128], op0=ALU.mult, op1=ALU.add),
                             reads=["ps3", "d_S32", "d_eglB"], writes=["d_S32"])
                        if ci % 8 == 7:
                            g8 = ci // 8
                            P.op("scalar", lambda e, po=po, g8=g8: e.copy(oT[:, g8 * 512:(g8 + 1) * 512], po[:, :]), reads=[pon], writes=["d_oT"])
                P.barrier()
                if "dn_dbg" in DBG_FLAGS and h == 0:
                    dl = [(kT[:, 0:128], 0), (qT[:, 0:128], 128), (gc_col[:], 256), (b_col[:], 288), (AT[:, 0, :], 320), (uin[:, 0, :], 448),
                          (WT[:, 0:128], 576), (oT[:, 0:128], 704), (vb_tm[:, 0, :], 832), (kbg_tm[:, 0, :], 960), (kdec_tm[:, 0, :], 1088), (qdT[:, 0:128], 1216), (eglB[:], 1344), (gcB[:, 2016:2144], 1408), (kT[:, 2048:2176], 1536), (qdT[:, 2048:2176], 1664), (AT[:, 16, :], 1792), (uin[:, 16, :], 1920), (WT[:, 2048:2176], 2048), (oT[:, 2048:2176], 2176), (kdec_tm[:, 16, :], 2304)]
                    for (ap_, o_) in dl:
                        P.dma("gpsimd", "dbgo", lambda e, ap_=ap_, o_=o_: e.dma_start(out=c.dbg1[:, o_:o_ + ap_.shape[-1]], in_=ap_), reads=[])
                    P.barrier()
                with contextlib.ExitStack() as st:
                    sq = sbt(nc, st, "d_sq2", [128, S], F32)
                    dgt = sbt(nc, st, "d_dgt", [128, S], F32)
                    dgs = sbt(nc, st, "d_dgs", [128, S], BF16)
                    rn = [sbt(nc, st, f"d_rn2{i}", [128, 512], F32) for i in range(2)]
                    yst = sbt(nc, st, "d_yst", [128, S], BF16)
                    P.dma("sync", "d_dgt", lambda e: e.dma_start(out=dgt[:], in_=c.fT[2048 + h * 128:2048 + (h + 1) * 128, :]), writes=["d_dgt"])
                    for tc in range(8):
                        P.op("scalar", lambda e, tc=tc: e.activation(out=dgs[:, tc * 512:(tc + 1) * 512], in_=dgt[:, tc * 512:(tc + 1) * 512], func=AF.Silu), reads=["d_dgt"], writes=["d_dgs"])
                    P.op("vector", lambda e: e.tensor_tensor(out=sq[:], in0=oT[:], in1=oT[:], op=ALU.mult), reads=["d_oT"], writes=["d_sq2"])
                    for tc in range(8):
                        pb, pn = k.ps[tc % 4], f"ps{tc % 4}"
                        rb, rbn = rn[tc % 2], f"d_rn2{tc % 2}"
                        cs = slice(tc * 512, (tc + 1) * 512)
                        P.op("tensor", lambda e, pb=pb, cs=cs: e.matmul(pb[:, :], lhsT=k.ones[:], rhs=sq[:, cs], start=True, stop=True), reads=["c_ones", "d_sq2"], writes=[pn])
                        P.op("scalar", lambda e, pb=pb, rb=rb: e.activation(out=rb[:], in_=pb[:, :], func=AF.Sqrt, bias=eps6[:, 0:1], scale=1.0 / 128.0), reads=[pn, "d_eps6"], writes=[rbn])
                        P.op("vector", lambda e, rb=rb: e.reciprocal(rb[:], rb[:]), reads=[rbn], writes=[rbn])
                        P.op("vector", lambda e, rb=rb, cs=cs: e.scalar_tensor_tensor(out=oT[:, cs], in0=oT[:, cs], scalar=nw[:, 0:1], in1=rb[:], op0=ALU.mult, op1=ALU.mult),
                             reads=["d_oT", "d_nw", rbn], writes=[f"d_oTn{tc}"])
                        P.op("gpsimd", lambda e, cs=cs: e.tensor_tensor(out=yst[:, cs], in0=oT[:, cs], in1=dgs[:, cs], op=ALU.mult), reads=[f"d_oTn{tc}", "d_dgs"], writes=[f"d_yst{tc}"])
                    P.dma("sync", "d_yout", lambda e: e.dma_start(out=c.yT[1024 + h * 128:1024 + (h + 1) * 128, :], in_=yst[:]), reads=[f"d_yst{tc}" for tc in range(8)])
            P.barrier()
        for h in range(4):
            do_head(h)
    P.barrier()


MOE_C = 512


def stage6_moe_sparse(nc, P, k, c, li, dst):
    C = MOE_C
    NB = C // 128
    psb6 = k.ps[6][:, :].bitcast(BF16)
    psb7 = k.ps[7][:, :].bitcast(BF16)
    with contextlib.ExitStack() as st0:
        gam = sbt(nc, st0, "s6_gam", [128, D], F32)
        bet = sbt(nc, st0, "s6_bet", [128, D], F32)
        eps = sbt(nc, st0, "s6_eps", [128, 1], F32)
        P.dma("sync", "lnp", lambda e: e.dma_start(out=gam[:], in_=c.ln2_g[li].partition_broadcast(128)), writes=["lnp"])
        P.dma("sync", "lnp", lambda e: e.dma_start(out=bet[:], in_=c.ln2_b[li].partition_broadcast(128)), writes=["lnp"])
        P.op("vector", lambda e: e.memset(eps[:], 1e-5), writes=["s6_eps"])
        with contextlib.ExitStack() as st:
            wr = sbt(nc, st, "s6_wr", [128, 8, 36], F32)
            brb = sbt(nc, st, "s6_brb", [128, 36], F32)
            xin = [sbt(nc, st, f"s6_xin{i}", [128, D], F32) for i in range(2)]
            xb16 = [sbt(nc, st, f"s6_xb{i}", [128, D], BF16) for i in range(2)]
            xTf = sbt(nc, st, "s6_xTf", [128, 8, 128], F32)
            LG = sbt(nc, st, "s6_LG", [128, NT, 36], F32)
            mg = sbt(nc, st, "s6_mg", [128, NT], F32)
            ohg = sbt(nc, st, "s6_ohg", [128, NT, 4], F32)
            e4 = sbt(nc, st, "s6_e4", [128, NT, 4], F32)
            pgp = sbt(nc, st, "s6_pgp", [128, NT], F32)
            MK = sbt(nc, st, "s6_MK", [128, NT, 32], F32)
            MK2 = sbt(nc, st, "s6_MK2", [128, NT, 32], F32)
            m1 = sbt(nc, st, "s6_m1", [128, NT], F32)
            m2 = sbt(nc, st, "s6_m2", [128, NT], F32)
            OH1 = sbt(nc, st, "s6_OH1", [128, NT, 32], F32)
            OH2 = sbt(nc, st, "s6_OH2", [128, NT, 32], F32)
            Gt = sbt(nc, st, "s6_Gt", [128, NT, 2], F32)
            sm = sbt(nc, st, "s6_sm", [128, 16], F32)
            t4 = sbt(nc, st, "s6_t4", [128, 4], F32)
            pen = sbt(nc, st, "s6_pen", [128, NT, 4], F32)
            P.dma("sync", "s6_wr", lambda e: e.dma_start(out=wr[:], in_=c.w_r[li].rearrange("(kc kp) n -> kp kc n", kp=128)), writes=["s6_wr"])
            P.dma("sync", "s6_brb", lambda e: e.dma_start(out=brb[:], in_=c.b_r[li].partition_broadcast(128)), writes=["s6_brb"])
            nps = 0
            V = lambda fn, r, w: P.op("vector", fn, reads=r, writes=w)
            for t in range(NT):
                xb, xn = xin[t % 2], f"s6_xin{t % 2}"
                P.dma("sync", xn, lambda e, xb=xb, t=t: e.dma_start(out=xb[:], in_=c.x1s[t * 128:(t + 1) * 128, :]), writes=[xn])
                x16, x16n = xb16[t % 2], f"s6_xb{t % 2}"
                P.op("gpsimd", lambda e, xb=xb, x16=x16: e.tensor_copy(x16[:], xb[:]), reads=[xn], writes=[x16n])
                P.dma("sync", "s6_x1bout", lambda e, x16=x16, t=t: e.dma_start(out=c.x1b[t * 128:(t + 1) * 128, :], in_=x16[:]), reads=[x16n], writes=[f"d_x1b{t}"])
                for half in range(2):
                    pb, pn = k.ps[nps % 4], f"ps{nps % 4}"
                    nps += 1
                    for q in range(4):
                        kc = half * 4 + q
                        P.op("tensor", lambda e, pb=pb, xb=xb, kc=kc, q=q: e.transpose(pb[:, q * 128:(q + 1) * 128], xb[:, kc * 128:(kc + 1) * 128], k.ident[:]),
                             reads=[xn, "c_ident"], writes=[pn])
                    P.op("scalar", lambda e, pb=pb, half=half: e.copy(xTf[:, half * 4:(half + 1) * 4, :], pb[:].rearrange("p (a b) -> p a b", a=4)),
                         reads=[pn], writes=[f"s6_xTf{half}"])
                pr, prn = k.ps[4 + t % 2], f"ps{4 + t % 2}"
                for kc in range(8):
                    P.op("tensor", lambda e, pr=pr, kc=kc: e.matmul(pr[:, 0:36], lhsT=xTf[:, kc, :], rhs=wr[:, kc, :], start=(kc == 0), stop=(kc == 7)),
                         reads=[f"s6_xTf{kc // 4}", "s6_wr"], writes=[prn])
                V(lambda e, pr=pr, t=t: e.tensor_tensor(out=LG[:, t, :], in0=pr[:, 0:36], in1=brb[:], op=ALU.add), [prn, "s6_brb"], ["s6_LG"])
            bc = lambda a, n: a.unsqueeze(2).to_broadcast([128, NT, n])
            LGg = LG[:, :, 0:4]
            V(lambda e: e.reduce_max(out=mg[:], in_=LGg, axis=AX.X), ["s6_LG"], ["s6_mg"])
            V(lambda e: e.tensor_tensor(out=ohg[:], in0=LGg, in1=bc(mg[:, :], 4), op=ALU.is_equal), ["s6_LG", "s6_mg"], ["s6_ohg"])
            V(lambda e: e.tensor_scalar(out=pen[:], in0=ohg[:], scalar1=-1.0, scalar2=1e30, op0=ALU.add, op1=ALU.mult), ["s6_ohg"], ["s6_pen"])
            V(lambda e: e.tensor_tensor(out=e4[:], in0=LGg, in1=bc(mg[:, :], 4), op=ALU.subtract), ["s6_LG", "s6_mg"], ["s6_e4"])
            P.op("scalar", lambda e: e.activation(out=e4[:], in_=e4[:], func=AF.Exp), reads=["s6_e4"], writes=["s6_e4"])
            V(lambda e: e.reduce_sum(out=pgp[:], in_=e4[:], axis=AX.X), ["s6_e4"], ["s6_pgp"])
            V(lambda e: e.reciprocal(pgp[:], pgp[:]), ["s6_pgp"], ["s6_pgp"])
            for g in range(4):
                V(lambda e, g=g: e.tensor_tensor(out=MK[:, :, g * 8:(g + 1) * 8], in0=LG[:, :, 4 + g * 8:4 + (g + 1) * 8], in1=pen[:, :, g:g + 1].to_broadcast([128, NT, 8]), op=ALU.add),
                  ["s6_LG", "s6_pen"], [f"s6_MK{g}"])
            mkres = [f"s6_MK{g}" for g in range(4)]
            V(lambda e: e.reduce_max(out=m1[:], in_=MK[:], axis=AX.X), mkres, ["s6_m1"])
            V(lambda e: e.tensor_tensor(out=OH1[:], in0=MK[:], in1=bc(m1[:, :], 32), op=ALU.is_equal), mkres + ["s6_m1"], ["s6_OH1"])
            V(lambda e: e.scalar_tensor_tensor(out=MK2[:], in0=OH1[:], scalar=-1e30, in1=MK[:], op0=ALU.mult, op1=ALU.add), mkres + ["s6_OH1"], ["s6_MK2"])
            V(lambda e: e.reduce_max(out=m2[:], in_=MK2[:], axis=AX.X), ["s6_MK2"], ["s6_m2"])
            V(lambda e: e.tensor_tensor(out=OH2[:], in0=MK2[:], in1=bc(m2[:, :], 32), op=ALU.is_equal), ["s6_MK2", "s6_m2"], ["s6_OH2"])
            V(lambda e: e.tensor_tensor(out=m1[:], in0=m1[:], in1=m2[:], op=ALU.subtract), ["s6_m1", "s6_m2", "s6_OH1"], ["s6_m1"])
            P.op("scalar", lambda e: e.activation(out=m1[:], in_=m1[:], func=AF.Sigmoid), reads=["s6_m1"], writes=["s6_m1"])
            V(lambda e: e.tensor_tensor(out=Gt[:, :, 0], in0=m1[:], in1=pgp[:], op=ALU.mult), ["s6_m1", "s6_pgp"], ["s6_Gt"])
            V(lambda e: e.tensor_tensor(out=Gt[:, :, 1], in0=pgp[:], in1=Gt[:, :, 0], op=ALU.subtract), ["s6_pgp", "s6_Gt"], ["s6_Gt"])
            A16 = sbt(nc, st, "s6_A16", [128, NT * 32], BF16)
            Tri = sbt(nc, st, "s6_Tri", [128, 128], BF16)
            INC = sbt(nc, st, "s6_INC", [128, NT, 32], F32)
            X = [sbt(nc, st, f"s6_X{i}", [128, NT, 32], F32) for i in range(2)]
            TB = sbt(nc, st, "s6_TB", [128, NT, 32], F32)
            ECf = sbt(nc, st, "s6_ECf", [128, NT, 32], F32)
            tmp3 = sbt(nc, st, "s6_tmp3", [128, NT, 32], F32)
            Sf = sbt(nc, st, "s6_Sf", [128, 2, NT], F32)
            Si = sbt(nc, st, "s6_Si", [128, 2, NT], I32)
            REC = sbt(nc, st, "s6_REC", [128, NT, 2, 16], I32)
            INIT = sbt(nc, st, "s6_INIT", [128, 32 * C // 128, 16], I32)
            io_t = sbt(nc, st, "s6_iot", [128, NT], I32)
            io_d = sbt(nc, st, "s6_iod", [128, 2, NT], I32)
            flat = lambda a: a[:].rearrange("p a b -> p (a b)")
            V(lambda e: e.tensor_tensor(out=A16[:], in0=flat(OH1), in1=flat(OH2), op=ALU.add), ["s6_OH1", "s6_OH2"], ["s6_A16"])
            P.op("gpsimd", lambda e: e.affine_select(out=Tri[:], in_=k.onesb[:], pattern=[[1, 128]], compare_op=ALU.is_ge, fill=0.0, base=0, channel_multiplier=-1), reads=["c_onesb"], writes=["s6_Tri"])
            for hf in range(2):
                P.op("tensor", lambda e, hf=hf: e.matmul(k.ps[hf][:, :], lhsT=Tri[:], rhs=A16[:, hf * 512:(hf + 1) * 512], start=True, stop=True), reads=["s6_Tri", "s6_A16"], writes=[f"ps{hf}"])
                P.op("tensor", lambda e, hf=hf: e.matmul(k.ps[2 + hf][:, :], lhsT=k.onesb[:], rhs=A16[:, hf * 512:(hf + 1) * 512], start=True, stop=True), reads=["c_onesb", "s6_A16"], writes=[f"ps{2 + hf}"])
                P.op("scalar", lambda e, hf=hf: e.copy(flat(INC)[:, hf * 512:(hf + 1) * 512], k.ps[hf][:, :]), reads=[f"ps{hf}"], writes=[f"s6_INC{hf}"])
                V(lambda e, hf=hf: e.tensor_copy(flat(TB)[:, hf * 512:(hf + 1) * 512], k.ps[2 + hf][:, :]), [f"ps{2 + hf}"], [f"s6_TB{hf}"])
            V(lambda e: e.tensor_copy(X[0][:], TB[:]), ["s6_TB0", "s6_TB1"], ["s6_X0"])
            cur = 0
            for s_ in (1, 2, 4, 8, 16):
                nxt = 1 - cur
                V(lambda e, s_=s_, cur=cur, nxt=nxt: e.tensor_tensor(out=X[nxt][:, s_:, :], in0=X[cur][:, s_:, :], in1=X[cur][:, 0:NT - s_, :], op=ALU.add), [f"s6_X{cur}"], [f"s6_X{nxt}"])
                V(lambda e, s_=s_, cur=cur, nxt=nxt: e.tensor_copy(X[nxt][:, 0:s_, :], X[cur][:, 0:s_, :]), [f"s6_X{cur}", f"s6_X{nxt}"], [f"s6_X{nxt}"])
                cur = nxt
            V(lambda e, cur=cur: e.tensor_tensor(out=tmp3[:], in0=X[cur][:], in1=TB[:], op=ALU.subtract), [f"s6_X{cur}", "s6_TB0", "s6_TB1"], ["s6_tmp3"])
            V(lambda e: e.tensor_tensor(out=tmp3[:], in0=tmp3[:], in1=INC[:], op=ALU.add), ["s6_tmp3", "s6_INC0", "s6_INC1"], ["s6_tmp3"])
            V(lambda e: e.tensor_scalar(out=tmp3[:], in0=tmp3[:], scalar1=-1.0, scalar2=float(C - 1), op0=ALU.add, op1=ALU.min), ["s6_tmp3"], ["s6_tmp3"])
            P.op("gpsimd", lambda e: e.iota(ECf[:], pattern=[[0, NT], [C, 32]], base=0, channel_multiplier=0, allow_small_or_imprecise_dtypes=True), writes=["s6_ECf"])
            V(lambda e: e.tensor_tensor(out=tmp3[:], in0=tmp3[:], in1=ECf[:], op=ALU.add), ["s6_tmp3", "s6_ECf"], ["s6_tmp3"])
            for kk, OH in ((0, OH1), (1, OH2)):
                ohn = "s6_OH1" if kk == 0 else "s6_OH2"
                V(lambda e, OH=OH: e.tensor_tensor(out=X[0][:], in0=OH[:], in1=tmp3[:], op=ALU.mult), [ohn, "s6_tmp3", "s6_X0", "s6_X1"], ["s6_X0"])
                V(lambda e, kk=kk: e.reduce_sum(out=Sf[:, kk, :], in_=X[0][:], axis=AX.X), ["s6_X0"], ["s6_Sf"])
            V(lambda e: e.tensor_copy(Si[:], Sf[:]), ["s6_Sf"], ["s6_Si"])
            P.op("gpsimd", lambda e: e.memset(REC[:], 0), writes=["s6_REC"])
            P.op("gpsimd", lambda e: e.iota(io_t[:], pattern=[[128, NT]], base=0, channel_multiplier=1), writes=["s6_iot"])
            for kk in range(2):
                P.op("gpsimd", lambda e, kk=kk: e.iota(io_d[:, kk, :], pattern=[[256, NT]], base=kk, channel_multiplier=2), writes=["s6_iod"])
            RECf = REC[:].bitcast(F32)
            for kk in range(2):
                P.op("gpsimd", lambda e, kk=kk: e.tensor_copy(REC[:, :, kk, 0], io_t[:]), reads=["s6_iot", "s6_REC"], writes=["s6_REC"])
                P.op("gpsimd", lambda e, kk=kk: e.tensor_copy(REC[:, :, kk, 1], io_d[:, kk, :]), reads=["s6_iod", "s6_REC"], writes=["s6_REC"])
                P.op("gpsimd", lambda e, kk=kk: e.tensor_copy(RECf[:, :, kk, 2], Gt[:, :, kk]), reads=["s6_Gt", "s6_REC"], writes=["s6_REC"])
            P.op("gpsimd", lambda e: e.memset(INIT[:], 0), writes=["s6_INIT"])
            P.op("gpsimd", lambda e: e.memset(INIT[:, :, 1:2], 2 * S), reads=["s6_INIT"], writes=["s6_INIT"])
            P.dma("gpsimd", "s6_tabinit", lambda e: e.dma_start(out=c.tab[:, :].rearrange("(p b) w -> p (b w)", p=128), in_=INIT[:].rearrange("p b w -> p (b w)")), reads=["s6_INIT"], writes=["d_tab"])
            for t in range(NT):
                for kk in range(2):
                    P.dma("gpsimd", "s6_tabsc", lambda e, t=t, kk=kk: e.indirect_dma_start(
                        out=c.tab[:, :], out_offset=bass.IndirectOffsetOnAxis(ap=Si[:, kk, t:t + 1], axis=0), in_=REC[:, t, kk, :], in_offset=None),
                        reads=["s6_Si", "s6_REC", "d_tab"], writes=[f"d_tabs{t}_{kk}"])
            tab_res = [f"d_tabs{t}_{kk}" for t in range(NT) for kk in range(2)]
            x1b_res = [f"d_x1b{t}" for t in range(NT)]
        P.barrier()
        wpg = sbt(nc, st0, "s6_wpg", [128, 8, D], BF16)
        wpp = sbt(nc, st0, "s6_wpp", [128, 2, D], BF16)
        wpgv = c.w_pg[li].rearrange("(kc kp) n -> kp kc n", kp=128)
        for q in range(2):
            P.dma("gpsimd", "s6_wpg", lambda e, q=q: e.dma_start(out=wpg[:, q * 4:(q + 1) * 4, :], in_=wpgv[:, q * 4:(q + 1) * 4, :]), writes=["s6_wpg"])
        P.dma("gpsimd", "s6_wpp", lambda e: e.dma_start(out=wpp[:], in_=c.w_pp[li].rearrange("(kc kp) n -> kp kc n", kp=128)), writes=["s6_wpp"])
        with contextlib.ExitStack() as st:
            wg_ = [sbt(nc, st, f"s6_wg{i}", [128, 8, 512], BF16) for i in range(2)]
            wu_ = [sbt(nc, st, f"s6_wu{i}", [128, 8, 512], BF16) for i in range(2)]
            wd = [sbt(nc, st, f"s6_wd{i}", [128, 4, D], BF16) for i in range(2)]
            tabt = [sbt(nc, st, f"s6_tabt{i}", [128, NB, 16], I32) for i in range(2)]
            xg = [sbt(nc, st, f"s6_xg{i}", [128, NB, D], BF16) for i in range(2)]
            xgT = [sbt(nc, st, f"s6_xgT{i}", [128, 8, C], BF16) for i in range(2)]
            hT = [sbt(nc, st, f"s6_hT{i}", [128, 4, C], BF16) for i in range(2)]
            sgl = [sbt(nc, st, f"s6_sg{i}", [128, C], F32) for i in range(2)]
            yg = [sbt(nc, st, f"s6_yg{i}", [128, D], F32) for i in range(2)]

            def loads(ex):
                i2 = ex % 2
                if "moe_noload" in DBG_FLAGS and ex >= 2:
                    return loads_now(ex, i2)
                P.dma("gpsimd", f"s6_wg{i2}", lambda e: e.dma_start(out=wg_[i2][:].rearrange("p a b -> p (a b)"), in_=c.w_eg[li, ex]), writes=[f"s6_wg{i2}"])
                P.dma("gpsimd", f"s6_wu{i2}", lambda e: e.dma_start(out=wu_[i2][:].rearrange("p a b -> p (a b)"), in_=c.w_eu[li, ex]), writes=[f"s6_wu{i2}"])
                P.dma("gpsimd", f"s6_wd{i2}", lambda e: e.dma_start(out=wd[i2][:].rearrange("p a b -> p (a b)"), in_=c.w_ed[li, ex]), writes=[f"s6_wd{i2}"])
                P.dma("gpsimd", f"s6_tabt{i2}", lambda e: e.dma_start(out=tabt[i2][:], in_=c.tab[ex * C:(ex + 1) * C, :].rearrange("(b p) w -> p b w", p=128)), reads=["s6_wpg", "s6_wpp"], writes=[f"s6_tabt{i2}"])
                for b in range(NB):
                    P.dma("gpsimd", f"s6_xg{i2}", lambda e, b=b: e.indirect_dma_start(
                        out=xg[i2][:, b, :], out_offset=None, in_=c.x1b[:, :], in_offset=bass.IndirectOffsetOnAxis(ap=tabt[i2][:, b, 0:1], axis=0)),
                        reads=[f"s6_tabt{i2}"], writes=[f"s6_xg{i2}_{b}"])

            def loads_now(ex, i2):
                P.dma("gpsimd", f"s6_tabt{i2}", lambda e: e.dma_start(out=tabt[i2][:], in_=c.tab[ex * C:(ex + 1) * C, :].rearrange("(b p) w -> p b w", p=128)), reads=["s6_wpg", "s6_wpp"], writes=[f"s6_tabt{i2}"])
                for b in range(NB):
                    P.dma("gpsimd", f"s6_xg{i2}", lambda e, b=b: e.indirect_dma_start(
                        out=xg[i2][:, b, :], out_offset=None, in_=c.x1b[:, :], in_offset=bass.IndirectOffsetOnAxis(ap=tabt[i2][:, b, 0:1], axis=0)),
                        reads=[f"s6_tabt{i2}"], writes=[f"s6_xg{i2}_{b}"])

            nps = [0]

            def compute(ex):
                i2 = ex % 2
                tabf = tabt[i2][:].bitcast(F32)
                for b in range(NB):
                    pq, pqn = (psb6, "ps6") if b % 2 == 0 else (psb7, "ps7")
                    for kc in range(8):
                        P.op("tensor", lambda e, b=b, kc=kc, pq=pq: e.transpose(pq[:, kc * 128:(kc + 1) * 128], xg[i2][:, b, kc * 128:(kc + 1) * 128], k.identb[:]),
                             reads=[f"s6_xg{i2}_{b}", "c_identb"], writes=[pqn])
                    fn = (lambda e, b=b, pq=pq: e.tensor_copy(xgT[i2][:, :, b * 128:(b + 1) * 128], pq[:, :].rearrange("p (a b) -> p a b", a=8))) if b % 2 == 0 else \
                         (lambda e, b=b, pq=pq: e.copy(xgT[i2][:, :, b * 128:(b + 1) * 128], pq[:, :].rearrange("p (a b) -> p a b", a=8)))
                    P.op("vector" if b % 2 == 0 else "scalar", fn, reads=[pqn], writes=[f"s6_xgT{i2}_{b}"])
                xres = [f"s6_xgT{i2}_{b}" for b in range(NB)]
                for fc in range(4):
                    pg, pgn = k.ps[nps[0] % 6], f"ps{nps[0] % 6}"
                    nps[0] += 1
                    pu, pun = k.ps[nps[0] % 6], f"ps{nps[0] % 6}"
                    nps[0] += 1
                    for kc in range(8):
                        P.op("tensor", lambda e, pg=pg, kc=kc, fc=fc: e.matmul(pg[:, :], lhsT=wg_[i2][:, kc, fc * 128:(fc + 1) * 128], rhs=xgT[i2][:, kc, :], start=(kc == 0), stop=(kc == 7)),
                             reads=[f"s6_wg{i2}"] + xres, writes=[pgn])
                    for kc in range(8):
                        P.op("tensor", lambda e, pu=pu, kc=kc, fc=fc: e.matmul(pu[:, :], lhsT=wu_[i2][:, kc, fc * 128:(fc + 1) * 128], rhs=xgT[i2][:, kc, :], start=(kc == 0), stop=(kc == 7)),
                             reads=[f"s6_wu{i2}"] + xres, writes=[pun])
                    sg, sgn = sgl[fc % 2], f"s6_sg{fc % 2}"
                    P.op("scalar", lambda e, pg=pg, sg=sg: e.activation(out=sg[:], in_=pg[:, :], func=AF.Silu), reads=[pgn], writes=[sgn])
                    P.op("vector", lambda e, pu=pu, sg=sg, fc=fc: e.tensor_tensor(out=hT[i2][:, fc, :], in0=pu[:, :], in1=sg[:], op=ALU.mult), reads=[pun, sgn], writes=[f"s6_hT{i2}_{fc}"])
                for b in range(NB):
                    ygb, ygn = yg[b % 2], f"s6_yg{b % 2}"
                    for nh in range(2):
                        py, pyn = k.ps[nps[0] % 6], f"ps{nps[0] % 6}"
                        nps[0] += 1
                        for fc in range(4):
                            P.op("tensor", lambda e, py=py, fc=fc, b=b, nh=nh: e.matmul(py[:, :], lhsT=hT[i2][:, fc, b * 128:(b + 1) * 128], rhs=wd[i2][:, fc, nh * 512:(nh + 1) * 512], start=(fc == 0), stop=(fc == 3)),
                                 reads=[f"s6_wd{i2}", f"s6_hT{i2}_{fc}"], writes=[pyn])
                        P.op("vector", lambda e, py=py, b=b, nh=nh, ygb=ygb: e.tensor_scalar(out=ygb[:, nh * 512:(nh + 1) * 512], in0=py[:, :], scalar1=tabf[:, b, 2:3], scalar2=None, op0=ALU.mult),
                             reads=[pyn, f"s6_tabt{i2}"], writes=[ygn + f"_{nh}"])
                    P.dma("gpsimd", "s6_ysc", lambda e, b=b, ygb=ygb: e.indirect_dma_start(
                        out=c.ybuf[:, :], out_offset=bass.IndirectOffsetOnAxis(ap=tabt[i2][:, b, 1:2], axis=0), in_=ygb[:, :], in_offset=None),
                        reads=[ygn + "_0", ygn + "_1", f"s6_tabt{i2}"], writes=[f"d_ybuf{ex}_{b}"])

            def castw(ex):
                i2 = ex % 2
                P.op("vector", lambda e: e.tensor_copy(wd[i2][:].rearrange("p a b -> p (a b)"), wd32[i2][:].rearrange("p a b -> p (a b)")), reads=[f"s6_wd32{i2}"], writes=[f"s6_wd{i2}"])

            loads(0)
            for ex in range(32):
                if ex + 1 < 32:
                    loads(ex + 1)
                if "moe_nocomp" not in DBG_FLAGS:
                    compute(ex)
        P.barrier()
        with contextlib.ExitStack() as st:
            xin = [sbt(nc, st, f"s6_xin2{i}", [128, D], F32) for i in range(2)]
            xTt = [sbt(nc, st, f"s6_xTt{i}", [128, 8, 128], BF16) for i in range(2)]
            ym = [sbt(nc, st, f"s6_ym{i}", [128, 2, D], F32) for i in range(2)]
            pin = [sbt(nc, st, f"s6_pin{i}", [128, 256], F32) for i in range(2)]
            pT = [sbt(nc, st, f"s6_pT{i}", [128, 2, 128], BF16) for i in range(2)]
            sgt = sbt(nc, st, "s6_sgt", [128, D], F32)
            z = [sbt(nc, st, f"s6_z{i}", [128, D], F32) for i in range(2)]
            xo = [sbt(nc, st, f"s6_xo{i}", [128, D], F32) for i in range(2)]
            stats = sbt(nc, st, "s6_stats", [128, 2, 6], F32)
            mv = sbt(nc, st, "s6_mv", [128, 2], F32)
            rstd = sbt(nc, st, "s6_rstd", [128, 1], F32)
            P.dma("sync", "s6_dummy", lambda e: e.dma_start(out=c.dummy[:, :], in_=c.x1s[0:512, :]), writes=["d_dummy"])
            nps = 0
            for t in range(NT):
                i2 = t % 2
                xb, xn = xin[i2], f"s6_xin2{i2}"
                P.dma("sync", xn, lambda e, xb=xb, t=t: e.dma_start(out=xb[:], in_=c.x1s[t * 128:(t + 1) * 128, :]), writes=[xn])
                P.dma("sync", f"s6_pin{i2}", lambda e, t=t, i2=i2: e.dma_start(out=pin[i2][:], in_=c.p[li, t * 128:(t + 1) * 128, :]), writes=[f"s6_pin{i2}"])
                P.dma("sync", f"s6_ym{i2}", lambda e, t=t, i2=i2: e.dma_start(out=ym[i2][:], in_=c.ybuf[t * 256:(t + 1) * 256, :].rearrange("(p two) n -> p two n", two=2)), reads=["d_dummy"], writes=[f"s6_ym{i2}"])
                for half in range(2):
                    pb, pn = k.ps[nps % 6], f"ps{nps % 6}"
                    nps += 1
                    for q in range(4):
                        kc = half * 4 + q
                        P.op("tensor", lambda e, pb=pb, xb=xb, kc=kc, q=q: e.transpose(pb[:, q * 128:(q + 1) * 128], xb[:, kc * 128:(kc + 1) * 128], k.ident[:]),
                             reads=[xn, "c_ident"], writes=[pn])
                    fn = (lambda e, pb=pb, half=half, i2=i2: e.tensor_copy(xTt[i2][:, half * 4:(half + 1) * 4, :], pb[:].rearrange("p (a b) -> p a b", a=4))) if half == 0 else \
                         (lambda e, pb=pb, half=half, i2=i2: e.copy(xTt[i2][:, half * 4:(half + 1) * 4, :], pb[:].rearrange("p (a b) -> p a b", a=4)))
                    P.op("vector" if half == 0 else "scalar", fn, reads=[pn], writes=[f"s6_xTt{i2}_{half}"])
                pb, pn = k.ps[nps % 6], f"ps{nps % 6}"
                nps += 1
                for q in range(2):
                    P.op("tensor", lambda e, pb=pb, q=q, i2=i2: e.transpose(pb[:, q * 128:(q + 1) * 128], pin[i2][:, q * 128:(q + 1) * 128], k.ident[:]), reads=[f"s6_pin{i2}", "c_ident"], writes=[pn])
                P.op("scalar", lambda e, pb=pb, i2=i2: e.copy(pT[i2][:], pb[:, 0:256].rearrange("p (a b) -> p a b", a=2)), reads=[pn], writes=[f"s6_pT{i2}"])
                zb, zn = z[i2], f"s6_z{i2}"
                for nh in range(2):
                    pgt, pgtn = k.ps[nps % 6], f"ps{nps % 6}"
                    nps += 1
                    pp, ppn = k.ps[nps % 6], f"ps{nps % 6}"
                    nps += 1
                    for kc in range(8):
                        P.op("tensor", lambda e, pgt=pgt, kc=kc, nh=nh, i2=i2: e.matmul(pgt[:, :], lhsT=xTt[i2][:, kc, :], rhs=wpg[:, kc, nh * 512:(nh + 1) * 512], start=(kc == 0), stop=(kc == 7)),
                             reads=["s6_wpg", f"s6_xTt{i2}_{kc // 4}"], writes=[pgtn])
                    for kc in range(2):
                        P.op("tensor", lambda e, pp=pp, kc=kc, nh=nh, i2=i2: e.matmul(pp[:, :], lhsT=pT[i2][:, kc, :], rhs=wpp[:, kc, nh * 512:(nh + 1) * 512], start=(kc == 0), stop=(kc == 1)),
                             reads=["s6_wpp", f"s6_pT{i2}"], writes=[ppn])
                    P.op("scalar", lambda e, pgt=pgt, nh=nh: e.activation(out=sgt[:, nh * 512:(nh + 1) * 512], in_=pgt[:, :], func=AF.Sigmoid), reads=[pgtn], writes=[f"s6_sgt{nh}"])
                    P.op("vector", lambda e, pp=pp, nh=nh, zb=zb: e.tensor_tensor(out=zb[:, nh * 512:(nh + 1) * 512], in0=pp[:, :], in1=sgt[:, nh * 512:(nh + 1) * 512], op=ALU.mult),
                         reads=[ppn, f"s6_sgt{nh}"], writes=[zn + f"_{nh}"])
                P.op("gpsimd", lambda e, zb=zb, i2=i2: e.tensor_tensor(out=zb[:], in0=zb[:], in1=ym[i2][:, 0, :], op=ALU.add), reads=[zn + "_0", zn + "_1", f"s6_ym{i2}"], writes=[zn])
                P.op("gpsimd", lambda e, zb=zb, i2=i2: e.tensor_tensor(out=zb[:], in0=zb[:], in1=ym[i2][:, 1, :], op=ALU.add), reads=[zn, f"s6_ym{i2}"], writes=[zn])
                P.op("vector", lambda e, zb=zb, xb=xb: e.scalar_tensor_tensor(out=zb[:], in0=xb[:], scalar=ALPHA, in1=zb[:], op0=ALU.mult, op1=ALU.add), reads=[xn, zn], writes=[zn])
                ob, on = xo[i2], f"s6_xo{i2}"
                layer_norm_tile(P, k, zb, zn, gam, bet, stats, mv, rstd, ob, on, eps[:, 0:1], "s6")
                P.dma("sync", "s6_out", lambda e, t=t, ob=ob: e.dma_start(out=dst[t * 128:(t + 1) * 128, :], in_=ob[:]), reads=[on])
    P.barrier()


def build_layers(nc, layers_local, first, last):
    with contextlib.ExitStack() as st:
        P = Prog(nc, st)
        c = declare_io(nc, layers_local, first, last)
        k = make_consts(nc, st, P)
        n = len(layers_local)
        for li in range(n):
            src = c.x_in if li == 0 else c.xs
            dst = c.out if li == n - 1 else c.xs
            stage1_proj(nc, P, k, c, li, src)
            stage2_attn(nc, P, k, c, li)
            stage3_pool(nc, P, k, c, li)
            stage4_dn(nc, P, k, c, li)
            stage5_merge(nc, P, k, c, li, src)
            stage6_moe_sparse(nc, P, k, c, li, dst)
        P.wait_all("sync")
        P.emit()
        return P.nops


def _kmaj(w, nchunk):
    n, e, kk, f = w.shape
    return np.ascontiguousarray(w.reshape(n, e, nchunk, 128, f).transpose(0, 1, 3, 2, 4)).reshape(n, e, 128, nchunk * f)


def _core_map(inp, xb, b, layers):
    L = list(layers)
    n = len(L)
    g = lambda nm: np.ascontiguousarray(inp[nm][L])
    return {
        "x_in": np.ascontiguousarray(xb, dtype=np.float32),
        "p": np.ascontiguousarray(inp["p"][L][:, b]),
        "w_in": g("w_in"), "b_forget": g("b_forget").reshape(n, 8, 1),
        "pool_w": g("pool_w"), "pool_scale": g("pool_scale").reshape(n, 4, 128, 1),
        "dn_conv": g("dn_conv"), "dn_a_log": g("dn_a_log").reshape(n, 4, 1),
        "dn_dt_bias": g("dn_dt_bias").reshape(n, 4, 1), "dn_norm_w": g("dn_norm_w").reshape(n, 128, 1),
        "w_br": np.ascontiguousarray(np.concatenate([inp["w_br_attn"][L], inp["w_br_pool"][L], inp["w_br_dn"][L]], axis=1)),
        "w_out": g("w_out"), "ln1_g": g("ln1_g"), "ln1_b": g("ln1_b"),
        "w_r": np.ascontiguousarray(np.concatenate([inp["w_router_group"][L], inp["w_router_expert"][L]], axis=2)),
        "b_r": np.ascontiguousarray(np.concatenate([inp["b_router_group"][L], inp["b_router_expert"][L]], axis=1)),
        "w_eg": _kmaj(g("w_exp_gate"), 8), "w_eu": _kmaj(g("w_exp_up"), 8), "w_ed": _kmaj(g("w_exp_down"), 4),
        "w_pp": g("w_ple_proj"), "w_pg": g("w_ple_gate"), "ln2_g": g("ln2_g"), "ln2_b": g("ln2_b"),
    }


LAYER_GROUPS = [[0, 1, 2, 3]]


def kernel(**inputs):
    inp = {k: np.asarray(v) for k, v in inputs.items()}
    cur = [inp["x"][b] for b in range(4)]
    for grp in LAYER_GROUPS:
        nc = bass.Bass("TRN2", target_bir_lowering=False)
        build_layers(nc, list(range(len(grp))), True, True)
        base = [_core_map(inp, cur[b], b, grp) for b in range(4)]
        maps = [base[c % 4] for c in range(8)]
        res = run_bass_kernel_spmd(nc, maps, core_ids=list(range(8)))
        cur = [np.asarray(res.results[b]["out"], dtype=np.float32) for b in range(4)]
    return np.stack(cur).astype(np.float32)
```
